# Optimizing a Trainium2 kernel written in Bass

```python
import jax
import jax.numpy as jnp
from jax import lax
import numpy as np

D_MODEL = 1024
BATCH = 8
SEQ = 4096
DEPTH = 2

GRID_W = 64
CTX_LEN = 256
N_EVEN = (DEPTH + 1) // 2
N_ODD = DEPTH // 2
EPS = 1e-6

MLSTM_HEADS = 4
MLSTM_DH = 256
MLSTM_W = MLSTM_HEADS * MLSTM_DH
MLSTM_CHUNK = 64
M_INIT = -1e30
LRU_W = 1024
LRU_BLOCKS = 16
LRU_BW = LRU_W // LRU_BLOCKS
LRU_C = 8.0
CONV_W = 4
CONV_LEFT = 2
EVEN_SPLITS = (MLSTM_W, 2 * MLSTM_W, 3 * MLSTM_W, 4 * MLSTM_W, 4 * MLSTM_W + 4 * MLSTM_HEADS, 4 * MLSTM_W + 4 * MLSTM_HEADS + LRU_W)
EVEN_IN = 4 * MLSTM_W + 4 * MLSTM_HEADS + 2 * LRU_W
EVEN_MIX = MLSTM_W + LRU_W
ATT_HEADS = 8
ATT_KV_HEADS = 2
ATT_GROUP = ATT_HEADS // ATT_KV_HEADS
ATT_DH = 128
ATT_Q_W = ATT_HEADS * ATT_DH
ATT_KV_W = ATT_KV_HEADS * ATT_DH
ATT_IN = ATT_Q_W + 2 * ATT_KV_W
ATT_BLOCK = 128
ROPE_AXIS_DIM = ATT_DH // 2
ROPE_THETA = 10000.0
N_EXPERTS = 32
TOP_K = 4
D_FF = 1024
SWIGLU_ALPHA = 1.702
SWIGLU_LIMIT = 7.0
MOE_BLOCK = 128

kernel_name = 'hybrid_mlstm_rglru_gqa_moe_diffusion'


def rmsnorm(x, g):
    xf = x.astype(jnp.float32)
    y = xf * lax.rsqrt(jnp.mean(xf * xf, axis=-1, keepdims=True) + EPS)
    return (y * g.astype(jnp.float32)).astype(x.dtype)


def dwconv_centred(x, w, b):
    y = lax.conv_general_dilated(
        x, w[:, None, :].astype(x.dtype), window_strides=(1,),
        padding=[(CONV_LEFT, CONV_W - 1 - CONV_LEFT)],
        dimension_numbers=('NWC', 'WIO', 'NWC'), feature_group_count=x.shape[-1])
    return y + b.astype(x.dtype)


def _maybe_flip(t, axis, rev):
    return jnp.flip(t, axis) if rev else t


def mlstm_chunk_scan(q, k, v, ig, lf, state):
    bsz, nh, seqlen, dh = q.shape
    n_chunks = seqlen // MLSTM_CHUNK

    def to_chunks(a):
        a = a.reshape(bsz, nh, n_chunks, MLSTM_CHUNK, *a.shape[3:])
        return jnp.moveaxis(a, 2, 0)

    lower = jnp.tril(jnp.ones((MLSTM_CHUNK, MLSTM_CHUNK), dtype=bool))

    def step(carry, inp):
        c_mat, n_vec, m_prev = carry
        qc, kc, vc, ic, fc = inp
        cum_f = jnp.cumsum(fc, axis=-1)
        log_intra = cum_f[..., :, None] - cum_f[..., None, :] + ic[..., None, :]
        log_intra = jnp.where(lower, log_intra, -jnp.inf)
        log_inter = cum_f + m_prev[..., None]
        m_t = jnp.maximum(log_inter, jnp.max(log_intra, axis=-1))
        w_inter = jnp.exp(log_inter - m_t)
        scores = jnp.einsum('bhtd,bhsd->bhts', qc, kc) * jnp.exp(log_intra - m_t[..., None])
        num = (w_inter[..., None] * jnp.einsum('bhed,bhtd->bhte', c_mat, qc)
               + jnp.einsum('bhts,bhse->bhte', scores, vc))
        den = w_inter * jnp.einsum('bhd,bhtd->bht', n_vec, qc) + jnp.sum(scores, axis=-1)
        h = num / jnp.maximum(jnp.abs(den), jnp.exp(-m_t))[..., None]
        total_f = cum_f[..., -1]
        log_w = total_f[..., None] - cum_f + ic
        m_new = jnp.maximum(total_f + m_prev, jnp.max(log_w, axis=-1))
        decay = jnp.exp(total_f + m_prev - m_new)
        w_state = jnp.exp(log_w - m_new[..., None])
        c_new = decay[..., None, None] * c_mat + jnp.einsum('bhse,bhsd->bhed', vc * w_state[..., None], kc)
        n_new = decay[..., None] * n_vec + jnp.einsum('bhs,bhsd->bhd', w_state, kc)
        return (c_new, n_new, m_new), h

    state, h = lax.scan(step, state, (to_chunks(q), to_chunks(k), to_chunks(v), to_chunks(ig), to_chunks(lf)))
    h = jnp.moveaxis(h, 0, 2).reshape(bsz, nh, seqlen, dh)
    return h, state


def linear_scan(a, b, h0):
    def combine(left, right):
        return left[0] * right[0], right[0] * left[1] + right[1]
    a_cum, b_cum = lax.associative_scan(combine, (a, b), axis=1)
    h = a_cum * h0[:, None, :] + b_cum
    return h, h[:, -1]


def even_prepare(u, w_in, qk_conv_w, qk_conv_b, gate_b, lru_conv_w, lru_conv_b, lru_wa, lru_ba, lru_wx, lru_bx, lru_lam):
    bsz, n, _ = u.shape
    z = u @ w_in
    q_pre, k_pre, v, o_pre, g, xr, y_gate = jnp.split(z, EVEN_SPLITS, axis=-1)
    qk = jax.nn.silu(dwconv_centred(jnp.concatenate([q_pre, k_pre], axis=-1), qk_conv_w, qk_conv_b))
    q, k = jnp.split(qk, 2, axis=-1)

    def heads(t):
        return t.reshape(bsz, n, MLSTM_HEADS, MLSTM_DH).transpose(0, 2, 1, 3).astype(jnp.float32)

    q, k, v = heads(q), heads(k) * (MLSTM_DH ** -0.5), heads(v)
    g = (g + gate_b).astype(jnp.float32).reshape(bsz, n, 2, 2, MLSTM_HEADS).transpose(2, 3, 0, 4, 1)
    ig, lf = g[:, 0], jax.nn.log_sigmoid(g[:, 1])
    xr = dwconv_centred(xr, lru_conv_w, lru_conv_b).astype(jnp.float32)
    xb = xr.reshape(bsz, n, LRU_BLOCKS, LRU_BW)
    r = jax.nn.sigmoid(jnp.einsum('blnc,zncd->zblnd', xb, lru_wa.astype(jnp.float32)) + lru_ba.astype(jnp.float32)[:, None, None])
    i = jax.nn.sigmoid(jnp.einsum('blnc,zncd->zblnd', xb, lru_wx.astype(jnp.float32)) + lru_bx.astype(jnp.float32)[:, None, None])
    log_a = -LRU_C * r.reshape(2, bsz, n, LRU_W) * jax.nn.softplus(-lru_lam.astype(jnp.float32))[:, None, None, :]
    a = jnp.exp(log_a)
    bx = jnp.sqrt(-jnp.expm1(2.0 * log_a)) * i.reshape(2, bsz, n, LRU_W) * xr
    return q, k, v, ig, lf, a, bx, o_pre, y_gate


def even_recur(q, k, v, ig, lf, a, bx, states):
    h_m, h_l, finals = 0.0, 0.0, []
    for d in range(2):
        rev = d == 1
        m_state, l_state = states[d]
        hm, m_state = mlstm_chunk_scan(_maybe_flip(q, 2, rev), _maybe_flip(k, 2, rev), _maybe_flip(v, 2, rev),
                                       _maybe_flip(ig[d], 2, rev), _maybe_flip(lf[d], 2, rev), m_state)
        hl, l_state = linear_scan(_maybe_flip(a[d], 1, rev), _maybe_flip(bx[d], 1, rev), l_state)
        h_m = h_m + _maybe_flip(hm, 2, rev)
        h_l = h_l + _maybe_flip(hl, 1, rev)
        finals.append((m_state, l_state))
    return h_m, h_l, finals


def even_output(h_m, h_l, o_pre, y_gate, mnorm_g, w_out, dtype):
    bsz, nh, n, dh = h_m.shape
    hm = h_m.transpose(0, 2, 1, 3)
    hm = hm * lax.rsqrt(jnp.mean(hm * hm, axis=-1, keepdims=True) + EPS) * mnorm_g.astype(jnp.float32).reshape(nh, dh)
    hm = hm.reshape(bsz, n, MLSTM_W) * jax.nn.sigmoid(o_pre.astype(jnp.float32))
    hl = h_l * jax.nn.gelu(y_gate.astype(jnp.float32))
    return jnp.concatenate([hm, hl], axis=-1).astype(dtype) @ w_out


def even_mixer(u_lat, u_ctx, w_in, qk_conv_w, qk_conv_b, gate_b, mnorm_g, lru_conv_w, lru_conv_b,
               lru_wa, lru_ba, lru_wx, lru_bx, lru_lam, w_out, ctx_out):
    pc = even_prepare(u_ctx, w_in, qk_conv_w, qk_conv_b, gate_b, lru_conv_w, lru_conv_b, lru_wa, lru_ba, lru_wx, lru_bx, lru_lam)
    pl = even_prepare(u_lat, w_in, qk_conv_w, qk_conv_b, gate_b, lru_conv_w, lru_conv_b, lru_wa, lru_ba, lru_wx, lru_bx, lru_lam)
    bsz = u_lat.shape[0]
    zero_m = (jnp.zeros((bsz, MLSTM_HEADS, MLSTM_DH, MLSTM_DH), jnp.float32),
              jnp.zeros((bsz, MLSTM_HEADS, MLSTM_DH), jnp.float32),
              jnp.full((bsz, MLSTM_HEADS), M_INIT, jnp.float32))
    zero_l = jnp.zeros((bsz, LRU_W), jnp.float32)
    hm_c, hl_c, ctx_states = even_recur(*pc[:7], [(zero_m, zero_l), (zero_m, zero_l)])
    hm_l, hl_l, _ = even_recur(*pl[:7], ctx_states)
    y_lat = even_output(hm_l, hl_l, pl[7], pl[8], mnorm_g, w_out, u_lat.dtype)
    y_ctx = even_output(hm_c, hl_c, pc[7], pc[8], mnorm_g, w_out, u_ctx.dtype) if ctx_out else None
    return y_lat, y_ctx


def odd_project(u, w_in, q_norm_g, k_norm_g, with_q):
    bsz, n, _ = u.shape
    if with_q:
        q, k, v = jnp.split(u @ w_in, [ATT_Q_W, ATT_Q_W + ATT_KV_W], axis=-1)
        q = rmsnorm(q.reshape(bsz, n, ATT_HEADS, ATT_DH), q_norm_g)
    else:
        k, v = jnp.split(u @ w_in[:, ATT_Q_W:], [ATT_KV_W], axis=-1)
        q = None
    k = rmsnorm(k.reshape(bsz, n, ATT_KV_HEADS, ATT_DH), k_norm_g)
    v = v.reshape(bsz, n, ATT_KV_HEADS, ATT_DH)
    return q, k, v


def axial_rope(t):
    n = t.shape[1]
    rows = n // GRID_W
    row = jnp.repeat(jnp.arange(rows), GRID_W)
    col = jnp.tile(jnp.arange(GRID_W), rows)
    inv_freq = ROPE_THETA ** (-jnp.arange(0, ROPE_AXIS_DIM, 2, dtype=jnp.float32) / ROPE_AXIS_DIM)
    tf = t.astype(jnp.float32)

    def rotate(seg, pos):
        ang = pos.astype(jnp.float32)[:, None] * inv_freq
        cos = jnp.cos(ang)[None, :, None, :]
        sin = jnp.sin(ang)[None, :, None, :]
        s1, s2 = jnp.split(seg, 2, axis=-1)
        return jnp.concatenate([s1 * cos - s2 * sin, s1 * sin + s2 * cos], axis=-1)

    out = jnp.concatenate([rotate(tf[..., :ROPE_AXIS_DIM], row), rotate(tf[..., ROPE_AXIS_DIM:], col)], axis=-1)
    return out.astype(t.dtype)


def attend_blocks(q, k, v):
    bsz, lq = q.shape[:2]
    nb = lq // ATT_BLOCK
    qb = q.reshape(bsz, nb, ATT_BLOCK, ATT_KV_HEADS, ATT_GROUP, ATT_DH).transpose(1, 0, 2, 3, 4, 5)
    scale = ATT_DH ** -0.5

    def block(qi):
        s = jnp.einsum('bqhgd,bkhd->bhgqk', qi, k).astype(jnp.float32) * scale
        p = jax.nn.softmax(s, axis=-1).astype(v.dtype)
        return jnp.einsum('bhgqk,bkhd->bqhgd', p, v)

    o = lax.map(block, qb)
    return o.transpose(1, 0, 2, 3, 4, 5).reshape(bsz, lq, ATT_Q_W)


def odd_mixer(u_lat, u_ctx, w_in, q_norm_g, k_norm_g, w_out, ctx_out):
    q, k, v = odd_project(u_lat, w_in, q_norm_g, k_norm_g, True)
    qc, kc, vc = odd_project(u_ctx, w_in, q_norm_g, k_norm_g, ctx_out)
    q, k = axial_rope(q), axial_rope(k)
    y_lat = attend_blocks(q, jnp.concatenate([kc, k], axis=1), jnp.concatenate([vc, v], axis=1)) @ w_out
    y_ctx = attend_blocks(qc, kc, vc) @ w_out if ctx_out else None
    return y_lat, y_ctx


def moe_ffn(u, w_r, b_r, w1, b1, w2, b2):
    d = u.shape[-1]

    def per_sample(xt):
        n_tok = xt.shape[0]
        n_rows = n_tok * TOP_K
        n_blocks = -(-n_rows // MOE_BLOCK) + N_EXPERTS
        logits = (xt @ w_r + b_r).astype(jnp.float32)
        top_logit, top_idx = lax.top_k(logits, TOP_K)
        weight = jax.nn.softmax(top_logit, axis=-1)
        expert = top_idx.reshape(-1)
        order = jnp.argsort(expert)
        expert_sorted = expert[order]
        token = order // TOP_K
        sizes = jnp.bincount(expert, length=N_EXPERTS)
        start = jnp.cumsum(sizes) - sizes
        padded = (sizes + MOE_BLOCK - 1) // MOE_BLOCK * MOE_BLOCK
        pad_end = jnp.cumsum(padded)
        pos = pad_end[expert_sorted] - padded[expert_sorted] + jnp.arange(n_rows) - start[expert_sorted]
        buf = jnp.zeros((n_blocks * MOE_BLOCK, d), xt.dtype).at[pos].set(xt[token])
        block_expert = jnp.minimum(jnp.searchsorted(pad_end, jnp.arange(n_blocks) * MOE_BLOCK, side='right'), N_EXPERTS - 1)

        def expert_block(args):
            xb, e = args
            hid = xb @ w1[e] + b1[e]
            gate = jnp.minimum(hid[:, :D_FF], SWIGLU_LIMIT)
            up = jnp.clip(hid[:, D_FF:], -SWIGLU_LIMIT, SWIGLU_LIMIT)
            act = (up + 1) * gate * jax.nn.sigmoid(SWIGLU_ALPHA * gate)
            return act @ w2[e] + b2[e]

        out_buf = lax.map(expert_block, (buf.reshape(n_blocks, MOE_BLOCK, d), block_expert))
        out = out_buf.reshape(-1, d)[pos] * weight.reshape(-1)[order][:, None].astype(xt.dtype)
        return jax.ops.segment_sum(out, token, num_segments=n_tok)

    return lax.map(per_sample, u)


def setup_inputs(seed: int = 0) -> dict:
    key = jax.random.key(seed)
    ks = jax.random.split(key, 32)
    D = D_MODEL

    def nrm(k, shape, scale):
        return jax.random.normal(k, shape, jnp.float32) * scale

    f_bias = jnp.stack([jnp.zeros((MLSTM_HEADS,), jnp.float32), jnp.linspace(3.0, 6.0, MLSTM_HEADS, dtype=jnp.float32)])
    a0 = jax.random.uniform(ks[20], (N_EVEN, 2, LRU_W), jnp.float32, minval=0.9, maxval=0.999)
    s0 = a0 ** (1.0 / LRU_C)
    return {
        'x': nrm(ks[0], (BATCH, SEQ, D), 1.0),
        'c': nrm(ks[1], (BATCH, D), 1.0),
        'ctx': nrm(ks[2], (BATCH, CTX_LEN, D), 1.0),
        'c_ctx': nrm(ks[3], (D,), 1.0),
        'mod_w': nrm(ks[4], (DEPTH, D, 6 * D), 0.5 * D ** -0.5),
        'mod_b': nrm(ks[5], (DEPTH, 6 * D), 0.02),
        'norm1_g': 1.0 + nrm(ks[6], (DEPTH, D), 0.05),
        'norm2_g': 1.0 + nrm(ks[7], (DEPTH, D), 0.05),
        'final_g': 1.0 + nrm(ks[8], (D,), 0.05),
        'ev_w_in': nrm(ks[9], (N_EVEN, D, EVEN_IN), D ** -0.5),
        'ev_qk_conv_w': nrm(ks[10], (N_EVEN, CONV_W, 2 * MLSTM_W), CONV_W ** -0.5),
        'ev_qk_conv_b': nrm(ks[11], (N_EVEN, 2 * MLSTM_W), 0.02),
        'ev_gate_b': (nrm(ks[12], (N_EVEN, 2, 2, MLSTM_HEADS), 0.1) + f_bias).reshape(N_EVEN, 4 * MLSTM_HEADS),
        'ev_mnorm_g': 1.0 + nrm(ks[13], (N_EVEN, MLSTM_W), 0.05),
        'ev_lru_conv_w': nrm(ks[14], (N_EVEN, CONV_W, LRU_W), CONV_W ** -0.5),
        'ev_lru_conv_b': nrm(ks[15], (N_EVEN, LRU_W), 0.02),
        'ev_lru_wa': nrm(ks[16], (N_EVEN, 2, LRU_BLOCKS, LRU_BW, LRU_BW), LRU_BW ** -0.5),
        'ev_lru_ba': nrm(ks[17], (N_EVEN, 2, LRU_BLOCKS, LRU_BW), 0.02),
        'ev_lru_wx': nrm(ks[18], (N_EVEN, 2, LRU_BLOCKS, LRU_BW, LRU_BW), LRU_BW ** -0.5),
        'ev_lru_bx': nrm(ks[19], (N_EVEN, 2, LRU_BLOCKS, LRU_BW), 0.02),
        'ev_lru_lam': jnp.log(s0) - jnp.log1p(-s0),
        'ev_w_out': nrm(ks[21], (N_EVEN, EVEN_MIX, D), EVEN_MIX ** -0.5),
        'od_w_in': nrm(ks[22], (N_ODD, D, ATT_IN), D ** -0.5),
        'od_q_norm_g': 1.0 + nrm(ks[23], (N_ODD, ATT_DH), 0.05),
        'od_k_norm_g': 1.0 + nrm(ks[24], (N_ODD, ATT_DH), 0.05),
        'od_w_out': nrm(ks[25], (N_ODD, ATT_Q_W, D), ATT_Q_W ** -0.5),
        'moe_w_r': nrm(ks[26], (DEPTH, D, N_EXPERTS), D ** -0.5),
        'moe_b_r': nrm(ks[27], (DEPTH, N_EXPERTS), 0.01),
        'moe_w1': nrm(ks[28], (DEPTH, N_EXPERTS, D, 2 * D_FF), D ** -0.5),
        'moe_b1': nrm(ks[29], (DEPTH, N_EXPERTS, 2 * D_FF), 0.02),
        'moe_w2': nrm(ks[30], (DEPTH, N_EXPERTS, D_FF, D), D_FF ** -0.5),
        'moe_b2': nrm(ks[31], (DEPTH, N_EXPERTS, D), 0.02),
    }


def reference(x, c, ctx, c_ctx, mod_w, mod_b, norm1_g, norm2_g, final_g,
              ev_w_in, ev_qk_conv_w, ev_qk_conv_b, ev_gate_b, ev_mnorm_g, ev_lru_conv_w, ev_lru_conv_b,
              ev_lru_wa, ev_lru_ba, ev_lru_wx, ev_lru_bx, ev_lru_lam, ev_w_out,
              od_w_in, od_q_norm_g, od_k_norm_g, od_w_out,
              moe_w_r, moe_b_r, moe_w1, moe_b1, moe_w2, moe_b2):
    h_ctx = ctx
    for layer in range(DEPTH):
        last = layer == DEPTH - 1
        j = layer // 2
        mod_lat = (jax.nn.silu(c) @ mod_w[layer] + mod_b[layer])[:, None, :]
        mod_ctx = (jax.nn.silu(c_ctx) @ mod_w[layer] + mod_b[layer])[None, None, :]
        sh1, sc1, g1, sh2, sc2, g2 = jnp.split(mod_lat, 6, axis=-1)
        csh1, csc1, cg1, csh2, csc2, cg2 = jnp.split(mod_ctx, 6, axis=-1)
        u_lat = rmsnorm(x, norm1_g[layer]) * (1 + sc1) + sh1
        u_ctx = rmsnorm(h_ctx, norm1_g[layer]) * (1 + csc1) + csh1
        if layer % 2 == 0:
            y_lat, y_ctx = even_mixer(u_lat, u_ctx, ev_w_in[j], ev_qk_conv_w[j], ev_qk_conv_b[j], ev_gate_b[j],
                                      ev_mnorm_g[j], ev_lru_conv_w[j], ev_lru_conv_b[j], ev_lru_wa[j], ev_lru_ba[j],
                                      ev_lru_wx[j], ev_lru_bx[j], ev_lru_lam[j], ev_w_out[j], not last)
        else:
            y_lat, y_ctx = odd_mixer(u_lat, u_ctx, od_w_in[j], od_q_norm_g[j], od_k_norm_g[j], od_w_out[j], not last)
        x = x + g1 * y_lat
        v_lat = rmsnorm(x, norm2_g[layer]) * (1 + sc2) + sh2
        if last:
            x = x + g2 * moe_ffn(v_lat, moe_w_r[layer], moe_b_r[layer], moe_w1[layer], moe_b1[layer], moe_w2[layer], moe_b2[layer])
        else:
            h_ctx = h_ctx + cg1 * y_ctx
            v_ctx = rmsnorm(h_ctx, norm2_g[layer]) * (1 + csc2) + csh2
            n_ctx = h_ctx.shape[1]
            f = moe_ffn(jnp.concatenate([v_ctx, v_lat], axis=1), moe_w_r[layer], moe_b_r[layer],
                        moe_w1[layer], moe_b1[layer], moe_w2[layer], moe_b2[layer])
            h_ctx = h_ctx + cg2 * f[:, :n_ctx]
            x = x + g2 * f[:, n_ctx:]
    return rmsnorm(x, final_g)
```

```python
import contextlib
import numpy as np
import concourse.bass as bass
import concourse.mybir as mybir
from concourse.bass_utils import run_bass_kernel_spmd

F32 = mybir.dt.float32
BF16 = mybir.dt.bfloat16
I32 = mybir.dt.int32
U32 = mybir.dt.uint32
ALU = mybir.AluOpType
AF = mybir.ActivationFunctionType

T = 4352
NT = 34
D = 1024
NCTX = 256
EPS = 1e-6


class Sched:
    NSLOT = 6

    def __init__(self, nc):
        self.nc = nc
        self.engs = {'pe': nc.tensor, 'act': nc.scalar, 'dve': nc.vector,
                     'pool': nc.gpsimd, 'sp': nc.sync}
        self.sem = {}
        self.cnt = {}
        for k in self.engs:
            self.sem[k] = nc.alloc_semaphore('s_' + k)
            self.cnt[k] = 0
        self.dslots = {}
        self.dcnt = {}
        self.dnext = {}
        self.nslot = {'sp': 8, 'pool': 8, 'act': 2}
        for q in ('sp', 'pool', 'act'):
            self.dslots[q] = [nc.alloc_semaphore('d_%s%d' % (q, i)) for i in range(self.nslot[q])]
            self.dcnt[q] = [0] * self.nslot[q]
            self.dnext[q] = 0
        self.waited = {k: {} for k in self.engs}
        self.res = {}

    def _semof(self, tok):
        if tok[0] == 'e':
            return self.sem[tok[1]]
        return self.dslots[tok[1][0]][tok[1][1]]

    def _wait(self, e, tok):
        key = (tok[0], tok[1])
        if self.waited[e].get(key, 0) >= tok[2]:
            return
        self.engs[e].wait_ge(self._semof(tok), tok[2])
        self.waited[e][key] = tok[2]

    def _deps(self, e, reads, writes):
        deps = []
        for r in reads:
            st = self.res.get(r)
            if st and st['w'] is not None:
                deps.append(st['w'])
        for w in writes:
            st = self.res.get(w)
            if st:
                if st['w'] is not None:
                    deps.append(st['w'])
                deps.extend(st['r'].values())
        for tok in deps:
            if e == 'pe' and tok[0] == 'e' and tok[1] == 'pe':
                continue
            self._wait(e, tok)

    def _commit(self, tok, reads, writes):
        for r in reads:
            st = self.res.setdefault(r, {'w': None, 'r': {}})
            st['r'][(tok[0], tok[1])] = tok
        for w in writes:
            self.res[w] = {'w': tok, 'r': {}}

    def op(self, e, fn, reads=(), writes=()):
        self._deps(e, reads, writes)
        inst = fn(self.engs[e])
        self.cnt[e] += 1
        inst.then_inc(self.sem[e], 1)
        self._commit(('e', e, self.cnt[e]), reads, writes)
        return inst

    def dma(self, q, fn, reads=(), writes=()):
        s = self.dnext[q]
        self.dnext[q] = (s + 1) % self.nslot[q]
        if self.dcnt[q][s] > 0:
            self._wait(q, ('d', (q, s), 16 * self.dcnt[q][s]))
        self._deps(q, reads, writes)
        inst = fn(self.engs[q])
        self.dcnt[q][s] += 1
        inst.then_inc(self.dslots[q][s], 16)
        self._commit(('d', (q, s), 16 * self.dcnt[q][s]), reads, writes)
        return inst

    def barrier(self):
        toks = []
        for q in self.dslots:
            for s in range(self.nslot[q]):
                if self.dcnt[q][s] > 0:
                    toks.append(('d', (q, s), 16 * self.dcnt[q][s]))
        for k in self.engs:
            if self.cnt[k] > 0:
                toks.append(('e', k, self.cnt[k]))
        for e in self.engs:
            for tok in toks:
                self._wait(e, tok)
        self.res = {}


class Rot:
    def __init__(self, bufs, name):
        self.bufs = bufs
        self.name = name
        self.i = 0

    def next(self):
        b = self.bufs[self.i % len(self.bufs)]
        k = (self.name, self.i % len(self.bufs))
        self.i += 1
        return b, k


AX = mybir.AxisListType


def build(stop_after=None, layers=(0, 1)):
    nc = bass.Bass("TRN2", target_bir_lowering=False)
    S = Sched(nc)
    used_inputs = {}
    _ctr = [0]

    def SBT(name, shape, dt):
        _ctr[0] += 1
        return nc.sbuf_tensor("%s_u%d" % (name, _ctr[0]), shape, dt)

    IN_SPECS = {
        "xin": [T, D], "ccols": [128, 16], "mod_w": [2, D, 6 * D], "mod_b": [2, 6 * D],
        "norm1_g": [2, D], "norm2_g": [2, D], "final_g": [1, D],
        "ev_w_in": [D, 6160], "ev_qkcw": [128, 16, 5], "ev_gate_b": [1, 16], "ev_mnorm_g": [1, D],
        "ev_lrucw": [128, 8, 5], "ev_lru_w": [4, 8, 128, 128], "ev_lru_b": [128, 8, 4], "ev_lru_lam": [128, 8, 2],
        "ev_w_out": [2 * D, D], "od_w_in": [D, 1536], "od_qk_g": [2, 128], "od_w_out": [D, D],
        "rope_cos": [4096, 128], "rope_sin": [4096, 128],
        "moe_w_r": [2, D, 32], "moe_b_r": [2, 32],
        "moe_w1_0": [32 * 128, 8, 2048], "moe_w1_1": [32 * 128, 8, 2048],
        "moe_b1_0": [32 * 128, 16], "moe_b1_1": [32 * 128, 16],
        "moe_w2_0": [32 * 128, 8, 1024], "moe_w2_1": [32 * 128, 8, 1024],
        "moe_b2_0": [32, 1024], "moe_b2_1": [32, 1024],
    }

    def I(name):
        if name not in used_inputs:
            used_inputs[name] = nc.dram_tensor(name, list(IN_SPECS[name]), F32, kind="ExternalInput").ap()
        return used_inputs[name]

    def dscr(name, shape, dt=F32):
        return nc.dram_tensor(name, list(shape), dt, kind="Internal").ap()

    dbg_out = {}

    def dbgt(name, shape, dt=F32):
        dbg_out[name] = nc.dram_tensor("dbg_" + name, list(shape), dt, kind="ExternalOutput").ap()
        return dbg_out[name]

    def finish_dbg(pairs):
        S.barrier()
        for name, src, shape, dt in pairs:
            d = dbgt(name, shape, dt)
            rows, cols = shape[0], int(np.prod(shape[1:]))
            s2 = src if len(shape) == 2 else src.rearrange("a b c -> a (b c)")
            d2 = d if len(shape) == 2 else d.rearrange("a b c -> a (b c)")
            with SBT("dbgbuf_" + name, [128, cols], dt) as buf:
                for r0 in range(0, rows, 128):
                    n = min(128, rows - r0)
                    S.dma('sp', lambda e: e.dma_start(out=buf[0:n, :], in_=s2[r0:r0 + n, :]), writes=['dbgbuf'])
                    S.dma('sp', lambda e: e.dma_start(out=d2[r0:r0 + n, :], in_=buf[0:n, :]), reads=['dbgbuf'], writes=[('dbgo', name, r0)])
                S.barrier()
        return nc, dbg_out, used_inputs

    out_d = nc.dram_tensor("out", [4096, D], F32, kind="ExternalOutput").ap()
    modrow = dscr("modrow", [2, 2, 6 * D])
    xres = dscr("xres", [T, D])

    ident = nc.alloc_sbuf_tensor("ident", [128, 128], F32)
    identb = nc.alloc_sbuf_tensor("identb", [128, 128], BF16)
    ones1 = nc.alloc_sbuf_tensor("ones1", [1, 128], F32)
    onesm = nc.alloc_sbuf_tensor("onesm", [128, 128], F32)
    triF = nc.alloc_sbuf_tensor("triF", [128, 128], F32)
    triB = nc.alloc_sbuf_tensor("triB", [128, 128], F32)
    triS = nc.alloc_sbuf_tensor("triS", [128, 128], F32)
    selF = nc.alloc_sbuf_tensor("selF", [128, 128], F32)
    selB = nc.alloc_sbuf_tensor("selB", [128, 128], F32)
    pidx = nc.alloc_sbuf_tensor("pidx", [128, 1], F32)
    pidx_i = nc.alloc_sbuf_tensor("pidx_i", [128, 1], I32)
    S.op('pool', lambda e: e.memset(ident[:], 0.0), writes=['ident'])
    S.op('pool', lambda e: e.affine_select(ident[:], ident[:], pattern=[[-1, 128]], compare_op=ALU.not_equal,
                                          fill=1.0, base=0, channel_multiplier=1), reads=['ident'], writes=['ident'])
    S.op('dve', lambda e: e.tensor_copy(identb[:], ident[:]), reads=['ident'], writes=['identb'])
    S.op('pool', lambda e: e.memset(ones1[:], 1.0), writes=['ones1'])
    S.op('pool', lambda e: e.memset(onesm[:], 1.0), writes=['onesm'])

    def mk_mask(t, pattern, cm, base, cmp):
        S.op('pool', lambda e: e.memset(t[:], 1.0), writes=[t.name])
        S.op('pool', lambda e: e.affine_select(t[:], t[:], pattern=pattern, compare_op=cmp, fill=0.0, base=base,
                                              channel_multiplier=cm), writes=[t.name])

    mk_mask(triF, [[1, 128]], -1, 0, ALU.is_ge)
    mk_mask(triB, [[-1, 128]], 1, 0, ALU.is_ge)
    mk_mask(triS, [[1, 128]], -1, 0, ALU.is_gt)
    mk_mask(selF, [[0, 128]], 1, -127, ALU.is_equal)
    mk_mask(selB, [[0, 128]], 1, 0, ALU.is_equal)
    S.op('pool', lambda e: e.iota(pidx_i[:], pattern=[[0, 1]], base=0, channel_multiplier=1), writes=['pidx_i'])
    S.op('dve', lambda e: e.tensor_copy(pidx[:], pidx_i[:]), reads=['pidx_i'], writes=['pidx'])

    psum = [nc.alloc_psum_tensor("ps%d" % i, [128, 512], F32) for i in range(6)]
    psb = [nc.alloc_psum_tensor("psb%d" % i, [128, 1024], BF16) for i in range(2)]

    def PB(i):
        return ('psum', i)

    def PBB(i):
        return ('psb', i)

    def bcast_row(dst, src_row, key, q='sp'):
        S.dma(q, lambda e: e.dma_start(out=dst, in_=src_row.partition_broadcast(dst.shape[0])), writes=[key])

    def phase_mod():
        mod_w, mod_b = I("mod_w"), I("mod_b")
        with SBT("cc", [128, 16], F32) as cc, SBT("csil", [128, 16], F32) as csil, \
                SBT("mw0", [128, 8, 512], F32) as mw0, SBT("mw1", [128, 8, 512], F32) as mw1, \
                SBT("mb0", [1, 512], F32) as mb0, SBT("mb1", [1, 512], F32) as mb1, \
                SBT("mo0", [1, 512], F32) as mo0, SBT("mo1", [1, 512], F32) as mo1:
            S.dma('sp', lambda e: e.dma_start(out=cc[:], in_=I("ccols")), writes=['cc'])
            S.op('act', lambda e: e.activation(out=csil[:], in_=cc[:], func=AF.Silu), reads=['cc'], writes=['csil'])
            mws = Rot([mw0, mw1], 'mw')
            mbs = Rot([mb0, mb1], 'mb')
            mos = Rot([mo0, mo1], 'mo')
            pi = 0
            for layer in range(2):
                mwv = mod_w[layer].rearrange("(kc p) n -> p kc n", p=128)
                for cb in range(12):
                    mw, mwk = mws.next()
                    mb, mbk = mbs.next()
                    S.dma('sp', lambda e: e.dma_start(out=mw[:], in_=mwv[:, :, cb * 512:(cb + 1) * 512]), writes=[mwk])
                    S.dma('sp', lambda e: e.dma_start(out=mb[:], in_=mod_b[layer:layer + 1, cb * 512:(cb + 1) * 512]), writes=[mbk])
                    for r in range(2):
                        ps = psum[pi % 2]
                        pk = PB(pi % 2)
                        pi += 1
                        for kc in range(8):
                            S.op('pe', lambda e: e.matmul(ps[0:1, :], csil[:, r * 8 + kc:r * 8 + kc + 1], mw[:, kc, :],
                                                          start=(kc == 0), stop=False), reads=['csil', mwk], writes=[pk])
                        S.op('pe', lambda e: e.matmul(ps[0:1, :], ones1[0:1, 0:1], mb[:], start=False, stop=True),
                             reads=['ones1', mbk], writes=[pk])
                        mo, mok = mos.next()
                        S.op('act', lambda e: e.copy(mo[:], ps[0:1, :]), writes=[pk, mok])
                        S.dma('sp', lambda e: e.dma_start(out=modrow[layer, r:r + 1, cb * 512:(cb + 1) * 512], in_=mo[:]),
                              reads=[mok], writes=[('modrow', layer, r, cb)])
        S.barrier()

    def norm_tiles(layer, which, src, tiles, consume):
        gsrc = I("norm1_g") if which == 0 else I("norm2_g")
        sh_off = 0 if which == 0 else 3 * D
        sc_off = D if which == 0 else 4 * D
        with SBT("nA", [128, 2, D], F32) as A, SBT("nSH", [128, 2, D], F32) as SH, \
                SBT("nG", [128, D], F32) as G, \
                SBT("nx0", [128, D], F32) as x0, SBT("nx1", [128, D], F32) as x1, \
                SBT("nu0", [128, D], F32) as u0, SBT("nu1", [128, D], F32) as u1, \
                SBT("nsq", [128, D], F32) as sq, SBT("nst", [128, 8], F32) as st:
            bcast_row(G[:], gsrc[layer:layer + 1, :], 'nG')
            for r in range(2):
                bcast_row(A[:, r, :], modrow[layer, r:r + 1, sc_off:sc_off + D], ('nA', r))
                bcast_row(SH[:, r, :], modrow[layer, r:r + 1, sh_off:sh_off + D], ('nSH', r))
                S.op('dve', lambda e: e.scalar_tensor_tensor(out=A[:, r, :], in0=A[:, r, :], scalar=1.0, in1=G[:],
                                                             op0=ALU.add, op1=ALU.mult), reads=['nG'], writes=[('nA', r)])
            xs = Rot([x0, x1], 'nx')
            us = Rot([u0, u1], 'nu')
            for i in tiles:
                r = 1 if i < 2 else 0
                xt, xk = xs.next()
                ut, uk = us.next()
                S.dma('sp', lambda e: e.dma_start(out=xt[:], in_=src[i * 128:(i + 1) * 128, :]), writes=[xk])
                S.op('act', lambda e: e.activation(out=sq[:], in_=xt[:], func=AF.Square, accum_out=st[:, 0:1]),
                     reads=[xk], writes=['nsq', 'nst'])
                S.op('dve', lambda e: e.tensor_scalar(out=st[:, 1:2], in0=st[:, 0:1], scalar1=1.0 / D, scalar2=EPS,
                                                      op0=ALU.mult, op1=ALU.add), writes=['nst'])
                S.op('act', lambda e: e.activation(out=st[:, 2:3], in_=st[:, 1:2], func=AF.Sqrt), writes=['nst'])
                S.op('dve', lambda e: e.reciprocal(st[:, 3:4], st[:, 2:3]), writes=['nst'])
                S.op('dve', lambda e: e.scalar_tensor_tensor(out=ut[:], in0=xt[:], scalar=st[:, 3:4], in1=A[:, r, :],
                                                             op0=ALU.mult, op1=ALU.mult), reads=[xk, ('nA', r)], writes=[uk, 'nst'])
                S.op('pool', lambda e: e.tensor_tensor(out=ut[:], in0=ut[:], in1=SH[:, r, :], op=ALU.add),
                     reads=[('nSH', r)], writes=[uk])
                consume(i, ut, uk)

    def phase_norm_T(layer, src, uT, tiles=range(NT)):
        def consume(i, ut, uk):
            for half in range(2):
                pb = (i * 2 + half) % 4
                for j in range(4):
                    kc = half * 4 + j
                    S.op('pe', lambda e: e.transpose(psum[pb][:, j * 128:(j + 1) * 128], ut[:, kc * 128:(kc + 1) * 128], ident[:]),
                         reads=[uk, 'ident'], writes=[PB(pb)])
                S.op('act', lambda e: e.copy(uT[:, half * 4:half * 4 + 4, i * 128:(i + 1) * 128],
                                             psum[pb][:].rearrange("p (a b) -> p a b", a=4)),
                     writes=[PB(pb), ('uT', i, half)])
        norm_tiles(layer, 0, src, tiles, consume)
        S.barrier()

    qkT = dscr("qkT", [2048, T], BF16)
    v_tm = dscr("v_tm", [T, D], BF16)
    so_tm = dscr("so_tm", [T, D], BF16)
    g_tm = dscr("g_tm", [T, 16])
    xrT = dscr("xrT", [D, T])
    ygT = dscr("ygT", [D, T], BF16)
    hlT = dscr("hlT", [D, T], BF16)
    hm_d = [dscr("hm_f", [T, D]), dscr("hm_b", [T, D])]
    TG = [(0, 256)] + [(256 + 512 * j, 512) for j in range(8)]
    ZW = 4358
    CN = 4355

    def zcol(t0):
        return 2 + t0 if t0 < 256 else t0 + 5

    def phase_E1(uT):
        wv = I("ev_w_in").rearrange("(kc p) n -> p kc n", p=128)
        with SBT("wb0", [128, 8, 512], BF16) as wb0, SBT("wb1", [128, 8, 512], BF16) as wb1, \
                SBT("zp0", [128, ZW], F32) as zp0, SBT("zp1", [128, ZW], F32) as zp1, \
                SBT("co", [128, ZW], F32) as co, \
                SBT("ob0", [128, ZW], BF16) as ob0, SBT("ob1", [128, ZW], BF16) as ob1, \
                SBT("cwq", [128, 16, 5], F32) as cwq, SBT("cwl", [128, 8, 5], F32) as cwl, \
                SBT("tms0", [128, 512], BF16) as tms0, SBT("tms1", [128, 512], BF16) as tms1, \
                SBT("wg", [128, 8, 16], BF16) as wg, SBT("gbr", [128, 16], F32) as gbr, \
                SBT("GT", [128, NT, 16], F32) as GT:
            S.dma('sp', lambda e: e.dma_start(out=cwq[:], in_=I("ev_qkcw")), writes=['cwq'])
            S.dma('sp', lambda e: e.dma_start(out=cwl[:], in_=I("ev_lrucw")), writes=['cwl'])
            S.op('pool', lambda e: e.memset(zp0[:], 0.0), writes=[('zp', 0)])
            S.op('pool', lambda e: e.memset(zp1[:], 0.0), writes=[('zp', 1)])
            wbs = Rot([wb0, wb1], 'wb')
            zps = Rot([zp0, zp1], 'zp')
            obs = Rot([ob0, ob1], 'ob')
            pi = [0]
            fm_blocks = [(c0, 'qk', c0 // 128) for c0 in range(0, 2048, 512)] + \
                        [(4112 + j * 512, 'xr', j * 4) for j in range(2)] + \
                        [(5136 + j * 512, 'yg', j * 4) for j in range(2)]
            for c0, kind, cbase in fm_blocks:
                wb, wbk = wbs.next()
                S.dma('pool', lambda e: e.dma_start(out=wb[:], in_=wv[:, :, c0:c0 + 512]), writes=[wbk])
                for jj in range(4):
                    cidx = cbase + jj
                    zp, zpk = zps.next()
                    for (t0, n) in TG:
                        pb = pi[0] % 2
                        pi[0] += 1
                        for kc in range(8):
                            S.op('pe', lambda e: e.matmul(psum[pb][:, 0:n], wb[:, kc, jj * 128:(jj + 1) * 128], uT[:, kc, t0:t0 + n],
                                                          start=(kc == 0), stop=(kc == 7)), reads=[wbk], writes=[PB(pb)])
                        z0 = zcol(t0)
                        S.op('act', lambda e: e.copy(zp[:, z0:z0 + n], psum[pb][:, 0:n]), writes=[PB(pb), zpk])
                    ob, obk = obs.next()
                    if kind in ('qk', 'xr'):
                        cw = cwq if kind == 'qk' else cwl
                        S.op('dve', lambda e: e.tensor_scalar(out=co[:, 0:CN], in0=zp[:, 0:CN], scalar1=cw[:, cidx, 0:1], scalar2=None,
                                                              op0=ALU.mult), reads=[zpk, 'cwq', 'cwl'], writes=['co'])
                        for j in range(1, 4):
                            S.op('dve', lambda e: e.scalar_tensor_tensor(out=co[:, 0:CN], in0=zp[:, j:j + CN], scalar=cw[:, cidx, j:j + 1],
                                                                         in1=co[:, 0:CN], op0=ALU.mult, op1=ALU.add),
                                 reads=[zpk], writes=['co'])
                        if kind == 'qk':
                            S.op('act', lambda e: e.activation(out=ob[:, 0:CN], in_=co[:, 0:CN], func=AF.Silu, bias=cw[:, cidx, 4:5], scale=1.0),
                                 reads=['co'], writes=[obk])
                            rows = qkT[cidx * 128:(cidx + 1) * 128, :]
                            S.dma('sp', lambda e: e.dma_start(out=rows[:, 0:256], in_=ob[:, 0:256]), reads=[obk], writes=[('qkT', cidx, 0)])
                            S.dma('sp', lambda e: e.dma_start(out=rows[:, 256:T], in_=ob[:, 259:CN]), reads=[obk], writes=[('qkT', cidx, 1)])
                        else:
                            S.op('act', lambda e: e.activation(out=co[:, 0:CN], in_=co[:, 0:CN], func=AF.Identity, bias=cw[:, cidx, 4:5], scale=1.0),
                                 writes=['co'])
                            rows = xrT[cidx * 128:(cidx + 1) * 128, :]
                            S.dma('sp', lambda e: e.dma_start(out=rows[:, 0:256], in_=co[:, 0:256]), reads=['co'], writes=[('xrT', cidx, 0)])
                            S.dma('sp', lambda e: e.dma_start(out=rows[:, 256:T], in_=co[:, 259:CN]), reads=['co'], writes=[('xrT', cidx, 1)])
                    else:
                        S.op('act', lambda e: e.activation(out=co[:], in_=zp[:], func=AF.Square), reads=[zpk], writes=['co'])
                        S.op('dve', lambda e: e.tensor_scalar(out=co[:], in0=co[:], scalar1=0.044715, scalar2=1.0, op0=ALU.mult, op1=ALU.add),
                             writes=['co'])
                        S.op('dve', lambda e: e.tensor_tensor(out=co[:], in0=co[:], in1=zp[:], op=ALU.mult), reads=[zpk], writes=['co'])
                        S.op('act', lambda e: e.activation(out=co[:], in_=co[:], func=AF.Sigmoid, scale=1.5957691216), writes=['co'])
                        S.op('pool', lambda e: e.tensor_tensor(out=ob[:], in0=co[:], in1=zp[:], op=ALU.mult), reads=['co', zpk], writes=[obk])
                        rows = ygT[cidx * 128:(cidx + 1) * 128, :]
                        S.dma('sp', lambda e: e.dma_start(out=rows[:, 0:256], in_=ob[:, 2:258]), reads=[obk], writes=[('ygT', cidx, 0)])
                        S.dma('sp', lambda e: e.dma_start(out=rows[:, 256:T], in_=ob[:, 261:4357]), reads=[obk], writes=[('ygT', cidx, 1)])
            tms = Rot([tms0, tms1], 'tms')
            for blk in range(4):
                c0 = 2048 + blk * 512
                wb, wbk = wbs.next()
                S.dma('pool', lambda e: e.dma_start(out=wb[:], in_=wv[:, :, c0:c0 + 512]), writes=[wbk])
                dst = v_tm if blk < 2 else so_tm
                dc0 = (blk % 2) * 512
                for i in range(NT):
                    pb = 2 + i % 2
                    for kc in range(8):
                        S.op('pe', lambda e: e.matmul(psum[pb][:], uT[:, kc, i * 128:(i + 1) * 128], wb[:, kc, :],
                                                      start=(kc == 0), stop=(kc == 7)), reads=[wbk], writes=[PB(pb)])
                    st_, stk = tms.next()
                    if blk < 2:
                        S.op('act', lambda e: e.copy(st_[:], psum[pb][:]), writes=[PB(pb), stk])
                    else:
                        S.op('act', lambda e: e.activation(out=st_[:], in_=psum[pb][:], func=AF.Sigmoid), writes=[PB(pb), stk])
                    S.dma('sp', lambda e: e.dma_start(out=dst[i * 128:(i + 1) * 128, dc0:dc0 + 512], in_=st_[:]), reads=[stk],
                          writes=[('tmout', blk, i)])
            S.dma('pool', lambda e: e.dma_start(out=wg[:], in_=wv[:, :, 4096:4112]), writes=['wg'])
            bcast_row(gbr[:], I("ev_gate_b")[0:1, :], 'gbr')
            for i in range(NT):
                pb = 4 + i % 2
                for kc in range(8):
                    S.op('pe', lambda e: e.matmul(psum[pb][:, 0:16], uT[:, kc, i * 128:(i + 1) * 128], wg[:, kc, :],
                                                  start=(kc == 0), stop=(kc == 7)), reads=['wg'], writes=[PB(pb)])
                S.op('dve', lambda e: e.tensor_tensor(out=GT[:, i, :], in0=psum[pb][:, 0:16], in1=gbr[:], op=ALU.add),
                     reads=['gbr'], writes=[PB(pb), 'GT'])
            S.dma('sp', lambda e: e.dma_start(out=g_tm.rearrange("(n p) c -> p n c", p=128), in_=GT[:]), reads=['GT'], writes=['g_tm'])
        S.barrier()

    def phase_E2():
        lw_d, lb_d, lam_d = I("ev_lru_w"), I("ev_lru_b"), I("ev_lru_lam")
        with SBT("lxr", [128, T], F32) as xr, SBT("lyg", [128, T], BF16) as yg, \
                SBT("lA", [128, T], F32) as A, SBT("lB", [128, T], F32) as Bx, \
                SBT("ltmp", [128, T], F32) as tmp, \
                SBT("lH0", [128, T], F32) as H0, SBT("lH1", [128, T], F32) as H1, \
                SBT("lho", [128, T], BF16) as ho, \
                SBT("lw", [128, 4, 128], F32) as lw, SBT("lb", [128, 8, 4], F32) as lb, \
                SBT("lam", [128, 8, 2], F32) as lam, SBT("cA", [128, 8, 2], F32) as cA:
            S.dma('sp', lambda e: e.dma_start(out=lb[:], in_=lb_d), writes=['lb'])
            S.dma('sp', lambda e: e.dma_start(out=lam[:], in_=lam_d), writes=['lam'])
            S.op('act', lambda e: e.activation(out=cA[:], in_=lam[:], func=AF.Exp, scale=-1.0), reads=['lam'], writes=['cA'])
            S.op('act', lambda e: e.activation(out=cA[:], in_=cA[:], func=AF.Ln, bias=1.0, scale=1.0), writes=['cA'])
            S.op('dve', lambda e: e.tensor_scalar(out=cA[:], in0=cA[:], scalar1=-8.0, scalar2=None, op0=ALU.mult), writes=['cA'])
            pi = 0
            for cc in range(8):
                S.dma('sp', lambda e: e.dma_start(out=xr[:], in_=xrT[cc * 128:(cc + 1) * 128, :]), writes=['xr'])
                S.dma('sp', lambda e: e.dma_start(out=yg[:], in_=ygT[cc * 128:(cc + 1) * 128, :]), writes=['yg'])
                S.dma('sp', lambda e: e.dma_start(out=lw[:], in_=lw_d[:, cc].rearrange("g k m -> k g m")), writes=['lw'])
                for z in range(2):
                    H = H0 if z == 0 else H1
                    hk = ('H', z)
                    for gi, dst, dk in ((0, A, 'A'), (1, Bx, 'Bx')):
                        for (t0, n) in TG:
                            pb = pi % 2
                            pi += 1
                            S.op('pe', lambda e: e.matmul(psum[pb][:, 0:n], lw[:, z * 2 + gi, :], xr[:, t0:t0 + n], start=True, stop=True),
                                 reads=['lw', 'xr'], writes=[PB(pb)])
                            S.op('act', lambda e: e.activation(out=dst[:, t0:t0 + n], in_=psum[pb][:, 0:n], func=AF.Sigmoid,
                                                               bias=lb[:, cc, z * 2 + gi:z * 2 + gi + 1], scale=1.0),
                                 reads=['lb'], writes=[PB(pb), dk])
                    S.op('act', lambda e: e.activation(out=A[:], in_=A[:], func=AF.Exp, scale=cA[:, cc, z:z + 1]), reads=['cA'], writes=['A'])
                    S.op('act', lambda e: e.activation(out=tmp[:], in_=A[:], func=AF.Square), reads=['A'], writes=['tmp'])
                    S.op('act', lambda e: e.activation(out=tmp[:], in_=tmp[:], func=AF.Sqrt, bias=1.0, scale=-1.0), writes=['tmp'])
                    S.op('pool', lambda e: e.tensor_tensor(out=Bx[:], in0=Bx[:], in1=xr[:], op=ALU.mult), reads=['xr'], writes=['Bx'])
                    S.op('dve', lambda e: e.tensor_tensor(out=Bx[:], in0=Bx[:], in1=tmp[:], op=ALU.mult), reads=['tmp'], writes=['Bx'])
                    if z == 0:
                        S.op('dve', lambda e: e.tensor_tensor_scan(H[:], A[:], Bx[:], 0.0, ALU.mult, ALU.add), reads=['A', 'Bx'], writes=[hk])
                    else:
                        S.op('dve', lambda e: e.tensor_tensor_scan(H[:, 0:256][:, ::-1], A[:, 0:256][:, ::-1], Bx[:, 0:256][:, ::-1], 0.0,
                                                                   ALU.mult, ALU.add), reads=['A', 'Bx'], writes=[hk])
                        S.op('dve', lambda e: e.tensor_tensor_scan(H[:, 256:T][:, ::-1], A[:, 256:T][:, ::-1], Bx[:, 256:T][:, ::-1], H[:, 0:1],
                                                                   ALU.mult, ALU.add), reads=['A', 'Bx'], writes=[hk])
                S.op('pool', lambda e: e.tensor_tensor(out=H0[:], in0=H0[:], in1=H1[:], op=ALU.add), reads=[('H', 1)], writes=[('H', 0)])
                S.op('dve', lambda e: e.tensor_tensor(out=ho[:], in0=H0[:], in1=yg[:], op=ALU.mult), reads=[('H', 0), 'yg'], writes=['ho'])
                S.dma('pool', lambda e: e.dma_start(out=hlT[cc * 128:(cc + 1) * 128, :], in_=ho[:]), reads=['ho'], writes=[('hlT', cc)])
        S.barrier()

    def phase_E3():
        NLN16 = -2.772588722239781
        with contextlib.ExitStack() as es:
            def al(name, shape, dt):
                return es.enter_context(SBT(name, shape, dt))
            G = al("G", [128, NT, 16], F32)
            LF = al("LF", [128, 2, NT, 4], F32)
            CF = al("CF", [128, 2, NT, 4], F32)
            EB16 = al("EB16", [128, 2, NT, 4], F32)
            ECF = al("ECF", [128, 2, NT, 4], F32)
            DEC = al("DEC", [128, 2, NT, 4], F32)
            WS16 = al("WS16", [128, 2, NT, 4], F32)
            qT0 = al("qT0", [128, 2, T], BF16)
            qT1 = al("qT1", [128, 2, T], BF16)
            kT0 = al("kT0", [128, 2, T], BF16)
            kT1 = al("kT1", [128, 2, T], BF16)
            V0 = al("V0", [128, NT, 257], BF16)
            V1 = al("V1", [128, NT, 257], BF16)
            CTs = al("CTs", [128, 4, 2, 257], F32)
            CTbs = al("CTbs", [128, 4, 2, 257], BF16)
            PTs = al("PTs", [128, 4, 128], BF16)
            ktms = al("ktms", [128, 4, 256], BF16)
            vws = al("vws", [128, 4, 257], BF16)
            sms = al("sms", [128, 4, 8], F32)
            houts = al("houts", [128, 4, 256], F32)
            S.dma('sp', lambda e: e.dma_start(out=G[:], in_=g_tm.rearrange("(n p) c -> p n c", p=128)), writes=['G'])
            Gv = G[:].rearrange("p n (d g h) -> p d g n h", d=2, g=2, h=4)
            fl = lambda t, d: t[:, d].rearrange("p n h -> p (n h)")
            for d in range(2):
                S.op('act', lambda e: e.activation(out=LF[:, d], in_=Gv[:, d, 1], func=AF.Exp, scale=-1.0), reads=['G'], writes=['LF'])
                S.op('act', lambda e: e.activation(out=LF[:, d], in_=LF[:, d], func=AF.Ln, bias=1.0, scale=1.0), writes=['LF'])
                S.op('dve', lambda e: e.tensor_scalar(out=LF[:, d], in0=LF[:, d], scalar1=-1.0, scalar2=None, op0=ALU.mult), writes=['LF'])
                tri = triF if d == 0 else triB
                sel = selF if d == 0 else selB
                S.op('pe', lambda e: e.matmul(psum[0][:, 0:NT * 4], tri[:], fl(LF, d), start=True, stop=True),
                     reads=['LF', tri.name], writes=[PB(0)])
                S.op('act', lambda e: e.copy(fl(CF, d), psum[0][:, 0:NT * 4]), writes=[PB(0), 'CF'])
                S.op('pe', lambda e: e.matmul(psum[1][:, 0:NT * 4], sel[:], fl(CF, d), start=True, stop=True),
                     reads=['CF', sel.name], writes=[PB(1)])
                S.op('act', lambda e: e.activation(out=fl(DEC, d), in_=psum[1][:, 0:NT * 4], func=AF.Exp), writes=[PB(1), 'DEC'])
                S.op('dve', lambda e: e.tensor_tensor(out=EB16[:, d], in0=Gv[:, d, 0], in1=CF[:, d], op=ALU.subtract), reads=['G', 'CF'], writes=['EB16'])
                S.op('dve', lambda e: e.tensor_scalar(out=EB16[:, d], in0=EB16[:, d], scalar1=NLN16, scalar2=None, op0=ALU.add), writes=['EB16'])
                S.op('act', lambda e: e.activation(out=EB16[:, d], in_=EB16[:, d], func=AF.Exp), writes=['EB16'])
                S.op('act', lambda e: e.activation(out=ECF[:, d], in_=CF[:, d], func=AF.Exp), reads=['CF'], writes=['ECF'])
                S.op('dve', lambda e: e.tensor_tensor(out=WS16[:, d], in0=EB16[:, d], in1=DEC[:, d], op=ALU.mult), reads=['EB16', 'DEC'], writes=['WS16'])
            S.barrier()
            qTs, kTs, Vs = [qT0, qT1], [kT0, kT1], [V0, V1]
            order = [list(range(NT)), [1, 0] + list(range(NT - 1, 1, -1))]
            cnt = {'st': 0, 'num': 0}
            for hp in range(2):
                for hh in range(2):
                    h = hp * 2 + hh
                    S.dma('sp', lambda e: e.dma_start(out=qTs[hh][:], in_=qkT[h * 256:(h + 1) * 256, :].rearrange("(dh p) t -> p dh t", p=128)),
                          writes=[('qT', hh)])
                    S.dma('sp', lambda e: e.dma_start(out=kTs[hh][:], in_=qkT[1024 + h * 256:1024 + (h + 1) * 256, :].rearrange("(dh p) t -> p dh t", p=128)),
                          writes=[('kT', hh)])
                    S.dma('sp', lambda e: e.dma_start(out=Vs[hh][:, :, 0:256], in_=v_tm[:, h * 256:(h + 1) * 256].rearrange("(n p) e -> p n e", p=128)),
                          writes=[('V', hh)])
                    S.op('pool', lambda e: e.memset(Vs[hh][:, :, 256:257], 1.0), writes=[('V1', hh)])
                S.op('pool', lambda e: e.memset(CTs[:], 0.0), writes=[('CT', i) for i in range(4)])
                S.op('pool', lambda e: e.memset(CTbs[:], 0.0), writes=[('CTb', i) for i in range(4)])
                for step in range(NT):
                    for ci, (z, hh) in enumerate([(0, 0), (0, 1), (1, 0), (1, 1)]):
                        h = hp * 2 + hh
                        c = order[z][step]
                        sl = slice(c * 128, (c + 1) * 128)
                        qT, kT, V = qTs[hh], kTs[hh], Vs[hh]
                        rk = [('qT', hh), ('kT', hh), ('V', hh), ('V1', hh)]
                        tri = triF if z == 0 else triB
                        pst = cnt['st'] % 2
                        cnt['st'] += 1
                        for dh in range(2):
                            S.op('pe', lambda e: e.matmul(psum[pst][:, 0:128], kT[:, dh, sl], qT[:, dh, sl], start=(dh == 0), stop=(dh == 1)),
                                 reads=rk, writes=[PB(pst)])
                        S.op('dve', lambda e: e.scalar_tensor_tensor(out=PTs[:, ci, :], in0=psum[pst][:, 0:128], scalar=EB16[:, z, c, h:h + 1],
                                                                     in1=tri[:], op0=ALU.mult, op1=ALU.mult), writes=[PB(pst), ('PT', ci)])
                        for dh in range(2):
                            S.op('pe', lambda e: e.transpose(psb[0][:, dh * 128:(dh + 1) * 128], kT[:, dh, sl], identb[:]),
                                 reads=rk, writes=[PBB(0)])
                        S.op('act', lambda e: e.copy(ktms[:, ci, :], psb[0][:, 0:256]), writes=[PBB(0), ('ktm', ci)])
                        S.op('pool', lambda e: e.tensor_scalar(out=vws[:, ci, :], in0=V[:, c, :], scalar1=WS16[:, z, c, h:h + 1], scalar2=None,
                                                               op0=ALU.mult), reads=rk, writes=[('vw', ci)])
                        pn = 2 + cnt['num'] % 2
                        cnt['num'] += 1
                        S.op('pe', lambda e: e.matmul(psum[pn][:, 0:257], PTs[:, ci, :], V[:, c, :], start=True, stop=False),
                             reads=rk + [('PT', ci)], writes=[PB(pn)])
                        for dh in range(2):
                            S.op('pe', lambda e: e.matmul(psum[pn][:, 0:257], qT[:, dh, sl], CTbs[:, ci, dh, :], start=False, stop=(dh == 1)),
                                 reads=rk + [('CTb', ci)], writes=[PB(pn)])
                        sm = sms[:, ci, :]
                        ecf = ECF[:, z, c, h:h + 1]
                        S.op('dve', lambda e: e.tensor_tensor(out=sm[:, 0:1], in0=psum[pn][:, 256:257], in1=ecf, op=ALU.mult), writes=[PB(pn), ('sm', ci)])
                        S.op('dve', lambda e: e.tensor_scalar(out=sm[:, 1:2], in0=sm[:, 0:1], scalar1=-1.0, scalar2=None, op0=ALU.mult), writes=[('sm', ci)])
                        S.op('dve', lambda e: e.tensor_tensor(out=sm[:, 2:3], in0=sm[:, 0:1], in1=sm[:, 1:2], op=ALU.max), writes=[('sm', ci)])
                        S.op('dve', lambda e: e.tensor_scalar(out=sm[:, 3:4], in0=sm[:, 2:3], scalar1=1.0, scalar2=None, op0=ALU.max), writes=[('sm', ci)])
                        S.op('dve', lambda e: e.reciprocal(sm[:, 4:5], sm[:, 3:4]), writes=[('sm', ci)])
                        S.op('dve', lambda e: e.tensor_tensor(out=sm[:, 5:6], in0=sm[:, 4:5], in1=ecf, op=ALU.mult), writes=[('sm', ci)])
                        S.op('act', lambda e: e.activation(out=houts[:, ci, :], in_=psum[pn][:, 0:256], func=AF.Identity, scale=sm[:, 5:6]),
                             reads=[('sm', ci)], writes=[PB(pn), ('hout', ci)])
                        S.dma('sp', lambda e: e.dma_start(out=hm_d[z][sl, h * 256:(h + 1) * 256], in_=houts[:, ci, :]), reads=[('hout', ci)],
                              writes=[('hm', z, h, c)])
                        for dh in range(2):
                            S.op('pe', lambda e: e.matmul(psum[4 + dh][:, 0:257], ktms[:, ci, dh * 128:(dh + 1) * 128], vws[:, ci, :], start=True, stop=True),
                                 reads=[('ktm', ci), ('vw', ci)], writes=[PB(4 + dh)])
                            S.op('dve', lambda e: e.scalar_tensor_tensor(out=CTs[:, ci, dh, :], in0=CTs[:, ci, dh, :], scalar=DEC[:, z, c, h:h + 1],
                                                                         in1=psum[4 + dh][:, 0:257], op0=ALU.mult, op1=ALU.add),
                                 writes=[PB(4 + dh), ('CT', ci)])
                        S.op('act', lambda e: e.copy(CTbs[:, ci], CTs[:, ci]), reads=[('CT', ci)], writes=[('CTb', ci)])
        S.barrier()

    def mixer_out(i, r, G1, wo, nk, lhs_list, src_rows, pbase):
        return

    def phase_E4():
        with contextlib.ExitStack() as es:
            def al(name, shape, dt):
                return es.enter_context(SBT(name, shape, dt))
            wo = al("wo", [128, 16, D], BF16)
            MG = al("MG", [128, D], F32)
            G1 = al("G1", [128, 2, D], F32)
            hf = [al("hf%d" % j, [128, D], F32) for j in range(2)]
            hb = [al("hb%d" % j, [128, D], F32) for j in range(2)]
            so = [al("so%d" % j, [128, D], BF16) for j in range(2)]
            sq = al("e4sq", [128, 256], F32)
            st = al("e4st", [128, 16], F32)
            hmb = al("hmb", [128, D], BF16)
            hmT = [al("hmT%d" % j, [128, 8, 128], BF16) for j in range(2)]
            hl = [al("hl%d" % j, [128, 8, 128], BF16) for j in range(2)]
            xt = [al("e4x%d" % j, [128, D], F32) for j in range(2)]
            tmp = al("e4tmp", [128, D], F32)
            wov = I("ev_w_out").rearrange("(kc p) n -> p kc n", p=128)
            for hlf in range(2):
                S.dma('pool', lambda e: e.dma_start(out=wo[:, hlf * 8:(hlf + 1) * 8, :], in_=wov[:, hlf * 8:(hlf + 1) * 8, :]), writes=[('wo', hlf)])
            bcast_row(MG[:], I("ev_mnorm_g")[0:1, :], 'MG')
            for r in range(2):
                bcast_row(G1[:, r, :], modrow[0, r:r + 1, 2 * D:3 * D], ('G1', r))
            hlv = hlT.rearrange("(cc p) t -> p cc t", p=128)
            xin = I("xin")
            for i in range(NT):
                r = 1 if i < 2 else 0
                j = i % 2
                rows = slice(i * 128, (i + 1) * 128)
                S.dma('sp', lambda e: e.dma_start(out=hf[j][:], in_=hm_d[0][rows, :]), writes=[('hf', j)])
                S.dma('sp', lambda e: e.dma_start(out=hb[j][:], in_=hm_d[1][rows, :]), writes=[('hb', j)])
                S.dma('sp', lambda e: e.dma_start(out=so[j][:], in_=so_tm[rows, :]), writes=[('so', j)])
                S.dma('sp', lambda e: e.dma_start(out=hl[j][:], in_=hlv[:, :, rows]), writes=[('hl', j)])
                S.dma('sp', lambda e: e.dma_start(out=xt[j][:], in_=xin[rows, :]), writes=[('xt', j)])
                S.op('pool', lambda e: e.tensor_tensor(out=hf[j][:], in0=hf[j][:], in1=hb[j][:], op=ALU.add), reads=[('hb', j)], writes=[('hf', j)])
                for h in range(4):
                    S.op('act', lambda e: e.activation(out=sq[:], in_=hf[j][:, h * 256:(h + 1) * 256], func=AF.Square, accum_out=st[:, h:h + 1]),
                         reads=[('hf', j)], writes=['e4sq', 'e4st'])
                S.op('dve', lambda e: e.tensor_scalar(out=st[:, 4:8], in0=st[:, 0:4], scalar1=1.0 / 256, scalar2=EPS, op0=ALU.mult, op1=ALU.add), writes=['e4st'])
                S.op('act', lambda e: e.activation(out=st[:, 8:12], in_=st[:, 4:8], func=AF.Sqrt), writes=['e4st'])
                S.op('dve', lambda e: e.reciprocal(st[:, 12:16], st[:, 8:12]), writes=['e4st'])
                for h in range(4):
                    hs = slice(h * 256, (h + 1) * 256)
                    S.op('dve', lambda e: e.scalar_tensor_tensor(out=hf[j][:, hs], in0=hf[j][:, hs], scalar=st[:, 12 + h:13 + h], in1=MG[:, hs],
                                                                 op0=ALU.mult, op1=ALU.mult), reads=['MG'], writes=[('hf', j), 'e4st'])
                S.op('pool', lambda e: e.tensor_tensor(out=hmb[:], in0=hf[j][:], in1=so[j][:], op=ALU.mult), reads=[('hf', j), ('so', j)], writes=['hmb'])
                for kc in range(8):
                    S.op('pe', lambda e: e.transpose(psb[j][:, kc * 128:(kc + 1) * 128], hmb[:, kc * 128:(kc + 1) * 128], identb[:]),
                         reads=['hmb'], writes=[PBB(j)])
                S.op('act', lambda e: e.copy(hmT[j][:].rearrange("p a b -> p (a b)"), psb[j][:]), writes=[PBB(j), ('hmT', j)])
                for nb in range(2):
                    pb = (2 * i + nb) % 4
                    for kc in range(16):
                        lhs = hmT[j][:, kc, :] if kc < 8 else hl[j][:, kc - 8, :]
                        S.op('pe', lambda e: e.matmul(psum[pb][:], lhs, wo[:, kc, nb * 512:(nb + 1) * 512], start=(kc == 0), stop=(kc == 15)),
                             reads=[('hmT', j), ('hl', j), ('wo', 0), ('wo', 1)], writes=[PB(pb)])
                    cs = slice(nb * 512, (nb + 1) * 512)
                    S.op('dve', lambda e: e.tensor_tensor(out=tmp[:, cs], in0=psum[pb][:], in1=G1[:, r, cs], op=ALU.mult), reads=[('G1', r)],
                         writes=[PB(pb), ('e4tmp', nb)])
                    S.op('pool', lambda e: e.tensor_tensor(out=tmp[:, cs], in0=tmp[:, cs], in1=xt[j][:, cs], op=ALU.add), reads=[('xt', j)],
                         writes=[('e4tmp', nb)])
                S.dma('pool', lambda e: e.dma_start(out=xres[rows, :], in_=tmp[:]), reads=[('e4tmp', 0), ('e4tmp', 1)], writes=[('xres', i)])
        S.barrier()

    v2_tm = dscr("v2_tm", [T, D], BF16)

    def phase_N2(layer, tiles, LOG):
        with contextlib.ExitStack() as es:
            def al(name, shape, dt):
                return es.enter_context(SBT(name, shape, dt))
            wr = al("wr", [128, 8, 32], F32)
            br = al("br", [1, 32], F32)
            vT32 = [al("vT32_%d" % j, [128, 8, 128], F32) for j in range(2)]
            vb = [al("vb%d" % j, [128, D], BF16) for j in range(2)]
            S.dma('sp', lambda e: e.dma_start(out=wr[:], in_=I("moe_w_r")[layer].rearrange("(kc p) n -> p kc n", p=128)), writes=['wr'])
            S.dma('sp', lambda e: e.dma_start(out=br[:], in_=I("moe_b_r")[layer:layer + 1, :]), writes=['br'])
            cnt = [0]

            def consume(i, ut, uk):
                j = cnt[0] % 2
                cnt[0] += 1
                S.op('pool', lambda e: e.tensor_copy(vb[j][:], ut[:]), reads=[uk], writes=[('vb', j)])
                S.dma('pool', lambda e: e.dma_start(out=v2_tm[i * 128:(i + 1) * 128, :], in_=vb[j][:]), reads=[('vb', j)], writes=[('v2', i)])
                for half in range(2):
                    pb = (i * 2 + half) % 4
                    for jj in range(4):
                        kc = half * 4 + jj
                        S.op('pe', lambda e: e.transpose(psum[pb][:, jj * 128:(jj + 1) * 128], ut[:, kc * 128:(kc + 1) * 128], ident[:]),
                             reads=[uk, 'ident'], writes=[PB(pb)])
                    S.op('act', lambda e: e.copy(vT32[j][:, half * 4:half * 4 + 4, :].rearrange("p a b -> p (a b)"), psum[pb][:]),
                         writes=[PB(pb), ('vT32', j, half)])
                pl = 4 + i % 2
                for kc in range(8):
                    S.op('pe', lambda e: e.matmul(psum[pl][:, 0:32], vT32[j][:, kc, :], wr[:, kc, :], start=(kc == 0), stop=False),
                         reads=[('vT32', j, 0), ('vT32', j, 1), 'wr'], writes=[PB(pl)])
                S.op('pe', lambda e: e.matmul(psum[pl][:, 0:32], ones1[0:1, :], br[:], start=False, stop=True), reads=['br', 'ones1'], writes=[PB(pl)])
                S.op('dve', lambda e: e.tensor_copy(LOG[:, i, :], psum[pl][:, 0:32]), writes=[PB(pl), 'LOG'])
            norm_tiles(layer, 1, xres, tiles, consume)
        S.barrier()

    SB = 512
    SHIFT = 9
    NSUB = SB // 128
    NBMAX = 66
    xs_d = dscr("xs_d", [NBMAX * SB, D], BF16)
    ys_d = dscr("ys_d", [NBMAX * SB, D], F32)
    RT = {}
    for nm, shp, dt in (("LOG", [128, NT, 32], F32), ("TOP8", [128, NT, 8], F32), ("WG", [128, NT, 4], F32),
                        ("SLOTI", [128, NT, 4], I32), ("IDXI", [128, NBMAX], I32), ("BIDX", [128, NBMAX], I32),
                        ("IDX8", [128, NBMAX, 8], I32), ("IDX4", [128, NBMAX, 4], I32)):
        RT[nm] = nc.alloc_sbuf_tensor("rt_" + nm, shp, dt)

    def phase_route(tiles, NB):
        LOG, TOP8, WG, SLOTI, IDXI, BIDX = (RT[k] for k in ("LOG", "TOP8", "WG", "SLOTI", "IDXI", "BIDX"))
        with contextlib.ExitStack() as es:
            def al(name, shape, dt):
                return es.enter_context(SBT(name, shape, dt))
            MASK = al("MASK", [128, NT, 32], F32)
            RANK = al("RANK", [128, NT, 32], F32)
            CAR = al("CAR", [128, 32], F32)
            sm = al("rsm", [128, 8], F32)
            ci = al("rci", [128, 32], I32)
            PADDED = al("PADDED", [128, 32], F32)
            PEND = al("PEND", [128, 32], F32)
            BASE = al("BASE", [128, 32], F32)
            ones32 = al("ones32", [128, 32], F32)
            junk = al("rjunk", [128, 32], F32)
            SLOTF = al("SLOTF", [128, NT, 4], F32)
            BSTi = al("BSTi", [128, NBMAX], I32)
            BST = al("BST", [128, NBMAX], F32)
            BLKE = al("BLKE", [128, NBMAX], F32)
            IDXF = al("IDXF", [128, NBMAX], F32)
            IDXF2 = al("IDXF2", [128, NBMAX], F32)
            S.op('pool', lambda e: e.memset(CAR[:], 0.0), writes=['CAR'])
            S.op('pool', lambda e: e.memset(ones32[:], 1.0), writes=['ones32'])
            S.op('pool', lambda e: e.memset(SLOTF[:], 0.0), writes=['SLOTF'])
            for i in tiles:
                S.op('dve', lambda e: e.max(out=TOP8[:, i, :], in_=LOG[:, i, :]), writes=['TOP8'])
                S.op('dve', lambda e: e.tensor_scalar(out=MASK[:, i, :], in0=LOG[:, i, :], scalar1=TOP8[:, i, 3:4], scalar2=None, op0=ALU.is_ge),
                     reads=['TOP8'], writes=[('MASK', i)])
                S.op('dve', lambda e: e.tensor_scalar(out=sm[:, 0:1], in0=TOP8[:, i, 0:1], scalar1=-1.0, scalar2=None, op0=ALU.mult), reads=['TOP8'], writes=['rsm'])
                S.op('act', lambda e: e.activation(out=WG[:, i, :], in_=TOP8[:, i, 0:4], func=AF.Exp, bias=sm[:, 0:1], scale=1.0, accum_out=sm[:, 1:2]),
                     reads=['TOP8'], writes=['rsm', 'WG'])
                S.op('dve', lambda e: e.reciprocal(sm[:, 2:3], sm[:, 1:2]), writes=['rsm'])
                S.op('dve', lambda e: e.tensor_scalar(out=WG[:, i, :], in0=WG[:, i, :], scalar1=sm[:, 2:3], scalar2=None, op0=ALU.mult), writes=['rsm', 'WG'])
                S.op('pe', lambda e: e.matmul(psum[0][:, 0:32], triS[:], MASK[:, i, :], start=True, stop=True), reads=[('MASK', i)], writes=[PB(0)])
                S.op('pe', lambda e: e.matmul(psum[1][:, 0:32], onesm[:], MASK[:, i, :], start=True, stop=True), reads=[('MASK', i)], writes=[PB(1)])
                S.op('dve', lambda e: e.tensor_tensor(out=RANK[:, i, :], in0=psum[0][:, 0:32], in1=CAR[:], op=ALU.add), reads=['CAR'], writes=[PB(0), 'RANK'])
                S.op('dve', lambda e: e.tensor_tensor(out=CAR[:], in0=psum[1][:, 0:32], in1=CAR[:], op=ALU.add), writes=[PB(1), 'CAR'])
            S.op('dve', lambda e: e.tensor_scalar(out=junk[:], in0=CAR[:], scalar1=float(SB - 1), scalar2=None, op0=ALU.add), reads=['CAR'], writes=['rjunk'])
            S.op('dve', lambda e: e.tensor_copy(ci[:], junk[:]), reads=['rjunk'], writes=['rci'])
            S.op('dve', lambda e: e.tensor_scalar(out=ci[:], in0=ci[:], scalar1=SHIFT, scalar2=SHIFT, op0=ALU.arith_shift_right, op1=ALU.logical_shift_left),
                 writes=['rci'])
            S.op('dve', lambda e: e.tensor_copy(PADDED[:], ci[:]), reads=['rci'], writes=['PADDED'])
            S.op('dve', lambda e: e.tensor_tensor_scan(PEND[:], ones32[:], PADDED[:], 0.0, ALU.mult, ALU.add), reads=['ones32', 'PADDED'], writes=['PEND'])
            S.op('dve', lambda e: e.tensor_tensor(out=BASE[:], in0=PEND[:], in1=PADDED[:], op=ALU.subtract), reads=['PEND', 'PADDED'], writes=['BASE'])
            for i in tiles:
                S.op('dve', lambda e: e.tensor_tensor(out=RANK[:, i, :], in0=RANK[:, i, :], in1=BASE[:], op=ALU.add), reads=['BASE'], writes=['RANK'])
                for k in range(4):
                    S.op('dve', lambda e: e.scalar_tensor_tensor(out=junk[:], in0=LOG[:, i, :], scalar=TOP8[:, i, k:k + 1], in1=RANK[:, i, :],
                                                                 op0=ALU.is_equal, op1=ALU.mult, accum_out=SLOTF[:, i, k:k + 1]),
                         reads=['TOP8'], writes=['rjunk', 'RANK', 'SLOTF'])
            S.op('dve', lambda e: e.tensor_copy(SLOTI[:], SLOTF[:]), reads=['SLOTF'], writes=['SLOTI'])
            S.op('pool', lambda e: e.iota(BSTi[:], pattern=[[SB, NBMAX]], base=0, channel_multiplier=0), writes=['BSTi'])
            S.op('dve', lambda e: e.tensor_copy(BST[:], BSTi[:]), reads=['BSTi'], writes=['BST'])
            S.op('pool', lambda e: e.memset(BLKE[:], 0.0), writes=['BLKE'])
            for ex in range(32):
                S.op('dve', lambda e: e.scalar_tensor_tensor(out=BLKE[:], in0=BST[:], scalar=PEND[:, ex:ex + 1], in1=BLKE[:], op0=ALU.is_ge, op1=ALU.add),
                     reads=['BST', 'PEND'], writes=['BLKE'])
            S.op('dve', lambda e: e.tensor_scalar(out=BLKE[:], in0=BLKE[:], scalar1=31.0, scalar2=None, op0=ALU.min), writes=['BLKE'])
            S.op('dve', lambda e: e.tensor_scalar(out=IDXF[:], in0=BLKE[:], scalar1=128.0, scalar2=pidx[:, 0:1], op0=ALU.mult, op1=ALU.add),
                 reads=['pidx'], writes=['IDXF'])
            S.op('dve', lambda e: e.tensor_copy(IDXI[:], IDXF[:]), reads=['IDXF'], writes=['IDXI'])
            S.op('dve', lambda e: e.tensor_copy(BIDX[:], BLKE[:]), reads=['BLKE'], writes=['BIDX'])
            S.op('pool', lambda e: e.memset(BSTi[:], 0), writes=['BSTi'])
            S.op('dve', lambda e: e.tensor_copy(IDXF2[:], BSTi[:]), reads=['BSTi'], writes=['IDXF2'])
            S.op('dve', lambda e: e.tensor_tensor(out=IDXF2[:, 2:NBMAX], in0=BLKE[:, 2:NBMAX], in1=BLKE[:, 0:NBMAX - 2], op=ALU.is_equal),
                 reads=['BLKE'], writes=['IDXF2'])
            S.op('dve', lambda e: e.tensor_scalar(out=IDXF2[:], in0=IDXF2[:], scalar1=float(1 << 20), scalar2=None, op0=ALU.mult), writes=['IDXF2'])
            for kc in range(8):
                S.op('dve', lambda e: e.tensor_scalar(out=BST[:], in0=IDXF[:], scalar1=8.0, scalar2=float(kc), op0=ALU.mult, op1=ALU.add),
                     reads=['IDXF'], writes=['BST'])
                S.op('dve', lambda e: e.tensor_tensor(out=BST[:], in0=BST[:], in1=IDXF2[:], op=ALU.add), reads=['IDXF2'], writes=['BST'])
                S.op('dve', lambda e: e.tensor_copy(RT["IDX8"][:, :, kc], BST[:]), reads=['BST'], writes=['IDX8'])
            for q in range(4):
                S.op('dve', lambda e: e.tensor_scalar(out=BST[:], in0=IDXF[:], scalar1=4.0, scalar2=float(q), op0=ALU.mult, op1=ALU.add),
                     reads=['IDXF'], writes=['BST'])
                S.op('dve', lambda e: e.tensor_tensor(out=BST[:], in0=BST[:], in1=IDXF2[:], op=ALU.add), reads=['IDXF2'], writes=['BST'])
                S.op('dve', lambda e: e.tensor_copy(RT["IDX4"][:, :, q], BST[:]), reads=['BST'], writes=['IDX4'])
            if RT.get('dbg') is not None:
                dd = RT['dbg']
                S.op('dve', lambda e: e.tensor_copy(dd[:, 0:32], CAR[:]), reads=['CAR'], writes=['dd'])
                S.op('dve', lambda e: e.tensor_copy(dd[:, 32:64], PADDED[:]), reads=['PADDED'], writes=['dd'])
                S.op('dve', lambda e: e.tensor_copy(dd[:, 64:96], PEND[:]), reads=['PEND'], writes=['dd'])
                S.op('dve', lambda e: e.tensor_copy(dd[:, 96:128], MASK[:, 0, :]), writes=['dd'])
                S.op('dve', lambda e: e.tensor_copy(dd[:, 128:160], RANK[:, 1, :]), writes=['dd'])
                S.op('dve', lambda e: e.tensor_copy(dd[:, 160:192], ci[:]), writes=['dd'])
        S.barrier()

    def phase_scatter(tiles, NB):
        SLOTI = RT["SLOTI"]
        with contextlib.ExitStack() as es:
            def al(name, shape, dt):
                return es.enter_context(SBT(name, shape, dt))
            zt = al("zt", [128, D], BF16)
            vt = [al("svt%d" % j, [128, D], BF16) for j in range(3)]
            S.op('pool', lambda e: e.memset(zt[:], 0.0), writes=['zt'])
            for b in range(NB * NSUB):
                S.dma('sp', lambda e: e.dma_start(out=xs_d[b * 128:(b + 1) * 128, :], in_=zt[:]), reads=['zt'], writes=[('xsz', b)])
            S.barrier()
            for n, i in enumerate(tiles):
                j = n % 3
                S.dma('sp', lambda e: e.dma_start(out=vt[j][:], in_=v2_tm[i * 128:(i + 1) * 128, :]), writes=[('svt', j)])
                for k in range(4):
                    S.dma('pool', lambda e: e.indirect_dma_start(out=xs_d, out_offset=bass.IndirectOffsetOnAxis(ap=SLOTI[:, i, k:k + 1], axis=0),
                                                                 in_=vt[j][:], in_offset=None), reads=[('svt', j)], writes=[('xs', i, k)])
        S.barrier()

    def phase_moe(layer, NB):
        w1d = I("moe_w1_%d" % layer)
        w2d = I("moe_w2_%d" % layer)
        b1d = I("moe_b1_%d" % layer)
        b2d = I("moe_b2_%d" % layer)
        IDXI, BIDX = RT["IDXI"], RT["BIDX"]
        w1v = w1d.rearrange("a b c -> (a b) c")
        w2v = w2d.rearrange("a (q t) c -> (a q) (t c)", t=2)
        with contextlib.ExitStack() as es:
            def al(name, shape, dt):
                return es.enter_context(SBT(name, shape, dt))
            W1 = [al("W1_%d" % j, [128, 8, 2048], BF16) for j in range(2)]
            W2 = [al("W2_%d" % j, [128, 8, 1024], BF16) for j in range(2)]
            B1C = [al("B1C_%d" % j, [128, 16], F32) for j in range(2)]
            B2R = [al("B2R_%d" % j, [128, D], F32) for j in range(2)]
            XS = [al("XS_%d" % j, [128, D], BF16) for j in range(2 * NSUB)]
            XST = al("XST", [128, 8, SB], BF16)
            ACTT = al("ACTT", [128, 8, SB], BF16)
            tg = [al("tg_%d" % j, [128, SB], F32) for j in range(2)]
            sg = [al("sg_%d" % j, [128, SB], F32) for j in range(2)]
            tu = [al("tu_%d" % j, [128, SB], F32) for j in range(2)]
            YT = [al("YT_%d" % j, [128, D], F32) for j in range(2)]
            ec = [0]
            xc = [0]
            yc = [0]
            bc8 = nc.gpsimd.to_reg(32 * 128 * 8 - 1)
            bc4 = nc.gpsimd.to_reg(32 * 128 * 4 - 1)

            def gathers(b):
                j = b % 2
                idx = bass.IndirectOffsetOnAxis(ap=IDXI[:, b:b + 1], axis=0)
                bidx = bass.IndirectOffsetOnAxis(ap=BIDX[:, b:b + 1], axis=0)
                for kc in range(8):
                    i8 = bass.IndirectOffsetOnAxis(ap=RT["IDX8"][:, b, kc:kc + 1], axis=0)
                    S.dma('pool', lambda e: e.indirect_dma_start(out=W1[j][:, kc, :], out_offset=None, in_=w1v, in_offset=i8, bounds_check=bc8, oob_is_err=False), writes=[('W1', j, kc)])
                for q in range(4):
                    i4 = bass.IndirectOffsetOnAxis(ap=RT["IDX4"][:, b, q:q + 1], axis=0)
                    S.dma('pool', lambda e: e.indirect_dma_start(out=W2[j][:, 2 * q:2 * q + 2, :].rearrange("p a b -> p (a b)"), out_offset=None,
                                                                 in_=w2v, in_offset=i4, bounds_check=bc4, oob_is_err=False), writes=[('W2', j, q)])
                S.dma('pool', lambda e: e.indirect_dma_start(out=B1C[j][:], out_offset=None, in_=b1d, in_offset=idx), writes=[('B1C', j)])
                S.dma('pool', lambda e: e.indirect_dma_start(out=B2R[j][:], out_offset=None, in_=b2d, in_offset=bidx), writes=[('B2R', j)])

            def xloads(b):
                for st_ in range(NSUB):
                    xj = (b % 2) * NSUB + st_
                    r0 = b * SB + st_ * 128
                    S.dma('sp', lambda e: e.dma_start(out=XS[xj][:], in_=xs_d[r0:r0 + 128, :]), writes=[('XS', xj)])

            gathers(0)
            xloads(0)
            for b in range(NB):
                j = b % 2
                if b + 1 < NB:
                    gathers(b + 1)
                    xloads(b + 1)
                for st_ in range(NSUB):
                    xj = (b % 2) * NSUB + st_
                    pbb = st_ % 2
                    for kc in range(8):
                        S.op('pe', lambda e: e.transpose(psb[pbb][:, kc * 128:(kc + 1) * 128], XS[xj][:, kc::8], identb[:]), reads=[('XS', xj)], writes=[PBB(pbb)])
                    S.op('act', lambda e: e.copy(XST[:, :, st_ * 128:(st_ + 1) * 128], psb[pbb][:].rearrange("p (a b) -> p a b", a=8)),
                         writes=[PBB(pbb), ('XST', st_)])
                xk = [('XST', s_) for s_ in range(NSUB)]
                for jj in range(8):
                    t = ec[0] % 2
                    ec[0] += 1
                    for half in range(2):
                        bank = 2 * t + half
                        for kc in range(8):
                            S.op('pe', lambda e: e.matmul(psum[bank][:, 0:SB], W1[j][:, kc, half * 1024:(half + 1) * 1024][:, jj::8], XST[:, kc, :],
                                                          start=(kc == 0), stop=(kc == 7)), reads=[('W1', j, kc)] + xk, writes=[PB(bank)])
                    S.op('dve', lambda e: e.tensor_scalar(out=tg[t][:], in0=psum[2 * t][:, 0:SB], scalar1=B1C[j][:, jj:jj + 1], scalar2=7.0,
                                                          op0=ALU.add, op1=ALU.min), reads=[('B1C', j)], writes=[PB(2 * t), ('tg', t)])
                    S.op('dve', lambda e: e.tensor_scalar(out=tu[t][:], in0=psum[2 * t + 1][:, 0:SB], scalar1=B1C[j][:, 8 + jj:9 + jj], scalar2=7.0,
                                                          op0=ALU.add, op1=ALU.min), reads=[('B1C', j)], writes=[PB(2 * t + 1), ('tu', t)])
                    S.op('act', lambda e: e.activation(out=sg[t][:], in_=tg[t][:], func=AF.Sigmoid, scale=1.702), reads=[('tg', t)], writes=[('sg', t)])
                    S.op('dve', lambda e: e.tensor_scalar(out=tu[t][:], in0=tu[t][:], scalar1=-7.0, scalar2=1.0, op0=ALU.max, op1=ALU.add), writes=[('tu', t)])
                    S.op('dve', lambda e: e.tensor_tensor(out=tu[t][:], in0=tu[t][:], in1=tg[t][:], op=ALU.mult), reads=[('tg', t)], writes=[('tu', t)])
                    S.op('dve', lambda e: e.tensor_tensor(out=ACTT[:, jj, :], in0=tu[t][:], in1=sg[t][:], op=ALU.mult),
                         reads=[('tu', t), ('sg', t)], writes=[('ACTT', jj)])
                ak = [('ACTT', jj) for jj in range(8)]
                for st_ in range(NSUB):
                    yj = yc[0] % 2
                    yc[0] += 1
                    for nb in range(2):
                        for jj in range(8):
                            S.op('pe', lambda e: e.matmul(psum[4 + nb][:], ACTT[:, jj, st_ * 128:(st_ + 1) * 128], W2[j][:, jj, nb * 512:(nb + 1) * 512],
                                                          start=(jj == 0), stop=(jj == 7)), reads=ak + [('W2', j, jj // 2)], writes=[PB(4 + nb)])
                        cs = slice(nb * 512, (nb + 1) * 512)
                        S.op('dve', lambda e: e.tensor_tensor(out=YT[yj][:, cs], in0=psum[4 + nb][:], in1=B2R[j][:, cs], op=ALU.add), reads=[('B2R', j)],
                             writes=[PB(4 + nb), ('YT', yj, nb)])
                    r0 = b * SB + st_ * 128
                    S.dma('sp', lambda e: e.dma_start(out=ys_d[r0:r0 + 128, :], in_=YT[yj][:]), reads=[('YT', yj, 0), ('YT', yj, 1)], writes=[('ys', b, st_)])
        S.barrier()

    def phase_combine(layer, tiles, final):
        WG, SLOTI = RT["WG"], RT["SLOTI"]
        with contextlib.ExitStack() as es:
            def al(name, shape, dt):
                return es.enter_context(SBT(name, shape, dt))
            G2 = al("G2", [128, 2, D], F32)
            FG = al("FG", [128, D], F32)
            xt = [al("cxt%d" % j, [128, D], F32) for j in range(2)]
            Y = [[al("cY%d_%d" % (j, k), [128, D], F32) for k in range(4)] for j in range(2)]
            acc = [al("cacc%d" % j, [128, D], F32) for j in range(2)]
            sq = al("csq", [128, D], F32)
            st = al("cst", [128, 8], F32)
            for r in range(2):
                bcast_row(G2[:, r, :], modrow[layer, r:r + 1, 5 * D:6 * D], ('G2', r))
            if final:
                bcast_row(FG[:], I("final_g")[0:1, :], 'FG')
            for n, i in enumerate(tiles):
                j = n % 2
                r = 1 if i < 2 else 0
                rows = slice(i * 128, (i + 1) * 128)
                S.dma('sp', lambda e: e.dma_start(out=xt[j][:], in_=xres[rows, :]), writes=[('cxt', j)])
                for k in range(4):
                    off = bass.IndirectOffsetOnAxis(ap=SLOTI[:, i, k:k + 1], axis=0)
                    S.dma('pool', lambda e: e.indirect_dma_start(out=Y[j][k][:], out_offset=None, in_=ys_d, in_offset=off), writes=[('cY', j, k)])
                S.op('dve', lambda e: e.tensor_scalar(out=acc[j][:], in0=Y[j][0][:], scalar1=WG[:, i, 0:1], scalar2=None, op0=ALU.mult),
                     reads=[('cY', j, 0)], writes=[('cacc', j)])
                for k in range(1, 4):
                    S.op('dve', lambda e: e.scalar_tensor_tensor(out=acc[j][:], in0=Y[j][k][:], scalar=WG[:, i, k:k + 1], in1=acc[j][:],
                                                                 op0=ALU.mult, op1=ALU.add), reads=[('cY', j, k)], writes=[('cacc', j)])
                S.op('pool', lambda e: e.tensor_tensor(out=acc[j][:], in0=acc[j][:], in1=G2[:, r, :], op=ALU.mult), reads=[('G2', r)], writes=[('cacc', j)])
                S.op('pool', lambda e: e.tensor_tensor(out=acc[j][:], in0=acc[j][:], in1=xt[j][:], op=ALU.add), reads=[('cxt', j)], writes=[('cacc', j)])
                if not final:
                    S.dma('act', lambda e: e.dma_start(out=xres[rows, :], in_=acc[j][:]), reads=[('cacc', j)], writes=[('xres', i)])
                else:
                    S.op('act', lambda e: e.activation(out=sq[:], in_=acc[j][:], func=AF.Square, accum_out=st[:, 0:1]), reads=[('cacc', j)], writes=['csq', 'cst'])
                    S.op('dve', lambda e: e.tensor_scalar(out=st[:, 1:2], in0=st[:, 0:1], scalar1=1.0 / D, scalar2=EPS, op0=ALU.mult, op1=ALU.add), writes=['cst'])
                    S.op('act', lambda e: e.activation(out=st[:, 2:3], in_=st[:, 1:2], func=AF.Sqrt), writes=['cst'])
                    S.op('dve', lambda e: e.reciprocal(st[:, 3:4], st[:, 2:3]), writes=['cst'])
                    S.op('dve', lambda e: e.scalar_tensor_tensor(out=acc[j][:], in0=acc[j][:], scalar=st[:, 3:4], in1=FG[:], op0=ALU.mult, op1=ALU.mult),
                         reads=['FG'], writes=[('cacc', j), 'cst'])
                    S.dma('act', lambda e: e.dma_start(out=out_d[(i - 2) * 128:(i - 1) * 128, :], in_=acc[j][:]), reads=[('cacc', j)], writes=[('out', i)])
        S.barrier()

    qT_d = dscr("qT_d", [D, 4096], BF16)
    kT_d = dscr("kT_d", [256, T], BF16)
    vA_d = dscr("vA_d", [T, 256], BF16)
    o_d = dscr("o_d", [4096, D], BF16)

    def phase_A1(uT):
        wv = I("od_w_in").rearrange("(kc p) n -> p kc n", p=128)
        with contextlib.ExitStack() as es:
            def al(name, shape, dt):
                return es.enter_context(SBT(name, shape, dt))
            W = al("aW", [128, 8, 1536], BF16)
            GQ = al("aGQ", [128, 128], F32)
            GK = al("aGK", [128, 128], F32)
            COS = [al("aCOS%d" % j, [128, 128], F32) for j in range(2)]
            SIN = [al("aSIN%d" % j, [128, 128], F32) for j in range(2)]
            xf = [al("axf%d" % j, [128, 1536], F32) for j in range(2)]
            sq = al("asq", [128, 1280], F32)
            st = al("ast", [128, 40], F32)
            kn = al("akn", [128, 1280], F32)
            t1 = al("at1", [128, 1280], F32)
            t2 = al("at2", [128, 1280], F32)
            kr = [al("akr%d" % j, [128, 1280], BF16) for j in range(2)]
            vb = [al("avb%d" % j, [128, 256], BF16) for j in range(2)]
            xT = [al("axT%d" % j, [128, 10, 128], BF16) for j in range(2)]
            for blk in range(3):
                S.dma('pool', lambda e: e.dma_start(out=W[:, :, blk * 512:(blk + 1) * 512], in_=wv[:, :, blk * 512:(blk + 1) * 512]), writes=[('aW', blk)])
            bcast_row(GQ[:], I("od_qk_g")[0:1, :], 'aGQ')
            bcast_row(GK[:], I("od_qk_g")[1:2, :], 'aGK')
            for i in range(NT):
                lat = i >= 2
                j = i % 2
                rows = slice(i * 128, (i + 1) * 128)
                blks = [0, 1, 2] if lat else [2]
                h0 = 0 if lat else 8
                if lat:
                    tr = slice((i - 2) * 128, (i - 1) * 128)
                    S.dma('pool', lambda e: e.dma_start(out=COS[j][:], in_=I("rope_cos")[tr, :]), writes=[('aCOS', j)])
                    S.dma('pool', lambda e: e.dma_start(out=SIN[j][:], in_=I("rope_sin")[tr, :]), writes=[('aSIN', j)])
                for blk in blks:
                    pb = blk
                    for kc in range(8):
                        S.op('pe', lambda e: e.matmul(psum[pb][:], uT[:, kc, rows], W[:, kc, blk * 512:(blk + 1) * 512], start=(kc == 0), stop=(kc == 7)),
                             reads=[('aW', blk)], writes=[PB(pb)])
                    S.op('act', lambda e: e.copy(xf[j][:, blk * 512:(blk + 1) * 512], psum[pb][:]), writes=[PB(pb), ('axf', j, blk)])
                S.op('pool', lambda e: e.tensor_copy(vb[j][:], xf[j][:, 1280:1536]), reads=[('axf', j, 2)], writes=[('avb', j)])
                S.dma('sp', lambda e: e.dma_start(out=vA_d[rows, :], in_=vb[j][:]), reads=[('avb', j)], writes=[('vA', i)])
                c0 = h0 * 128
                nh = 10 - h0
                S.op('act', lambda e: e.activation(out=sq[:, c0:1280], in_=xf[j][:, c0:1280], func=AF.Square),
                     reads=[('axf', j, 0), ('axf', j, 1), ('axf', j, 2)], writes=['asq'])
                S.op('dve', lambda e: e.tensor_reduce(out=st[:, h0:10], in_=sq[:, c0:1280].rearrange("p (h d) -> p h d", d=128), axis=AX.X, op=ALU.add),
                     reads=['asq'], writes=['ast'])
                S.op('dve', lambda e: e.tensor_scalar(out=st[:, 10 + h0:20], in0=st[:, h0:10], scalar1=1.0 / 128, scalar2=EPS, op0=ALU.mult, op1=ALU.add), writes=['ast'])
                S.op('act', lambda e: e.activation(out=st[:, 20 + h0:30], in_=st[:, 10 + h0:20], func=AF.Sqrt), writes=['ast'])
                S.op('dve', lambda e: e.reciprocal(st[:, 30 + h0:40], st[:, 20 + h0:30]), writes=['ast'])
                for h in range(h0, 10):
                    hs = slice(h * 128, (h + 1) * 128)
                    G = GQ if h < 8 else GK
                    eng = 'dve' if h % 2 == 0 else 'pool'
                    if lat:
                        S.op('dve', lambda e: e.scalar_tensor_tensor(out=kn[:, hs], in0=xf[j][:, hs], scalar=st[:, 30 + h:31 + h], in1=G[:], op0=ALU.mult, op1=ALU.mult),
                             reads=['aGQ', 'aGK', 'ast'], writes=[('akn', h)])
                        knv = kn[:, hs].rearrange("p (b c) -> p b c", b=2)
                        t2v = t2[:, hs].rearrange("p (b c) -> p b c", b=2)
                        snv = SIN[j][:].rearrange("p (b c) -> p b c", b=2)
                        S.op(eng, lambda e: e.tensor_tensor(out=t1[:, hs], in0=kn[:, hs], in1=COS[j][:], op=ALU.mult), reads=[('akn', h), ('aCOS', j)], writes=[('at1', h)])
                        S.op('pool', lambda e: e.tensor_tensor(out=t2v[:, :, 0:32], in0=knv[:, :, 32:64], in1=snv[:, :, 0:32], op=ALU.mult),
                             reads=[('akn', h), ('aSIN', j)], writes=[('at2', h)])
                        S.op('pool', lambda e: e.tensor_tensor(out=t2v[:, :, 32:64], in0=knv[:, :, 0:32], in1=snv[:, :, 32:64], op=ALU.mult),
                             reads=[('akn', h), ('aSIN', j)], writes=[('at2', h)])
                        S.op('dve', lambda e: e.tensor_tensor(out=kr[j][:, hs], in0=t1[:, hs], in1=t2[:, hs], op=ALU.add), reads=[('at1', h), ('at2', h)],
                             writes=[('akr', j, h)])
                    else:
                        S.op('dve', lambda e: e.scalar_tensor_tensor(out=kr[j][:, hs], in0=xf[j][:, hs], scalar=st[:, 30 + h:31 + h], in1=G[:], op0=ALU.mult, op1=ALU.mult),
                             reads=['aGQ', 'aGK', 'ast'], writes=[('akr', j, h)])
                if lat:
                    for h in range(8):
                        S.op('pe', lambda e: e.transpose(psb[0][:, h * 128:(h + 1) * 128], kr[j][:, h * 128:(h + 1) * 128], identb[:]),
                             reads=[('akr', j, h)], writes=[PBB(0)])
                    S.op('act', lambda e: e.copy(xT[j][:, 0:8, :].rearrange("p a b -> p (a b)"), psb[0][:]), writes=[PBB(0), ('axT', j, 0)])
                    S.dma('sp', lambda e: e.dma_start(out=qT_d[:, (i - 2) * 128:(i - 1) * 128].rearrange("(h d) t -> d h t", d=128), in_=xT[j][:, 0:8, :]),
                          reads=[('axT', j, 0)], writes=[('qT_d', i)])
                for h in range(8, 10):
                    S.op('pe', lambda e: e.transpose(psb[1][:, (h - 8) * 128:(h - 7) * 128], kr[j][:, h * 128:(h + 1) * 128], identb[:]),
                         reads=[('akr', j, h)], writes=[PBB(1)])
                S.op('act', lambda e: e.copy(xT[j][:, 8:10, :].rearrange("p a b -> p (a b)"), psb[1][:, 0:256]), writes=[PBB(1), ('axT', j, 1)])
                S.dma('sp', lambda e: e.dma_start(out=kT_d[:, rows].rearrange("(h d) t -> d h t", d=128), in_=xT[j][:, 8:10, :]),
                      reads=[('axT', j, 1)], writes=[('kT_d', i)])
        S.barrier()

    def phase_A2():
        scale = 128.0 ** -0.5
        with contextlib.ExitStack() as es:
            def al(name, shape, dt):
                return es.enter_context(SBT(name, shape, dt))
            KT = al("bKT", [128, T], BF16)
            V = al("bV", [128, NT, 129], BF16)
            QT4 = [al("bQT%d" % j, [128, 4, 128], BF16) for j in range(2)]
            PT = [al("bPT%d" % j, [128, 512], BF16) for j in range(3)]
            ot = [al("bot%d" % j, [128, 512], BF16) for j in range(2)]
            sm = al("bsm", [128, 8], F32)
            pc = 0
            for g in range(2):
                S.dma('sp', lambda e: e.dma_start(out=KT[:], in_=kT_d[g * 128:(g + 1) * 128, :]), writes=['bKT'])
                S.dma('sp', lambda e: e.dma_start(out=V[:, :, 0:128], in_=vA_d[:, g * 128:(g + 1) * 128].rearrange("(n p) d -> p n d", p=128)), writes=['bV'])
                S.op('pool', lambda e: e.memset(V[:, :, 128:129], 1.0), writes=['bV1'])
                for qi in range(32):
                    j = qi % 2
                    S.dma('sp', lambda e: e.dma_start(out=QT4[j][:], in_=qT_d[g * 512:(g + 1) * 512, qi * 128:(qi + 1) * 128].rearrange("(h d) t -> d h t", d=128)),
                          writes=[('bQT', j)])
                    def st_mm(kt):
                        bank = kt % 2
                        S.op('pe', lambda e: e.matmul(psum[bank][:], KT[:, kt * 128:(kt + 1) * 128], QT4[j][:].rearrange("p a b -> p (a b)"), start=True, stop=True),
                             reads=['bKT', ('bQT', j)], writes=[PB(bank)])
                    st_mm(0)
                    for kt in range(NT):
                        bank = kt % 2
                        p_ = pc % 3
                        pc += 1
                        S.op('act', lambda e: e.activation(out=PT[p_][:], in_=psum[bank][:], func=AF.Exp, scale=scale), writes=[PB(bank), ('bPT', p_)])
                        if kt + 1 < NT:
                            st_mm(kt + 1)
                        for hh in range(4):
                            S.op('pe', lambda e: e.matmul(psum[2 + hh][:, 0:129], PT[p_][:, hh * 128:(hh + 1) * 128], V[:, kt, :], start=(kt == 0), stop=(kt == NT - 1)),
                                 reads=[('bPT', p_), 'bV', 'bV1'], writes=[PB(2 + hh)])
                    for hh in range(4):
                        S.op('dve', lambda e: e.reciprocal(sm[:, hh:hh + 1], psum[2 + hh][:, 128:129]), writes=[PB(2 + hh), 'bsm'])
                        S.op('act', lambda e: e.activation(out=ot[j][:, hh * 128:(hh + 1) * 128], in_=psum[2 + hh][:, 0:128], func=AF.Identity, scale=sm[:, hh:hh + 1]),
                             reads=['bsm'], writes=[PB(2 + hh), ('bot', j)])
                    S.dma('sp', lambda e: e.dma_start(out=o_d[qi * 128:(qi + 1) * 128, g * 512:(g + 1) * 512], in_=ot[j][:]), reads=[('bot', j)], writes=[('o_d', g, qi)])
        S.barrier()

    def phase_A3():
        with contextlib.ExitStack() as es:
            def al(name, shape, dt):
                return es.enter_context(SBT(name, shape, dt))
            WO = al("cWO", [128, 8, D], BF16)
            G1 = al("cG1", [128, D], F32)
            ob = [al("cob%d" % j, [128, D], BF16) for j in range(2)]
            OT = [al("cOT%d" % j, [128, 8, 128], BF16) for j in range(2)]
            xt = [al("cx%d" % j, [128, D], F32) for j in range(2)]
            tmp = al("ctmp", [128, D], F32)
            S.dma('pool', lambda e: e.dma_start(out=WO[:], in_=I("od_w_out").rearrange("(kc p) n -> p kc n", p=128)), writes=['cWO'])
            bcast_row(G1[:], modrow[1, 0:1, 2 * D:3 * D], 'cG1')
            for qi in range(32):
                j = qi % 2
                rows = slice((qi + 2) * 128, (qi + 3) * 128)
                S.dma('sp', lambda e: e.dma_start(out=ob[j][:], in_=o_d[qi * 128:(qi + 1) * 128, :]), writes=[('cob', j)])
                S.dma('sp', lambda e: e.dma_start(out=xt[j][:], in_=xres[rows, :]), writes=[('cx', j)])
                for kc in range(8):
                    S.op('pe', lambda e: e.transpose(psb[j][:, kc * 128:(kc + 1) * 128], ob[j][:, kc * 128:(kc + 1) * 128], identb[:]), reads=[('cob', j)], writes=[PBB(j)])
                S.op('act', lambda e: e.copy(OT[j][:].rearrange("p a b -> p (a b)"), psb[j][:]), writes=[PBB(j), ('cOT', j)])
                for nb in range(2):
                    pb = (2 * qi + nb) % 4
                    for kc in range(8):
                        S.op('pe', lambda e: e.matmul(psum[pb][:], OT[j][:, kc, :], WO[:, kc, nb * 512:(nb + 1) * 512], start=(kc == 0), stop=(kc == 7)),
                             reads=[('cOT', j), 'cWO'], writes=[PB(pb)])
                    cs = slice(nb * 512, (nb + 1) * 512)
                    S.op('dve', lambda e: e.tensor_tensor(out=tmp[:, cs], in0=psum[pb][:], in1=G1[:, cs], op=ALU.mult), reads=['cG1'], writes=[PB(pb), ('ctmp', nb)])
                    S.op('pool', lambda e: e.tensor_tensor(out=tmp[:, cs], in0=tmp[:, cs], in1=xt[j][:, cs], op=ALU.add), reads=[('cx', j)], writes=[('ctmp', nb)])
                S.dma('pool', lambda e: e.dma_start(out=xres[rows, :], in_=tmp[:]), reads=[('ctmp', 0), ('ctmp', 1)], writes=[('xres', qi)])
        S.barrier()

    def small_dump():
        W_ = NT * 32 + NT * 4 + NT * 4 + 2 * NBMAX
        dl = dscr("dsmall_scr", [128, W_], F32)
        with SBT("dsm", [128, W_], F32) as dsm:
            o = 0
            for nm, n in (("LOG", NT * 32), ("WG", NT * 4), ("SLOTI", NT * 4)):
                S.op('dve', lambda e: e.tensor_copy(dsm[:, o:o + n], RT[nm][:].rearrange("p a b -> p (a b)")), writes=['dsm'])
                o += n
            for nm in ("IDXI", "BIDX"):
                S.op('dve', lambda e: e.tensor_copy(dsm[:, o:o + NBMAX], RT[nm][:]), writes=['dsm'])
                o += NBMAX
            S.dma('sp', lambda e: e.dma_start(out=dl, in_=dsm[:]), reads=['dsm'], writes=['dl'])
            S.barrier()
        return ('small', dl, [128, W_], F32)

    def copy_xin_to_xres():
        with SBT("cpx", [128, D], F32) as cpx:
            for i in range(NT):
                S.dma('sp', lambda e: e.dma_start(out=cpx[:], in_=I("xin")[i * 128:(i + 1) * 128, :]), writes=['cpx'])
                S.dma('sp', lambda e: e.dma_start(out=xres[i * 128:(i + 1) * 128, :], in_=cpx[:]), reads=['cpx'], writes=[('xres', i)])
        S.barrier()

    phase_mod()
    if stop_after == 'mod':
        return finish_dbg([('modrow', modrow.rearrange("a b c -> (a b) c"), [4, 6 * D], F32)])

    if stop_after not in ('A_only', 'M1_only'):
        uT_guard = SBT("uT", [128, 8, T], BF16)
        uT = uT_guard.__enter__()
        phase_norm_T(0, I("xin"), uT)
        phase_E1(uT)
        if stop_after == 'E1':
            return finish_dbg([('qkT', qkT, [2048, T], BF16), ('v_tm', v_tm, [T, D], BF16), ('so_tm', so_tm, [T, D], BF16),
                               ('g_tm', g_tm, [T, 16], F32), ('xrT', xrT, [D, T], F32), ('ygT', ygT, [D, T], BF16)])
        uT_guard.__exit__(None, None, None)
        phase_E2()
        if stop_after == 'E2':
            return finish_dbg([('hlT', hlT, [D, T], BF16)])
        phase_E3()
        if stop_after == 'E3':
            return finish_dbg([('hm_f', hm_d[0], [T, D], F32), ('hm_b', hm_d[1], [T, D], F32)])
        phase_E4()
        if stop_after == 'E4':
            return finish_dbg([('xres', xres, [T, D], F32)])
        NB0 = (4 * T) // SB + 32
        phase_N2(0, range(NT), RT["LOG"])
        phase_route(range(NT), NB0)
        phase_scatter(range(NT), NB0)
        if stop_after == 'R0':
            return finish_dbg([small_dump(), ('xs', xs_d, [NBMAX * SB, D], BF16), ('v2', v2_tm, [T, D], BF16)])
        phase_moe(0, NB0)
        phase_combine(0, range(NT), False)
        if stop_after == 'M0':
            return finish_dbg([('xres', xres, [T, D], F32)])
    else:
        copy_xin_to_xres()

    if stop_after != 'M1_only':
        uT_guard = SBT("uT", [128, 8, T], BF16)
        uT = uT_guard.__enter__()
        phase_norm_T(1, xres, uT)
        phase_A1(uT)
        uT_guard.__exit__(None, None, None)
        phase_A2()
        phase_A3()
        if stop_after in ('A', 'A_only'):
            return finish_dbg([('xres', xres, [T, D], F32), ('o_d', o_d, [4096, D], BF16)])
    NB1 = (4 * 4096) // SB + 32
    lat_tiles = range(2, NT)
    phase_N2(1, lat_tiles, RT["LOG"])
    phase_route(lat_tiles, NB1)
    phase_scatter(lat_tiles, NB1)
    phase_moe(1, NB1)
    phase_combine(1, lat_tiles, True)
    if stop_after == 'M1_only':
        return finish_dbg([('outc', out_d, [4096, D], F32)])
    S.barrier()
    return nc, dbg_out, used_inputs


def host_inputs(b, inp, names=None):
    f = lambda a: np.ascontiguousarray(a, dtype=np.float32)
    want = lambda k: names is None or k in names
    m = {}
    if want("xin"):
        m["xin"] = f(np.concatenate([inp["ctx"][b], inp["x"][b]], axis=0))
    if want("ccols"):
        m["ccols"] = f(np.concatenate([inp["c"][b].reshape(8, 128).T, inp["c_ctx"].reshape(8, 128).T], axis=1))
    for k in ["mod_w", "mod_b", "norm1_g", "norm2_g", "moe_w_r", "moe_b_r"]:
        if want(k):
            m[k] = f(inp[k])
    if want("final_g"):
        m["final_g"] = f(inp["final_g"].reshape(1, D))
    if want("ev_w_in"):
        m["ev_w_in"] = f(inp["ev_w_in"][0])
    if want("ev_qkcw"):
        qk = np.concatenate([inp["ev_qk_conv_w"][0], inp["ev_qk_conv_b"][0][None]], axis=0)
        m["ev_qkcw"] = f(qk.T.reshape(16, 128, 5).transpose(1, 0, 2))
    if want("ev_gate_b"):
        m["ev_gate_b"] = f(inp["ev_gate_b"][0].reshape(1, 16))
    if want("ev_mnorm_g"):
        m["ev_mnorm_g"] = f(inp["ev_mnorm_g"][0].reshape(1, D))
    if want("ev_lrucw"):
        lc = np.concatenate([inp["ev_lru_conv_w"][0], inp["ev_lru_conv_b"][0][None]], axis=0)
        m["ev_lrucw"] = f(lc.T.reshape(8, 128, 5).transpose(1, 0, 2))
    if want("ev_lru_w") or want("ev_lru_b"):
        lw = np.zeros((4, 8, 128, 128), np.float32)
        lb = np.zeros((128, 8, 4), np.float32)
        for z in range(2):
            for gi, (wk, bk) in enumerate([("ev_lru_wa", "ev_lru_ba"), ("ev_lru_wx", "ev_lru_bx")]):
                for cc_ in range(8):
                    for h in range(2):
                        n = cc_ * 2 + h
                        lw[z * 2 + gi, cc_, h * 64:(h + 1) * 64, h * 64:(h + 1) * 64] = inp[wk][0, z, n]
                        lb[h * 64:(h + 1) * 64, cc_, z * 2 + gi] = inp[bk][0, z, n]
        m["ev_lru_w"] = lw
        m["ev_lru_b"] = lb
    if want("ev_lru_lam"):
        m["ev_lru_lam"] = f(inp["ev_lru_lam"][0].reshape(2, 8, 128).transpose(2, 1, 0))
    if want("ev_w_out"):
        m["ev_w_out"] = f(inp["ev_w_out"][0])
    if want("od_w_in"):
        m["od_w_in"] = f(inp["od_w_in"][0])
    if want("od_qk_g"):
        m["od_qk_g"] = f(np.stack([inp["od_q_norm_g"][0], inp["od_k_norm_g"][0]]))
    if want("od_w_out"):
        m["od_w_out"] = f(inp["od_w_out"][0])
    if want("rope_cos") or want("rope_sin"):
        pos = np.arange(4096)
        row = (pos // 64).astype(np.float32)
        col = (pos % 64).astype(np.float32)
        inv = (10000.0 ** (-np.arange(0, 64, 2, dtype=np.float32) / 64)).astype(np.float32)
        ar = row[:, None] * inv[None]
        ac = col[:, None] * inv[None]
        m["rope_cos"] = f(np.concatenate([np.cos(ar), np.cos(ar), np.cos(ac), np.cos(ac)], axis=1))
        m["rope_sin"] = f(np.concatenate([-np.sin(ar), np.sin(ar), -np.sin(ac), np.sin(ac)], axis=1))
    for l in range(2):
        if want("moe_w1_%d" % l):
            m["moe_w1_%d" % l] = f(inp["moe_w1"][l]).reshape(32 * 128, 8, 2048)
        if want("moe_w2_%d" % l):
            m["moe_w2_%d" % l] = f(inp["moe_w2"][l]).reshape(32 * 128, 8, 1024)
    for l in range(2):
        if want("moe_b1_%d" % l):
            m["moe_b1_%d" % l] = f(inp["moe_b1"][l].reshape(32, 2, 128, 8).transpose(0, 2, 1, 3).reshape(32 * 128, 16))
        if want("moe_b2_%d" % l):
            m["moe_b2_%d" % l] = f(inp["moe_b2"][l])
    if names is not None:
        m = {k: v for k, v in m.items() if k in names}
    return m


_CACHE = {}


def kernel(**inputs):
    if 'nc' not in _CACHE:
        _CACHE['nc'] = build()
    nc, _, used = _CACHE['nc']
    names = set(used.keys())
    in_maps = [host_inputs(b, inputs, names) for b in range(8)]
    res = run_bass_kernel_spmd(nc, in_maps, core_ids=list(range(8)))
    return np.stack([r["out"] for r in res.results], axis=0).astype(np.float32)
```

```python
import contextlib
import numpy as np
import concourse.bass as bass
import concourse.mybir as mybir
from concourse.bass_utils import run_bass_kernel_spmd

F32 = mybir.dt.float32
BF16 = mybir.dt.bfloat16
I32 = mybir.dt.int32
U32 = mybir.dt.uint32
ALU = mybir.AluOpType
AF = mybir.ActivationFunctionType

T = 4352
NT = 34
D = 1024
NCTX = 256
EPS = 1e-6


class Sched:
    NSLOT = 6

    def __init__(self, nc):
        self.nc = nc
        self.engs = {'pe': nc.tensor, 'act': nc.scalar, 'dve': nc.vector,
                     'pool': nc.gpsimd, 'sp': nc.sync}
        self.sem = {}
        self.cnt = {}
        for k in self.engs:
            self.sem[k] = nc.alloc_semaphore('s_' + k)
            self.cnt[k] = 0
        self.dslots = {}
        self.dcnt = {}
        self.dnext = {}
        self.nslot = {'sp': 8, 'pool': 8, 'act': 2}
        for q in ('sp', 'pool', 'act'):
            self.dslots[q] = [nc.alloc_semaphore('d_%s%d' % (q, i)) for i in range(self.nslot[q])]
            self.dcnt[q] = [0] * self.nslot[q]
            self.dnext[q] = 0
        self.waited = {k: {} for k in self.engs}
        self.res = {}

    def _semof(self, tok):
        if tok[0] == 'e':
            return self.sem[tok[1]]
        return self.dslots[tok[1][0]][tok[1][1]]

    def _wait(self, e, tok):
        key = (tok[0], tok[1])
        if self.waited[e].get(key, 0) >= tok[2]:
            return
        self.engs[e].wait_ge(self._semof(tok), tok[2])
        self.waited[e][key] = tok[2]

    def _deps(self, e, reads, writes):
        deps = []
        for r in reads:
            st = self.res.get(r)
            if st and st['w'] is not None:
                deps.append(st['w'])
        for w in writes:
            st = self.res.get(w)
            if st:
                if st['w'] is not None:
                    deps.append(st['w'])
                deps.extend(st['r'].values())
        for tok in deps:
            if e == 'pe' and tok[0] == 'e' and tok[1] == 'pe':
                continue
            self._wait(e, tok)

    def _commit(self, tok, reads, writes):
        for r in reads:
            st = self.res.setdefault(r, {'w': None, 'r': {}})
            st['r'][(tok[0], tok[1])] = tok
        for w in writes:
            self.res[w] = {'w': tok, 'r': {}}

    def op(self, e, fn, reads=(), writes=()):
        self._deps(e, reads, writes)
        inst = fn(self.engs[e])
        self.cnt[e] += 1
        inst.then_inc(self.sem[e], 1)
        self._commit(('e', e, self.cnt[e]), reads, writes)
        return inst

    def dma(self, q, fn, reads=(), writes=()):
        s = self.dnext[q]
        self.dnext[q] = (s + 1) % self.nslot[q]
        if self.dcnt[q][s] > 0:
            self._wait(q, ('d', (q, s), 16 * self.dcnt[q][s]))
        self._deps(q, reads, writes)
        inst = fn(self.engs[q])
        self.dcnt[q][s] += 1
        inst.then_inc(self.dslots[q][s], 16)
        self._commit(('d', (q, s), 16 * self.dcnt[q][s]), reads, writes)
        return inst

    def barrier(self):
        toks = []
        for q in self.dslots:
            for s in range(self.nslot[q]):
                if self.dcnt[q][s] > 0:
                    toks.append(('d', (q, s), 16 * self.dcnt[q][s]))
        for k in self.engs:
            if self.cnt[k] > 0:
                toks.append(('e', k, self.cnt[k]))
        for e in self.engs:
            for tok in toks:
                self._wait(e, tok)
        self.res = {}


class Rot:
    def __init__(self, bufs, name):
        self.bufs = bufs
        self.name = name
        self.i = 0

    def next(self):
        b = self.bufs[self.i % len(self.bufs)]
        k = (self.name, self.i % len(self.bufs))
        self.i += 1
        return b, k


AX = mybir.AxisListType


def build(stop_after=None, layers=(0, 1)):
    nc = bass.Bass("TRN2", target_bir_lowering=False)
    S = Sched(nc)
    used_inputs = {}
    _ctr = [0]

    def SBT(name, shape, dt):
        _ctr[0] += 1
        return nc.sbuf_tensor("%s_u%d" % (name, _ctr[0]), shape, dt)

    IN_SPECS = {
        "xin": [T, D], "ccols": [128, 16], "mod_w": [2, D, 6 * D], "mod_b": [2, 6 * D],
        "norm1_g": [2, D], "norm2_g": [2, D], "final_g": [1, D],
        "ev_w_in": [D, 6160], "ev_qkcw": [128, 16, 5], "ev_gate_b": [1, 16], "ev_mnorm_g": [1, D],
        "ev_lrucw": [128, 8, 5], "ev_lru_w": [4, 8, 128, 128], "ev_lru_b": [128, 8, 4], "ev_lru_lam": [128, 8, 2],
        "ev_w_out": [2 * D, D], "od_w_in": [D, 1536], "od_qk_g": [2, 128], "od_w_out": [D, D],
        "rope_cos": [4096, 128], "rope_sin": [4096, 128],
        "moe_w_r": [2, D, 32], "moe_b_r": [2, 32],
        "moe_w1_0": [32 * 128, 8, 2048], "moe_w1_1": [32 * 128, 8, 2048],
        "moe_b1_0": [32 * 128, 16], "moe_b1_1": [32 * 128, 16],
        "moe_w2_0": [32 * 128, 8, 1024], "moe_w2_1": [32 * 128, 8, 1024],
        "moe_b2_0": [32, 1024], "moe_b2_1": [32, 1024],
    }

    def I(name):
        if name not in used_inputs:
            used_inputs[name] = nc.dram_tensor(name, list(IN_SPECS[name]), F32, kind="ExternalInput").ap()
        return used_inputs[name]

    def dscr(name, shape, dt=F32):
        return nc.dram_tensor(name, list(shape), dt, kind="Internal").ap()

    dbg_out = {}

    def dbgt(name, shape, dt=F32):
        dbg_out[name] = nc.dram_tensor("dbg_" + name, list(shape), dt, kind="ExternalOutput").ap()
        return dbg_out[name]

    def finish_dbg(pairs):
        S.barrier()
        for name, src, shape, dt in pairs:
            d = dbgt(name, shape, dt)
            rows, cols = shape[0], int(np.prod(shape[1:]))
            s2 = src if len(shape) == 2 else src.rearrange("a b c -> a (b c)")
            d2 = d if len(shape) == 2 else d.rearrange("a b c -> a (b c)")
            with SBT("dbgbuf_" + name, [128, cols], dt) as buf:
                for r0 in range(0, rows, 128):
                    n = min(128, rows - r0)
                    S.dma('sp', lambda e: e.dma_start(out=buf[0:n, :], in_=s2[r0:r0 + n, :]), writes=['dbgbuf'])
                    S.dma('sp', lambda e: e.dma_start(out=d2[r0:r0 + n, :], in_=buf[0:n, :]), reads=['dbgbuf'], writes=[('dbgo', name, r0)])
                S.barrier()
        return nc, dbg_out, used_inputs

    out_d = nc.dram_tensor("out", [4096, D], F32, kind="ExternalOutput").ap()
    modrow = dscr("modrow", [2, 2, 6 * D])
    xres = dscr("xres", [T, D])

    ident = nc.alloc_sbuf_tensor("ident", [128, 128], F32)
    identb = nc.alloc_sbuf_tensor("identb", [128, 128], BF16)
    ones1 = nc.alloc_sbuf_tensor("ones1", [1, 128], F32)
    onesm = nc.alloc_sbuf_tensor("onesm", [128, 128], F32)
    triF = nc.alloc_sbuf_tensor("triF", [128, 128], F32)
    triB = nc.alloc_sbuf_tensor("triB", [128, 128], F32)
    triS = nc.alloc_sbuf_tensor("triS", [128, 128], F32)
    selF = nc.alloc_sbuf_tensor("selF", [128, 128], F32)
    selB = nc.alloc_sbuf_tensor("selB", [128, 128], F32)
    pidx = nc.alloc_sbuf_tensor("pidx", [128, 1], F32)
    pidx_i = nc.alloc_sbuf_tensor("pidx_i", [128, 1], I32)
    S.op('pool', lambda e: e.memset(ident[:], 0.0), writes=['ident'])
    S.op('pool', lambda e: e.affine_select(ident[:], ident[:], pattern=[[-1, 128]], compare_op=ALU.not_equal,
                                          fill=1.0, base=0, channel_multiplier=1), reads=['ident'], writes=['ident'])
    S.op('dve', lambda e: e.tensor_copy(identb[:], ident[:]), reads=['ident'], writes=['identb'])
    S.op('pool', lambda e: e.memset(ones1[:], 1.0), writes=['ones1'])
    S.op('pool', lambda e: e.memset(onesm[:], 1.0), writes=['onesm'])

    def mk_mask(t, pattern, cm, base, cmp):
        S.op('pool', lambda e: e.memset(t[:], 1.0), writes=[t.name])
        S.op('pool', lambda e: e.affine_select(t[:], t[:], pattern=pattern, compare_op=cmp, fill=0.0, base=base,
                                              channel_multiplier=cm), writes=[t.name])

    mk_mask(triF, [[1, 128]], -1, 0, ALU.is_ge)
    mk_mask(triB, [[-1, 128]], 1, 0, ALU.is_ge)
    mk_mask(triS, [[1, 128]], -1, 0, ALU.is_gt)
    mk_mask(selF, [[0, 128]], 1, -127, ALU.is_equal)
    mk_mask(selB, [[0, 128]], 1, 0, ALU.is_equal)
    S.op('pool', lambda e: e.iota(pidx_i[:], pattern=[[0, 1]], base=0, channel_multiplier=1), writes=['pidx_i'])
    S.op('dve', lambda e: e.tensor_copy(pidx[:], pidx_i[:]), reads=['pidx_i'], writes=['pidx'])

    psum = [nc.alloc_psum_tensor("ps%d" % i, [128, 512], F32) for i in range(6)]
    psb = [nc.alloc_psum_tensor("psb%d" % i, [128, 1024], BF16) for i in range(2)]

    def PB(i):
        return ('psum', i)

    def PBB(i):
        return ('psb', i)

    def bcast_row(dst, src_row, key, q='sp'):
        S.dma(q, lambda e: e.dma_start(out=dst, in_=src_row.partition_broadcast(dst.shape[0])), writes=[key])

    def phase_mod():
        mod_w, mod_b = I("mod_w"), I("mod_b")
        with SBT("cc", [128, 16], F32) as cc, SBT("csil", [128, 16], F32) as csil, \
                SBT("mw0", [128, 8, 512], F32) as mw0, SBT("mw1", [128, 8, 512], F32) as mw1, \
                SBT("mb0", [1, 512], F32) as mb0, SBT("mb1", [1, 512], F32) as mb1, \
                SBT("mo0", [1, 512], F32) as mo0, SBT("mo1", [1, 512], F32) as mo1:
            S.dma('sp', lambda e: e.dma_start(out=cc[:], in_=I("ccols")), writes=['cc'])
            S.op('act', lambda e: e.activation(out=csil[:], in_=cc[:], func=AF.Silu), reads=['cc'], writes=['csil'])
            mws = Rot([mw0, mw1], 'mw')
            mbs = Rot([mb0, mb1], 'mb')
            mos = Rot([mo0, mo1], 'mo')
            pi = 0
            for layer in range(2):
                mwv = mod_w[layer].rearrange("(kc p) n -> p kc n", p=128)
                for cb in range(12):
                    mw, mwk = mws.next()
                    mb, mbk = mbs.next()
                    S.dma('sp', lambda e: e.dma_start(out=mw[:], in_=mwv[:, :, cb * 512:(cb + 1) * 512]), writes=[mwk])
                    S.dma('sp', lambda e: e.dma_start(out=mb[:], in_=mod_b[layer:layer + 1, cb * 512:(cb + 1) * 512]), writes=[mbk])
                    for r in range(2):
                        ps = psum[pi % 2]
                        pk = PB(pi % 2)
                        pi += 1
                        for kc in range(8):
                            S.op('pe', lambda e: e.matmul(ps[0:1, :], csil[:, r * 8 + kc:r * 8 + kc + 1], mw[:, kc, :],
                                                          start=(kc == 0), stop=False), reads=['csil', mwk], writes=[pk])
                        S.op('pe', lambda e: e.matmul(ps[0:1, :], ones1[0:1, 0:1], mb[:], start=False, stop=True),
                             reads=['ones1', mbk], writes=[pk])
                        mo, mok = mos.next()
                        S.op('act', lambda e: e.copy(mo[:], ps[0:1, :]), writes=[pk, mok])
                        S.dma('sp', lambda e: e.dma_start(out=modrow[layer, r:r + 1, cb * 512:(cb + 1) * 512], in_=mo[:]),
                              reads=[mok], writes=[('modrow', layer, r, cb)])
        S.barrier()

    def norm_tiles(layer, which, src, tiles, consume):
        gsrc = I("norm1_g") if which == 0 else I("norm2_g")
        sh_off = 0 if which == 0 else 3 * D
        sc_off = D if which == 0 else 4 * D
        with SBT("nA", [128, 2, D], F32) as A, SBT("nSH", [128, 2, D], F32) as SH, \
                SBT("nG", [128, D], F32) as G, \
                SBT("nx0", [128, D], F32) as x0, SBT("nx1", [128, D], F32) as x1, \
                SBT("nu0", [128, D], F32) as u0, SBT("nu1", [128, D], F32) as u1, \
                SBT("nsq", [128, D], F32) as sq, SBT("nst", [128, 8], F32) as st:
            bcast_row(G[:], gsrc[layer:layer + 1, :], 'nG')
            for r in range(2):
                bcast_row(A[:, r, :], modrow[layer, r:r + 1, sc_off:sc_off + D], ('nA', r))
                bcast_row(SH[:, r, :], modrow[layer, r:r + 1, sh_off:sh_off + D], ('nSH', r))
                S.op('dve', lambda e: e.scalar_tensor_tensor(out=A[:, r, :], in0=A[:, r, :], scalar=1.0, in1=G[:],
                                                             op0=ALU.add, op1=ALU.mult), reads=['nG'], writes=[('nA', r)])
            xs = Rot([x0, x1], 'nx')
            us = Rot([u0, u1], 'nu')
            for i in tiles:
                r = 1 if i < 2 else 0
                xt, xk = xs.next()
                ut, uk = us.next()
                S.dma('sp', lambda e: e.dma_start(out=xt[:], in_=src[i * 128:(i + 1) * 128, :]), writes=[xk])
                S.op('act', lambda e: e.activation(out=sq[:], in_=xt[:], func=AF.Square, accum_out=st[:, 0:1]),
                     reads=[xk], writes=['nsq', 'nst'])
                S.op('dve', lambda e: e.tensor_scalar(out=st[:, 1:2], in0=st[:, 0:1], scalar1=1.0 / D, scalar2=EPS,
                                                      op0=ALU.mult, op1=ALU.add), writes=['nst'])
                S.op('act', lambda e: e.activation(out=st[:, 2:3], in_=st[:, 1:2], func=AF.Sqrt), writes=['nst'])
                S.op('dve', lambda e: e.reciprocal(st[:, 3:4], st[:, 2:3]), writes=['nst'])
                S.op('dve', lambda e: e.scalar_tensor_tensor(out=ut[:], in0=xt[:], scalar=st[:, 3:4], in1=A[:, r, :],
                                                             op0=ALU.mult, op1=ALU.mult), reads=[xk, ('nA', r)], writes=[uk, 'nst'])
                S.op('pool', lambda e: e.tensor_tensor(out=ut[:], in0=ut[:], in1=SH[:, r, :], op=ALU.add),
                     reads=[('nSH', r)], writes=[uk])
                consume(i, ut, uk)

    def phase_norm_T(layer, src, uT, tiles=range(NT)):
        def consume(i, ut, uk):
            for half in range(2):
                pb = (i * 2 + half) % 4
                for j in range(4):
                    kc = half * 4 + j
                    S.op('pe', lambda e: e.transpose(psum[pb][:, j * 128:(j + 1) * 128], ut[:, kc * 128:(kc + 1) * 128], ident[:]),
                         reads=[uk, 'ident'], writes=[PB(pb)])
                S.op('act', lambda e: e.copy(uT[:, half * 4:half * 4 + 4, i * 128:(i + 1) * 128],
                                             psum[pb][:].rearrange("p (a b) -> p a b", a=4)),
                     writes=[PB(pb), ('uT', i, half)])
        norm_tiles(layer, 0, src, tiles, consume)
        S.barrier()

    qkT = dscr("qkT", [2048, T], BF16)
    v_tm = dscr("v_tm", [T, D], BF16)
    so_tm = dscr("so_tm", [T, D], BF16)
    g_tm = dscr("g_tm", [T, 16])
    xrT = dscr("xrT", [D, T])
    ygT = dscr("ygT", [D, T], BF16)
    hlT = dscr("hlT", [D, T], BF16)
    hm_d = [dscr("hm_f", [T, D]), dscr("hm_b", [T, D])]
    TG = [(0, 256)] + [(256 + 512 * j, 512) for j in range(8)]
    ZW = 4358
    CN = 4355

    def zcol(t0):
        return 2 + t0 if t0 < 256 else t0 + 5

    def phase_E1(uT):
        wv = I("ev_w_in").rearrange("(kc p) n -> p kc n", p=128)
        with SBT("wb0", [128, 8, 512], BF16) as wb0, SBT("wb1", [128, 8, 512], BF16) as wb1, \
                SBT("zp0", [128, ZW], F32) as zp0, SBT("zp1", [128, ZW], F32) as zp1, \
                SBT("co", [128, ZW], F32) as co, \
                SBT("ob0", [128, ZW], BF16) as ob0, SBT("ob1", [128, ZW], BF16) as ob1, \
                SBT("cwq", [128, 16, 5], F32) as cwq, SBT("cwl", [128, 8, 5], F32) as cwl, \
                SBT("tms0", [128, 512], BF16) as tms0, SBT("tms1", [128, 512], BF16) as tms1, \
                SBT("wg", [128, 8, 16], BF16) as wg, SBT("gbr", [128, 16], F32) as gbr, \
                SBT("GT", [128, NT, 16], F32) as GT:
            S.dma('sp', lambda e: e.dma_start(out=cwq[:], in_=I("ev_qkcw")), writes=['cwq'])
            S.dma('sp', lambda e: e.dma_start(out=cwl[:], in_=I("ev_lrucw")), writes=['cwl'])
            S.op('pool', lambda e: e.memset(zp0[:], 0.0), writes=[('zp', 0)])
            S.op('pool', lambda e: e.memset(zp1[:], 0.0), writes=[('zp', 1)])
            wbs = Rot([wb0, wb1], 'wb')
            zps = Rot([zp0, zp1], 'zp')
            obs = Rot([ob0, ob1], 'ob')
            pi = [0]
            fm_blocks = [(c0, 'qk', c0 // 128) for c0 in range(0, 2048, 512)] + \
                        [(4112 + j * 512, 'xr', j * 4) for j in range(2)] + \
                        [(5136 + j * 512, 'yg', j * 4) for j in range(2)]
            for c0, kind, cbase in fm_blocks:
                wb, wbk = wbs.next()
                S.dma('pool', lambda e: e.dma_start(out=wb[:], in_=wv[:, :, c0:c0 + 512]), writes=[wbk])
                for jj in range(4):
                    cidx = cbase + jj
                    zp, zpk = zps.next()
                    for (t0, n) in TG:
                        pb = pi[0] % 2
                        pi[0] += 1
                        for kc in range(8):
                            S.op('pe', lambda e: e.matmul(psum[pb][:, 0:n], wb[:, kc, jj * 128:(jj + 1) * 128], uT[:, kc, t0:t0 + n],
                                                          start=(kc == 0), stop=(kc == 7)), reads=[wbk], writes=[PB(pb)])
                        z0 = zcol(t0)
                        S.op('act', lambda e: e.copy(zp[:, z0:z0 + n], psum[pb][:, 0:n]), writes=[PB(pb), zpk])
                    ob, obk = obs.next()
                    if kind in ('qk', 'xr'):
                        cw = cwq if kind == 'qk' else cwl
                        S.op('dve', lambda e: e.tensor_scalar(out=co[:, 0:CN], in0=zp[:, 0:CN], scalar1=cw[:, cidx, 0:1], scalar2=None,
                                                              op0=ALU.mult), reads=[zpk, 'cwq', 'cwl'], writes=['co'])
                        for j in range(1, 4):
                            S.op('dve', lambda e: e.scalar_tensor_tensor(out=co[:, 0:CN], in0=zp[:, j:j + CN], scalar=cw[:, cidx, j:j + 1],
                                                                         in1=co[:, 0:CN], op0=ALU.mult, op1=ALU.add),
                                 reads=[zpk], writes=['co'])
                        if kind == 'qk':
                            S.op('act', lambda e: e.activation(out=ob[:, 0:CN], in_=co[:, 0:CN], func=AF.Silu, bias=cw[:, cidx, 4:5], scale=1.0),
                                 reads=['co'], writes=[obk])
                            rows = qkT[cidx * 128:(cidx + 1) * 128, :]
                            S.dma('sp', lambda e: e.dma_start(out=rows[:, 0:256], in_=ob[:, 0:256]), reads=[obk], writes=[('qkT', cidx, 0)])
                            S.dma('sp', lambda e: e.dma_start(out=rows[:, 256:T], in_=ob[:, 259:CN]), reads=[obk], writes=[('qkT', cidx, 1)])
                        else:
                            S.op('act', lambda e: e.activation(out=co[:, 0:CN], in_=co[:, 0:CN], func=AF.Identity, bias=cw[:, cidx, 4:5], scale=1.0),
                                 writes=['co'])
                            rows = xrT[cidx * 128:(cidx + 1) * 128, :]
                            S.dma('sp', lambda e: e.dma_start(out=rows[:, 0:256], in_=co[:, 0:256]), reads=['co'], writes=[('xrT', cidx, 0)])
                            S.dma('sp', lambda e: e.dma_start(out=rows[:, 256:T], in_=co[:, 259:CN]), reads=['co'], writes=[('xrT', cidx, 1)])
                    else:
                        S.op('act', lambda e: e.activation(out=co[:], in_=zp[:], func=AF.Square), reads=[zpk], writes=['co'])
                        S.op('dve', lambda e: e.tensor_scalar(out=co[:], in0=co[:], scalar1=0.044715, scalar2=1.0, op0=ALU.mult, op1=ALU.add),
                             writes=['co'])
                        S.op('dve', lambda e: e.tensor_tensor(out=co[:], in0=co[:], in1=zp[:], op=ALU.mult), reads=[zpk], writes=['co'])
                        S.op('act', lambda e: e.activation(out=co[:], in_=co[:], func=AF.Sigmoid, scale=1.5957691216), writes=['co'])
                        S.op('pool', lambda e: e.tensor_tensor(out=ob[:], in0=co[:], in1=zp[:], op=ALU.mult), reads=['co', zpk], writes=[obk])
                        rows = ygT[cidx * 128:(cidx + 1) * 128, :]
                        S.dma('sp', lambda e: e.dma_start(out=rows[:, 0:256], in_=ob[:, 2:258]), reads=[obk], writes=[('ygT', cidx, 0)])
                        S.dma('sp', lambda e: e.dma_start(out=rows[:, 256:T], in_=ob[:, 261:4357]), reads=[obk], writes=[('ygT', cidx, 1)])
            tms = Rot([tms0, tms1], 'tms')
            for blk in range(4):
                c0 = 2048 + blk * 512
                wb, wbk = wbs.next()
                S.dma('pool', lambda e: e.dma_start(out=wb[:], in_=wv[:, :, c0:c0 + 512]), writes=[wbk])
                dst = v_tm if blk < 2 else so_tm
                dc0 = (blk % 2) * 512
                for i in range(NT):
                    pb = 2 + i % 2
                    for kc in range(8):
                        S.op('pe', lambda e: e.matmul(psum[pb][:], uT[:, kc, i * 128:(i + 1) * 128], wb[:, kc, :],
                                                      start=(kc == 0), stop=(kc == 7)), reads=[wbk], writes=[PB(pb)])
                    st_, stk = tms.next()
                    if blk < 2:
                        S.op('act', lambda e: e.copy(st_[:], psum[pb][:]), writes=[PB(pb), stk])
                    else:
                        S.op('act', lambda e: e.activation(out=st_[:], in_=psum[pb][:], func=AF.Sigmoid), writes=[PB(pb), stk])
                    S.dma('sp', lambda e: e.dma_start(out=dst[i * 128:(i + 1) * 128, dc0:dc0 + 512], in_=st_[:]), reads=[stk],
                          writes=[('tmout', blk, i)])
            S.dma('pool', lambda e: e.dma_start(out=wg[:], in_=wv[:, :, 4096:4112]), writes=['wg'])
            bcast_row(gbr[:], I("ev_gate_b")[0:1, :], 'gbr')
            for i in range(NT):
                pb = 4 + i % 2
                for kc in range(8):
                    S.op('pe', lambda e: e.matmul(psum[pb][:, 0:16], uT[:, kc, i * 128:(i + 1) * 128], wg[:, kc, :],
                                                  start=(kc == 0), stop=(kc == 7)), reads=['wg'], writes=[PB(pb)])
                S.op('dve', lambda e: e.tensor_tensor(out=GT[:, i, :], in0=psum[pb][:, 0:16], in1=gbr[:], op=ALU.add),
                     reads=['gbr'], writes=[PB(pb), 'GT'])
            S.dma('sp', lambda e: e.dma_start(out=g_tm.rearrange("(n p) c -> p n c", p=128), in_=GT[:]), reads=['GT'], writes=['g_tm'])
        S.barrier()

    def phase_E2():
        lw_d, lb_d, lam_d = I("ev_lru_w"), I("ev_lru_b"), I("ev_lru_lam")
        with SBT("lxr", [128, T], F32) as xr, SBT("lyg", [128, T], BF16) as yg, \
                SBT("lA", [128, T], F32) as A, SBT("lB", [128, T], F32) as Bx, \
                SBT("ltmp", [128, T], F32) as tmp, \
                SBT("lH0", [128, T], F32) as H0, SBT("lH1", [128, T], F32) as H1, \
                SBT("lho", [128, T], BF16) as ho, \
                SBT("lw", [128, 4, 128], F32) as lw, SBT("lb", [128, 8, 4], F32) as lb, \
                SBT("lam", [128, 8, 2], F32) as lam, SBT("cA", [128, 8, 2], F32) as cA:
            S.dma('sp', lambda e: e.dma_start(out=lb[:], in_=lb_d), writes=['lb'])
            S.dma('sp', lambda e: e.dma_start(out=lam[:], in_=lam_d), writes=['lam'])
            S.op('act', lambda e: e.activation(out=cA[:], in_=lam[:], func=AF.Exp, scale=-1.0), reads=['lam'], writes=['cA'])
            S.op('act', lambda e: e.activation(out=cA[:], in_=cA[:], func=AF.Ln, bias=1.0, scale=1.0), writes=['cA'])
            S.op('dve', lambda e: e.tensor_scalar(out=cA[:], in0=cA[:], scalar1=-8.0, scalar2=None, op0=ALU.mult), writes=['cA'])
            pi = 0
            for cc in range(8):
                S.dma('sp', lambda e: e.dma_start(out=xr[:], in_=xrT[cc * 128:(cc + 1) * 128, :]), writes=['xr'])
                S.dma('sp', lambda e: e.dma_start(out=yg[:], in_=ygT[cc * 128:(cc + 1) * 128, :]), writes=['yg'])
                S.dma('sp', lambda e: e.dma_start(out=lw[:], in_=lw_d[:, cc].rearrange("g k m -> k g m")), writes=['lw'])
                for z in range(2):
                    H = H0 if z == 0 else H1
                    hk = ('H', z)
                    for gi, dst, dk in ((0, A, 'A'), (1, Bx, 'Bx')):
                        for (t0, n) in TG:
                            pb = pi % 2
                            pi += 1
                            S.op('pe', lambda e: e.matmul(psum[pb][:, 0:n], lw[:, z * 2 + gi, :], xr[:, t0:t0 + n], start=True, stop=True),
                                 reads=['lw', 'xr'], writes=[PB(pb)])
                            S.op('act', lambda e: e.activation(out=dst[:, t0:t0 + n], in_=psum[pb][:, 0:n], func=AF.Sigmoid,
                                                               bias=lb[:, cc, z * 2 + gi:z * 2 + gi + 1], scale=1.0),
                                 reads=['lb'], writes=[PB(pb), dk])
                    S.op('act', lambda e: e.activation(out=A[:], in_=A[:], func=AF.Exp, scale=cA[:, cc, z:z + 1]), reads=['cA'], writes=['A'])
                    S.op('act', lambda e: e.activation(out=tmp[:], in_=A[:], func=AF.Square), reads=['A'], writes=['tmp'])
                    S.op('act', lambda e: e.activation(out=tmp[:], in_=tmp[:], func=AF.Sqrt, bias=1.0, scale=-1.0), writes=['tmp'])
                    S.op('pool', lambda e: e.tensor_tensor(out=Bx[:], in0=Bx[:], in1=xr[:], op=ALU.mult), reads=['xr'], writes=['Bx'])
                    S.op('dve', lambda e: e.tensor_tensor(out=Bx[:], in0=Bx[:], in1=tmp[:], op=ALU.mult), reads=['tmp'], writes=['Bx'])
                    if z == 0:
                        S.op('dve', lambda e: e.tensor_tensor_scan(H[:], A[:], Bx[:], 0.0, ALU.mult, ALU.add), reads=['A', 'Bx'], writes=[hk])
                    else:
                        S.op('dve', lambda e: e.tensor_tensor_scan(H[:, 0:256][:, ::-1], A[:, 0:256][:, ::-1], Bx[:, 0:256][:, ::-1], 0.0,
                                                                   ALU.mult, ALU.add), reads=['A', 'Bx'], writes=[hk])
                        S.op('dve', lambda e: e.tensor_tensor_scan(H[:, 256:T][:, ::-1], A[:, 256:T][:, ::-1], Bx[:, 256:T][:, ::-1], H[:, 0:1],
                                                                   ALU.mult, ALU.add), reads=['A', 'Bx'], writes=[hk])
                S.op('pool', lambda e: e.tensor_tensor(out=H0[:], in0=H0[:], in1=H1[:], op=ALU.add), reads=[('H', 1)], writes=[('H', 0)])
                S.op('dve', lambda e: e.tensor_tensor(out=ho[:], in0=H0[:], in1=yg[:], op=ALU.mult), reads=[('H', 0), 'yg'], writes=['ho'])
                S.dma('pool', lambda e: e.dma_start(out=hlT[cc * 128:(cc + 1) * 128, :], in_=ho[:]), reads=['ho'], writes=[('hlT', cc)])
        S.barrier()

    def phase_E3():
        NLN16 = -2.772588722239781
        with contextlib.ExitStack() as es:
            def al(name, shape, dt):
                return es.enter_context(SBT(name, shape, dt))
            G = al("G", [128, NT, 16], F32)
            LF = al("LF", [128, 2, NT, 4], F32)
            CF = al("CF", [128, 2, NT, 4], F32)
            EB16 = al("EB16", [128, 2, NT, 4], F32)
            ECF = al("ECF", [128, 2, NT, 4], F32)
            DEC = al("DEC", [128, 2, NT, 4], F32)
            WS16 = al("WS16", [128, 2, NT, 4], F32)
            qT0 = al("qT0", [128, 2, T], BF16)
            qT1 = al("qT1", [128, 2, T], BF16)
            kT0 = al("kT0", [128, 2, T], BF16)
            kT1 = al("kT1", [128, 2, T], BF16)
            V0 = al("V0", [128, NT, 257], BF16)
            V1 = al("V1", [128, NT, 257], BF16)
            CTs = al("CTs", [128, 4, 2, 257], F32)
            CTbs = al("CTbs", [128, 4, 2, 257], BF16)
            PTs = al("PTs", [128, 4, 128], BF16)
            ktms = al("ktms", [128, 4, 256], BF16)
            vws = al("vws", [128, 4, 257], BF16)
            sms = al("sms", [128, 4, 8], F32)
            houts = al("houts", [128, 4, 256], F32)
            S.dma('sp', lambda e: e.dma_start(out=G[:], in_=g_tm.rearrange("(n p) c -> p n c", p=128)), writes=['G'])
            Gv = G[:].rearrange("p n (d g h) -> p d g n h", d=2, g=2, h=4)
            fl = lambda t, d: t[:, d].rearrange("p n h -> p (n h)")
            for d in range(2):
                S.op('act', lambda e: e.activation(out=LF[:, d], in_=Gv[:, d, 1], func=AF.Exp, scale=-1.0), reads=['G'], writes=['LF'])
                S.op('act', lambda e: e.activation(out=LF[:, d], in_=LF[:, d], func=AF.Ln, bias=1.0, scale=1.0), writes=['LF'])
                S.op('dve', lambda e: e.tensor_scalar(out=LF[:, d], in0=LF[:, d], scalar1=-1.0, scalar2=None, op0=ALU.mult), writes=['LF'])
                tri = triF if d == 0 else triB
                sel = selF if d == 0 else selB
                S.op('pe', lambda e: e.matmul(psum[0][:, 0:NT * 4], tri[:], fl(LF, d), start=True, stop=True),
                     reads=['LF', tri.name], writes=[PB(0)])
                S.op('act', lambda e: e.copy(fl(CF, d), psum[0][:, 0:NT * 4]), writes=[PB(0), 'CF'])
                S.op('pe', lambda e: e.matmul(psum[1][:, 0:NT * 4], sel[:], fl(CF, d), start=True, stop=True),
                     reads=['CF', sel.name], writes=[PB(1)])
                S.op('act', lambda e: e.activation(out=fl(DEC, d), in_=psum[1][:, 0:NT * 4], func=AF.Exp), writes=[PB(1), 'DEC'])
                S.op('dve', lambda e: e.tensor_tensor(out=EB16[:, d], in0=Gv[:, d, 0], in1=CF[:, d], op=ALU.subtract), reads=['G', 'CF'], writes=['EB16'])
                S.op('dve', lambda e: e.tensor_scalar(out=EB16[:, d], in0=EB16[:, d], scalar1=NLN16, scalar2=None, op0=ALU.add), writes=['EB16'])
                S.op('act', lambda e: e.activation(out=EB16[:, d], in_=EB16[:, d], func=AF.Exp), writes=['EB16'])
                S.op('act', lambda e: e.activation(out=ECF[:, d], in_=CF[:, d], func=AF.Exp), reads=['CF'], writes=['ECF'])
                S.op('dve', lambda e: e.tensor_tensor(out=WS16[:, d], in0=EB16[:, d], in1=DEC[:, d], op=ALU.mult), reads=['EB16', 'DEC'], writes=['WS16'])
            S.barrier()
            qTs, kTs, Vs = [qT0, qT1], [kT0, kT1], [V0, V1]
            order = [list(range(NT)), [1, 0] + list(range(NT - 1, 1, -1))]
            cnt = {'st': 0, 'num': 0}
            for hp in range(2):
                for hh in range(2):
                    h = hp * 2 + hh
                    S.dma('sp', lambda e: e.dma_start(out=qTs[hh][:], in_=qkT[h * 256:(h + 1) * 256, :].rearrange("(dh p) t -> p dh t", p=128)),
                          writes=[('qT', hh)])
                    S.dma('sp', lambda e: e.dma_start(out=kTs[hh][:], in_=qkT[1024 + h * 256:1024 + (h + 1) * 256, :].rearrange("(dh p) t -> p dh t", p=128)),
                          writes=[('kT', hh)])
                    S.dma('sp', lambda e: e.dma_start(out=Vs[hh][:, :, 0:256], in_=v_tm[:, h * 256:(h + 1) * 256].rearrange("(n p) e -> p n e", p=128)),
                          writes=[('V', hh)])
                    S.op('pool', lambda e: e.memset(Vs[hh][:, :, 256:257], 1.0), writes=[('V1', hh)])
                S.op('pool', lambda e: e.memset(CTs[:], 0.0), writes=[('CT', i) for i in range(4)])
                S.op('pool', lambda e: e.memset(CTbs[:], 0.0), writes=[('CTb', i) for i in range(4)])
                for step in range(NT):
                    for ci, (z, hh) in enumerate([(0, 0), (0, 1), (1, 0), (1, 1)]):
                        h = hp * 2 + hh
                        c = order[z][step]
                        sl = slice(c * 128, (c + 1) * 128)
                        qT, kT, V = qTs[hh], kTs[hh], Vs[hh]
                        rk = [('qT', hh), ('kT', hh), ('V', hh), ('V1', hh)]
                        tri = triF if z == 0 else triB
                        pst = cnt['st'] % 2
                        cnt['st'] += 1
                        for dh in range(2):
                            S.op('pe', lambda e: e.matmul(psum[pst][:, 0:128], kT[:, dh, sl], qT[:, dh, sl], start=(dh == 0), stop=(dh == 1)),
                                 reads=rk, writes=[PB(pst)])
                        S.op('dve', lambda e: e.scalar_tensor_tensor(out=PTs[:, ci, :], in0=psum[pst][:, 0:128], scalar=EB16[:, z, c, h:h + 1],
                                                                     in1=tri[:], op0=ALU.mult, op1=ALU.mult), writes=[PB(pst), ('PT', ci)])
                        for dh in range(2):
                            S.op('pe', lambda e: e.transpose(psb[0][:, dh * 128:(dh + 1) * 128], kT[:, dh, sl], identb[:]),
                                 reads=rk, writes=[PBB(0)])
                        S.op('act', lambda e: e.copy(ktms[:, ci, :], psb[0][:, 0:256]), writes=[PBB(0), ('ktm', ci)])
                        S.op('act', lambda e: e.activation(out=vws[:, ci, :], in_=V[:, c, :], func=AF.Identity, scale=WS16[:, z, c, h:h + 1]),
                             reads=rk, writes=[('vw', ci)])
                        pn = 2 + cnt['num'] % 2
                        cnt['num'] += 1
                        S.op('pe', lambda e: e.matmul(psum[pn][:, 0:257], PTs[:, ci, :], V[:, c, :], start=True, stop=False),
                             reads=rk + [('PT', ci)], writes=[PB(pn)])
                        for dh in range(2):
                            S.op('pe', lambda e: e.matmul(psum[pn][:, 0:257], qT[:, dh, sl], CTbs[:, ci, dh, :], start=False, stop=(dh == 1)),
                                 reads=rk + [('CTb', ci)], writes=[PB(pn)])
                        sm = sms[:, ci, :]
                        ecf = ECF[:, z, c, h:h + 1]
                        S.op('dve', lambda e: e.tensor_tensor(out=sm[:, 0:1], in0=psum[pn][:, 256:257], in1=ecf, op=ALU.mult), writes=[PB(pn), ('sm', ci)])
                        S.op('dve', lambda e: e.tensor_scalar(out=sm[:, 2:3], in0=sm[:, 0:1], scalar1=1.0, scalar2=None, op0=ALU.max), writes=[('sm', ci)])
                        S.op('dve', lambda e: e.tensor_scalar(out=sm[:, 3:4], in0=sm[:, 0:1], scalar1=-1.0, scalar2=sm[:, 2:3], op0=ALU.mult, op1=ALU.max),
                             writes=[('sm', ci)])
                        S.op('dve', lambda e: e.reciprocal(sm[:, 4:5], sm[:, 3:4]), writes=[('sm', ci)])
                        S.op('dve', lambda e: e.tensor_tensor(out=sm[:, 5:6], in0=sm[:, 4:5], in1=ecf, op=ALU.mult), writes=[('sm', ci)])
                        S.op('act', lambda e: e.activation(out=houts[:, ci, :], in_=psum[pn][:, 0:256], func=AF.Identity, scale=sm[:, 5:6]),
                             reads=[('sm', ci)], writes=[PB(pn), ('hout', ci)])
                        S.dma('sp', lambda e: e.dma_start(out=hm_d[z][sl, h * 256:(h + 1) * 256], in_=houts[:, ci, :]), reads=[('hout', ci)],
                              writes=[('hm', z, h, c)])
                        for dh in range(2):
                            S.op('pe', lambda e: e.matmul(psum[4 + dh][:, 0:257], ktms[:, ci, dh * 128:(dh + 1) * 128], vws[:, ci, :], start=True, stop=True),
                                 reads=[('ktm', ci), ('vw', ci)], writes=[PB(4 + dh)])
                            S.op('dve', lambda e: e.scalar_tensor_tensor(out=CTs[:, ci, dh, :], in0=CTs[:, ci, dh, :], scalar=DEC[:, z, c, h:h + 1],
                                                                         in1=psum[4 + dh][:, 0:257], op0=ALU.mult, op1=ALU.add),
                                 writes=[PB(4 + dh), ('CT', ci)])
                        S.op('act', lambda e: e.copy(CTbs[:, ci], CTs[:, ci]), reads=[('CT', ci)], writes=[('CTb', ci)])
        S.barrier()

    def mixer_out(i, r, G1, wo, nk, lhs_list, src_rows, pbase):
        return

    def phase_E4():
        with contextlib.ExitStack() as es:
            def al(name, shape, dt):
                return es.enter_context(SBT(name, shape, dt))
            wo = al("wo", [128, 16, D], BF16)
            MG = al("MG", [128, D], F32)
            G1 = al("G1", [128, 2, D], F32)
            hf = [al("hf%d" % j, [128, D], F32) for j in range(2)]
            hb = [al("hb%d" % j, [128, D], F32) for j in range(2)]
            so = [al("so%d" % j, [128, D], BF16) for j in range(2)]
            sq = al("e4sq", [128, 256], F32)
            st = al("e4st", [128, 16], F32)
            hmb = al("hmb", [128, D], BF16)
            hmT = [al("hmT%d" % j, [128, 8, 128], BF16) for j in range(2)]
            hl = [al("hl%d" % j, [128, 8, 128], BF16) for j in range(2)]
            xt = [al("e4x%d" % j, [128, D], F32) for j in range(2)]
            tmp = al("e4tmp", [128, D], F32)
            wov = I("ev_w_out").rearrange("(kc p) n -> p kc n", p=128)
            for hlf in range(2):
                S.dma('pool', lambda e: e.dma_start(out=wo[:, hlf * 8:(hlf + 1) * 8, :], in_=wov[:, hlf * 8:(hlf + 1) * 8, :]), writes=[('wo', hlf)])
            bcast_row(MG[:], I("ev_mnorm_g")[0:1, :], 'MG')
            for r in range(2):
                bcast_row(G1[:, r, :], modrow[0, r:r + 1, 2 * D:3 * D], ('G1', r))
            hlv = hlT.rearrange("(cc p) t -> p cc t", p=128)
            xin = I("xin")
            for i in range(NT):
                r = 1 if i < 2 else 0
                j = i % 2
                rows = slice(i * 128, (i + 1) * 128)
                S.dma('sp', lambda e: e.dma_start(out=hf[j][:], in_=hm_d[0][rows, :]), writes=[('hf', j)])
                S.dma('sp', lambda e: e.dma_start(out=hb[j][:], in_=hm_d[1][rows, :]), writes=[('hb', j)])
                S.dma('sp', lambda e: e.dma_start(out=so[j][:], in_=so_tm[rows, :]), writes=[('so', j)])
                S.dma('sp', lambda e: e.dma_start(out=hl[j][:], in_=hlv[:, :, rows]), writes=[('hl', j)])
                S.dma('sp', lambda e: e.dma_start(out=xt[j][:], in_=xin[rows, :]), writes=[('xt', j)])
                S.op('pool', lambda e: e.tensor_tensor(out=hf[j][:], in0=hf[j][:], in1=hb[j][:], op=ALU.add), reads=[('hb', j)], writes=[('hf', j)])
                for h in range(4):
                    S.op('act', lambda e: e.activation(out=sq[:], in_=hf[j][:, h * 256:(h + 1) * 256], func=AF.Square, accum_out=st[:, h:h + 1]),
                         reads=[('hf', j)], writes=['e4sq', 'e4st'])
                S.op('dve', lambda e: e.tensor_scalar(out=st[:, 4:8], in0=st[:, 0:4], scalar1=1.0 / 256, scalar2=EPS, op0=ALU.mult, op1=ALU.add), writes=['e4st'])
                S.op('act', lambda e: e.activation(out=st[:, 8:12], in_=st[:, 4:8], func=AF.Sqrt), writes=['e4st'])
                S.op('dve', lambda e: e.reciprocal(st[:, 12:16], st[:, 8:12]), writes=['e4st'])
                for h in range(4):
                    hs = slice(h * 256, (h + 1) * 256)
                    S.op('dve', lambda e: e.scalar_tensor_tensor(out=hf[j][:, hs], in0=hf[j][:, hs], scalar=st[:, 12 + h:13 + h], in1=MG[:, hs],
                                                                 op0=ALU.mult, op1=ALU.mult), reads=['MG'], writes=[('hf', j), 'e4st'])
                S.op('pool', lambda e: e.tensor_tensor(out=hmb[:], in0=hf[j][:], in1=so[j][:], op=ALU.mult), reads=[('hf', j), ('so', j)], writes=['hmb'])
                for kc in range(8):
                    S.op('pe', lambda e: e.transpose(psb[j][:, kc * 128:(kc + 1) * 128], hmb[:, kc * 128:(kc + 1) * 128], identb[:]),
                         reads=['hmb'], writes=[PBB(j)])
                S.op('act', lambda e: e.copy(hmT[j][:].rearrange("p a b -> p (a b)"), psb[j][:]), writes=[PBB(j), ('hmT', j)])
                for nb in range(2):
                    pb = (2 * i + nb) % 4
                    for kc in range(16):
                        lhs = hmT[j][:, kc, :] if kc < 8 else hl[j][:, kc - 8, :]
                        S.op('pe', lambda e: e.matmul(psum[pb][:], lhs, wo[:, kc, nb * 512:(nb + 1) * 512], start=(kc == 0), stop=(kc == 15)),
                             reads=[('hmT', j), ('hl', j), ('wo', 0), ('wo', 1)], writes=[PB(pb)])
                    cs = slice(nb * 512, (nb + 1) * 512)
                    S.op('dve', lambda e: e.tensor_tensor(out=tmp[:, cs], in0=psum[pb][:], in1=G1[:, r, cs], op=ALU.mult), reads=[('G1', r)],
                         writes=[PB(pb), ('e4tmp', nb)])
                    S.op('pool', lambda e: e.tensor_tensor(out=tmp[:, cs], in0=tmp[:, cs], in1=xt[j][:, cs], op=ALU.add), reads=[('xt', j)],
                         writes=[('e4tmp', nb)])
                S.dma('pool', lambda e: e.dma_start(out=xres[rows, :], in_=tmp[:]), reads=[('e4tmp', 0), ('e4tmp', 1)], writes=[('xres', i)])
        S.barrier()

    v2_tm = dscr("v2_tm", [T, D], BF16)

    def phase_N2(layer, tiles, LOG):
        with contextlib.ExitStack() as es:
            def al(name, shape, dt):
                return es.enter_context(SBT(name, shape, dt))
            wr = al("wr", [128, 8, 32], F32)
            br = al("br", [1, 32], F32)
            vT32 = [al("vT32_%d" % j, [128, 8, 128], F32) for j in range(2)]
            vb = [al("vb%d" % j, [128, D], BF16) for j in range(2)]
            S.dma('sp', lambda e: e.dma_start(out=wr[:], in_=I("moe_w_r")[layer].rearrange("(kc p) n -> p kc n", p=128)), writes=['wr'])
            S.dma('sp', lambda e: e.dma_start(out=br[:], in_=I("moe_b_r")[layer:layer + 1, :]), writes=['br'])
            cnt = [0]

            def consume(i, ut, uk):
                j = cnt[0] % 2
                cnt[0] += 1
                S.op('pool', lambda e: e.tensor_copy(vb[j][:], ut[:]), reads=[uk], writes=[('vb', j)])
                S.dma('pool', lambda e: e.dma_start(out=v2_tm[i * 128:(i + 1) * 128, :], in_=vb[j][:]), reads=[('vb', j)], writes=[('v2', i)])
                for half in range(2):
                    pb = (i * 2 + half) % 4
                    for jj in range(4):
                        kc = half * 4 + jj
                        S.op('pe', lambda e: e.transpose(psum[pb][:, jj * 128:(jj + 1) * 128], ut[:, kc * 128:(kc + 1) * 128], ident[:]),
                             reads=[uk, 'ident'], writes=[PB(pb)])
                    S.op('act', lambda e: e.copy(vT32[j][:, half * 4:half * 4 + 4, :].rearrange("p a b -> p (a b)"), psum[pb][:]),
                         writes=[PB(pb), ('vT32', j, half)])
                pl = 4 + i % 2
                for kc in range(8):
                    S.op('pe', lambda e: e.matmul(psum[pl][:, 0:32], vT32[j][:, kc, :], wr[:, kc, :], start=(kc == 0), stop=False),
                         reads=[('vT32', j, 0), ('vT32', j, 1), 'wr'], writes=[PB(pl)])
                S.op('pe', lambda e: e.matmul(psum[pl][:, 0:32], ones1[0:1, :], br[:], start=False, stop=True), reads=['br', 'ones1'], writes=[PB(pl)])
                S.op('dve', lambda e: e.tensor_copy(LOG[:, i, :], psum[pl][:, 0:32]), writes=[PB(pl), 'LOG'])
            norm_tiles(layer, 1, xres, tiles, consume)
        S.barrier()

    SB = 512
    SHIFT = 9
    NSUB = SB // 128
    NBMAX = 66
    xs_d = dscr("xs_d", [NBMAX * SB, D], BF16)
    ys_d = dscr("ys_d", [NBMAX * SB, D], F32)
    RT = {}
    for nm, shp, dt in (("LOG", [128, NT, 32], F32), ("TOP8", [128, NT, 8], F32), ("WG", [128, NT, 4], F32),
                        ("SLOTI", [128, NT, 4], I32), ("IDXI", [128, NBMAX], I32), ("BIDX", [128, NBMAX], I32),
                        ("IDX8", [128, NBMAX, 8], I32), ("IDX4", [128, NBMAX, 4], I32)):
        RT[nm] = nc.alloc_sbuf_tensor("rt_" + nm, shp, dt)

    def phase_route(tiles, NB):
        LOG, TOP8, WG, SLOTI, IDXI, BIDX = (RT[k] for k in ("LOG", "TOP8", "WG", "SLOTI", "IDXI", "BIDX"))
        with contextlib.ExitStack() as es:
            def al(name, shape, dt):
                return es.enter_context(SBT(name, shape, dt))
            MASK = al("MASK", [128, NT, 32], F32)
            RANK = al("RANK", [128, NT, 32], F32)
            CAR = al("CAR", [128, 32], F32)
            sm = al("rsm", [128, 8], F32)
            ci = al("rci", [128, 32], I32)
            PADDED = al("PADDED", [128, 32], F32)
            PEND = al("PEND", [128, 32], F32)
            BASE = al("BASE", [128, 32], F32)
            ones32 = al("ones32", [128, 32], F32)
            junk = al("rjunk", [128, 32], F32)
            SLOTF = al("SLOTF", [128, NT, 4], F32)
            BSTi = al("BSTi", [128, NBMAX], I32)
            BST = al("BST", [128, NBMAX], F32)
            BLKE = al("BLKE", [128, NBMAX], F32)
            IDXF = al("IDXF", [128, NBMAX], F32)
            IDXF2 = al("IDXF2", [128, NBMAX], F32)
            S.op('pool', lambda e: e.memset(CAR[:], 0.0), writes=['CAR'])
            S.op('pool', lambda e: e.memset(ones32[:], 1.0), writes=['ones32'])
            S.op('pool', lambda e: e.memset(SLOTF[:], 0.0), writes=['SLOTF'])
            for i in tiles:
                S.op('dve', lambda e: e.max(out=TOP8[:, i, :], in_=LOG[:, i, :]), writes=['TOP8'])
                S.op('dve', lambda e: e.tensor_scalar(out=MASK[:, i, :], in0=LOG[:, i, :], scalar1=TOP8[:, i, 3:4], scalar2=None, op0=ALU.is_ge),
                     reads=['TOP8'], writes=[('MASK', i)])
                S.op('dve', lambda e: e.tensor_scalar(out=sm[:, 0:1], in0=TOP8[:, i, 0:1], scalar1=-1.0, scalar2=None, op0=ALU.mult), reads=['TOP8'], writes=['rsm'])
                S.op('act', lambda e: e.activation(out=WG[:, i, :], in_=TOP8[:, i, 0:4], func=AF.Exp, bias=sm[:, 0:1], scale=1.0, accum_out=sm[:, 1:2]),
                     reads=['TOP8'], writes=['rsm', 'WG'])
                S.op('dve', lambda e: e.reciprocal(sm[:, 2:3], sm[:, 1:2]), writes=['rsm'])
                S.op('dve', lambda e: e.tensor_scalar(out=WG[:, i, :], in0=WG[:, i, :], scalar1=sm[:, 2:3], scalar2=None, op0=ALU.mult), writes=['rsm', 'WG'])
                S.op('pe', lambda e: e.matmul(psum[0][:, 0:32], triS[:], MASK[:, i, :], start=True, stop=True), reads=[('MASK', i)], writes=[PB(0)])
                S.op('pe', lambda e: e.matmul(psum[1][:, 0:32], onesm[:], MASK[:, i, :], start=True, stop=True), reads=[('MASK', i)], writes=[PB(1)])
                S.op('dve', lambda e: e.tensor_tensor(out=RANK[:, i, :], in0=psum[0][:, 0:32], in1=CAR[:], op=ALU.add), reads=['CAR'], writes=[PB(0), 'RANK'])
                S.op('dve', lambda e: e.tensor_tensor(out=CAR[:], in0=psum[1][:, 0:32], in1=CAR[:], op=ALU.add), writes=[PB(1), 'CAR'])
            S.op('dve', lambda e: e.tensor_scalar(out=junk[:], in0=CAR[:], scalar1=float(SB - 1), scalar2=None, op0=ALU.add), reads=['CAR'], writes=['rjunk'])
            S.op('dve', lambda e: e.tensor_copy(ci[:], junk[:]), reads=['rjunk'], writes=['rci'])
            S.op('dve', lambda e: e.tensor_scalar(out=ci[:], in0=ci[:], scalar1=SHIFT, scalar2=SHIFT, op0=ALU.arith_shift_right, op1=ALU.logical_shift_left),
                 writes=['rci'])
            S.op('dve', lambda e: e.tensor_copy(PADDED[:], ci[:]), reads=['rci'], writes=['PADDED'])
            S.op('dve', lambda e: e.tensor_tensor_scan(PEND[:], ones32[:], PADDED[:], 0.0, ALU.mult, ALU.add), reads=['ones32', 'PADDED'], writes=['PEND'])
            S.op('dve', lambda e: e.tensor_tensor(out=BASE[:], in0=PEND[:], in1=PADDED[:], op=ALU.subtract), reads=['PEND', 'PADDED'], writes=['BASE'])
            for i in tiles:
                S.op('dve', lambda e: e.tensor_tensor(out=RANK[:, i, :], in0=RANK[:, i, :], in1=BASE[:], op=ALU.add), reads=['BASE'], writes=['RANK'])
                for k in range(4):
                    S.op('dve', lambda e: e.scalar_tensor_tensor(out=junk[:], in0=LOG[:, i, :], scalar=TOP8[:, i, k:k + 1], in1=RANK[:, i, :],
                                                                 op0=ALU.is_equal, op1=ALU.mult, accum_out=SLOTF[:, i, k:k + 1]),
                         reads=['TOP8'], writes=['rjunk', 'RANK', 'SLOTF'])
            S.op('dve', lambda e: e.tensor_copy(SLOTI[:], SLOTF[:]), reads=['SLOTF'], writes=['SLOTI'])
            S.op('pool', lambda e: e.iota(BSTi[:], pattern=[[SB, NBMAX]], base=0, channel_multiplier=0), writes=['BSTi'])
            S.op('dve', lambda e: e.tensor_copy(BST[:], BSTi[:]), reads=['BSTi'], writes=['BST'])
            S.op('pool', lambda e: e.memset(BLKE[:], 0.0), writes=['BLKE'])
            for ex in range(32):
                S.op('dve', lambda e: e.scalar_tensor_tensor(out=BLKE[:], in0=BST[:], scalar=PEND[:, ex:ex + 1], in1=BLKE[:], op0=ALU.is_ge, op1=ALU.add),
                     reads=['BST', 'PEND'], writes=['BLKE'])
            S.op('dve', lambda e: e.tensor_scalar(out=BLKE[:], in0=BLKE[:], scalar1=31.0, scalar2=None, op0=ALU.min), writes=['BLKE'])
            S.op('dve', lambda e: e.tensor_scalar(out=IDXF[:], in0=BLKE[:], scalar1=128.0, scalar2=pidx[:, 0:1], op0=ALU.mult, op1=ALU.add),
                 reads=['pidx'], writes=['IDXF'])
            S.op('dve', lambda e: e.tensor_copy(IDXI[:], IDXF[:]), reads=['IDXF'], writes=['IDXI'])
            S.op('dve', lambda e: e.tensor_copy(BIDX[:], BLKE[:]), reads=['BLKE'], writes=['BIDX'])
            S.op('pool', lambda e: e.memset(BSTi[:], 0), writes=['BSTi'])
            S.op('dve', lambda e: e.tensor_copy(IDXF2[:], BSTi[:]), reads=['BSTi'], writes=['IDXF2'])
            S.op('dve', lambda e: e.tensor_tensor(out=IDXF2[:, 2:NBMAX], in0=BLKE[:, 2:NBMAX], in1=BLKE[:, 0:NBMAX - 2], op=ALU.is_equal),
                 reads=['BLKE'], writes=['IDXF2'])
            S.op('dve', lambda e: e.tensor_scalar(out=IDXF2[:], in0=IDXF2[:], scalar1=float(1 << 20), scalar2=None, op0=ALU.mult), writes=['IDXF2'])
            for kc in range(8):
                S.op('dve', lambda e: e.tensor_scalar(out=BST[:], in0=IDXF[:], scalar1=8.0, scalar2=float(kc), op0=ALU.mult, op1=ALU.add),
                     reads=['IDXF'], writes=['BST'])
                S.op('dve', lambda e: e.tensor_tensor(out=BST[:], in0=BST[:], in1=IDXF2[:], op=ALU.add), reads=['IDXF2'], writes=['BST'])
                S.op('dve', lambda e: e.tensor_copy(RT["IDX8"][:, :, kc], BST[:]), reads=['BST'], writes=['IDX8'])
            for q in range(4):
                S.op('dve', lambda e: e.tensor_scalar(out=BST[:], in0=IDXF[:], scalar1=4.0, scalar2=float(q), op0=ALU.mult, op1=ALU.add),
                     reads=['IDXF'], writes=['BST'])
                S.op('dve', lambda e: e.tensor_tensor(out=BST[:], in0=BST[:], in1=IDXF2[:], op=ALU.add), reads=['IDXF2'], writes=['BST'])
                S.op('dve', lambda e: e.tensor_copy(RT["IDX4"][:, :, q], BST[:]), reads=['BST'], writes=['IDX4'])
            if RT.get('dbg') is not None:
                dd = RT['dbg']
                S.op('dve', lambda e: e.tensor_copy(dd[:, 0:32], CAR[:]), reads=['CAR'], writes=['dd'])
                S.op('dve', lambda e: e.tensor_copy(dd[:, 32:64], PADDED[:]), reads=['PADDED'], writes=['dd'])
                S.op('dve', lambda e: e.tensor_copy(dd[:, 64:96], PEND[:]), reads=['PEND'], writes=['dd'])
                S.op('dve', lambda e: e.tensor_copy(dd[:, 96:128], MASK[:, 0, :]), writes=['dd'])
                S.op('dve', lambda e: e.tensor_copy(dd[:, 128:160], RANK[:, 1, :]), writes=['dd'])
                S.op('dve', lambda e: e.tensor_copy(dd[:, 160:192], ci[:]), writes=['dd'])
        S.barrier()

    def phase_scatter(tiles, NB):
        SLOTI = RT["SLOTI"]
        with contextlib.ExitStack() as es:
            def al(name, shape, dt):
                return es.enter_context(SBT(name, shape, dt))
            zt = al("zt", [128, D], BF16)
            vt = [al("svt%d" % j, [128, D], BF16) for j in range(3)]
            S.op('pool', lambda e: e.memset(zt[:], 0.0), writes=['zt'])
            for b in range(NB * NSUB):
                S.dma('sp', lambda e: e.dma_start(out=xs_d[b * 128:(b + 1) * 128, :], in_=zt[:]), reads=['zt'], writes=[('xsz', b)])
            S.barrier()
            for n, i in enumerate(tiles):
                j = n % 3
                S.dma('sp', lambda e: e.dma_start(out=vt[j][:], in_=v2_tm[i * 128:(i + 1) * 128, :]), writes=[('svt', j)])
                for k in range(4):
                    S.dma('pool', lambda e: e.indirect_dma_start(out=xs_d, out_offset=bass.IndirectOffsetOnAxis(ap=SLOTI[:, i, k:k + 1], axis=0),
                                                                 in_=vt[j][:], in_offset=None), reads=[('svt', j)], writes=[('xs', i, k)])
        S.barrier()

    def phase_moe(layer, NB):
        w1d = I("moe_w1_%d" % layer)
        w2d = I("moe_w2_%d" % layer)
        b1d = I("moe_b1_%d" % layer)
        b2d = I("moe_b2_%d" % layer)
        IDXI, BIDX = RT["IDXI"], RT["BIDX"]
        w1v = w1d.rearrange("a b c -> (a b) c")
        w2v = w2d.rearrange("a (q t) c -> (a q) (t c)", t=2)
        with contextlib.ExitStack() as es:
            def al(name, shape, dt):
                return es.enter_context(SBT(name, shape, dt))
            W1 = [al("W1_%d" % j, [128, 8, 2048], BF16) for j in range(2)]
            W2 = [al("W2_%d" % j, [128, 8, 1024], BF16) for j in range(2)]
            B1C = [al("B1C_%d" % j, [128, 16], F32) for j in range(2)]
            B2R = [al("B2R_%d" % j, [128, D], F32) for j in range(2)]
            XS = [al("XS_%d" % j, [128, D], BF16) for j in range(2 * NSUB)]
            XST = al("XST", [128, 8, SB], BF16)
            ACTT = al("ACTT", [128, 8, SB], BF16)
            tg = [al("tg_%d" % j, [128, SB], F32) for j in range(2)]
            sg = [al("sg_%d" % j, [128, SB], F32) for j in range(2)]
            tu = [al("tu_%d" % j, [128, SB], F32) for j in range(2)]
            YT = [al("YT_%d" % j, [128, D], F32) for j in range(2)]
            ec = [0]
            xc = [0]
            yc = [0]
            bc8 = nc.gpsimd.to_reg(32 * 128 * 8 - 1)
            bc4 = nc.gpsimd.to_reg(32 * 128 * 4 - 1)

            def gathers(b):
                j = b % 2
                idx = bass.IndirectOffsetOnAxis(ap=IDXI[:, b:b + 1], axis=0)
                bidx = bass.IndirectOffsetOnAxis(ap=BIDX[:, b:b + 1], axis=0)
                for kc in range(8):
                    i8 = bass.IndirectOffsetOnAxis(ap=RT["IDX8"][:, b, kc:kc + 1], axis=0)
                    S.dma('pool', lambda e: e.indirect_dma_start(out=W1[j][:, kc, :], out_offset=None, in_=w1v, in_offset=i8, bounds_check=bc8, oob_is_err=False), writes=[('W1', j, kc)])
                for q in range(4):
                    i4 = bass.IndirectOffsetOnAxis(ap=RT["IDX4"][:, b, q:q + 1], axis=0)
                    S.dma('pool', lambda e: e.indirect_dma_start(out=W2[j][:, 2 * q:2 * q + 2, :].rearrange("p a b -> p (a b)"), out_offset=None,
                                                                 in_=w2v, in_offset=i4, bounds_check=bc4, oob_is_err=False), writes=[('W2', j, q)])
                S.dma('pool', lambda e: e.indirect_dma_start(out=B1C[j][:], out_offset=None, in_=b1d, in_offset=idx), writes=[('B1C', j)])
                S.dma('pool', lambda e: e.indirect_dma_start(out=B2R[j][:], out_offset=None, in_=b2d, in_offset=bidx), writes=[('B2R', j)])

            def xloads(b):
                for st_ in range(NSUB):
                    xj = (b % 2) * NSUB + st_
                    r0 = b * SB + st_ * 128
                    S.dma('sp', lambda e: e.dma_start(out=XS[xj][:], in_=xs_d[r0:r0 + 128, :]), writes=[('XS', xj)])

            gathers(0)
            xloads(0)
            for b in range(NB):
                j = b % 2
                if b + 1 < NB:
                    gathers(b + 1)
                    xloads(b + 1)
                for st_ in range(NSUB):
                    xj = (b % 2) * NSUB + st_
                    pbb = st_ % 2
                    for kc in range(8):
                        S.op('pe', lambda e: e.transpose(psb[pbb][:, kc * 128:(kc + 1) * 128], XS[xj][:, kc::8], identb[:]), reads=[('XS', xj)], writes=[PBB(pbb)])
                    S.op('act', lambda e: e.copy(XST[:, :, st_ * 128:(st_ + 1) * 128], psb[pbb][:].rearrange("p (a b) -> p a b", a=8)),
                         writes=[PBB(pbb), ('XST', st_)])
                xk = [('XST', s_) for s_ in range(NSUB)]
                for jj in range(8):
                    t = ec[0] % 2
                    ec[0] += 1
                    for half in range(2):
                        bank = 2 * t + half
                        for kc in range(8):
                            S.op('pe', lambda e: e.matmul(psum[bank][:, 0:SB], W1[j][:, kc, half * 1024:(half + 1) * 1024][:, jj::8], XST[:, kc, :],
                                                          start=(kc == 0), stop=(kc == 7)), reads=[('W1', j, kc)] + xk, writes=[PB(bank)])
                    S.op('dve', lambda e: e.tensor_scalar(out=tg[t][:], in0=psum[2 * t][:, 0:SB], scalar1=B1C[j][:, jj:jj + 1], scalar2=7.0,
                                                          op0=ALU.add, op1=ALU.min), reads=[('B1C', j)], writes=[PB(2 * t), ('tg', t)])
                    S.op('dve', lambda e: e.tensor_scalar(out=tu[t][:], in0=psum[2 * t + 1][:, 0:SB], scalar1=B1C[j][:, 8 + jj:9 + jj], scalar2=7.0,
                                                          op0=ALU.add, op1=ALU.min), reads=[('B1C', j)], writes=[PB(2 * t + 1), ('tu', t)])
                    S.op('act', lambda e: e.activation(out=sg[t][:], in_=tg[t][:], func=AF.Sigmoid, scale=1.702), reads=[('tg', t)], writes=[('sg', t)])
                    S.op('dve', lambda e: e.tensor_scalar(out=tu[t][:], in0=tu[t][:], scalar1=-7.0, scalar2=1.0, op0=ALU.max, op1=ALU.add), writes=[('tu', t)])
                    S.op('dve', lambda e: e.tensor_tensor(out=tu[t][:], in0=tu[t][:], in1=tg[t][:], op=ALU.mult), reads=[('tg', t)], writes=[('tu', t)])
                    S.op('dve', lambda e: e.tensor_tensor(out=ACTT[:, jj, :], in0=tu[t][:], in1=sg[t][:], op=ALU.mult),
                         reads=[('tu', t), ('sg', t)], writes=[('ACTT', jj)])
                ak = [('ACTT', jj) for jj in range(8)]
                for st_ in range(NSUB):
                    yj = yc[0] % 2
                    yc[0] += 1
                    for nb in range(2):
                        for jj in range(8):
                            S.op('pe', lambda e: e.matmul(psum[4 + nb][:], ACTT[:, jj, st_ * 128:(st_ + 1) * 128], W2[j][:, jj, nb * 512:(nb + 1) * 512],
                                                          start=(jj == 0), stop=(jj == 7)), reads=ak + [('W2', j, jj // 2)], writes=[PB(4 + nb)])
                        cs = slice(nb * 512, (nb + 1) * 512)
                        S.op('dve', lambda e: e.tensor_tensor(out=YT[yj][:, cs], in0=psum[4 + nb][:], in1=B2R[j][:, cs], op=ALU.add), reads=[('B2R', j)],
                             writes=[PB(4 + nb), ('YT', yj, nb)])
                    r0 = b * SB + st_ * 128
                    S.dma('sp', lambda e: e.dma_start(out=ys_d[r0:r0 + 128, :], in_=YT[yj][:]), reads=[('YT', yj, 0), ('YT', yj, 1)], writes=[('ys', b, st_)])
        S.barrier()

    def phase_combine(layer, tiles, final):
        WG, SLOTI = RT["WG"], RT["SLOTI"]
        with contextlib.ExitStack() as es:
            def al(name, shape, dt):
                return es.enter_context(SBT(name, shape, dt))
            G2 = al("G2", [128, 2, D], F32)
            FG = al("FG", [128, D], F32)
            xt = [al("cxt%d" % j, [128, D], F32) for j in range(2)]
            Y = [[al("cY%d_%d" % (j, k), [128, D], F32) for k in range(4)] for j in range(2)]
            acc = [al("cacc%d" % j, [128, D], F32) for j in range(2)]
            sq = al("csq", [128, D], F32)
            st = al("cst", [128, 8], F32)
            for r in range(2):
                bcast_row(G2[:, r, :], modrow[layer, r:r + 1, 5 * D:6 * D], ('G2', r))
            if final:
                bcast_row(FG[:], I("final_g")[0:1, :], 'FG')
            for n, i in enumerate(tiles):
                j = n % 2
                r = 1 if i < 2 else 0
                rows = slice(i * 128, (i + 1) * 128)
                S.dma('sp', lambda e: e.dma_start(out=xt[j][:], in_=xres[rows, :]), writes=[('cxt', j)])
                for k in range(4):
                    off = bass.IndirectOffsetOnAxis(ap=SLOTI[:, i, k:k + 1], axis=0)
                    S.dma('pool', lambda e: e.indirect_dma_start(out=Y[j][k][:], out_offset=None, in_=ys_d, in_offset=off), writes=[('cY', j, k)])
                S.op('dve', lambda e: e.tensor_scalar(out=acc[j][:], in0=Y[j][0][:], scalar1=WG[:, i, 0:1], scalar2=None, op0=ALU.mult),
                     reads=[('cY', j, 0)], writes=[('cacc', j)])
                for k in range(1, 4):
                    S.op('dve', lambda e: e.scalar_tensor_tensor(out=acc[j][:], in0=Y[j][k][:], scalar=WG[:, i, k:k + 1], in1=acc[j][:],
                                                                 op0=ALU.mult, op1=ALU.add), reads=[('cY', j, k)], writes=[('cacc', j)])
                S.op('pool', lambda e: e.tensor_tensor(out=acc[j][:], in0=acc[j][:], in1=G2[:, r, :], op=ALU.mult), reads=[('G2', r)], writes=[('cacc', j)])
                S.op('pool', lambda e: e.tensor_tensor(out=acc[j][:], in0=acc[j][:], in1=xt[j][:], op=ALU.add), reads=[('cxt', j)], writes=[('cacc', j)])
                if not final:
                    S.dma('act', lambda e: e.dma_start(out=xres[rows, :], in_=acc[j][:]), reads=[('cacc', j)], writes=[('xres', i)])
                else:
                    S.op('act', lambda e: e.activation(out=sq[:], in_=acc[j][:], func=AF.Square, accum_out=st[:, 0:1]), reads=[('cacc', j)], writes=['csq', 'cst'])
                    S.op('dve', lambda e: e.tensor_scalar(out=st[:, 1:2], in0=st[:, 0:1], scalar1=1.0 / D, scalar2=EPS, op0=ALU.mult, op1=ALU.add), writes=['cst'])
                    S.op('act', lambda e: e.activation(out=st[:, 2:3], in_=st[:, 1:2], func=AF.Sqrt), writes=['cst'])
                    S.op('dve', lambda e: e.reciprocal(st[:, 3:4], st[:, 2:3]), writes=['cst'])
                    S.op('dve', lambda e: e.scalar_tensor_tensor(out=acc[j][:], in0=acc[j][:], scalar=st[:, 3:4], in1=FG[:], op0=ALU.mult, op1=ALU.mult),
                         reads=['FG'], writes=[('cacc', j), 'cst'])
                    S.dma('act', lambda e: e.dma_start(out=out_d[(i - 2) * 128:(i - 1) * 128, :], in_=acc[j][:]), reads=[('cacc', j)], writes=[('out', i)])
        S.barrier()

    qT_d = dscr("qT_d", [D, 4096], BF16)
    kT_d = dscr("kT_d", [256, T], BF16)
    vA_d = dscr("vA_d", [T, 256], BF16)
    oT_d = dscr("oT_d", [D, 4096], BF16)

    def phase_A1(uT):
        wv = I("od_w_in").rearrange("(kc p) n -> p kc n", p=128)
        with contextlib.ExitStack() as es:
            def al(name, shape, dt):
                return es.enter_context(SBT(name, shape, dt))
            W = al("aW", [128, 8, 1536], BF16)
            GQ = al("aGQ", [128, 128], F32)
            GK = al("aGK", [128, 128], F32)
            COS = [al("aCOS%d" % j, [128, 128], F32) for j in range(2)]
            SIN = [al("aSIN%d" % j, [128, 128], F32) for j in range(2)]
            xf = [al("axf%d" % j, [128, 1536], F32) for j in range(2)]
            sq = al("asq", [128, 1280], F32)
            st = al("ast", [128, 40], F32)
            kn = al("akn", [128, 1280], F32)
            t1 = al("at1", [128, 1280], F32)
            t2 = al("at2", [128, 1280], F32)
            kr = [al("akr%d" % j, [128, 1280], BF16) for j in range(2)]
            vb = [al("avb%d" % j, [128, 256], BF16) for j in range(2)]
            xT = [al("axT%d" % j, [128, 10, 128], BF16) for j in range(2)]
            for blk in range(3):
                S.dma('pool', lambda e: e.dma_start(out=W[:, :, blk * 512:(blk + 1) * 512], in_=wv[:, :, blk * 512:(blk + 1) * 512]), writes=[('aW', blk)])
            bcast_row(GQ[:], I("od_qk_g")[0:1, :], 'aGQ')
            bcast_row(GK[:], I("od_qk_g")[1:2, :], 'aGK')
            for i in range(NT):
                lat = i >= 2
                j = i % 2
                rows = slice(i * 128, (i + 1) * 128)
                blks = [0, 1, 2] if lat else [2]
                h0 = 0 if lat else 8
                if lat:
                    tr = slice((i - 2) * 128, (i - 1) * 128)
                    S.dma('pool', lambda e: e.dma_start(out=COS[j][:], in_=I("rope_cos")[tr, :]), writes=[('aCOS', j)])
                    S.dma('pool', lambda e: e.dma_start(out=SIN[j][:], in_=I("rope_sin")[tr, :]), writes=[('aSIN', j)])
                for blk in blks:
                    pb = blk
                    for kc in range(8):
                        S.op('pe', lambda e: e.matmul(psum[pb][:], uT[:, kc, rows], W[:, kc, blk * 512:(blk + 1) * 512], start=(kc == 0), stop=(kc == 7)),
                             reads=[('aW', blk)], writes=[PB(pb)])
                    S.op('act', lambda e: e.copy(xf[j][:, blk * 512:(blk + 1) * 512], psum[pb][:]), writes=[PB(pb), ('axf', j, blk)])
                S.op('pool', lambda e: e.tensor_copy(vb[j][:], xf[j][:, 1280:1536]), reads=[('axf', j, 2)], writes=[('avb', j)])
                S.dma('sp', lambda e: e.dma_start(out=vA_d[rows, :], in_=vb[j][:]), reads=[('avb', j)], writes=[('vA', i)])
                c0 = h0 * 128
                nh = 10 - h0
                S.op('act', lambda e: e.activation(out=sq[:, c0:1280], in_=xf[j][:, c0:1280], func=AF.Square),
                     reads=[('axf', j, 0), ('axf', j, 1), ('axf', j, 2)], writes=['asq'])
                S.op('dve', lambda e: e.tensor_reduce(out=st[:, h0:10], in_=sq[:, c0:1280].rearrange("p (h d) -> p h d", d=128), axis=AX.X, op=ALU.add),
                     reads=['asq'], writes=['ast'])
                S.op('dve', lambda e: e.tensor_scalar(out=st[:, 10 + h0:20], in0=st[:, h0:10], scalar1=1.0 / 128, scalar2=EPS, op0=ALU.mult, op1=ALU.add), writes=['ast'])
                S.op('act', lambda e: e.activation(out=st[:, 20 + h0:30], in_=st[:, 10 + h0:20], func=AF.Sqrt), writes=['ast'])
                S.op('dve', lambda e: e.reciprocal(st[:, 30 + h0:40], st[:, 20 + h0:30]), writes=['ast'])
                for h in range(h0, 10):
                    hs = slice(h * 128, (h + 1) * 128)
                    G = GQ if h < 8 else GK
                    eng = 'dve' if h % 2 == 0 else 'pool'
                    if lat:
                        S.op('dve', lambda e: e.scalar_tensor_tensor(out=kn[:, hs], in0=xf[j][:, hs], scalar=st[:, 30 + h:31 + h], in1=G[:], op0=ALU.mult, op1=ALU.mult),
                             reads=['aGQ', 'aGK', 'ast'], writes=[('akn', h)])
                        knv = kn[:, hs].rearrange("p (b c) -> p b c", b=2)
                        t2v = t2[:, hs].rearrange("p (b c) -> p b c", b=2)
                        snv = SIN[j][:].rearrange("p (b c) -> p b c", b=2)
                        S.op(eng, lambda e: e.tensor_tensor(out=t1[:, hs], in0=kn[:, hs], in1=COS[j][:], op=ALU.mult), reads=[('akn', h), ('aCOS', j)], writes=[('at1', h)])
                        S.op('pool', lambda e: e.tensor_tensor(out=t2v[:, :, 0:32], in0=knv[:, :, 32:64], in1=snv[:, :, 0:32], op=ALU.mult),
                             reads=[('akn', h), ('aSIN', j)], writes=[('at2', h)])
                        S.op('pool', lambda e: e.tensor_tensor(out=t2v[:, :, 32:64], in0=knv[:, :, 0:32], in1=snv[:, :, 32:64], op=ALU.mult),
                             reads=[('akn', h), ('aSIN', j)], writes=[('at2', h)])
                        S.op('dve', lambda e: e.tensor_tensor(out=kr[j][:, hs], in0=t1[:, hs], in1=t2[:, hs], op=ALU.add), reads=[('at1', h), ('at2', h)],
                             writes=[('akr', j, h)])
                    else:
                        S.op('dve', lambda e: e.scalar_tensor_tensor(out=kr[j][:, hs], in0=xf[j][:, hs], scalar=st[:, 30 + h:31 + h], in1=G[:], op0=ALU.mult, op1=ALU.mult),
                             reads=['aGQ', 'aGK', 'ast'], writes=[('akr', j, h)])
                if lat:
                    for h in range(8):
                        S.op('pe', lambda e: e.transpose(psb[0][:, h * 128:(h + 1) * 128], kr[j][:, h * 128:(h + 1) * 128], identb[:]),
                             reads=[('akr', j, h)], writes=[PBB(0)])
                    S.op('act', lambda e: e.copy(xT[j][:, 0:8, :].rearrange("p a b -> p (a b)"), psb[0][:]), writes=[PBB(0), ('axT', j, 0)])
                    S.dma('sp', lambda e: e.dma_start(out=qT_d[:, (i - 2) * 128:(i - 1) * 128].rearrange("(h d) t -> d h t", d=128), in_=xT[j][:, 0:8, :]),
                          reads=[('axT', j, 0)], writes=[('qT_d', i)])
                for h in range(8, 10):
                    S.op('pe', lambda e: e.transpose(psb[1][:, (h - 8) * 128:(h - 7) * 128], kr[j][:, h * 128:(h + 1) * 128], identb[:]),
                         reads=[('akr', j, h)], writes=[PBB(1)])
                S.op('act', lambda e: e.copy(xT[j][:, 8:10, :].rearrange("p a b -> p (a b)"), psb[1][:, 0:256]), writes=[PBB(1), ('axT', j, 1)])
                S.dma('sp', lambda e: e.dma_start(out=kT_d[:, rows].rearrange("(h d) t -> d h t", d=128), in_=xT[j][:, 8:10, :]),
                      reads=[('axT', j, 1)], writes=[('kT_d', i)])
        S.barrier()

    def phase_A2():
        scale = 128.0 ** -0.5
        with contextlib.ExitStack() as es:
            def al(name, shape, dt):
                return es.enter_context(SBT(name, shape, dt))
            KT = al("bKT", [128, T], BF16)
            V = al("bV", [128, NT, 128], BF16)
            onesb = al("bones", [128, 128], BF16)
            QT4 = [al("bQT%d" % j, [128, 4, 128], BF16) for j in range(2)]
            PT = [al("bPT%d" % j, [128, 512], BF16) for j in range(4)]
            rs = [al("brs%d" % j, [128, 512], F32) for j in range(2)]
            ot = [al("bot%d" % j, [128, 4, 128], BF16) for j in range(2)]
            S.op('pool', lambda e: e.memset(onesb[:], 1.0), writes=['bones'])
            pc = 0
            it = 0
            for g in range(2):
                S.dma('sp', lambda e: e.dma_start(out=KT[:], in_=kT_d[g * 128:(g + 1) * 128, :]), writes=['bKT'])
                S.dma('sp', lambda e: e.dma_start(out=V[:], in_=vA_d[:, g * 128:(g + 1) * 128].rearrange("(n p) d -> p n d", p=128)), writes=['bV'])
                for qi in range(32):
                    j = it % 2
                    it += 1
                    bo, bs_ = 2 + 2 * j, 3 + 2 * j
                    S.dma('sp', lambda e: e.dma_start(out=QT4[j][:], in_=qT_d[g * 512:(g + 1) * 512, qi * 128:(qi + 1) * 128].rearrange("(h d) t -> d h t", d=128)),
                          writes=[('bQT', j)])

                    def st_mm(kt):
                        bank = kt % 2
                        S.op('pe', lambda e: e.matmul(psum[bank][:], KT[:, kt * 128:(kt + 1) * 128], QT4[j][:].rearrange("p a b -> p (a b)"), start=True, stop=True),
                             reads=['bKT', ('bQT', j)], writes=[PB(bank)])
                    st_mm(0)
                    for kt in range(NT):
                        bank = kt % 2
                        p_ = pc % 4
                        pc += 1
                        S.op('act', lambda e: e.activation(out=PT[p_][:], in_=psum[bank][:], func=AF.Exp, scale=scale), writes=[PB(bank), ('bPT', p_)])
                        if kt + 1 < NT:
                            st_mm(kt + 1)
                        S.op('pe', lambda e: e.matmul(psum[bo][:], V[:, kt, :], PT[p_][:], start=(kt == 0), stop=(kt == NT - 1)),
                             reads=[('bPT', p_), 'bV'], writes=[PB(bo)])
                        S.op('pe', lambda e: e.matmul(psum[bs_][:], onesb[:], PT[p_][:], start=(kt == 0), stop=(kt == NT - 1)),
                             reads=[('bPT', p_), 'bones'], writes=[PB(bs_)])
                    S.op('dve', lambda e: e.reciprocal(rs[j][:], psum[bs_][:]), writes=[PB(bs_), ('brs', j)])
                    S.op('dve', lambda e: e.tensor_tensor(out=ot[j][:].rearrange("p a b -> p (a b)"), in0=psum[bo][:], in1=rs[j][:], op=ALU.mult),
                         reads=[('brs', j)], writes=[PB(bo), ('bot', j)])
                    S.dma('pool', lambda e: e.dma_start(out=oT_d[g * 512:(g + 1) * 512, qi * 128:(qi + 1) * 128].rearrange("(h d) t -> d h t", d=128), in_=ot[j][:]),
                          reads=[('bot', j)], writes=[('oT_d', g, qi)])
        S.barrier()

    def phase_A3():
        with contextlib.ExitStack() as es:
            def al(name, shape, dt):
                return es.enter_context(SBT(name, shape, dt))
            WO = al("cWO", [128, 8, D], BF16)
            G1 = al("cG1", [128, D], F32)
            OT = [al("cOT%d" % j, [128, 8, 128], BF16) for j in range(2)]
            xt = [al("cx%d" % j, [128, D], F32) for j in range(2)]
            tmp = al("ctmp", [128, D], F32)
            S.dma('pool', lambda e: e.dma_start(out=WO[:], in_=I("od_w_out").rearrange("(kc p) n -> p kc n", p=128)), writes=['cWO'])
            bcast_row(G1[:], modrow[1, 0:1, 2 * D:3 * D], 'cG1')
            for qi in range(32):
                j = qi % 2
                rows = slice((qi + 2) * 128, (qi + 3) * 128)
                S.dma('sp', lambda e: e.dma_start(out=OT[j][:], in_=oT_d[:, qi * 128:(qi + 1) * 128].rearrange("(h d) t -> d h t", d=128)), writes=[('cOT', j)])
                S.dma('sp', lambda e: e.dma_start(out=xt[j][:], in_=xres[rows, :]), writes=[('cx', j)])
                for nb in range(2):
                    pb = (2 * qi + nb) % 4
                    for kc in range(8):
                        S.op('pe', lambda e: e.matmul(psum[pb][:], OT[j][:, kc, :], WO[:, kc, nb * 512:(nb + 1) * 512], start=(kc == 0), stop=(kc == 7)),
                             reads=[('cOT', j), 'cWO'], writes=[PB(pb)])
                    cs = slice(nb * 512, (nb + 1) * 512)
                    S.op('dve', lambda e: e.tensor_tensor(out=tmp[:, cs], in0=psum[pb][:], in1=G1[:, cs], op=ALU.mult), reads=['cG1'], writes=[PB(pb), ('ctmp', nb)])
                    S.op('pool', lambda e: e.tensor_tensor(out=tmp[:, cs], in0=tmp[:, cs], in1=xt[j][:, cs], op=ALU.add), reads=[('cx', j)], writes=[('ctmp', nb)])
                S.dma('pool', lambda e: e.dma_start(out=xres[rows, :], in_=tmp[:]), reads=[('ctmp', 0), ('ctmp', 1)], writes=[('xres', qi)])
        S.barrier()

    def small_dump():
        W_ = NT * 32 + NT * 4 + NT * 4 + 2 * NBMAX
        dl = dscr("dsmall_scr", [128, W_], F32)
        with SBT("dsm", [128, W_], F32) as dsm:
            o = 0
            for nm, n in (("LOG", NT * 32), ("WG", NT * 4), ("SLOTI", NT * 4)):
                S.op('dve', lambda e: e.tensor_copy(dsm[:, o:o + n], RT[nm][:].rearrange("p a b -> p (a b)")), writes=['dsm'])
                o += n
            for nm in ("IDXI", "BIDX"):
                S.op('dve', lambda e: e.tensor_copy(dsm[:, o:o + NBMAX], RT[nm][:]), writes=['dsm'])
                o += NBMAX
            S.dma('sp', lambda e: e.dma_start(out=dl, in_=dsm[:]), reads=['dsm'], writes=['dl'])
            S.barrier()
        return ('small', dl, [128, W_], F32)

    def copy_xin_to_xres():
        with SBT("cpx", [128, D], F32) as cpx:
            for i in range(NT):
                S.dma('sp', lambda e: e.dma_start(out=cpx[:], in_=I("xin")[i * 128:(i + 1) * 128, :]), writes=['cpx'])
                S.dma('sp', lambda e: e.dma_start(out=xres[i * 128:(i + 1) * 128, :], in_=cpx[:]), reads=['cpx'], writes=[('xres', i)])
        S.barrier()

    phase_mod()
    if stop_after == 'mod':
        return finish_dbg([('modrow', modrow.rearrange("a b c -> (a b) c"), [4, 6 * D], F32)])

    if stop_after not in ('A_only', 'M1_only'):
        uT_guard = SBT("uT", [128, 8, T], BF16)
        uT = uT_guard.__enter__()
        phase_norm_T(0, I("xin"), uT)
        phase_E1(uT)
        if stop_after == 'E1':
            return finish_dbg([('qkT', qkT, [2048, T], BF16), ('v_tm', v_tm, [T, D], BF16), ('so_tm', so_tm, [T, D], BF16),
                               ('g_tm', g_tm, [T, 16], F32), ('xrT', xrT, [D, T], F32), ('ygT', ygT, [D, T], BF16)])
        uT_guard.__exit__(None, None, None)
        phase_E2()
        if stop_after == 'E2':
            return finish_dbg([('hlT', hlT, [D, T], BF16)])
        phase_E3()
        if stop_after == 'E3':
            return finish_dbg([('hm_f', hm_d[0], [T, D], F32), ('hm_b', hm_d[1], [T, D], F32)])
        phase_E4()
        if stop_after == 'E4':
            return finish_dbg([('xres', xres, [T, D], F32)])
        NB0 = (4 * T) // SB + 32
        phase_N2(0, range(NT), RT["LOG"])
        phase_route(range(NT), NB0)
        phase_scatter(range(NT), NB0)
        if stop_after == 'R0':
            return finish_dbg([small_dump(), ('xs', xs_d, [NBMAX * SB, D], BF16), ('v2', v2_tm, [T, D], BF16)])
        phase_moe(0, NB0)
        phase_combine(0, range(NT), False)
        if stop_after == 'M0':
            return finish_dbg([('xres', xres, [T, D], F32)])
    else:
        copy_xin_to_xres()

    if stop_after != 'M1_only':
        uT_guard = SBT("uT", [128, 8, T], BF16)
        uT = uT_guard.__enter__()
        phase_norm_T(1, xres, uT)
        phase_A1(uT)
        uT_guard.__exit__(None, None, None)
        phase_A2()
        phase_A3()
        if stop_after in ('A', 'A_only'):
            return finish_dbg([('xres', xres, [T, D], F32)])
    NB1 = (4 * 4096) // SB + 32
    lat_tiles = range(2, NT)
    phase_N2(1, lat_tiles, RT["LOG"])
    phase_route(lat_tiles, NB1)
    phase_scatter(lat_tiles, NB1)
    phase_moe(1, NB1)
    phase_combine(1, lat_tiles, True)
    if stop_after == 'M1_only':
        return finish_dbg([('outc', out_d, [4096, D], F32)])
    S.barrier()
    return nc, dbg_out, used_inputs


def host_inputs(b, inp, names=None):
    f = lambda a: np.ascontiguousarray(a, dtype=np.float32)
    want = lambda k: names is None or k in names
    m = {}
    if want("xin"):
        m["xin"] = f(np.concatenate([inp["ctx"][b], inp["x"][b]], axis=0))
    if want("ccols"):
        m["ccols"] = f(np.concatenate([inp["c"][b].reshape(8, 128).T, inp["c_ctx"].reshape(8, 128).T], axis=1))
    for k in ["mod_w", "mod_b", "norm1_g", "norm2_g", "moe_w_r", "moe_b_r"]:
        if want(k):
            m[k] = f(inp[k])
    if want("final_g"):
        m["final_g"] = f(inp["final_g"].reshape(1, D))
    if want("ev_w_in"):
        m["ev_w_in"] = f(inp["ev_w_in"][0])
    if want("ev_qkcw"):
        qk = np.concatenate([inp["ev_qk_conv_w"][0], inp["ev_qk_conv_b"][0][None]], axis=0)
        m["ev_qkcw"] = f(qk.T.reshape(16, 128, 5).transpose(1, 0, 2))
    if want("ev_gate_b"):
        m["ev_gate_b"] = f(inp["ev_gate_b"][0].reshape(1, 16))
    if want("ev_mnorm_g"):
        m["ev_mnorm_g"] = f(inp["ev_mnorm_g"][0].reshape(1, D))
    if want("ev_lrucw"):
        lc = np.concatenate([inp["ev_lru_conv_w"][0], inp["ev_lru_conv_b"][0][None]], axis=0)
        m["ev_lrucw"] = f(lc.T.reshape(8, 128, 5).transpose(1, 0, 2))
    if want("ev_lru_w") or want("ev_lru_b"):
        lw = np.zeros((4, 8, 128, 128), np.float32)
        lb = np.zeros((128, 8, 4), np.float32)
        for z in range(2):
            for gi, (wk, bk) in enumerate([("ev_lru_wa", "ev_lru_ba"), ("ev_lru_wx", "ev_lru_bx")]):
                for cc_ in range(8):
                    for h in range(2):
                        n = cc_ * 2 + h
                        lw[z * 2 + gi, cc_, h * 64:(h + 1) * 64, h * 64:(h + 1) * 64] = inp[wk][0, z, n]
                        lb[h * 64:(h + 1) * 64, cc_, z * 2 + gi] = inp[bk][0, z, n]
        m["ev_lru_w"] = lw
        m["ev_lru_b"] = lb
    if want("ev_lru_lam"):
        m["ev_lru_lam"] = f(inp["ev_lru_lam"][0].reshape(2, 8, 128).transpose(2, 1, 0))
    if want("ev_w_out"):
        m["ev_w_out"] = f(inp["ev_w_out"][0])
    if want("od_w_in"):
        m["od_w_in"] = f(inp["od_w_in"][0])
    if want("od_qk_g"):
        m["od_qk_g"] = f(np.stack([inp["od_q_norm_g"][0], inp["od_k_norm_g"][0]]))
    if want("od_w_out"):
        m["od_w_out"] = f(inp["od_w_out"][0])
    if want("rope_cos") or want("rope_sin"):
        pos = np.arange(4096)
        row = (pos // 64).astype(np.float32)
        col = (pos % 64).astype(np.float32)
        inv = (10000.0 ** (-np.arange(0, 64, 2, dtype=np.float32) / 64)).astype(np.float32)
        ar = row[:, None] * inv[None]
        ac = col[:, None] * inv[None]
        m["rope_cos"] = f(np.concatenate([np.cos(ar), np.cos(ar), np.cos(ac), np.cos(ac)], axis=1))
        m["rope_sin"] = f(np.concatenate([-np.sin(ar), np.sin(ar), -np.sin(ac), np.sin(ac)], axis=1))
    for l in range(2):
        if want("moe_w1_%d" % l):
            m["moe_w1_%d" % l] = f(inp["moe_w1"][l]).reshape(32 * 128, 8, 2048)
        if want("moe_w2_%d" % l):
            m["moe_w2_%d" % l] = f(inp["moe_w2"][l]).reshape(32 * 128, 8, 1024)
    for l in range(2):
        if want("moe_b1_%d" % l):
            m["moe_b1_%d" % l] = f(inp["moe_b1"][l].reshape(32, 2, 128, 8).transpose(0, 2, 1, 3).reshape(32 * 128, 16))
        if want("moe_b2_%d" % l):
            m["moe_b2_%d" % l] = f(inp["moe_b2"][l])
    if names is not None:
        m = {k: v for k, v in m.items() if k in names}
    return m


_CACHE = {}


def kernel(**inputs):
    if 'nc' not in _CACHE:
        _CACHE['nc'] = build()
    nc, _, used = _CACHE['nc']
    names = set(used.keys())
    in_maps = [host_inputs(b, inputs, names) for b in range(8)]
    res = run_bass_kernel_spmd(nc, in_maps, core_ids=list(range(8)))
    return np.stack([r["out"] for r in res.results], axis=0).astype(np.float32)
```

```python
import contextlib
import numpy as np
import concourse.bass as bass
import concourse.mybir as mybir
from concourse.bass_utils import run_bass_kernel_spmd

F32 = mybir.dt.float32
BF16 = mybir.dt.bfloat16
I32 = mybir.dt.int32
U32 = mybir.dt.uint32
ALU = mybir.AluOpType
AF = mybir.ActivationFunctionType

T = 4352
NT = 34
D = 1024
NCTX = 256
EPS = 1e-6


class Sched:
    NSLOT = 6

    def __init__(self, nc):
        self.nc = nc
        self.engs = {'pe': nc.tensor, 'act': nc.scalar, 'dve': nc.vector,
                     'pool': nc.gpsimd, 'sp': nc.sync}
        self.sem = {}
        self.cnt = {}
        for k in self.engs:
            self.sem[k] = nc.alloc_semaphore('s_' + k)
            self.cnt[k] = 0
        self.dslots = {}
        self.dcnt = {}
        self.dnext = {}
        self.nslot = {'sp': 8, 'pool': 8, 'act': 2}
        for q in ('sp', 'pool', 'act'):
            self.dslots[q] = [nc.alloc_semaphore('d_%s%d' % (q, i)) for i in range(self.nslot[q])]
            self.dcnt[q] = [0] * self.nslot[q]
            self.dnext[q] = 0
        self.waited = {k: {} for k in self.engs}
        self.res = {}

    def _semof(self, tok):
        if tok[0] == 'e':
            return self.sem[tok[1]]
        return self.dslots[tok[1][0]][tok[1][1]]

    def _wait(self, e, tok):
        key = (tok[0], tok[1])
        if self.waited[e].get(key, 0) >= tok[2]:
            return
        self.engs[e].wait_ge(self._semof(tok), tok[2])
        self.waited[e][key] = tok[2]

    def _deps(self, e, reads, writes):
        deps = []
        for r in reads:
            st = self.res.get(r)
            if st and st['w'] is not None:
                deps.append(st['w'])
        for w in writes:
            st = self.res.get(w)
            if st:
                if st['w'] is not None:
                    deps.append(st['w'])
                deps.extend(st['r'].values())
        for tok in deps:
            if e == 'pe' and tok[0] == 'e' and tok[1] == 'pe':
                continue
            self._wait(e, tok)

    def _commit(self, tok, reads, writes):
        for r in reads:
            st = self.res.setdefault(r, {'w': None, 'r': {}})
            st['r'][(tok[0], tok[1])] = tok
        for w in writes:
            self.res[w] = {'w': tok, 'r': {}}

    def op(self, e, fn, reads=(), writes=()):
        self._deps(e, reads, writes)
        inst = fn(self.engs[e])
        self.cnt[e] += 1
        inst.then_inc(self.sem[e], 1)
        self._commit(('e', e, self.cnt[e]), reads, writes)
        return inst

    def dma(self, q, fn, reads=(), writes=()):
        s = self.dnext[q]
        self.dnext[q] = (s + 1) % self.nslot[q]
        if self.dcnt[q][s] > 0:
            self._wait(q, ('d', (q, s), 16 * self.dcnt[q][s]))
        self._deps(q, reads, writes)
        inst = fn(self.engs[q])
        self.dcnt[q][s] += 1
        inst.then_inc(self.dslots[q][s], 16)
        self._commit(('d', (q, s), 16 * self.dcnt[q][s]), reads, writes)
        return inst

    def barrier(self):
        toks = []
        for q in self.dslots:
            for s in range(self.nslot[q]):
                if self.dcnt[q][s] > 0:
                    toks.append(('d', (q, s), 16 * self.dcnt[q][s]))
        for k in self.engs:
            if self.cnt[k] > 0:
                toks.append(('e', k, self.cnt[k]))
        for e in self.engs:
            for tok in toks:
                self._wait(e, tok)
        self.res = {}


class Rot:
    def __init__(self, bufs, name):
        self.bufs = bufs
        self.name = name
        self.i = 0

    def next(self):
        b = self.bufs[self.i % len(self.bufs)]
        k = (self.name, self.i % len(self.bufs))
        self.i += 1
        return b, k


AX = mybir.AxisListType


def build(stop_after=None, layers=(0, 1)):
    nc = bass.Bass("TRN2", target_bir_lowering=False)
    S = Sched(nc)
    used_inputs = {}
    _ctr = [0]

    def SBT(name, shape, dt):
        _ctr[0] += 1
        return nc.sbuf_tensor("%s_u%d" % (name, _ctr[0]), shape, dt)

    IN_SPECS = {
        "xin": [T, D], "ccols": [128, 16], "mod_w": [2, D, 6 * D], "mod_b": [2, 6 * D],
        "norm1_g": [2, D], "norm2_g": [2, D], "final_g": [1, D],
        "ev_w_in": [D, 6160], "ev_qkcw": [128, 16, 5], "ev_gate_b": [1, 16], "ev_mnorm_g": [1, D],
        "ev_lrucw": [128, 8, 5], "ev_lru_w": [4, 8, 128, 128], "ev_lru_b": [128, 8, 4], "ev_lru_lam": [128, 8, 2],
        "ev_w_out": [2 * D, D], "od_w_in": [D, 1536], "od_qk_g": [2, 128], "od_w_out": [D, D],
        "rope_cos": [4096, 128], "rope_sin": [4096, 128],
        "moe_w_r": [2, D, 32], "moe_b_r": [2, 32],
        "moe_w1_0": [32 * 128, 8, 2048], "moe_w1_1": [32 * 128, 8, 2048],
        "moe_b1_0": [32 * 128, 16], "moe_b1_1": [32 * 128, 16],
        "moe_w2_0": [32 * 128, 8, 1024], "moe_w2_1": [32 * 128, 8, 1024],
        "moe_b2_0": [32, 1024], "moe_b2_1": [32, 1024],
    }

    def I(name):
        if name not in used_inputs:
            used_inputs[name] = nc.dram_tensor(name, list(IN_SPECS[name]), F32, kind="ExternalInput").ap()
        return used_inputs[name]

    def dscr(name, shape, dt=F32):
        return nc.dram_tensor(name, list(shape), dt, kind="Internal").ap()

    dbg_out = {}

    def dbgt(name, shape, dt=F32):
        dbg_out[name] = nc.dram_tensor("dbg_" + name, list(shape), dt, kind="ExternalOutput").ap()
        return dbg_out[name]

    def finish_dbg(pairs):
        S.barrier()
        for name, src, shape, dt in pairs:
            d = dbgt(name, shape, dt)
            rows, cols = shape[0], int(np.prod(shape[1:]))
            s2 = src if len(shape) == 2 else src.rearrange("a b c -> a (b c)")
            d2 = d if len(shape) == 2 else d.rearrange("a b c -> a (b c)")
            with SBT("dbgbuf_" + name, [128, cols], dt) as buf:
                for r0 in range(0, rows, 128):
                    n = min(128, rows - r0)
                    S.dma('sp', lambda e: e.dma_start(out=buf[0:n, :], in_=s2[r0:r0 + n, :]), writes=['dbgbuf'])
                    S.dma('sp', lambda e: e.dma_start(out=d2[r0:r0 + n, :], in_=buf[0:n, :]), reads=['dbgbuf'], writes=[('dbgo', name, r0)])
                S.barrier()
        return nc, dbg_out, used_inputs

    out_d = nc.dram_tensor("out", [4096, D], F32, kind="ExternalOutput").ap()
    modrow = dscr("modrow", [2, 2, 6 * D])
    xres = dscr("xres", [T, D])

    ident = nc.alloc_sbuf_tensor("ident", [128, 128], F32)
    identb = nc.alloc_sbuf_tensor("identb", [128, 128], BF16)
    ones1 = nc.alloc_sbuf_tensor("ones1", [1, 128], F32)
    onesm = nc.alloc_sbuf_tensor("onesm", [128, 128], F32)
    triF = nc.alloc_sbuf_tensor("triF", [128, 128], F32)
    triB = nc.alloc_sbuf_tensor("triB", [128, 128], F32)
    triS = nc.alloc_sbuf_tensor("triS", [128, 128], F32)
    selF = nc.alloc_sbuf_tensor("selF", [128, 128], F32)
    selB = nc.alloc_sbuf_tensor("selB", [128, 128], F32)
    pidx = nc.alloc_sbuf_tensor("pidx", [128, 1], F32)
    pidx_i = nc.alloc_sbuf_tensor("pidx_i", [128, 1], I32)
    S.op('pool', lambda e: e.memset(ident[:], 0.0), writes=['ident'])
    S.op('pool', lambda e: e.affine_select(ident[:], ident[:], pattern=[[-1, 128]], compare_op=ALU.not_equal,
                                          fill=1.0, base=0, channel_multiplier=1), reads=['ident'], writes=['ident'])
    S.op('dve', lambda e: e.tensor_copy(identb[:], ident[:]), reads=['ident'], writes=['identb'])
    S.op('pool', lambda e: e.memset(ones1[:], 1.0), writes=['ones1'])
    S.op('pool', lambda e: e.memset(onesm[:], 1.0), writes=['onesm'])

    def mk_mask(t, pattern, cm, base, cmp):
        S.op('pool', lambda e: e.memset(t[:], 1.0), writes=[t.name])
        S.op('pool', lambda e: e.affine_select(t[:], t[:], pattern=pattern, compare_op=cmp, fill=0.0, base=base,
                                              channel_multiplier=cm), writes=[t.name])

    mk_mask(triF, [[1, 128]], -1, 0, ALU.is_ge)
    mk_mask(triB, [[-1, 128]], 1, 0, ALU.is_ge)
    mk_mask(triS, [[1, 128]], -1, 0, ALU.is_gt)
    mk_mask(selF, [[0, 128]], 1, -127, ALU.is_equal)
    mk_mask(selB, [[0, 128]], 1, 0, ALU.is_equal)
    S.op('pool', lambda e: e.iota(pidx_i[:], pattern=[[0, 1]], base=0, channel_multiplier=1), writes=['pidx_i'])
    S.op('dve', lambda e: e.tensor_copy(pidx[:], pidx_i[:]), reads=['pidx_i'], writes=['pidx'])

    psum = [nc.alloc_psum_tensor("ps%d" % i, [128, 512], F32) for i in range(6)]
    psb = [nc.alloc_psum_tensor("psb%d" % i, [128, 1024], BF16) for i in range(2)]

    def PB(i):
        return ('psum', i)

    def PBB(i):
        return ('psb', i)

    def bcast_row(dst, src_row, key, q='sp'):
        S.dma(q, lambda e: e.dma_start(out=dst, in_=src_row.partition_broadcast(dst.shape[0])), writes=[key])

    def phase_mod():
        mod_w, mod_b = I("mod_w"), I("mod_b")
        with SBT("cc", [128, 16], F32) as cc, SBT("csil", [128, 16], F32) as csil, \
                SBT("mw0", [128, 8, 512], F32) as mw0, SBT("mw1", [128, 8, 512], F32) as mw1, \
                SBT("mb0", [1, 512], F32) as mb0, SBT("mb1", [1, 512], F32) as mb1, \
                SBT("mo0", [1, 512], F32) as mo0, SBT("mo1", [1, 512], F32) as mo1:
            S.dma('sp', lambda e: e.dma_start(out=cc[:], in_=I("ccols")), writes=['cc'])
            S.op('act', lambda e: e.activation(out=csil[:], in_=cc[:], func=AF.Silu), reads=['cc'], writes=['csil'])
            mws = Rot([mw0, mw1], 'mw')
            mbs = Rot([mb0, mb1], 'mb')
            mos = Rot([mo0, mo1], 'mo')
            pi = 0
            for layer in range(2):
                mwv = mod_w[layer].rearrange("(kc p) n -> p kc n", p=128)
                for cb in range(12):
                    mw, mwk = mws.next()
                    mb, mbk = mbs.next()
                    S.dma('sp', lambda e: e.dma_start(out=mw[:], in_=mwv[:, :, cb * 512:(cb + 1) * 512]), writes=[mwk])
                    S.dma('sp', lambda e: e.dma_start(out=mb[:], in_=mod_b[layer:layer + 1, cb * 512:(cb + 1) * 512]), writes=[mbk])
                    for r in range(2):
                        ps = psum[pi % 2]
                        pk = PB(pi % 2)
                        pi += 1
                        for kc in range(8):
                            S.op('pe', lambda e: e.matmul(ps[0:1, :], csil[:, r * 8 + kc:r * 8 + kc + 1], mw[:, kc, :],
                                                          start=(kc == 0), stop=False), reads=['csil', mwk], writes=[pk])
                        S.op('pe', lambda e: e.matmul(ps[0:1, :], ones1[0:1, 0:1], mb[:], start=False, stop=True),
                             reads=['ones1', mbk], writes=[pk])
                        mo, mok = mos.next()
                        S.op('act', lambda e: e.copy(mo[:], ps[0:1, :]), writes=[pk, mok])
                        S.dma('sp', lambda e: e.dma_start(out=modrow[layer, r:r + 1, cb * 512:(cb + 1) * 512], in_=mo[:]),
                              reads=[mok], writes=[('modrow', layer, r, cb)])
        S.barrier()

    def norm_tiles(layer, which, src, tiles, consume):
        gsrc = I("norm1_g") if which == 0 else I("norm2_g")
        sh_off = 0 if which == 0 else 3 * D
        sc_off = D if which == 0 else 4 * D
        with SBT("nA", [128, 2, D], F32) as A, SBT("nSH", [128, 2, D], F32) as SH, \
                SBT("nG", [128, D], F32) as G, \
                SBT("nx0", [128, D], F32) as x0, SBT("nx1", [128, D], F32) as x1, \
                SBT("nu0", [128, D], F32) as u0, SBT("nu1", [128, D], F32) as u1, \
                SBT("nsq", [128, 2, D], F32) as sq2, SBT("nst", [128, 2, 8], F32) as st2:
            bcast_row(G[:], gsrc[layer:layer + 1, :], 'nG')
            for r in range(2):
                bcast_row(A[:, r, :], modrow[layer, r:r + 1, sc_off:sc_off + D], ('nA', r))
                bcast_row(SH[:, r, :], modrow[layer, r:r + 1, sh_off:sh_off + D], ('nSH', r))
                S.op('dve', lambda e: e.scalar_tensor_tensor(out=A[:, r, :], in0=A[:, r, :], scalar=1.0, in1=G[:],
                                                             op0=ALU.add, op1=ALU.mult), reads=['nG'], writes=[('nA', r)])
            xs = Rot([x0, x1], 'nx')
            us = Rot([u0, u1], 'nu')
            tl = list(tiles)
            pend = {}

            def pre(n_):
                i = tl[n_]
                r = 1 if i < 2 else 0
                xt, xk = xs.next()
                ut, uk = us.next()
                sq = sq2[:, n_ % 2, :]
                st = st2[:, n_ % 2, :]
                NSQ, NST = ('nsq', n_ % 2), ('nst', n_ % 2)
                S.dma('sp', lambda e: e.dma_start(out=xt[:], in_=src[i * 128:(i + 1) * 128, :]), writes=[xk])
                S.op('act', lambda e: e.activation(out=sq, in_=xt[:], func=AF.Square, accum_out=st[:, 0:1]),
                     reads=[xk], writes=[NSQ, NST])
                S.op('dve', lambda e: e.tensor_scalar(out=st[:, 1:2], in0=st[:, 0:1], scalar1=1.0 / D, scalar2=EPS,
                                                      op0=ALU.mult, op1=ALU.add), writes=[NST])
                S.op('act', lambda e: e.activation(out=st[:, 2:3], in_=st[:, 1:2], func=AF.Sqrt), writes=[NST])
                S.op('dve', lambda e: e.reciprocal(st[:, 3:4], st[:, 2:3]), writes=[NST])
                S.op('dve', lambda e: e.scalar_tensor_tensor(out=ut[:], in0=xt[:], scalar=st[:, 3:4], in1=A[:, r, :],
                                                             op0=ALU.mult, op1=ALU.mult), reads=[xk, ('nA', r)], writes=[uk, NST])
                S.op('pool', lambda e: e.tensor_tensor(out=ut[:], in0=ut[:], in1=SH[:, r, :], op=ALU.add),
                     reads=[('nSH', r)], writes=[uk])
                pend[n_] = (i, ut, uk)

            pre(0)
            for n_ in range(len(tl)):
                if n_ + 1 < len(tl):
                    pre(n_ + 1)
                consume(*pend.pop(n_))

    def phase_norm_T(layer, src, uT, tiles=range(NT)):
        def consume(i, ut, uk):
            for half in range(2):
                pb = (i * 2 + half) % 4
                for j in range(4):
                    kc = half * 4 + j
                    S.op('pe', lambda e: e.transpose(psum[pb][:, j * 128:(j + 1) * 128], ut[:, kc * 128:(kc + 1) * 128], ident[:]),
                         reads=[uk, 'ident'], writes=[PB(pb)])
                S.op('act', lambda e: e.copy(uT[:, half * 4:half * 4 + 4, i * 128:(i + 1) * 128],
                                             psum[pb][:].rearrange("p (a b) -> p a b", a=4)),
                     writes=[PB(pb), ('uT', i, half)])
        norm_tiles(layer, 0, src, tiles, consume)
        S.barrier()

    qkT = dscr("qkT", [2048, T], BF16)
    v_tm = dscr("v_tm", [T, D], BF16)
    so_tm = dscr("so_tm", [T, D], BF16)
    g_tm = dscr("g_tm", [T, 16])
    xrT = dscr("xrT", [D, T])
    ygT = dscr("ygT", [D, T], BF16)
    hlT = dscr("hlT", [D, T], BF16)
    hm_d = [dscr("hm_f", [T, D]), dscr("hm_b", [T, D])]
    TG = [(0, 256)] + [(256 + 512 * j, 512) for j in range(8)]
    ZW = 4358
    CN = 4355

    def zcol(t0):
        return 2 + t0 if t0 < 256 else t0 + 5

    def phase_E1(uT):
        wv = I("ev_w_in").rearrange("(kc p) n -> p kc n", p=128)
        with SBT("wb0", [128, 8, 512], BF16) as wb0, SBT("wb1", [128, 8, 512], BF16) as wb1, \
                SBT("zp0", [128, ZW], F32) as zp0, SBT("zp1", [128, ZW], F32) as zp1, \
                SBT("co", [128, ZW], F32) as co, SBT("co_b", [128, ZW], F32) as co_b, \
                SBT("ob0", [128, ZW], BF16) as ob0, SBT("ob1", [128, ZW], BF16) as ob1, \
                SBT("cwq", [128, 16, 5], F32) as cwq, SBT("cwl", [128, 8, 5], F32) as cwl, \
                SBT("tms0", [128, 512], BF16) as tms0, SBT("tms1", [128, 512], BF16) as tms1, \
                SBT("wg", [128, 8, 16], BF16) as wg, SBT("gbr", [128, 16], F32) as gbr, \
                SBT("GT", [128, NT, 16], F32) as GT:
            S.dma('sp', lambda e: e.dma_start(out=cwq[:], in_=I("ev_qkcw")), writes=['cwq'])
            S.dma('sp', lambda e: e.dma_start(out=cwl[:], in_=I("ev_lrucw")), writes=['cwl'])
            S.op('pool', lambda e: e.memset(zp0[:], 0.0), writes=[('zp', 0)])
            S.op('pool', lambda e: e.memset(zp1[:], 0.0), writes=[('zp', 1)])
            wbs = Rot([wb0, wb1], 'wb')
            zps = Rot([zp0, zp1], 'zp')
            obs = Rot([ob0, ob1], 'ob')
            pi = [0]
            fm_blocks = [(c0, 'qk', c0 // 128) for c0 in range(0, 2048, 512)] + \
                        [(4112 + j * 512, 'xr', j * 4) for j in range(2)] + \
                        [(5136 + j * 512, 'yg', j * 4) for j in range(2)]
            chunks = []
            for c0, kind, cbase in fm_blocks:
                for jj in range(4):
                    chunks.append((c0, kind, cbase + jj, jj))
            cos_ = [co, co_b]
            state = {}

            def stage1(n):
                c0, kind, cidx, jj = chunks[n]
                if jj == 0:
                    wb, wbk = wbs.next()
                    S.dma('pool', lambda e: e.dma_start(out=wb[:], in_=wv[:, :, c0:c0 + 512]), writes=[wbk])
                    state['wb'] = (wb, wbk)
                wb, wbk = state['wb']
                zp, zpk = zps.next()
                cq = cos_[n % 2]
                ck = ('co', n % 2)
                for (t0, n_) in TG:
                    pb = pi[0] % 2
                    pi[0] += 1
                    for kc in range(8):
                        S.op('pe', lambda e: e.matmul(psum[pb][:, 0:n_], wb[:, kc, jj * 128:(jj + 1) * 128], uT[:, kc, t0:t0 + n_],
                                                      start=(kc == 0), stop=(kc == 7)), reads=[wbk], writes=[PB(pb)])
                    z0 = zcol(t0)
                    S.op('act', lambda e: e.copy(zp[:, z0:z0 + n_], psum[pb][:, 0:n_]), writes=[PB(pb), zpk])
                if kind in ('qk', 'xr'):
                    cw = cwq if kind == 'qk' else cwl
                    S.op('dve', lambda e: e.tensor_scalar(out=cq[:, 0:CN], in0=zp[:, 0:CN], scalar1=cw[:, cidx, 0:1], scalar2=None,
                                                          op0=ALU.mult), reads=[zpk, 'cwq', 'cwl'], writes=[ck])
                    for j in range(1, 4):
                        S.op('dve', lambda e: e.scalar_tensor_tensor(out=cq[:, 0:CN], in0=zp[:, j:j + CN], scalar=cw[:, cidx, j:j + 1],
                                                                     in1=cq[:, 0:CN], op0=ALU.mult, op1=ALU.add),
                             reads=[zpk], writes=[ck])
                state[n] = (zp, zpk, cq, ck)

            def stage2(n):
                c0, kind, cidx, jj = chunks[n]
                zp, zpk, cq, ck = state.pop(n)
                ob, obk = obs.next()
                if kind == 'qk':
                    S.op('act', lambda e: e.activation(out=ob[:, 0:CN], in_=cq[:, 0:CN], func=AF.Silu, bias=cwq[:, cidx, 4:5], scale=1.0),
                         reads=[ck], writes=[obk])
                    rows = qkT[cidx * 128:(cidx + 1) * 128, :]
                    S.dma('sp', lambda e: e.dma_start(out=rows[:, 0:256], in_=ob[:, 0:256]), reads=[obk], writes=[('qkT', cidx, 0)])
                    S.dma('sp', lambda e: e.dma_start(out=rows[:, 256:T], in_=ob[:, 259:CN]), reads=[obk], writes=[('qkT', cidx, 1)])
                elif kind == 'xr':
                    S.op('act', lambda e: e.activation(out=cq[:, 0:CN], in_=cq[:, 0:CN], func=AF.Identity, bias=cwl[:, cidx, 4:5], scale=1.0),
                         writes=[ck])
                    rows = xrT[cidx * 128:(cidx + 1) * 128, :]
                    S.dma('sp', lambda e: e.dma_start(out=rows[:, 0:256], in_=cq[:, 0:256]), reads=[ck], writes=[('xrT', cidx, 0)])
                    S.dma('sp', lambda e: e.dma_start(out=rows[:, 256:T], in_=cq[:, 259:CN]), reads=[ck], writes=[('xrT', cidx, 1)])
                else:
                    S.op('act', lambda e: e.activation(out=cq[:], in_=zp[:], func=AF.Square), reads=[zpk], writes=[ck])
                    S.op('dve', lambda e: e.tensor_scalar(out=cq[:], in0=cq[:], scalar1=0.044715, scalar2=1.0, op0=ALU.mult, op1=ALU.add),
                         writes=[ck])
                    S.op('dve', lambda e: e.tensor_tensor(out=cq[:], in0=cq[:], in1=zp[:], op=ALU.mult), reads=[zpk], writes=[ck])
                    S.op('act', lambda e: e.activation(out=cq[:], in_=cq[:], func=AF.Sigmoid, scale=1.5957691216), writes=[ck])
                    S.op('pool', lambda e: e.tensor_tensor(out=ob[:], in0=cq[:], in1=zp[:], op=ALU.mult), reads=[ck, zpk], writes=[obk])
                    rows = ygT[cidx * 128:(cidx + 1) * 128, :]
                    S.dma('sp', lambda e: e.dma_start(out=rows[:, 0:256], in_=ob[:, 2:258]), reads=[obk], writes=[('ygT', cidx, 0)])
                    S.dma('sp', lambda e: e.dma_start(out=rows[:, 256:T], in_=ob[:, 261:4357]), reads=[obk], writes=[('ygT', cidx, 1)])

            stage1(0)
            for n in range(len(chunks)):
                if n + 1 < len(chunks):
                    stage1(n + 1)
                stage2(n)
            tms = Rot([tms0, tms1], 'tms')
            for blk in range(4):
                c0 = 2048 + blk * 512
                wb, wbk = wbs.next()
                S.dma('pool', lambda e: e.dma_start(out=wb[:], in_=wv[:, :, c0:c0 + 512]), writes=[wbk])
                dst = v_tm if blk < 2 else so_tm
                dc0 = (blk % 2) * 512
                for i in range(NT):
                    pb = 2 + i % 2
                    for kc in range(8):
                        S.op('pe', lambda e: e.matmul(psum[pb][:], uT[:, kc, i * 128:(i + 1) * 128], wb[:, kc, :],
                                                      start=(kc == 0), stop=(kc == 7)), reads=[wbk], writes=[PB(pb)])
                    st_, stk = tms.next()
                    if blk < 2:
                        S.op('act', lambda e: e.copy(st_[:], psum[pb][:]), writes=[PB(pb), stk])
                    else:
                        S.op('act', lambda e: e.activation(out=st_[:], in_=psum[pb][:], func=AF.Sigmoid), writes=[PB(pb), stk])
                    S.dma('sp', lambda e: e.dma_start(out=dst[i * 128:(i + 1) * 128, dc0:dc0 + 512], in_=st_[:]), reads=[stk],
                          writes=[('tmout', blk, i)])
            S.dma('pool', lambda e: e.dma_start(out=wg[:], in_=wv[:, :, 4096:4112]), writes=['wg'])
            bcast_row(gbr[:], I("ev_gate_b")[0:1, :], 'gbr')
            for i in range(NT):
                pb = 4 + i % 2
                for kc in range(8):
                    S.op('pe', lambda e: e.matmul(psum[pb][:, 0:16], uT[:, kc, i * 128:(i + 1) * 128], wg[:, kc, :],
                                                  start=(kc == 0), stop=(kc == 7)), reads=['wg'], writes=[PB(pb)])
                S.op('dve', lambda e: e.tensor_tensor(out=GT[:, i, :], in0=psum[pb][:, 0:16], in1=gbr[:], op=ALU.add),
                     reads=['gbr'], writes=[PB(pb), 'GT'])
            S.dma('sp', lambda e: e.dma_start(out=g_tm.rearrange("(n p) c -> p n c", p=128), in_=GT[:]), reads=['GT'], writes=['g_tm'])
        S.barrier()

    def phase_E2():
        lw_d, lb_d, lam_d = I("ev_lru_w"), I("ev_lru_b"), I("ev_lru_lam")
        with SBT("lxr", [128, T], F32) as xr, SBT("lyg", [128, T], BF16) as yg, \
                SBT("lA", [128, T], F32) as A, SBT("lB", [128, T], F32) as Bx, \
                SBT("ltmp", [128, T], F32) as tmp, \
                SBT("lH0", [128, T], F32) as H0, SBT("lH1", [128, T], F32) as H1, \
                SBT("lho", [128, T], BF16) as ho, \
                SBT("lw", [128, 4, 128], F32) as lw, SBT("lb", [128, 8, 4], F32) as lb, \
                SBT("lam", [128, 8, 2], F32) as lam, SBT("cA", [128, 8, 2], F32) as cA:
            S.dma('sp', lambda e: e.dma_start(out=lb[:], in_=lb_d), writes=['lb'])
            S.dma('sp', lambda e: e.dma_start(out=lam[:], in_=lam_d), writes=['lam'])
            S.op('act', lambda e: e.activation(out=cA[:], in_=lam[:], func=AF.Exp, scale=-1.0), reads=['lam'], writes=['cA'])
            S.op('act', lambda e: e.activation(out=cA[:], in_=cA[:], func=AF.Ln, bias=1.0, scale=1.0), writes=['cA'])
            S.op('dve', lambda e: e.tensor_scalar(out=cA[:], in0=cA[:], scalar1=-8.0, scalar2=None, op0=ALU.mult), writes=['cA'])
            pi = 0
            for cc in range(8):
                S.dma('sp', lambda e: e.dma_start(out=xr[:], in_=xrT[cc * 128:(cc + 1) * 128, :]), writes=['xr'])
                S.dma('sp', lambda e: e.dma_start(out=yg[:], in_=ygT[cc * 128:(cc + 1) * 128, :]), writes=['yg'])
                S.dma('sp', lambda e: e.dma_start(out=lw[:], in_=lw_d[:, cc].rearrange("g k m -> k g m")), writes=['lw'])
                for z in range(2):
                    H = H0 if z == 0 else H1
                    hk = ('H', z)
                    for gi, dst, dk in ((0, A, 'A'), (1, Bx, 'Bx')):
                        for (t0, n) in TG:
                            pb = pi % 2
                            pi += 1
                            S.op('pe', lambda e: e.matmul(psum[pb][:, 0:n], lw[:, z * 2 + gi, :], xr[:, t0:t0 + n], start=True, stop=True),
                                 reads=['lw', 'xr'], writes=[PB(pb)])
                            S.op('act', lambda e: e.activation(out=dst[:, t0:t0 + n], in_=psum[pb][:, 0:n], func=AF.Sigmoid,
                                                               bias=lb[:, cc, z * 2 + gi:z * 2 + gi + 1], scale=1.0),
                                 reads=['lb'], writes=[PB(pb), dk])
                    S.op('act', lambda e: e.activation(out=A[:], in_=A[:], func=AF.Exp, scale=cA[:, cc, z:z + 1]), reads=['cA'], writes=['A'])
                    S.op('act', lambda e: e.activation(out=tmp[:], in_=A[:], func=AF.Square), reads=['A'], writes=['tmp'])
                    S.op('act', lambda e: e.activation(out=tmp[:], in_=tmp[:], func=AF.Sqrt, bias=1.0, scale=-1.0), writes=['tmp'])
                    S.op('pool', lambda e: e.tensor_tensor(out=Bx[:], in0=Bx[:], in1=xr[:], op=ALU.mult), reads=['xr'], writes=['Bx'])
                    S.op('dve', lambda e: e.tensor_tensor(out=Bx[:], in0=Bx[:], in1=tmp[:], op=ALU.mult), reads=['tmp'], writes=['Bx'])
                    if z == 0:
                        S.op('dve', lambda e: e.tensor_tensor_scan(H[:], A[:], Bx[:], 0.0, ALU.mult, ALU.add), reads=['A', 'Bx'], writes=[hk])
                    else:
                        S.op('dve', lambda e: e.tensor_tensor_scan(H[:, 0:256][:, ::-1], A[:, 0:256][:, ::-1], Bx[:, 0:256][:, ::-1], 0.0,
                                                                   ALU.mult, ALU.add), reads=['A', 'Bx'], writes=[hk])
                        S.op('dve', lambda e: e.tensor_tensor_scan(H[:, 256:T][:, ::-1], A[:, 256:T][:, ::-1], Bx[:, 256:T][:, ::-1], H[:, 0:1],
                                                                   ALU.mult, ALU.add), reads=['A', 'Bx'], writes=[hk])
                S.op('pool', lambda e: e.tensor_tensor(out=H0[:], in0=H0[:], in1=H1[:], op=ALU.add), reads=[('H', 1)], writes=[('H', 0)])
                S.op('dve', lambda e: e.tensor_tensor(out=ho[:], in0=H0[:], in1=yg[:], op=ALU.mult), reads=[('H', 0), 'yg'], writes=['ho'])
                S.dma('pool', lambda e: e.dma_start(out=hlT[cc * 128:(cc + 1) * 128, :], in_=ho[:]), reads=['ho'], writes=[('hlT', cc)])
        S.barrier()

    def phase_E3():
        NLN16 = -2.772588722239781
        with contextlib.ExitStack() as es:
            def al(name, shape, dt):
                return es.enter_context(SBT(name, shape, dt))
            G = al("G", [128, NT, 16], F32)
            LF = al("LF", [128, 2, NT, 4], F32)
            CF = al("CF", [128, 2, NT, 4], F32)
            EB16 = al("EB16", [128, 2, NT, 4], F32)
            ECF = al("ECF", [128, 2, NT, 4], F32)
            DEC = al("DEC", [128, 2, NT, 4], F32)
            WS16 = al("WS16", [128, 2, NT, 4], F32)
            qT0 = al("qT0", [128, 2, T], BF16)
            qT1 = al("qT1", [128, 2, T], BF16)
            kT0 = al("kT0", [128, 2, T], BF16)
            kT1 = al("kT1", [128, 2, T], BF16)
            V0 = al("V0", [128, NT, 257], BF16)
            V1 = al("V1", [128, NT, 257], BF16)
            CTs = al("CTs", [128, 4, 2, 257], F32)
            CTbs = al("CTbs", [128, 4, 2, 257], BF16)
            PTs = al("PTs", [128, 4, 128], BF16)
            ktms = al("ktms", [128, 4, 256], BF16)
            vws = al("vws", [128, 4, 257], BF16)
            sms = al("sms", [128, 4, 8], F32)
            houts = al("houts", [128, 4, 256], F32)
            S.dma('sp', lambda e: e.dma_start(out=G[:], in_=g_tm.rearrange("(n p) c -> p n c", p=128)), writes=['G'])
            Gv = G[:].rearrange("p n (d g h) -> p d g n h", d=2, g=2, h=4)
            fl = lambda t, d: t[:, d].rearrange("p n h -> p (n h)")
            for d in range(2):
                S.op('act', lambda e: e.activation(out=LF[:, d], in_=Gv[:, d, 1], func=AF.Exp, scale=-1.0), reads=['G'], writes=['LF'])
                S.op('act', lambda e: e.activation(out=LF[:, d], in_=LF[:, d], func=AF.Ln, bias=1.0, scale=1.0), writes=['LF'])
                S.op('dve', lambda e: e.tensor_scalar(out=LF[:, d], in0=LF[:, d], scalar1=-1.0, scalar2=None, op0=ALU.mult), writes=['LF'])
                tri = triF if d == 0 else triB
                sel = selF if d == 0 else selB
                S.op('pe', lambda e: e.matmul(psum[0][:, 0:NT * 4], tri[:], fl(LF, d), start=True, stop=True),
                     reads=['LF', tri.name], writes=[PB(0)])
                S.op('act', lambda e: e.copy(fl(CF, d), psum[0][:, 0:NT * 4]), writes=[PB(0), 'CF'])
                S.op('pe', lambda e: e.matmul(psum[1][:, 0:NT * 4], sel[:], fl(CF, d), start=True, stop=True),
                     reads=['CF', sel.name], writes=[PB(1)])
                S.op('act', lambda e: e.activation(out=fl(DEC, d), in_=psum[1][:, 0:NT * 4], func=AF.Exp), writes=[PB(1), 'DEC'])
                S.op('dve', lambda e: e.tensor_tensor(out=EB16[:, d], in0=Gv[:, d, 0], in1=CF[:, d], op=ALU.subtract), reads=['G', 'CF'], writes=['EB16'])
                S.op('dve', lambda e: e.tensor_scalar(out=EB16[:, d], in0=EB16[:, d], scalar1=NLN16, scalar2=None, op0=ALU.add), writes=['EB16'])
                S.op('act', lambda e: e.activation(out=EB16[:, d], in_=EB16[:, d], func=AF.Exp), writes=['EB16'])
                S.op('act', lambda e: e.activation(out=ECF[:, d], in_=CF[:, d], func=AF.Exp), reads=['CF'], writes=['ECF'])
                S.op('dve', lambda e: e.tensor_tensor(out=WS16[:, d], in0=EB16[:, d], in1=DEC[:, d], op=ALU.mult), reads=['EB16', 'DEC'], writes=['WS16'])
            S.barrier()
            qTs, kTs, Vs = [qT0, qT1], [kT0, kT1], [V0, V1]
            order = [list(range(NT)), [1, 0] + list(range(NT - 1, 1, -1))]
            cnt = {'st': 0, 'num': 0}
            for hp in range(2):
                for hh in range(2):
                    h = hp * 2 + hh
                    S.dma('sp', lambda e: e.dma_start(out=qTs[hh][:], in_=qkT[h * 256:(h + 1) * 256, :].rearrange("(dh p) t -> p dh t", p=128)),
                          writes=[('qT', hh)])
                    S.dma('sp', lambda e: e.dma_start(out=kTs[hh][:], in_=qkT[1024 + h * 256:1024 + (h + 1) * 256, :].rearrange("(dh p) t -> p dh t", p=128)),
                          writes=[('kT', hh)])
                    S.dma('sp', lambda e: e.dma_start(out=Vs[hh][:, :, 0:256], in_=v_tm[:, h * 256:(h + 1) * 256].rearrange("(n p) e -> p n e", p=128)),
                          writes=[('V', hh)])
                    S.op('pool', lambda e: e.memset(Vs[hh][:, :, 256:257], 1.0), writes=[('V1', hh)])
                S.op('pool', lambda e: e.memset(CTs[:], 0.0), writes=[('CT', i) for i in range(4)])
                S.op('pool', lambda e: e.memset(CTbs[:], 0.0), writes=[('CTb', i) for i in range(4)])
                for step in range(NT):
                    for ci, (z, hh) in enumerate([(0, 0), (0, 1), (1, 0), (1, 1)]):
                        h = hp * 2 + hh
                        c = order[z][step]
                        sl = slice(c * 128, (c + 1) * 128)
                        qT, kT, V = qTs[hh], kTs[hh], Vs[hh]
                        rk = [('qT', hh), ('kT', hh), ('V', hh), ('V1', hh)]
                        tri = triF if z == 0 else triB
                        pst = cnt['st'] % 2
                        cnt['st'] += 1
                        for dh in range(2):
                            S.op('pe', lambda e: e.matmul(psum[pst][:, 0:128], kT[:, dh, sl], qT[:, dh, sl], start=(dh == 0), stop=(dh == 1)),
                                 reads=rk, writes=[PB(pst)])
                        S.op('dve', lambda e: e.scalar_tensor_tensor(out=PTs[:, ci, :], in0=psum[pst][:, 0:128], scalar=EB16[:, z, c, h:h + 1],
                                                                     in1=tri[:], op0=ALU.mult, op1=ALU.mult), writes=[PB(pst), ('PT', ci)])
                        for dh in range(2):
                            S.op('pe', lambda e: e.transpose(psb[0][:, dh * 128:(dh + 1) * 128], kT[:, dh, sl], identb[:]),
                                 reads=rk, writes=[PBB(0)])
                        S.op('act', lambda e: e.copy(ktms[:, ci, :], psb[0][:, 0:256]), writes=[PBB(0), ('ktm', ci)])
                        S.op('act', lambda e: e.activation(out=vws[:, ci, :], in_=V[:, c, :], func=AF.Identity, scale=WS16[:, z, c, h:h + 1]),
                             reads=rk, writes=[('vw', ci)])
                        pn = 2 + cnt['num'] % 2
                        cnt['num'] += 1
                        S.op('pe', lambda e: e.matmul(psum[pn][:, 0:257], PTs[:, ci, :], V[:, c, :], start=True, stop=False),
                             reads=rk + [('PT', ci)], writes=[PB(pn)])
                        for dh in range(2):
                            S.op('pe', lambda e: e.matmul(psum[pn][:, 0:257], qT[:, dh, sl], CTbs[:, ci, dh, :], start=False, stop=(dh == 1)),
                                 reads=rk + [('CTb', ci)], writes=[PB(pn)])
                        sm = sms[:, ci, :]
                        ecf = ECF[:, z, c, h:h + 1]
                        S.op('dve', lambda e: e.tensor_tensor(out=sm[:, 0:1], in0=psum[pn][:, 256:257], in1=ecf, op=ALU.mult), writes=[PB(pn), ('sm', ci)])
                        S.op('dve', lambda e: e.tensor_scalar(out=sm[:, 2:3], in0=sm[:, 0:1], scalar1=1.0, scalar2=None, op0=ALU.max), writes=[('sm', ci)])
                        S.op('dve', lambda e: e.tensor_scalar(out=sm[:, 3:4], in0=sm[:, 0:1], scalar1=-1.0, scalar2=sm[:, 2:3], op0=ALU.mult, op1=ALU.max),
                             writes=[('sm', ci)])
                        S.op('dve', lambda e: e.reciprocal(sm[:, 4:5], sm[:, 3:4]), writes=[('sm', ci)])
                        S.op('dve', lambda e: e.tensor_tensor(out=sm[:, 5:6], in0=sm[:, 4:5], in1=ecf, op=ALU.mult), writes=[('sm', ci)])
                        S.op('act', lambda e: e.activation(out=houts[:, ci, :], in_=psum[pn][:, 0:256], func=AF.Identity, scale=sm[:, 5:6]),
                             reads=[('sm', ci)], writes=[PB(pn), ('hout', ci)])
                        S.dma('sp', lambda e: e.dma_start(out=hm_d[z][sl, h * 256:(h + 1) * 256], in_=houts[:, ci, :]), reads=[('hout', ci)],
                              writes=[('hm', z, h, c)])
                        for dh in range(2):
                            S.op('pe', lambda e: e.matmul(psum[4 + dh][:, 0:257], ktms[:, ci, dh * 128:(dh + 1) * 128], vws[:, ci, :], start=True, stop=True),
                                 reads=[('ktm', ci), ('vw', ci)], writes=[PB(4 + dh)])
                            S.op('dve', lambda e: e.scalar_tensor_tensor(out=CTs[:, ci, dh, :], in0=CTs[:, ci, dh, :], scalar=DEC[:, z, c, h:h + 1],
                                                                         in1=psum[4 + dh][:, 0:257], op0=ALU.mult, op1=ALU.add),
                                 writes=[PB(4 + dh), ('CT', ci)])
                        S.op('act', lambda e: e.copy(CTbs[:, ci], CTs[:, ci]), reads=[('CT', ci)], writes=[('CTb', ci)])
        S.barrier()

    def mixer_out(i, r, G1, wo, nk, lhs_list, src_rows, pbase):
        return

    def phase_E4():
        with contextlib.ExitStack() as es:
            def al(name, shape, dt):
                return es.enter_context(SBT(name, shape, dt))
            wo = al("wo", [128, 16, D], BF16)
            MG = al("MG", [128, D], F32)
            G1 = al("G1", [128, 2, D], F32)
            hf = [al("hf%d" % j, [128, D], F32) for j in range(2)]
            hb = [al("hb%d" % j, [128, D], F32) for j in range(2)]
            so = [al("so%d" % j, [128, D], BF16) for j in range(2)]
            sq2 = [al("e4sq%d" % j_, [128, 256], F32) for j_ in range(2)]
            st2 = [al("e4st%d" % j_, [128, 16], F32) for j_ in range(2)]
            hmb2 = [al("hmb%d" % j_, [128, D], BF16) for j_ in range(2)]
            hmT = [al("hmT%d" % j, [128, 8, 128], BF16) for j in range(2)]
            hl = [al("hl%d" % j, [128, 8, 128], BF16) for j in range(2)]
            xt = [al("e4x%d" % j, [128, D], F32) for j in range(2)]
            tmp2 = [al("e4tmp%d" % j_, [128, D], F32) for j_ in range(2)]
            wov = I("ev_w_out").rearrange("(kc p) n -> p kc n", p=128)
            for hlf in range(2):
                S.dma('pool', lambda e: e.dma_start(out=wo[:, hlf * 8:(hlf + 1) * 8, :], in_=wov[:, hlf * 8:(hlf + 1) * 8, :]), writes=[('wo', hlf)])
            bcast_row(MG[:], I("ev_mnorm_g")[0:1, :], 'MG')
            for r in range(2):
                bcast_row(G1[:, r, :], modrow[0, r:r + 1, 2 * D:3 * D], ('G1', r))
            hlv = hlT.rearrange("(cc p) t -> p cc t", p=128)
            xin = I("xin")
            def stL(i):
                r = 1 if i < 2 else 0
                j = i % 2
                rows = slice(i * 128, (i + 1) * 128)
                sq, st, hmb, tmp = sq2[j], st2[j], hmb2[j], tmp2[j]
                KSQ, KST, KHMB = ('e4sq', j), ('e4st', j), ('hmb', j)
                S.dma('sp', lambda e: e.dma_start(out=hf[j][:], in_=hm_d[0][rows, :]), writes=[('hf', j)])
                S.dma('sp', lambda e: e.dma_start(out=hb[j][:], in_=hm_d[1][rows, :]), writes=[('hb', j)])
                S.dma('sp', lambda e: e.dma_start(out=so[j][:], in_=so_tm[rows, :]), writes=[('so', j)])
                S.dma('sp', lambda e: e.dma_start(out=hl[j][:], in_=hlv[:, :, rows]), writes=[('hl', j)])
                S.dma('sp', lambda e: e.dma_start(out=xt[j][:], in_=xin[rows, :]), writes=[('xt', j)])

            def stB(i):
                r = 1 if i < 2 else 0
                j = i % 2
                rows = slice(i * 128, (i + 1) * 128)
                sq, st, hmb, tmp = sq2[j], st2[j], hmb2[j], tmp2[j]
                KSQ, KST, KHMB = ('e4sq', j), ('e4st', j), ('hmb', j)
                S.op('pool', lambda e: e.tensor_tensor(out=hf[j][:], in0=hf[j][:], in1=hb[j][:], op=ALU.add), reads=[('hb', j)], writes=[('hf', j)])
                for h in range(4):
                    S.op('act', lambda e: e.activation(out=sq[:], in_=hf[j][:, h * 256:(h + 1) * 256], func=AF.Square, accum_out=st[:, h:h + 1]),
                         reads=[('hf', j)], writes=[KSQ, KST])
                S.op('dve', lambda e: e.tensor_scalar(out=st[:, 4:8], in0=st[:, 0:4], scalar1=1.0 / 256, scalar2=EPS, op0=ALU.mult, op1=ALU.add), writes=[KST])
                S.op('act', lambda e: e.activation(out=st[:, 8:12], in_=st[:, 4:8], func=AF.Sqrt), writes=[KST])
                S.op('dve', lambda e: e.reciprocal(st[:, 12:16], st[:, 8:12]), writes=[KST])
                for h in range(4):
                    hs = slice(h * 256, (h + 1) * 256)
                    S.op('dve', lambda e: e.scalar_tensor_tensor(out=hf[j][:, hs], in0=hf[j][:, hs], scalar=st[:, 12 + h:13 + h], in1=MG[:, hs],
                                                                 op0=ALU.mult, op1=ALU.mult), reads=['MG'], writes=[('hf', j), KST])
                S.op('pool', lambda e: e.tensor_tensor(out=hmb[:], in0=hf[j][:], in1=so[j][:], op=ALU.mult), reads=[('hf', j), ('so', j)], writes=[KHMB])

            def stCD(i):
                r = 1 if i < 2 else 0
                j = i % 2
                rows = slice(i * 128, (i + 1) * 128)
                sq, st, hmb, tmp = sq2[j], st2[j], hmb2[j], tmp2[j]
                KSQ, KST, KHMB = ('e4sq', j), ('e4st', j), ('hmb', j)
                for kc in range(8):
                    S.op('pe', lambda e: e.transpose(psb[j][:, kc * 128:(kc + 1) * 128], hmb[:, kc * 128:(kc + 1) * 128], identb[:]),
                         reads=[KHMB], writes=[PBB(j)])
                S.op('act', lambda e: e.copy(hmT[j][:].rearrange("p a b -> p (a b)"), psb[j][:]), writes=[PBB(j), ('hmT', j)])
                for nb in range(2):
                    pb = (2 * i + nb) % 4
                    for kc in range(16):
                        lhs = hmT[j][:, kc, :] if kc < 8 else hl[j][:, kc - 8, :]
                        S.op('pe', lambda e: e.matmul(psum[pb][:], lhs, wo[:, kc, nb * 512:(nb + 1) * 512], start=(kc == 0), stop=(kc == 15)),
                             reads=[('hmT', j), ('hl', j), ('wo', 0), ('wo', 1)], writes=[PB(pb)])
                    cs = slice(nb * 512, (nb + 1) * 512)
                    S.op('dve', lambda e: e.tensor_tensor(out=tmp[:, cs], in0=psum[pb][:], in1=G1[:, r, cs], op=ALU.mult), reads=[('G1', r)],
                         writes=[PB(pb), ('e4tmp', j, nb)])
                    S.op('pool', lambda e: e.tensor_tensor(out=tmp[:, cs], in0=tmp[:, cs], in1=xt[j][:, cs], op=ALU.add), reads=[('xt', j)],
                         writes=[('e4tmp', j, nb)])
                S.dma('pool', lambda e: e.dma_start(out=xres[rows, :], in_=tmp[:]), reads=[('e4tmp', j, 0), ('e4tmp', j, 1)], writes=[('xres', i)])

            stL(0)
            stB(0)
            for i in range(NT):
                if i + 1 < NT:
                    stL(i + 1)
                    stB(i + 1)
                stCD(i)
        S.barrier()

    v2_tm = dscr("v2_tm", [T, D], BF16)

    def phase_N2(layer, tiles, LOG):
        with contextlib.ExitStack() as es:
            def al(name, shape, dt):
                return es.enter_context(SBT(name, shape, dt))
            wr = al("wr", [128, 8, 32], F32)
            br = al("br", [1, 32], F32)
            vT32 = [al("vT32_%d" % j, [128, 8, 128], F32) for j in range(2)]
            vb = [al("vb%d" % j, [128, D], BF16) for j in range(2)]
            S.dma('sp', lambda e: e.dma_start(out=wr[:], in_=I("moe_w_r")[layer].rearrange("(kc p) n -> p kc n", p=128)), writes=['wr'])
            S.dma('sp', lambda e: e.dma_start(out=br[:], in_=I("moe_b_r")[layer:layer + 1, :]), writes=['br'])
            cnt = [0]

            def consume(i, ut, uk):
                j = cnt[0] % 2
                cnt[0] += 1
                S.op('pool', lambda e: e.tensor_copy(vb[j][:], ut[:]), reads=[uk], writes=[('vb', j)])
                S.dma('pool', lambda e: e.dma_start(out=v2_tm[i * 128:(i + 1) * 128, :], in_=vb[j][:]), reads=[('vb', j)], writes=[('v2', i)])
                for half in range(2):
                    pb = (i * 2 + half) % 4
                    for jj in range(4):
                        kc = half * 4 + jj
                        S.op('pe', lambda e: e.transpose(psum[pb][:, jj * 128:(jj + 1) * 128], ut[:, kc * 128:(kc + 1) * 128], ident[:]),
                             reads=[uk, 'ident'], writes=[PB(pb)])
                    S.op('act', lambda e: e.copy(vT32[j][:, half * 4:half * 4 + 4, :].rearrange("p a b -> p (a b)"), psum[pb][:]),
                         writes=[PB(pb), ('vT32', j, half)])
                pl = 4 + i % 2
                for kc in range(8):
                    S.op('pe', lambda e: e.matmul(psum[pl][:, 0:32], vT32[j][:, kc, :], wr[:, kc, :], start=(kc == 0), stop=False),
                         reads=[('vT32', j, 0), ('vT32', j, 1), 'wr'], writes=[PB(pl)])
                S.op('pe', lambda e: e.matmul(psum[pl][:, 0:32], ones1[0:1, :], br[:], start=False, stop=True), reads=['br', 'ones1'], writes=[PB(pl)])
                S.op('dve', lambda e: e.tensor_copy(LOG[:, i, :], psum[pl][:, 0:32]), writes=[PB(pl), 'LOG'])
            norm_tiles(layer, 1, xres, tiles, consume)
        S.barrier()

    SB = 512
    SHIFT = 9
    NSUB = SB // 128
    NBMAX = 66
    xs_d = dscr("xs_d", [NBMAX * SB, D], BF16)
    ys_d = dscr("ys_d", [NBMAX * SB, D], F32)
    RT = {}
    for nm, shp, dt in (("LOG", [128, NT, 32], F32), ("TOP8", [128, NT, 8], F32), ("WG", [128, NT, 4], F32),
                        ("SLOTI", [128, NT, 4], I32), ("IDXI", [128, NBMAX], I32), ("BIDX", [128, NBMAX], I32),
                        ("IDX8", [128, NBMAX, 8], I32), ("IDX4", [128, NBMAX, 4], I32)):
        RT[nm] = nc.alloc_sbuf_tensor("rt_" + nm, shp, dt)

    def phase_route(tiles, NB):
        LOG, TOP8, WG, SLOTI, IDXI, BIDX = (RT[k] for k in ("LOG", "TOP8", "WG", "SLOTI", "IDXI", "BIDX"))
        with contextlib.ExitStack() as es:
            def al(name, shape, dt):
                return es.enter_context(SBT(name, shape, dt))
            MASK = al("MASK", [128, NT, 32], F32)
            RANK = al("RANK", [128, NT, 32], F32)
            CAR = al("CAR", [128, 32], F32)
            sm = al("rsm", [128, 8], F32)
            ci = al("rci", [128, 32], I32)
            PADDED = al("PADDED", [128, 32], F32)
            PEND = al("PEND", [128, 32], F32)
            BASE = al("BASE", [128, 32], F32)
            ones32 = al("ones32", [128, 32], F32)
            junk = al("rjunk", [128, 32], F32)
            SLOTF = al("SLOTF", [128, NT, 4], F32)
            BSTi = al("BSTi", [128, NBMAX], I32)
            BST = al("BST", [128, NBMAX], F32)
            BLKE = al("BLKE", [128, NBMAX], F32)
            IDXF = al("IDXF", [128, NBMAX], F32)
            IDXF2 = al("IDXF2", [128, NBMAX], F32)
            S.op('pool', lambda e: e.memset(CAR[:], 0.0), writes=['CAR'])
            S.op('pool', lambda e: e.memset(ones32[:], 1.0), writes=['ones32'])
            S.op('pool', lambda e: e.memset(SLOTF[:], 0.0), writes=['SLOTF'])
            for i in tiles:
                S.op('dve', lambda e: e.max(out=TOP8[:, i, :], in_=LOG[:, i, :]), writes=['TOP8'])
                S.op('dve', lambda e: e.tensor_scalar(out=MASK[:, i, :], in0=LOG[:, i, :], scalar1=TOP8[:, i, 3:4], scalar2=None, op0=ALU.is_ge),
                     reads=['TOP8'], writes=[('MASK', i)])
                S.op('dve', lambda e: e.tensor_scalar(out=sm[:, 0:1], in0=TOP8[:, i, 0:1], scalar1=-1.0, scalar2=None, op0=ALU.mult), reads=['TOP8'], writes=['rsm'])
                S.op('act', lambda e: e.activation(out=WG[:, i, :], in_=TOP8[:, i, 0:4], func=AF.Exp, bias=sm[:, 0:1], scale=1.0, accum_out=sm[:, 1:2]),
                     reads=['TOP8'], writes=['rsm', 'WG'])
                S.op('dve', lambda e: e.reciprocal(sm[:, 2:3], sm[:, 1:2]), writes=['rsm'])
                S.op('dve', lambda e: e.tensor_scalar(out=WG[:, i, :], in0=WG[:, i, :], scalar1=sm[:, 2:3], scalar2=None, op0=ALU.mult), writes=['rsm', 'WG'])
                S.op('pe', lambda e: e.matmul(psum[0][:, 0:32], triS[:], MASK[:, i, :], start=True, stop=True), reads=[('MASK', i)], writes=[PB(0)])
                S.op('pe', lambda e: e.matmul(psum[1][:, 0:32], onesm[:], MASK[:, i, :], start=True, stop=True), reads=[('MASK', i)], writes=[PB(1)])
                S.op('dve', lambda e: e.tensor_tensor(out=RANK[:, i, :], in0=psum[0][:, 0:32], in1=CAR[:], op=ALU.add), reads=['CAR'], writes=[PB(0), 'RANK'])
                S.op('dve', lambda e: e.tensor_tensor(out=CAR[:], in0=psum[1][:, 0:32], in1=CAR[:], op=ALU.add), writes=[PB(1), 'CAR'])
            S.op('dve', lambda e: e.tensor_scalar(out=junk[:], in0=CAR[:], scalar1=float(SB - 1), scalar2=None, op0=ALU.add), reads=['CAR'], writes=['rjunk'])
            S.op('dve', lambda e: e.tensor_copy(ci[:], junk[:]), reads=['rjunk'], writes=['rci'])
            S.op('dve', lambda e: e.tensor_scalar(out=ci[:], in0=ci[:], scalar1=SHIFT, scalar2=SHIFT, op0=ALU.arith_shift_right, op1=ALU.logical_shift_left),
                 writes=['rci'])
            S.op('dve', lambda e: e.tensor_copy(PADDED[:], ci[:]), reads=['rci'], writes=['PADDED'])
            S.op('dve', lambda e: e.tensor_tensor_scan(PEND[:], ones32[:], PADDED[:], 0.0, ALU.mult, ALU.add), reads=['ones32', 'PADDED'], writes=['PEND'])
            S.op('dve', lambda e: e.tensor_tensor(out=BASE[:], in0=PEND[:], in1=PADDED[:], op=ALU.subtract), reads=['PEND', 'PADDED'], writes=['BASE'])
            for i in tiles:
                S.op('dve', lambda e: e.tensor_tensor(out=RANK[:, i, :], in0=RANK[:, i, :], in1=BASE[:], op=ALU.add), reads=['BASE'], writes=['RANK'])
                for k in range(4):
                    S.op('dve', lambda e: e.scalar_tensor_tensor(out=junk[:], in0=LOG[:, i, :], scalar=TOP8[:, i, k:k + 1], in1=RANK[:, i, :],
                                                                 op0=ALU.is_equal, op1=ALU.mult, accum_out=SLOTF[:, i, k:k + 1]),
                         reads=['TOP8'], writes=['rjunk', 'RANK', 'SLOTF'])
            S.op('dve', lambda e: e.tensor_copy(SLOTI[:], SLOTF[:]), reads=['SLOTF'], writes=['SLOTI'])
            S.op('pool', lambda e: e.iota(BSTi[:], pattern=[[SB, NBMAX]], base=0, channel_multiplier=0), writes=['BSTi'])
            S.op('dve', lambda e: e.tensor_copy(BST[:], BSTi[:]), reads=['BSTi'], writes=['BST'])
            S.op('pool', lambda e: e.memset(BLKE[:], 0.0), writes=['BLKE'])
            for ex in range(32):
                S.op('dve', lambda e: e.scalar_tensor_tensor(out=BLKE[:], in0=BST[:], scalar=PEND[:, ex:ex + 1], in1=BLKE[:], op0=ALU.is_ge, op1=ALU.add),
                     reads=['BST', 'PEND'], writes=['BLKE'])
            S.op('dve', lambda e: e.tensor_scalar(out=BLKE[:], in0=BLKE[:], scalar1=31.0, scalar2=None, op0=ALU.min), writes=['BLKE'])
            S.op('dve', lambda e: e.tensor_scalar(out=IDXF[:], in0=BLKE[:], scalar1=128.0, scalar2=pidx[:, 0:1], op0=ALU.mult, op1=ALU.add),
                 reads=['pidx'], writes=['IDXF'])
            S.op('dve', lambda e: e.tensor_copy(IDXI[:], IDXF[:]), reads=['IDXF'], writes=['IDXI'])
            S.op('dve', lambda e: e.tensor_copy(BIDX[:], BLKE[:]), reads=['BLKE'], writes=['BIDX'])
            S.op('pool', lambda e: e.memset(BSTi[:], 0), writes=['BSTi'])
            S.op('dve', lambda e: e.tensor_copy(IDXF2[:], BSTi[:]), reads=['BSTi'], writes=['IDXF2'])
            S.op('dve', lambda e: e.tensor_tensor(out=IDXF2[:, 2:NBMAX], in0=BLKE[:, 2:NBMAX], in1=BLKE[:, 0:NBMAX - 2], op=ALU.is_equal),
                 reads=['BLKE'], writes=['IDXF2'])
            S.op('dve', lambda e: e.tensor_scalar(out=IDXF2[:], in0=IDXF2[:], scalar1=float(1 << 20), scalar2=None, op0=ALU.mult), writes=['IDXF2'])
            for kc in range(8):
                S.op('dve', lambda e: e.tensor_scalar(out=BST[:], in0=IDXF[:], scalar1=8.0, scalar2=float(kc), op0=ALU.mult, op1=ALU.add),
                     reads=['IDXF'], writes=['BST'])
                S.op('dve', lambda e: e.tensor_tensor(out=BST[:], in0=BST[:], in1=IDXF2[:], op=ALU.add), reads=['IDXF2'], writes=['BST'])
                S.op('dve', lambda e: e.tensor_copy(RT["IDX8"][:, :, kc], BST[:]), reads=['BST'], writes=['IDX8'])
            for q in range(4):
                S.op('dve', lambda e: e.tensor_scalar(out=BST[:], in0=IDXF[:], scalar1=4.0, scalar2=float(q), op0=ALU.mult, op1=ALU.add),
                     reads=['IDXF'], writes=['BST'])
                S.op('dve', lambda e: e.tensor_tensor(out=BST[:], in0=BST[:], in1=IDXF2[:], op=ALU.add), reads=['IDXF2'], writes=['BST'])
                S.op('dve', lambda e: e.tensor_copy(RT["IDX4"][:, :, q], BST[:]), reads=['BST'], writes=['IDX4'])
            if RT.get('dbg') is not None:
                dd = RT['dbg']
                S.op('dve', lambda e: e.tensor_copy(dd[:, 0:32], CAR[:]), reads=['CAR'], writes=['dd'])
                S.op('dve', lambda e: e.tensor_copy(dd[:, 32:64], PADDED[:]), reads=['PADDED'], writes=['dd'])
                S.op('dve', lambda e: e.tensor_copy(dd[:, 64:96], PEND[:]), reads=['PEND'], writes=['dd'])
                S.op('dve', lambda e: e.tensor_copy(dd[:, 96:128], MASK[:, 0, :]), writes=['dd'])
                S.op('dve', lambda e: e.tensor_copy(dd[:, 128:160], RANK[:, 1, :]), writes=['dd'])
                S.op('dve', lambda e: e.tensor_copy(dd[:, 160:192], ci[:]), writes=['dd'])
        S.barrier()

    def phase_scatter(tiles, NB):
        SLOTI = RT["SLOTI"]
        with contextlib.ExitStack() as es:
            def al(name, shape, dt):
                return es.enter_context(SBT(name, shape, dt))
            zt = al("zt", [128, D], BF16)
            vt = [al("svt%d" % j, [128, D], BF16) for j in range(3)]
            S.op('pool', lambda e: e.memset(zt[:], 0.0), writes=['zt'])
            for b in range(NB * NSUB):
                S.dma('sp', lambda e: e.dma_start(out=xs_d[b * 128:(b + 1) * 128, :], in_=zt[:]), reads=['zt'], writes=[('xsz', b)])
            S.barrier()
            for n, i in enumerate(tiles):
                j = n % 3
                S.dma('sp', lambda e: e.dma_start(out=vt[j][:], in_=v2_tm[i * 128:(i + 1) * 128, :]), writes=[('svt', j)])
                for k in range(4):
                    S.dma('pool', lambda e: e.indirect_dma_start(out=xs_d, out_offset=bass.IndirectOffsetOnAxis(ap=SLOTI[:, i, k:k + 1], axis=0),
                                                                 in_=vt[j][:], in_offset=None), reads=[('svt', j)], writes=[('xs', i, k)])
        S.barrier()

    def phase_moe(layer, NB):
        w1d = I("moe_w1_%d" % layer)
        w2d = I("moe_w2_%d" % layer)
        b1d = I("moe_b1_%d" % layer)
        b2d = I("moe_b2_%d" % layer)
        IDXI, BIDX = RT["IDXI"], RT["BIDX"]
        w1v = w1d.rearrange("a b c -> (a b) c")
        w2v = w2d.rearrange("a (q t) c -> (a q) (t c)", t=2)
        with contextlib.ExitStack() as es:
            def al(name, shape, dt):
                return es.enter_context(SBT(name, shape, dt))
            W1 = [al("W1_%d" % j, [128, 8, 2048], BF16) for j in range(2)]
            W2 = [al("W2_%d" % j, [128, 8, 1024], BF16) for j in range(2)]
            B1C = [al("B1C_%d" % j, [128, 16], F32) for j in range(2)]
            B2R = [al("B2R_%d" % j, [128, D], F32) for j in range(2)]
            XS = [al("XS_%d" % j, [128, D], BF16) for j in range(2 * NSUB)]
            XST = al("XST", [128, 8, SB], BF16)
            ACTT = al("ACTT", [128, 8, SB], BF16)
            tg = [al("tg_%d" % j, [128, SB], F32) for j in range(2)]
            sg = [al("sg_%d" % j, [128, SB], F32) for j in range(2)]
            tu = [al("tu_%d" % j, [128, SB], F32) for j in range(2)]
            YT = [al("YT_%d" % j, [128, D], F32) for j in range(2)]
            ec = [0]
            xc = [0]
            yc = [0]
            bc8 = nc.gpsimd.to_reg(32 * 128 * 8 - 1)
            bc4 = nc.gpsimd.to_reg(32 * 128 * 4 - 1)

            def gathers(b):
                j = b % 2
                idx = bass.IndirectOffsetOnAxis(ap=IDXI[:, b:b + 1], axis=0)
                bidx = bass.IndirectOffsetOnAxis(ap=BIDX[:, b:b + 1], axis=0)
                for kc in range(8):
                    i8 = bass.IndirectOffsetOnAxis(ap=RT["IDX8"][:, b, kc:kc + 1], axis=0)
                    S.dma('pool', lambda e: e.indirect_dma_start(out=W1[j][:, kc, :], out_offset=None, in_=w1v, in_offset=i8, bounds_check=bc8, oob_is_err=False), writes=[('W1', j, kc)])
                for q in range(4):
                    i4 = bass.IndirectOffsetOnAxis(ap=RT["IDX4"][:, b, q:q + 1], axis=0)
                    S.dma('pool', lambda e: e.indirect_dma_start(out=W2[j][:, 2 * q:2 * q + 2, :].rearrange("p a b -> p (a b)"), out_offset=None,
                                                                 in_=w2v, in_offset=i4, bounds_check=bc4, oob_is_err=False), writes=[('W2', j, q)])
                S.dma('pool', lambda e: e.indirect_dma_start(out=B1C[j][:], out_offset=None, in_=b1d, in_offset=idx), writes=[('B1C', j)])
                S.dma('pool', lambda e: e.indirect_dma_start(out=B2R[j][:], out_offset=None, in_=b2d, in_offset=bidx), writes=[('B2R', j)])

            def xloads(b):
                for st_ in range(NSUB):
                    xj = (b % 2) * NSUB + st_
                    r0 = b * SB + st_ * 128
                    S.dma('sp', lambda e: e.dma_start(out=XS[xj][:], in_=xs_d[r0:r0 + 128, :]), writes=[('XS', xj)])

            gathers(0)
            xloads(0)
            for b in range(NB):
                j = b % 2
                if b + 1 < NB:
                    gathers(b + 1)
                    xloads(b + 1)
                for st_ in range(NSUB):
                    xj = (b % 2) * NSUB + st_
                    pbb = st_ % 2
                    for kc in range(8):
                        S.op('pe', lambda e: e.transpose(psb[pbb][:, kc * 128:(kc + 1) * 128], XS[xj][:, kc::8], identb[:]), reads=[('XS', xj)], writes=[PBB(pbb)])
                    S.op('act', lambda e: e.copy(XST[:, :, st_ * 128:(st_ + 1) * 128], psb[pbb][:].rearrange("p (a b) -> p a b", a=8)),
                         writes=[PBB(pbb), ('XST', st_)])
                xk = [('XST', s_) for s_ in range(NSUB)]
                for jj in range(8):
                    t = ec[0] % 2
                    ec[0] += 1
                    for half in range(2):
                        bank = 2 * t + half
                        for kc in range(8):
                            S.op('pe', lambda e: e.matmul(psum[bank][:, 0:SB], W1[j][:, kc, half * 1024:(half + 1) * 1024][:, jj::8], XST[:, kc, :],
                                                          start=(kc == 0), stop=(kc == 7)), reads=[('W1', j, kc)] + xk, writes=[PB(bank)])
                    S.op('dve', lambda e: e.tensor_scalar(out=tg[t][:], in0=psum[2 * t][:, 0:SB], scalar1=B1C[j][:, jj:jj + 1], scalar2=7.0,
                                                          op0=ALU.add, op1=ALU.min), reads=[('B1C', j)], writes=[PB(2 * t), ('tg', t)])
                    S.op('dve', lambda e: e.tensor_scalar(out=tu[t][:], in0=psum[2 * t + 1][:, 0:SB], scalar1=B1C[j][:, 8 + jj:9 + jj], scalar2=7.0,
                                                          op0=ALU.add, op1=ALU.min), reads=[('B1C', j)], writes=[PB(2 * t + 1), ('tu', t)])
                    S.op('act', lambda e: e.activation(out=sg[t][:], in_=tg[t][:], func=AF.Sigmoid, scale=1.702), reads=[('tg', t)], writes=[('sg', t)])
                    S.op('dve', lambda e: e.tensor_scalar(out=tu[t][:], in0=tu[t][:], scalar1=-7.0, scalar2=1.0, op0=ALU.max, op1=ALU.add), writes=[('tu', t)])
                    S.op('dve', lambda e: e.tensor_tensor(out=tu[t][:], in0=tu[t][:], in1=tg[t][:], op=ALU.mult), reads=[('tg', t)], writes=[('tu', t)])
                    S.op('dve', lambda e: e.tensor_tensor(out=ACTT[:, jj, :], in0=tu[t][:], in1=sg[t][:], op=ALU.mult),
                         reads=[('tu', t), ('sg', t)], writes=[('ACTT', jj)])
                ak = [('ACTT', jj) for jj in range(8)]
                for st_ in range(NSUB):
                    yj = yc[0] % 2
                    yc[0] += 1
                    for nb in range(2):
                        for jj in range(8):
                            S.op('pe', lambda e: e.matmul(psum[4 + nb][:], ACTT[:, jj, st_ * 128:(st_ + 1) * 128], W2[j][:, jj, nb * 512:(nb + 1) * 512],
                                                          start=(jj == 0), stop=(jj == 7)), reads=ak + [('W2', j, jj // 2)], writes=[PB(4 + nb)])
                        cs = slice(nb * 512, (nb + 1) * 512)
                        S.op('dve', lambda e: e.tensor_tensor(out=YT[yj][:, cs], in0=psum[4 + nb][:], in1=B2R[j][:, cs], op=ALU.add), reads=[('B2R', j)],
                             writes=[PB(4 + nb), ('YT', yj, nb)])
                    r0 = b * SB + st_ * 128
                    S.dma('sp', lambda e: e.dma_start(out=ys_d[r0:r0 + 128, :], in_=YT[yj][:]), reads=[('YT', yj, 0), ('YT', yj, 1)], writes=[('ys', b, st_)])
        S.barrier()

    def phase_combine(layer, tiles, final):
        WG, SLOTI = RT["WG"], RT["SLOTI"]
        with contextlib.ExitStack() as es:
            def al(name, shape, dt):
                return es.enter_context(SBT(name, shape, dt))
            G2 = al("G2", [128, 2, D], F32)
            FG = al("FG", [128, D], F32)
            xt = [al("cxt%d" % j, [128, D], F32) for j in range(2)]
            Y = [[al("cY%d_%d" % (j, k), [128, D], F32) for k in range(4)] for j in range(2)]
            acc = [al("cacc%d" % j, [128, D], F32) for j in range(2)]
            sq = al("csq", [128, D], F32)
            st = al("cst", [128, 8], F32)
            for r in range(2):
                bcast_row(G2[:, r, :], modrow[layer, r:r + 1, 5 * D:6 * D], ('G2', r))
            if final:
                bcast_row(FG[:], I("final_g")[0:1, :], 'FG')
            for n, i in enumerate(tiles):
                j = n % 2
                r = 1 if i < 2 else 0
                rows = slice(i * 128, (i + 1) * 128)
                S.dma('sp', lambda e: e.dma_start(out=xt[j][:], in_=xres[rows, :]), writes=[('cxt', j)])
                for k in range(4):
                    off = bass.IndirectOffsetOnAxis(ap=SLOTI[:, i, k:k + 1], axis=0)
                    S.dma('pool', lambda e: e.indirect_dma_start(out=Y[j][k][:], out_offset=None, in_=ys_d, in_offset=off), writes=[('cY', j, k)])
                S.op('dve', lambda e: e.tensor_scalar(out=acc[j][:], in0=Y[j][0][:], scalar1=WG[:, i, 0:1], scalar2=None, op0=ALU.mult),
                     reads=[('cY', j, 0)], writes=[('cacc', j)])
                for k in range(1, 4):
                    S.op('dve', lambda e: e.scalar_tensor_tensor(out=acc[j][:], in0=Y[j][k][:], scalar=WG[:, i, k:k + 1], in1=acc[j][:],
                                                                 op0=ALU.mult, op1=ALU.add), reads=[('cY', j, k)], writes=[('cacc', j)])
                S.op('pool', lambda e: e.tensor_tensor(out=acc[j][:], in0=acc[j][:], in1=G2[:, r, :], op=ALU.mult), reads=[('G2', r)], writes=[('cacc', j)])
                S.op('pool', lambda e: e.tensor_tensor(out=acc[j][:], in0=acc[j][:], in1=xt[j][:], op=ALU.add), reads=[('cxt', j)], writes=[('cacc', j)])
                if not final:
                    S.dma('act', lambda e: e.dma_start(out=xres[rows, :], in_=acc[j][:]), reads=[('cacc', j)], writes=[('xres', i)])
                else:
                    S.op('act', lambda e: e.activation(out=sq[:], in_=acc[j][:], func=AF.Square, accum_out=st[:, 0:1]), reads=[('cacc', j)], writes=['csq', 'cst'])
                    S.op('dve', lambda e: e.tensor_scalar(out=st[:, 1:2], in0=st[:, 0:1], scalar1=1.0 / D, scalar2=EPS, op0=ALU.mult, op1=ALU.add), writes=['cst'])
                    S.op('act', lambda e: e.activation(out=st[:, 2:3], in_=st[:, 1:2], func=AF.Sqrt), writes=['cst'])
                    S.op('dve', lambda e: e.reciprocal(st[:, 3:4], st[:, 2:3]), writes=['cst'])
                    S.op('dve', lambda e: e.scalar_tensor_tensor(out=acc[j][:], in0=acc[j][:], scalar=st[:, 3:4], in1=FG[:], op0=ALU.mult, op1=ALU.mult),
                         reads=['FG'], writes=[('cacc', j), 'cst'])
                    S.dma('act', lambda e: e.dma_start(out=out_d[(i - 2) * 128:(i - 1) * 128, :], in_=acc[j][:]), reads=[('cacc', j)], writes=[('out', i)])
        S.barrier()

    qT_d = dscr("qT_d", [D, 4096], BF16)
    kT_d = dscr("kT_d", [256, T], BF16)
    vA_d = dscr("vA_d", [T, 256], BF16)
    oT_d = dscr("oT_d", [D, 4096], BF16)

    def phase_A1(uT):
        wv = I("od_w_in").rearrange("(kc p) n -> p kc n", p=128)
        with contextlib.ExitStack() as es:
            def al(name, shape, dt):
                return es.enter_context(SBT(name, shape, dt))
            W = al("aW", [128, 8, 1536], BF16)
            GQ = al("aGQ", [128, 128], F32)
            GK = al("aGK", [128, 128], F32)
            COS = [al("aCOS%d" % j, [128, 128], F32) for j in range(2)]
            SIN = [al("aSIN%d" % j, [128, 128], F32) for j in range(2)]
            xf = [al("axf%d" % j, [128, 1536], F32) for j in range(2)]
            sq2 = [al("asq%d" % j_, [128, 1280], F32) for j_ in range(2)]
            st2 = [al("ast%d" % j_, [128, 40], F32) for j_ in range(2)]
            kn2 = [al("akn%d" % j_, [128, 1280], F32) for j_ in range(2)]
            t12 = [al("at1%d" % j_, [128, 1280], F32) for j_ in range(2)]
            t22 = [al("at2%d" % j_, [128, 1280], F32) for j_ in range(2)]
            kr = [al("akr%d" % j, [128, 1280], BF16) for j in range(2)]
            vb = [al("avb%d" % j, [128, 256], BF16) for j in range(2)]
            xT = [al("axT%d" % j, [128, 10, 128], BF16) for j in range(2)]
            for blk in range(3):
                S.dma('pool', lambda e: e.dma_start(out=W[:, :, blk * 512:(blk + 1) * 512], in_=wv[:, :, blk * 512:(blk + 1) * 512]), writes=[('aW', blk)])
            bcast_row(GQ[:], I("od_qk_g")[0:1, :], 'aGQ')
            bcast_row(GK[:], I("od_qk_g")[1:2, :], 'aGK')
            def hdrvars(i):
                lat = i >= 2
                j = i % 2
                rows = slice(i * 128, (i + 1) * 128)
                blks = [0, 1, 2] if lat else [2]
                h0 = 0 if lat else 8
                sq, st, kn, t1, t2 = sq2[j], st2[j], kn2[j], t12[j], t22[j]
                KSQ, KST = ('asq', j), ('ast', j)
                return lat, j, rows, blks, h0, sq, st, kn, t1, t2, KSQ, KST

            def stageA(i):
                lat, j, rows, blks, h0, sq, st, kn, t1, t2, KSQ, KST = hdrvars(i)
                if lat:
                    tr = slice((i - 2) * 128, (i - 1) * 128)
                    S.dma('pool', lambda e: e.dma_start(out=COS[j][:], in_=I("rope_cos")[tr, :]), writes=[('aCOS', j)])
                    S.dma('pool', lambda e: e.dma_start(out=SIN[j][:], in_=I("rope_sin")[tr, :]), writes=[('aSIN', j)])
                for blk in blks:
                    pb = blk
                    for kc in range(8):
                        S.op('pe', lambda e: e.matmul(psum[pb][:], uT[:, kc, rows], W[:, kc, blk * 512:(blk + 1) * 512], start=(kc == 0), stop=(kc == 7)),
                             reads=[('aW', blk)], writes=[PB(pb)])
                    S.op('act', lambda e: e.copy(xf[j][:, blk * 512:(blk + 1) * 512], psum[pb][:]), writes=[PB(pb), ('axf', j, blk)])
                S.op('pool', lambda e: e.tensor_copy(vb[j][:], xf[j][:, 1280:1536]), reads=[('axf', j, 2)], writes=[('avb', j)])
                S.dma('sp', lambda e: e.dma_start(out=vA_d[rows, :], in_=vb[j][:]), reads=[('avb', j)], writes=[('vA', i)])

            def stageB(i):
                lat, j, rows, blks, h0, sq, st, kn, t1, t2, KSQ, KST = hdrvars(i)
                c0 = h0 * 128
                nh = 10 - h0
                S.op('act', lambda e: e.activation(out=sq[:, c0:1280], in_=xf[j][:, c0:1280], func=AF.Square),
                     reads=[('axf', j, 0), ('axf', j, 1), ('axf', j, 2)], writes=[KSQ])
                S.op('dve', lambda e: e.tensor_reduce(out=st[:, h0:10], in_=sq[:, c0:1280].rearrange("p (h d) -> p h d", d=128), axis=AX.X, op=ALU.add),
                     reads=[KSQ], writes=[KST])
                S.op('dve', lambda e: e.tensor_scalar(out=st[:, 10 + h0:20], in0=st[:, h0:10], scalar1=1.0 / 128, scalar2=EPS, op0=ALU.mult, op1=ALU.add), writes=[KST])
                S.op('act', lambda e: e.activation(out=st[:, 20 + h0:30], in_=st[:, 10 + h0:20], func=AF.Sqrt), writes=[KST])
                S.op('dve', lambda e: e.reciprocal(st[:, 30 + h0:40], st[:, 20 + h0:30]), writes=[KST])
                for h in range(h0, 10):
                    hs = slice(h * 128, (h + 1) * 128)
                    G = GQ if h < 8 else GK
                    dst = kn[:, hs] if lat else kr[j][:, hs]
                    dk = ('akn', j, h) if lat else ('akr', j, h)
                    S.op('dve', lambda e: e.scalar_tensor_tensor(out=dst, in0=xf[j][:, hs], scalar=st[:, 30 + h:31 + h], in1=G[:], op0=ALU.mult, op1=ALU.mult),
                         reads=['aGQ', 'aGK', KST], writes=[dk])
                if lat:
                    snv = SIN[j][:].rearrange("p (b c) -> p b c", b=2)
                    for h in range(h0, 10):
                        hs = slice(h * 128, (h + 1) * 128)
                        knv = kn[:, hs].rearrange("p (b c) -> p b c", b=2)
                        t2v = t2[:, hs].rearrange("p (b c) -> p b c", b=2)
                        S.op('dve', lambda e: e.tensor_tensor(out=t1[:, hs], in0=kn[:, hs], in1=COS[j][:], op=ALU.mult), reads=[('akn', j, h), ('aCOS', j)],
                             writes=[('at1', j, h)])
                        S.op('pool', lambda e: e.tensor_tensor(out=t2v[:, :, 0:32], in0=knv[:, :, 32:64], in1=snv[:, :, 0:32], op=ALU.mult),
                             reads=[('akn', j, h), ('aSIN', j)], writes=[('at2', j, h)])
                        S.op('pool', lambda e: e.tensor_tensor(out=t2v[:, :, 32:64], in0=knv[:, :, 0:32], in1=snv[:, :, 32:64], op=ALU.mult),
                             reads=[('akn', j, h), ('aSIN', j)], writes=[('at2', j, h)])
                    for h in range(h0, 10):
                        hs = slice(h * 128, (h + 1) * 128)
                        S.op('dve', lambda e: e.tensor_tensor(out=kr[j][:, hs], in0=t1[:, hs], in1=t2[:, hs], op=ALU.add), reads=[('at1', j, h), ('at2', j, h)],
                             writes=[('akr', j, h)])

            def stageC(i):
                lat, j, rows, blks, h0, sq, st, kn, t1, t2, KSQ, KST = hdrvars(i)
                if lat:
                    for h in range(8):
                        S.op('pe', lambda e: e.transpose(psb[0][:, h * 128:(h + 1) * 128], kr[j][:, h * 128:(h + 1) * 128], identb[:]),
                             reads=[('akr', j, h)], writes=[PBB(0)])
                    S.op('act', lambda e: e.copy(xT[j][:, 0:8, :].rearrange("p a b -> p (a b)"), psb[0][:]), writes=[PBB(0), ('axT', j, 0)])
                    S.dma('sp', lambda e: e.dma_start(out=qT_d[:, (i - 2) * 128:(i - 1) * 128].rearrange("(h d) t -> d h t", d=128), in_=xT[j][:, 0:8, :]),
                          reads=[('axT', j, 0)], writes=[('qT_d', i)])
                for h in range(8, 10):
                    S.op('pe', lambda e: e.transpose(psb[1][:, (h - 8) * 128:(h - 7) * 128], kr[j][:, h * 128:(h + 1) * 128], identb[:]),
                         reads=[('akr', j, h)], writes=[PBB(1)])
                S.op('act', lambda e: e.copy(xT[j][:, 8:10, :].rearrange("p a b -> p (a b)"), psb[1][:, 0:256]), writes=[PBB(1), ('axT', j, 1)])
                S.dma('sp', lambda e: e.dma_start(out=kT_d[:, rows].rearrange("(h d) t -> d h t", d=128), in_=xT[j][:, 8:10, :]),
                      reads=[('axT', j, 1)], writes=[('kT_d', i)])

            stageA(0)
            for i in range(NT):
                if i + 1 < NT:
                    stageA(i + 1)
                stageB(i)
                stageC(i)
        S.barrier()

    def phase_A2():
        scale = 128.0 ** -0.5
        with contextlib.ExitStack() as es:
            def al(name, shape, dt):
                return es.enter_context(SBT(name, shape, dt))
            KT = al("bKT", [128, T], BF16)
            V = al("bV", [128, NT, 128], BF16)
            onesb = al("bones", [128, 128], BF16)
            QT4 = [al("bQT%d" % j, [128, 4, 128], BF16) for j in range(2)]
            PT = [al("bPT%d" % j, [128, 512], BF16) for j in range(4)]
            rs = [al("brs%d" % j, [128, 512], F32) for j in range(2)]
            ot = [al("bot%d" % j, [128, 4, 128], BF16) for j in range(2)]
            S.op('pool', lambda e: e.memset(onesb[:], 1.0), writes=['bones'])
            pc = 0
            it = 0
            for g in range(2):
                S.dma('sp', lambda e: e.dma_start(out=KT[:], in_=kT_d[g * 128:(g + 1) * 128, :]), writes=['bKT'])
                S.dma('sp', lambda e: e.dma_start(out=V[:], in_=vA_d[:, g * 128:(g + 1) * 128].rearrange("(n p) d -> p n d", p=128)), writes=['bV'])
                for qi in range(32):
                    j = it % 2
                    it += 1
                    bo, bs_ = 2 + 2 * j, 3 + 2 * j
                    S.dma('sp', lambda e: e.dma_start(out=QT4[j][:], in_=qT_d[g * 512:(g + 1) * 512, qi * 128:(qi + 1) * 128].rearrange("(h d) t -> d h t", d=128)),
                          writes=[('bQT', j)])

                    def st_mm(kt):
                        bank = kt % 2
                        S.op('pe', lambda e: e.matmul(psum[bank][:], KT[:, kt * 128:(kt + 1) * 128], QT4[j][:].rearrange("p a b -> p (a b)"), start=True, stop=True),
                             reads=['bKT', ('bQT', j)], writes=[PB(bank)])
                    st_mm(0)
                    for kt in range(NT):
                        bank = kt % 2
                        p_ = pc % 4
                        pc += 1
                        S.op('act', lambda e: e.activation(out=PT[p_][:], in_=psum[bank][:], func=AF.Exp, scale=scale), writes=[PB(bank), ('bPT', p_)])
                        if kt + 1 < NT:
                            st_mm(kt + 1)
                        S.op('pe', lambda e: e.matmul(psum[bo][:], V[:, kt, :], PT[p_][:], start=(kt == 0), stop=(kt == NT - 1)),
                             reads=[('bPT', p_), 'bV'], writes=[PB(bo)])
                        S.op('pe', lambda e: e.matmul(psum[bs_][:], onesb[:], PT[p_][:], start=(kt == 0), stop=(kt == NT - 1)),
                             reads=[('bPT', p_), 'bones'], writes=[PB(bs_)])
                    S.op('dve', lambda e: e.reciprocal(rs[j][:], psum[bs_][:]), writes=[PB(bs_), ('brs', j)])
                    S.op('dve', lambda e: e.tensor_tensor(out=ot[j][:].rearrange("p a b -> p (a b)"), in0=psum[bo][:], in1=rs[j][:], op=ALU.mult),
                         reads=[('brs', j)], writes=[PB(bo), ('bot', j)])
                    S.dma('pool', lambda e: e.dma_start(out=oT_d[g * 512:(g + 1) * 512, qi * 128:(qi + 1) * 128].rearrange("(h d) t -> d h t", d=128), in_=ot[j][:]),
                          reads=[('bot', j)], writes=[('oT_d', g, qi)])
        S.barrier()

    def phase_A3():
        with contextlib.ExitStack() as es:
            def al(name, shape, dt):
                return es.enter_context(SBT(name, shape, dt))
            WO = al("cWO", [128, 8, D], BF16)
            G1 = al("cG1", [128, D], F32)
            OT = [al("cOT%d" % j, [128, 8, 128], BF16) for j in range(2)]
            xt = [al("cx%d" % j, [128, D], F32) for j in range(2)]
            tmp = al("ctmp", [128, D], F32)
            S.dma('pool', lambda e: e.dma_start(out=WO[:], in_=I("od_w_out").rearrange("(kc p) n -> p kc n", p=128)), writes=['cWO'])
            bcast_row(G1[:], modrow[1, 0:1, 2 * D:3 * D], 'cG1')
            for qi in range(32):
                j = qi % 2
                rows = slice((qi + 2) * 128, (qi + 3) * 128)
                S.dma('sp', lambda e: e.dma_start(out=OT[j][:], in_=oT_d[:, qi * 128:(qi + 1) * 128].rearrange("(h d) t -> d h t", d=128)), writes=[('cOT', j)])
                S.dma('sp', lambda e: e.dma_start(out=xt[j][:], in_=xres[rows, :]), writes=[('cx', j)])
                for nb in range(2):
                    pb = (2 * qi + nb) % 4
                    for kc in range(8):
                        S.op('pe', lambda e: e.matmul(psum[pb][:], OT[j][:, kc, :], WO[:, kc, nb * 512:(nb + 1) * 512], start=(kc == 0), stop=(kc == 7)),
                             reads=[('cOT', j), 'cWO'], writes=[PB(pb)])
                    cs = slice(nb * 512, (nb + 1) * 512)
                    S.op('dve', lambda e: e.tensor_tensor(out=tmp[:, cs], in0=psum[pb][:], in1=G1[:, cs], op=ALU.mult), reads=['cG1'], writes=[PB(pb), ('ctmp', nb)])
                    S.op('pool', lambda e: e.tensor_tensor(out=tmp[:, cs], in0=tmp[:, cs], in1=xt[j][:, cs], op=ALU.add), reads=[('cx', j)], writes=[('ctmp', nb)])
                S.dma('pool', lambda e: e.dma_start(out=xres[rows, :], in_=tmp[:]), reads=[('ctmp', 0), ('ctmp', 1)], writes=[('xres', qi)])
        S.barrier()

    def small_dump():
        W_ = NT * 32 + NT * 4 + NT * 4 + 2 * NBMAX
        dl = dscr("dsmall_scr", [128, W_], F32)
        with SBT("dsm", [128, W_], F32) as dsm:
            o = 0
            for nm, n in (("LOG", NT * 32), ("WG", NT * 4), ("SLOTI", NT * 4)):
                S.op('dve', lambda e: e.tensor_copy(dsm[:, o:o + n], RT[nm][:].rearrange("p a b -> p (a b)")), writes=['dsm'])
                o += n
            for nm in ("IDXI", "BIDX"):
                S.op('dve', lambda e: e.tensor_copy(dsm[:, o:o + NBMAX], RT[nm][:]), writes=['dsm'])
                o += NBMAX
            S.dma('sp', lambda e: e.dma_start(out=dl, in_=dsm[:]), reads=['dsm'], writes=['dl'])
            S.barrier()
        return ('small', dl, [128, W_], F32)

    def copy_xin_to_xres():
        with SBT("cpx", [128, D], F32) as cpx:
            for i in range(NT):
                S.dma('sp', lambda e: e.dma_start(out=cpx[:], in_=I("xin")[i * 128:(i + 1) * 128, :]), writes=['cpx'])
                S.dma('sp', lambda e: e.dma_start(out=xres[i * 128:(i + 1) * 128, :], in_=cpx[:]), reads=['cpx'], writes=[('xres', i)])
        S.barrier()

    phase_mod()
    if stop_after == 'mod':
        return finish_dbg([('modrow', modrow.rearrange("a b c -> (a b) c"), [4, 6 * D], F32)])

    if stop_after not in ('A_only', 'M1_only'):
        uT_guard = SBT("uT", [128, 8, T], BF16)
        uT = uT_guard.__enter__()
        phase_norm_T(0, I("xin"), uT)
        phase_E1(uT)
        if stop_after == 'E1':
            return finish_dbg([('qkT', qkT, [2048, T], BF16), ('v_tm', v_tm, [T, D], BF16), ('so_tm', so_tm, [T, D], BF16),
                               ('g_tm', g_tm, [T, 16], F32), ('xrT', xrT, [D, T], F32), ('ygT', ygT, [D, T], BF16)])
        uT_guard.__exit__(None, None, None)
        phase_E2()
        if stop_after == 'E2':
            return finish_dbg([('hlT', hlT, [D, T], BF16)])
        phase_E3()
        if stop_after == 'E3':
            return finish_dbg([('hm_f', hm_d[0], [T, D], F32), ('hm_b', hm_d[1], [T, D], F32)])
        phase_E4()
        if stop_after == 'E4':
            return finish_dbg([('xres', xres, [T, D], F32)])
        NB0 = (4 * T) // SB + 32
        phase_N2(0, range(NT), RT["LOG"])
        phase_route(range(NT), NB0)
        phase_scatter(range(NT), NB0)
        if stop_after == 'R0':
            return finish_dbg([small_dump(), ('xs', xs_d, [NBMAX * SB, D], BF16), ('v2', v2_tm, [T, D], BF16)])
        phase_moe(0, NB0)
        phase_combine(0, range(NT), False)
        if stop_after == 'M0':
            return finish_dbg([('xres', xres, [T, D], F32)])
    else:
        copy_xin_to_xres()

    if stop_after != 'M1_only':
        uT_guard = SBT("uT", [128, 8, T], BF16)
        uT = uT_guard.__enter__()
        phase_norm_T(1, xres, uT)
        phase_A1(uT)
        uT_guard.__exit__(None, None, None)
        phase_A2()
        phase_A3()
        if stop_after in ('A', 'A_only'):
            return finish_dbg([('xres', xres, [T, D], F32)])
    NB1 = (4 * 4096) // SB + 32
    lat_tiles = range(2, NT)
    phase_N2(1, lat_tiles, RT["LOG"])
    phase_route(lat_tiles, NB1)
    phase_scatter(lat_tiles, NB1)
    phase_moe(1, NB1)
    phase_combine(1, lat_tiles, True)
    if stop_after == 'M1_only':
        return finish_dbg([('outc', out_d, [4096, D], F32)])
    S.barrier()
    return nc, dbg_out, used_inputs


def host_inputs(b, inp, names=None):
    f = lambda a: np.ascontiguousarray(a, dtype=np.float32)
    want = lambda k: names is None or k in names
    m = {}
    if want("xin"):
        m["xin"] = f(np.concatenate([inp["ctx"][b], inp["x"][b]], axis=0))
    if want("ccols"):
        m["ccols"] = f(np.concatenate([inp["c"][b].reshape(8, 128).T, inp["c_ctx"].reshape(8, 128).T], axis=1))
    for k in ["mod_w", "mod_b", "norm1_g", "norm2_g", "moe_w_r", "moe_b_r"]:
        if want(k):
            m[k] = f(inp[k])
    if want("final_g"):
        m["final_g"] = f(inp["final_g"].reshape(1, D))
    if want("ev_w_in"):
        m["ev_w_in"] = f(inp["ev_w_in"][0])
    if want("ev_qkcw"):
        qk = np.concatenate([inp["ev_qk_conv_w"][0], inp["ev_qk_conv_b"][0][None]], axis=0)
        m["ev_qkcw"] = f(qk.T.reshape(16, 128, 5).transpose(1, 0, 2))
    if want("ev_gate_b"):
        m["ev_gate_b"] = f(inp["ev_gate_b"][0].reshape(1, 16))
    if want("ev_mnorm_g"):
        m["ev_mnorm_g"] = f(inp["ev_mnorm_g"][0].reshape(1, D))
    if want("ev_lrucw"):
        lc = np.concatenate([inp["ev_lru_conv_w"][0], inp["ev_lru_conv_b"][0][None]], axis=0)
        m["ev_lrucw"] = f(lc.T.reshape(8, 128, 5).transpose(1, 0, 2))
    if want("ev_lru_w") or want("ev_lru_b"):
        lw = np.zeros((4, 8, 128, 128), np.float32)
        lb = np.zeros((128, 8, 4), np.float32)
        for z in range(2):
            for gi, (wk, bk) in enumerate([("ev_lru_wa", "ev_lru_ba"), ("ev_lru_wx", "ev_lru_bx")]):
                for cc_ in range(8):
                    for h in range(2):
                        n = cc_ * 2 + h
                        lw[z * 2 + gi, cc_, h * 64:(h + 1) * 64, h * 64:(h + 1) * 64] = inp[wk][0, z, n]
                        lb[h * 64:(h + 1) * 64, cc_, z * 2 + gi] = inp[bk][0, z, n]
        m["ev_lru_w"] = lw
        m["ev_lru_b"] = lb
    if want("ev_lru_lam"):
        m["ev_lru_lam"] = f(inp["ev_lru_lam"][0].reshape(2, 8, 128).transpose(2, 1, 0))
    if want("ev_w_out"):
        m["ev_w_out"] = f(inp["ev_w_out"][0])
    if want("od_w_in"):
        m["od_w_in"] = f(inp["od_w_in"][0])
    if want("od_qk_g"):
        m["od_qk_g"] = f(np.stack([inp["od_q_norm_g"][0], inp["od_k_norm_g"][0]]))
    if want("od_w_out"):
        m["od_w_out"] = f(inp["od_w_out"][0])
    if want("rope_cos") or want("rope_sin"):
        pos = np.arange(4096)
        row = (pos // 64).astype(np.float32)
        col = (pos % 64).astype(np.float32)
        inv = (10000.0 ** (-np.arange(0, 64, 2, dtype=np.float32) / 64)).astype(np.float32)
        ar = row[:, None] * inv[None]
        ac = col[:, None] * inv[None]
        m["rope_cos"] = f(np.concatenate([np.cos(ar), np.cos(ar), np.cos(ac), np.cos(ac)], axis=1))
        m["rope_sin"] = f(np.concatenate([-np.sin(ar), np.sin(ar), -np.sin(ac), np.sin(ac)], axis=1))
    for l in range(2):
        if want("moe_w1_%d" % l):
            m["moe_w1_%d" % l] = f(inp["moe_w1"][l]).reshape(32 * 128, 8, 2048)
        if want("moe_w2_%d" % l):
            m["moe_w2_%d" % l] = f(inp["moe_w2"][l]).reshape(32 * 128, 8, 1024)
    for l in range(2):
        if want("moe_b1_%d" % l):
            m["moe_b1_%d" % l] = f(inp["moe_b1"][l].reshape(32, 2, 128, 8).transpose(0, 2, 1, 3).reshape(32 * 128, 16))
        if want("moe_b2_%d" % l):
            m["moe_b2_%d" % l] = f(inp["moe_b2"][l])
    if names is not None:
        m = {k: v for k, v in m.items() if k in names}
    return m


_CACHE = {}


def kernel(**inputs):
    if 'nc' not in _CACHE:
        _CACHE['nc'] = build()
    nc, _, used = _CACHE['nc']
    names = set(used.keys())
    in_maps = [host_inputs(b, inputs, names) for b in range(8)]
    res = run_bass_kernel_spmd(nc, in_maps, core_ids=list(range(8)))
    return np.stack([r["out"] for r in res.results], axis=0).astype(np.float32)
```

```python
import contextlib
import numpy as np
import concourse.bass as bass
import concourse.mybir as mybir
from concourse.bass_utils import run_bass_kernel_spmd

F32 = mybir.dt.float32
BF16 = mybir.dt.bfloat16
I32 = mybir.dt.int32
U32 = mybir.dt.uint32
ALU = mybir.AluOpType
AF = mybir.ActivationFunctionType

T = 4352
NT = 34
D = 1024
NCTX = 256
EPS = 1e-6


class Sched:
    NSLOT = 6

    def __init__(self, nc):
        self.nc = nc
        self.engs = {'pe': nc.tensor, 'act': nc.scalar, 'dve': nc.vector,
                     'pool': nc.gpsimd, 'sp': nc.sync}
        self.sem = {}
        self.cnt = {}
        for k in self.engs:
            self.sem[k] = nc.alloc_semaphore('s_' + k)
            self.cnt[k] = 0
        self.dslots = {}
        self.dcnt = {}
        self.dnext = {}
        self.nslot = {'sp': 8, 'pool': 8, 'act': 2}
        for q in ('sp', 'pool', 'act'):
            self.dslots[q] = [nc.alloc_semaphore('d_%s%d' % (q, i)) for i in range(self.nslot[q])]
            self.dcnt[q] = [0] * self.nslot[q]
            self.dnext[q] = 0
        self.waited = {k: {} for k in self.engs}
        self.res = {}

    def _semof(self, tok):
        if tok[0] == 'e':
            return self.sem[tok[1]]
        return self.dslots[tok[1][0]][tok[1][1]]

    def _wait(self, e, tok):
        key = (tok[0], tok[1])
        if self.waited[e].get(key, 0) >= tok[2]:
            return
        self.engs[e].wait_ge(self._semof(tok), tok[2])
        self.waited[e][key] = tok[2]

    def _deps(self, e, reads, writes):
        deps = []
        for r in reads:
            st = self.res.get(r)
            if st and st['w'] is not None:
                deps.append(st['w'])
        for w in writes:
            st = self.res.get(w)
            if st:
                if st['w'] is not None:
                    deps.append(st['w'])
                deps.extend(st['r'].values())
        for tok in deps:
            if e == 'pe' and tok[0] == 'e' and tok[1] == 'pe':
                continue
            self._wait(e, tok)

    def _commit(self, tok, reads, writes):
        for r in reads:
            st = self.res.setdefault(r, {'w': None, 'r': {}})
            st['r'][(tok[0], tok[1])] = tok
        for w in writes:
            self.res[w] = {'w': tok, 'r': {}}

    def op(self, e, fn, reads=(), writes=()):
        self._deps(e, reads, writes)
        inst = fn(self.engs[e])
        self.cnt[e] += 1
        inst.then_inc(self.sem[e], 1)
        self._commit(('e', e, self.cnt[e]), reads, writes)
        return inst

    def dma(self, q, fn, reads=(), writes=()):
        s = self.dnext[q]
        self.dnext[q] = (s + 1) % self.nslot[q]
        if self.dcnt[q][s] > 0:
            self._wait(q, ('d', (q, s), 16 * self.dcnt[q][s]))
        self._deps(q, reads, writes)
        inst = fn(self.engs[q])
        self.dcnt[q][s] += 1
        inst.then_inc(self.dslots[q][s], 16)
        self._commit(('d', (q, s), 16 * self.dcnt[q][s]), reads, writes)
        return inst

    def barrier(self):
        toks = []
        for q in self.dslots:
            for s in range(self.nslot[q]):
                if self.dcnt[q][s] > 0:
                    toks.append(('d', (q, s), 16 * self.dcnt[q][s]))
        for k in self.engs:
            if self.cnt[k] > 0:
                toks.append(('e', k, self.cnt[k]))
        for e in self.engs:
            for tok in toks:
                self._wait(e, tok)
        self.res = {}


class Rot:
    def __init__(self, bufs, name):
        self.bufs = bufs
        self.name = name
        self.i = 0

    def next(self):
        b = self.bufs[self.i % len(self.bufs)]
        k = (self.name, self.i % len(self.bufs))
        self.i += 1
        return b, k


AX = mybir.AxisListType


def build(stop_after=None, layers=(0, 1)):
    nc = bass.Bass("TRN2", target_bir_lowering=False)
    S = Sched(nc)
    used_inputs = {}
    _ctr = [0]

    def SBT(name, shape, dt):
        _ctr[0] += 1
        return nc.sbuf_tensor("%s_u%d" % (name, _ctr[0]), shape, dt)

    IN_SPECS = {
        "xin": [T, D], "ccols": [128, 16], "mod_w": [2, D, 6 * D], "mod_b": [2, 6 * D],
        "norm1_g": [2, D], "norm2_g": [2, D], "final_g": [1, D],
        "ev_w_in": [D, 6160], "ev_qkcw": [128, 16, 5], "ev_gate_b": [1, 16], "ev_mnorm_g": [1, D],
        "ev_lrucw": [128, 8, 5], "ev_lru_w": [4, 8, 128, 128], "ev_lru_b": [128, 8, 4], "ev_lru_lam": [128, 8, 2],
        "ev_w_out": [2 * D, D], "od_w_in": [D, 1536], "od_qk_g": [2, 128], "od_w_out": [D, D],
        "rope_cos": [4096, 128], "rope_sin": [4096, 128],
        "moe_w_r": [2, D, 32], "moe_b_r": [2, 32],
        "moe_w1_0": [32 * 128, 8, 2048], "moe_w1_1": [32 * 128, 8, 2048],
        "moe_b1_0": [32 * 128, 16], "moe_b1_1": [32 * 128, 16],
        "moe_w2_0": [32 * 128, 8, 1024], "moe_w2_1": [32 * 128, 8, 1024],
        "moe_b2_0": [32, 1024], "moe_b2_1": [32, 1024],
    }

    def I(name):
        if name not in used_inputs:
            used_inputs[name] = nc.dram_tensor(name, list(IN_SPECS[name]), F32, kind="ExternalInput").ap()
        return used_inputs[name]

    def dscr(name, shape, dt=F32):
        return nc.dram_tensor(name, list(shape), dt, kind="Internal").ap()

    dbg_out = {}

    def dbgt(name, shape, dt=F32):
        dbg_out[name] = nc.dram_tensor("dbg_" + name, list(shape), dt, kind="ExternalOutput").ap()
        return dbg_out[name]

    def finish_dbg(pairs):
        S.barrier()
        for name, src, shape, dt in pairs:
            d = dbgt(name, shape, dt)
            rows, cols = shape[0], int(np.prod(shape[1:]))
            s2 = src if len(shape) == 2 else src.rearrange("a b c -> a (b c)")
            d2 = d if len(shape) == 2 else d.rearrange("a b c -> a (b c)")
            with SBT("dbgbuf_" + name, [128, cols], dt) as buf:
                for r0 in range(0, rows, 128):
                    n = min(128, rows - r0)
                    S.dma('sp', lambda e: e.dma_start(out=buf[0:n, :], in_=s2[r0:r0 + n, :]), writes=['dbgbuf'])
                    S.dma('sp', lambda e: e.dma_start(out=d2[r0:r0 + n, :], in_=buf[0:n, :]), reads=['dbgbuf'], writes=[('dbgo', name, r0)])
                S.barrier()
        return nc, dbg_out, used_inputs

    out_d = nc.dram_tensor("out", [4096, D], F32, kind="ExternalOutput").ap()
    modrow = dscr("modrow", [2, 2, 6 * D])
    xres = dscr("xres", [T, D])

    ident = nc.alloc_sbuf_tensor("ident", [128, 128], F32)
    identb = nc.alloc_sbuf_tensor("identb", [128, 128], BF16)
    ones1 = nc.alloc_sbuf_tensor("ones1", [1, 128], F32)
    onesm = nc.alloc_sbuf_tensor("onesm", [128, 128], F32)
    triF = nc.alloc_sbuf_tensor("triF", [128, 128], F32)
    triB = nc.alloc_sbuf_tensor("triB", [128, 128], F32)
    triS = nc.alloc_sbuf_tensor("triS", [128, 128], F32)
    selF = nc.alloc_sbuf_tensor("selF", [128, 128], F32)
    selB = nc.alloc_sbuf_tensor("selB", [128, 128], F32)
    pidx = nc.alloc_sbuf_tensor("pidx", [128, 1], F32)
    pidx_i = nc.alloc_sbuf_tensor("pidx_i", [128, 1], I32)
    S.op('pool', lambda e: e.memset(ident[:], 0.0), writes=['ident'])
    S.op('pool', lambda e: e.affine_select(ident[:], ident[:], pattern=[[-1, 128]], compare_op=ALU.not_equal,
                                          fill=1.0, base=0, channel_multiplier=1), reads=['ident'], writes=['ident'])
    S.op('dve', lambda e: e.tensor_copy(identb[:], ident[:]), reads=['ident'], writes=['identb'])
    S.op('pool', lambda e: e.memset(ones1[:], 1.0), writes=['ones1'])
    S.op('pool', lambda e: e.memset(onesm[:], 1.0), writes=['onesm'])

    def mk_mask(t, pattern, cm, base, cmp):
        S.op('pool', lambda e: e.memset(t[:], 1.0), writes=[t.name])
        S.op('pool', lambda e: e.affine_select(t[:], t[:], pattern=pattern, compare_op=cmp, fill=0.0, base=base,
                                              channel_multiplier=cm), writes=[t.name])

    mk_mask(triF, [[1, 128]], -1, 0, ALU.is_ge)
    mk_mask(triB, [[-1, 128]], 1, 0, ALU.is_ge)
    mk_mask(triS, [[1, 128]], -1, 0, ALU.is_gt)
    mk_mask(selF, [[0, 128]], 1, -127, ALU.is_equal)
    mk_mask(selB, [[0, 128]], 1, 0, ALU.is_equal)
    S.op('pool', lambda e: e.iota(pidx_i[:], pattern=[[0, 1]], base=0, channel_multiplier=1), writes=['pidx_i'])
    S.op('dve', lambda e: e.tensor_copy(pidx[:], pidx_i[:]), reads=['pidx_i'], writes=['pidx'])

    psum = [nc.alloc_psum_tensor("ps%d" % i, [128, 512], F32) for i in range(6)]
    psb = [nc.alloc_psum_tensor("psb%d" % i, [128, 1024], BF16) for i in range(2)]

    def PB(i):
        return ('psum', i)

    def PBB(i):
        return ('psb', i)

    def bcast_row(dst, src_row, key, q='sp'):
        S.dma(q, lambda e: e.dma_start(out=dst, in_=src_row.partition_broadcast(dst.shape[0])), writes=[key])

    def phase_mod():
        mod_w, mod_b = I("mod_w"), I("mod_b")
        with SBT("cc", [128, 16], F32) as cc, SBT("csil", [128, 16], BF16) as csil, \
                SBT("mw0", [128, 8, 512], BF16) as mw0, SBT("mw1", [128, 8, 512], BF16) as mw1, SBT("mw2", [128, 8, 512], BF16) as mw2, \
                SBT("mb0", [1, 512], F32) as mb0, SBT("mb1", [1, 512], F32) as mb1, \
                SBT("mo0", [1, 512], F32) as mo0, SBT("mo1", [1, 512], F32) as mo1:
            S.dma('sp', lambda e: e.dma_start(out=cc[:], in_=I("ccols")), writes=['cc'])
            S.op('act', lambda e: e.activation(out=csil[:], in_=cc[:], func=AF.Silu), reads=['cc'], writes=['csil'])
            mws = Rot([mw0, mw1, mw2], 'mw')
            mbs = Rot([mb0, mb1], 'mb')
            mos = Rot([mo0, mo1], 'mo')
            pi = 0
            for layer in range(2):
                mwv = mod_w[layer].rearrange("(kc p) n -> p kc n", p=128)
                for cb in range(12):
                    mw, mwk = mws.next()
                    mb, mbk = mbs.next()
                    S.dma('pool', lambda e: e.dma_start(out=mw[:], in_=mwv[:, :, cb * 512:(cb + 1) * 512]), writes=[mwk])
                    S.dma('sp', lambda e: e.dma_start(out=mb[:], in_=mod_b[layer:layer + 1, cb * 512:(cb + 1) * 512]), writes=[mbk])
                    for r in range(2):
                        ps = psum[pi % 4]
                        pk = PB(pi % 4)
                        pi += 1
                        for kc in range(8):
                            S.op('pe', lambda e: e.matmul(ps[0:1, :], csil[:, r * 8 + kc:r * 8 + kc + 1], mw[:, kc, :],
                                                          start=(kc == 0), stop=(kc == 7)), reads=['csil', mwk], writes=[pk])
                        mo, mok = mos.next()
                        S.op('dve', lambda e: e.tensor_tensor(out=mo[:], in0=ps[0:1, :], in1=mb[:], op=ALU.add), reads=[mbk], writes=[pk, mok])
                        S.dma('sp', lambda e: e.dma_start(out=modrow[layer, r:r + 1, cb * 512:(cb + 1) * 512], in_=mo[:]),
                              reads=[mok], writes=[('modrow', layer, r, cb)])
        S.barrier()

    def norm_tiles(layer, which, src, tiles, consume):
        gsrc = I("norm1_g") if which == 0 else I("norm2_g")
        sh_off = 0 if which == 0 else 3 * D
        sc_off = D if which == 0 else 4 * D
        with SBT("nA", [128, 2, D], F32) as A, SBT("nSH", [128, 2, D], F32) as SH, \
                SBT("nG", [128, D], F32) as G, \
                SBT("nx0", [128, D], F32) as x0, SBT("nx1", [128, D], F32) as x1, \
                SBT("nu0", [128, D], F32) as u0, SBT("nu1", [128, D], F32) as u1, \
                SBT("nsq", [128, 2, D], F32) as sq2, SBT("nst", [128, 2, 8], F32) as st2:
            bcast_row(G[:], gsrc[layer:layer + 1, :], 'nG')
            for r in range(2):
                bcast_row(A[:, r, :], modrow[layer, r:r + 1, sc_off:sc_off + D], ('nA', r))
                bcast_row(SH[:, r, :], modrow[layer, r:r + 1, sh_off:sh_off + D], ('nSH', r))
                S.op('dve', lambda e: e.scalar_tensor_tensor(out=A[:, r, :], in0=A[:, r, :], scalar=1.0, in1=G[:],
                                                             op0=ALU.add, op1=ALU.mult), reads=['nG'], writes=[('nA', r)])
            xs = Rot([x0, x1], 'nx')
            us = Rot([u0, u1], 'nu')
            tl = list(tiles)
            pend = {}

            def pre(n_):
                i = tl[n_]
                r = 1 if i < 2 else 0
                xt, xk = xs.next()
                ut, uk = us.next()
                sq = sq2[:, n_ % 2, :]
                st = st2[:, n_ % 2, :]
                NSQ, NST = ('nsq', n_ % 2), ('nst', n_ % 2)
                S.dma('sp', lambda e: e.dma_start(out=xt[:], in_=src[i * 128:(i + 1) * 128, :]), writes=[xk])
                S.op('act', lambda e: e.activation(out=sq, in_=xt[:], func=AF.Square, accum_out=st[:, 0:1]),
                     reads=[xk], writes=[NSQ, NST])
                S.op('dve', lambda e: e.tensor_scalar(out=st[:, 1:2], in0=st[:, 0:1], scalar1=1.0 / D, scalar2=EPS,
                                                      op0=ALU.mult, op1=ALU.add), writes=[NST])
                S.op('act', lambda e: e.activation(out=st[:, 2:3], in_=st[:, 1:2], func=AF.Sqrt), writes=[NST])
                S.op('dve', lambda e: e.reciprocal(st[:, 3:4], st[:, 2:3]), writes=[NST])
                S.op('dve', lambda e: e.scalar_tensor_tensor(out=ut[:], in0=xt[:], scalar=st[:, 3:4], in1=A[:, r, :],
                                                             op0=ALU.mult, op1=ALU.mult), reads=[xk, ('nA', r)], writes=[uk, NST])
                S.op('pool', lambda e: e.tensor_tensor(out=ut[:], in0=ut[:], in1=SH[:, r, :], op=ALU.add),
                     reads=[('nSH', r)], writes=[uk])
                pend[n_] = (i, ut, uk)

            pre(0)
            for n_ in range(len(tl)):
                if n_ + 1 < len(tl):
                    pre(n_ + 1)
                consume(*pend.pop(n_))

    def phase_norm_T(layer, src, uT, tiles=range(NT)):
        def consume(i, ut, uk):
            for half in range(2):
                pb = (i * 2 + half) % 4
                for j in range(4):
                    kc = half * 4 + j
                    S.op('pe', lambda e: e.transpose(psum[pb][:, j * 128:(j + 1) * 128], ut[:, kc * 128:(kc + 1) * 128], ident[:]),
                         reads=[uk, 'ident'], writes=[PB(pb)])
                S.op('act', lambda e: e.copy(uT[:, half * 4:half * 4 + 4, i * 128:(i + 1) * 128],
                                             psum[pb][:].rearrange("p (a b) -> p a b", a=4)),
                     writes=[PB(pb), ('uT', i, half)])
        norm_tiles(layer, 0, src, tiles, consume)
        S.barrier()

    qkT = dscr("qkT", [2048, T], BF16)
    v_tm = dscr("v_tm", [T, D], BF16)
    so_tm = dscr("so_tm", [T, D], BF16)
    g_tm = dscr("g_tm", [T, 16])
    xrT = dscr("xrT", [D, T])
    ygT = dscr("ygT", [D, T], BF16)
    hlT = dscr("hlT", [D, T], BF16)
    hm_d = [dscr("hm_f", [T, D]), dscr("hm_b", [T, D])]
    TG = [(0, 256)] + [(256 + 512 * j, 512) for j in range(8)]
    ZW = 4358
    CN = 4355

    def zcol(t0):
        return 2 + t0 if t0 < 256 else t0 + 5

    def phase_E1(uT):
        wv = I("ev_w_in").rearrange("(kc p) n -> p kc n", p=128)
        with SBT("wb0", [128, 8, 512], BF16) as wb0, SBT("wb1", [128, 8, 512], BF16) as wb1, \
                SBT("zp0", [128, ZW], F32) as zp0, SBT("zp1", [128, ZW], F32) as zp1, \
                SBT("co", [128, ZW], F32) as co, SBT("co_b", [128, ZW], F32) as co_b, \
                SBT("ob0", [128, ZW], BF16) as ob0, SBT("ob1", [128, ZW], BF16) as ob1, \
                SBT("cwq", [128, 16, 5], F32) as cwq, SBT("cwl", [128, 8, 5], F32) as cwl, \
                SBT("tms0", [128, 512], BF16) as tms0, SBT("tms1", [128, 512], BF16) as tms1, \
                SBT("wg", [128, 8, 16], BF16) as wg, SBT("gbr", [128, 16], F32) as gbr, \
                SBT("GT", [128, NT, 16], F32) as GT:
            S.dma('sp', lambda e: e.dma_start(out=cwq[:], in_=I("ev_qkcw")), writes=['cwq'])
            S.dma('sp', lambda e: e.dma_start(out=cwl[:], in_=I("ev_lrucw")), writes=['cwl'])
            S.op('pool', lambda e: e.memset(zp0[:], 0.0), writes=[('zp', 0)])
            S.op('pool', lambda e: e.memset(zp1[:], 0.0), writes=[('zp', 1)])
            wbs = Rot([wb0, wb1], 'wb')
            zps = Rot([zp0, zp1], 'zp')
            obs = Rot([ob0, ob1], 'ob')
            pi = [0]
            fm_blocks = [(c0, 'qk', c0 // 128) for c0 in range(0, 2048, 512)] + \
                        [(4112 + j * 512, 'xr', j * 4) for j in range(2)] + \
                        [(5136 + j * 512, 'yg', j * 4) for j in range(2)]
            chunks = []
            for c0, kind, cbase in fm_blocks:
                for jj in range(4):
                    chunks.append((c0, kind, cbase + jj, jj))
            cos_ = [co, co_b]
            state = {}

            def stage1(n):
                c0, kind, cidx, jj = chunks[n]
                if jj == 0:
                    wb, wbk = wbs.next()
                    S.dma('pool', lambda e: e.dma_start(out=wb[:], in_=wv[:, :, c0:c0 + 512]), writes=[wbk])
                    state['wb'] = (wb, wbk)
                wb, wbk = state['wb']
                zp, zpk = zps.next()
                cq = cos_[n % 2]
                ck = ('co', n % 2)
                for (t0, n_) in TG:
                    pb = pi[0] % 2
                    pi[0] += 1
                    for kc in range(8):
                        S.op('pe', lambda e: e.matmul(psum[pb][:, 0:n_], wb[:, kc, jj * 128:(jj + 1) * 128], uT[:, kc, t0:t0 + n_],
                                                      start=(kc == 0), stop=(kc == 7)), reads=[wbk], writes=[PB(pb)])
                    z0 = zcol(t0)
                    S.op('act', lambda e: e.copy(zp[:, z0:z0 + n_], psum[pb][:, 0:n_]), writes=[PB(pb), zpk])
                if kind in ('qk', 'xr'):
                    cw = cwq if kind == 'qk' else cwl
                    S.op('dve', lambda e: e.tensor_scalar(out=cq[:, 0:CN], in0=zp[:, 0:CN], scalar1=cw[:, cidx, 0:1], scalar2=None,
                                                          op0=ALU.mult), reads=[zpk, 'cwq', 'cwl'], writes=[ck])
                    for j in range(1, 4):
                        S.op('dve', lambda e: e.scalar_tensor_tensor(out=cq[:, 0:CN], in0=zp[:, j:j + CN], scalar=cw[:, cidx, j:j + 1],
                                                                     in1=cq[:, 0:CN], op0=ALU.mult, op1=ALU.add),
                             reads=[zpk], writes=[ck])
                state[n] = (zp, zpk, cq, ck)

            def stage2(n):
                c0, kind, cidx, jj = chunks[n]
                zp, zpk, cq, ck = state.pop(n)
                ob, obk = obs.next()
                if kind == 'qk':
                    S.op('act', lambda e: e.activation(out=ob[:, 0:CN], in_=cq[:, 0:CN], func=AF.Silu, bias=cwq[:, cidx, 4:5], scale=1.0),
                         reads=[ck], writes=[obk])
                    rows = qkT[cidx * 128:(cidx + 1) * 128, :]
                    S.dma('sp', lambda e: e.dma_start(out=rows[:, 0:256], in_=ob[:, 0:256]), reads=[obk], writes=[('qkT', cidx, 0)])
                    S.dma('sp', lambda e: e.dma_start(out=rows[:, 256:T], in_=ob[:, 259:CN]), reads=[obk], writes=[('qkT', cidx, 1)])
                elif kind == 'xr':
                    S.op('act', lambda e: e.activation(out=cq[:, 0:CN], in_=cq[:, 0:CN], func=AF.Identity, bias=cwl[:, cidx, 4:5], scale=1.0),
                         writes=[ck])
                    rows = xrT[cidx * 128:(cidx + 1) * 128, :]
                    S.dma('sp', lambda e: e.dma_start(out=rows[:, 0:256], in_=cq[:, 0:256]), reads=[ck], writes=[('xrT', cidx, 0)])
                    S.dma('sp', lambda e: e.dma_start(out=rows[:, 256:T], in_=cq[:, 259:CN]), reads=[ck], writes=[('xrT', cidx, 1)])
                else:
                    S.op('act', lambda e: e.activation(out=cq[:], in_=zp[:], func=AF.Square), reads=[zpk], writes=[ck])
                    S.op('dve', lambda e: e.tensor_scalar(out=cq[:], in0=cq[:], scalar1=0.044715, scalar2=1.0, op0=ALU.mult, op1=ALU.add),
                         writes=[ck])
                    S.op('dve', lambda e: e.tensor_tensor(out=cq[:], in0=cq[:], in1=zp[:], op=ALU.mult), reads=[zpk], writes=[ck])
                    S.op('act', lambda e: e.activation(out=cq[:], in_=cq[:], func=AF.Sigmoid, scale=1.5957691216), writes=[ck])
                    S.op('pool', lambda e: e.tensor_tensor(out=ob[:], in0=cq[:], in1=zp[:], op=ALU.mult), reads=[ck, zpk], writes=[obk])
                    rows = ygT[cidx * 128:(cidx + 1) * 128, :]
                    S.dma('sp', lambda e: e.dma_start(out=rows[:, 0:256], in_=ob[:, 2:258]), reads=[obk], writes=[('ygT', cidx, 0)])
                    S.dma('sp', lambda e: e.dma_start(out=rows[:, 256:T], in_=ob[:, 261:4357]), reads=[obk], writes=[('ygT', cidx, 1)])

            stage1(0)
            for n in range(len(chunks)):
                if n + 1 < len(chunks):
                    stage1(n + 1)
                stage2(n)
            tms = Rot([tms0, tms1], 'tms')
            for blk in range(4):
                c0 = 2048 + blk * 512
                wb, wbk = wbs.next()
                S.dma('pool', lambda e: e.dma_start(out=wb[:], in_=wv[:, :, c0:c0 + 512]), writes=[wbk])
                dst = v_tm if blk < 2 else so_tm
                dc0 = (blk % 2) * 512
                for i in range(NT):
                    pb = 2 + i % 2
                    for kc in range(8):
                        S.op('pe', lambda e: e.matmul(psum[pb][:], uT[:, kc, i * 128:(i + 1) * 128], wb[:, kc, :],
                                                      start=(kc == 0), stop=(kc == 7)), reads=[wbk], writes=[PB(pb)])
                    st_, stk = tms.next()
                    if blk < 2:
                        S.op('act', lambda e: e.copy(st_[:], psum[pb][:]), writes=[PB(pb), stk])
                    else:
                        S.op('act', lambda e: e.activation(out=st_[:], in_=psum[pb][:], func=AF.Sigmoid), writes=[PB(pb), stk])
                    S.dma('sp', lambda e: e.dma_start(out=dst[i * 128:(i + 1) * 128, dc0:dc0 + 512], in_=st_[:]), reads=[stk],
                          writes=[('tmout', blk, i)])
            S.dma('pool', lambda e: e.dma_start(out=wg[:], in_=wv[:, :, 4096:4112]), writes=['wg'])
            bcast_row(gbr[:], I("ev_gate_b")[0:1, :], 'gbr')
            for i in range(NT):
                pb = 4 + i % 2
                for kc in range(8):
                    S.op('pe', lambda e: e.matmul(psum[pb][:, 0:16], uT[:, kc, i * 128:(i + 1) * 128], wg[:, kc, :],
                                                  start=(kc == 0), stop=(kc == 7)), reads=['wg'], writes=[PB(pb)])
                S.op('dve', lambda e: e.tensor_tensor(out=GT[:, i, :], in0=psum[pb][:, 0:16], in1=gbr[:], op=ALU.add),
                     reads=['gbr'], writes=[PB(pb), 'GT'])
            S.dma('sp', lambda e: e.dma_start(out=g_tm.rearrange("(n p) c -> p n c", p=128), in_=GT[:]), reads=['GT'], writes=['g_tm'])
        S.barrier()

    def phase_E2():
        lw_d, lb_d, lam_d = I("ev_lru_w"), I("ev_lru_b"), I("ev_lru_lam")
        with SBT("lxr", [128, T], F32) as xr, SBT("lyg", [128, T], BF16) as yg, \
                SBT("lA", [128, T], F32) as A, SBT("lB", [128, T], F32) as Bx, \
                SBT("ltmp", [128, T], F32) as tmp, \
                SBT("lH0", [128, T], F32) as H0, SBT("lH1", [128, T], F32) as H1, \
                SBT("lho", [128, T], BF16) as ho, \
                SBT("lw", [128, 4, 128], F32) as lw, SBT("lwb", [128, 4, 128], BF16) as lwb, SBT("xrb", [128, T], BF16) as xrb, SBT("lb", [128, 8, 4], F32) as lb, \
                SBT("lam", [128, 8, 2], F32) as lam, SBT("cA", [128, 8, 2], F32) as cA:
            S.dma('sp', lambda e: e.dma_start(out=lb[:], in_=lb_d), writes=['lb'])
            S.dma('sp', lambda e: e.dma_start(out=lam[:], in_=lam_d), writes=['lam'])
            S.op('act', lambda e: e.activation(out=cA[:], in_=lam[:], func=AF.Exp, scale=-1.0), reads=['lam'], writes=['cA'])
            S.op('act', lambda e: e.activation(out=cA[:], in_=cA[:], func=AF.Ln, bias=1.0, scale=1.0), writes=['cA'])
            S.op('dve', lambda e: e.tensor_scalar(out=cA[:], in0=cA[:], scalar1=-8.0, scalar2=None, op0=ALU.mult), writes=['cA'])
            pi = 0
            for cc in range(8):
                S.dma('sp', lambda e: e.dma_start(out=xr[:], in_=xrT[cc * 128:(cc + 1) * 128, :]), writes=['xr'])
                S.dma('sp', lambda e: e.dma_start(out=yg[:], in_=ygT[cc * 128:(cc + 1) * 128, :]), writes=['yg'])
                S.dma('sp', lambda e: e.dma_start(out=lw[:], in_=lw_d[:, cc].rearrange("g k m -> k g m")), writes=['lw'])
                S.op('pool', lambda e: e.tensor_copy(lwb[:], lw[:]), reads=['lw'], writes=['lwb'])
                S.op('pool', lambda e: e.tensor_copy(xrb[:], xr[:]), reads=['xr'], writes=['xrb'])
                for z in range(2):
                    H = H0 if z == 0 else H1
                    hk = ('H', z)
                    for gi, dst, dk in ((0, A, 'A'), (1, Bx, 'Bx')):
                        for (t0, n) in TG:
                            pb = pi % 2
                            pi += 1
                            S.op('pe', lambda e: e.matmul(psum[pb][:, 0:n], lwb[:, z * 2 + gi, :], xrb[:, t0:t0 + n], start=True, stop=True),
                                 reads=['lwb', 'xrb'], writes=[PB(pb)])
                            S.op('act', lambda e: e.activation(out=dst[:, t0:t0 + n], in_=psum[pb][:, 0:n], func=AF.Sigmoid,
                                                               bias=lb[:, cc, z * 2 + gi:z * 2 + gi + 1], scale=1.0),
                                 reads=['lb'], writes=[PB(pb), dk])
                    S.op('act', lambda e: e.activation(out=A[:], in_=A[:], func=AF.Exp, scale=cA[:, cc, z:z + 1]), reads=['cA'], writes=['A'])
                    S.op('act', lambda e: e.activation(out=tmp[:], in_=A[:], func=AF.Square), reads=['A'], writes=['tmp'])
                    S.op('act', lambda e: e.activation(out=tmp[:], in_=tmp[:], func=AF.Sqrt, bias=1.0, scale=-1.0), writes=['tmp'])
                    S.op('pool', lambda e: e.tensor_tensor(out=Bx[:], in0=Bx[:], in1=xr[:], op=ALU.mult), reads=['xr'], writes=['Bx'])
                    S.op('dve', lambda e: e.tensor_tensor(out=Bx[:], in0=Bx[:], in1=tmp[:], op=ALU.mult), reads=['tmp'], writes=['Bx'])
                    if z == 0:
                        S.op('dve', lambda e: e.tensor_tensor_scan(H[:], A[:], Bx[:], 0.0, ALU.mult, ALU.add), reads=['A', 'Bx'], writes=[hk])
                    else:
                        S.op('dve', lambda e: e.tensor_tensor_scan(H[:, 0:256][:, ::-1], A[:, 0:256][:, ::-1], Bx[:, 0:256][:, ::-1], 0.0,
                                                                   ALU.mult, ALU.add), reads=['A', 'Bx'], writes=[hk])
                        S.op('dve', lambda e: e.tensor_tensor_scan(H[:, 256:T][:, ::-1], A[:, 256:T][:, ::-1], Bx[:, 256:T][:, ::-1], H[:, 0:1],
                                                                   ALU.mult, ALU.add), reads=['A', 'Bx'], writes=[hk])
                S.op('pool', lambda e: e.tensor_tensor(out=H0[:], in0=H0[:], in1=H1[:], op=ALU.add), reads=[('H', 1)], writes=[('H', 0)])
                S.op('dve', lambda e: e.tensor_tensor(out=ho[:], in0=H0[:], in1=yg[:], op=ALU.mult), reads=[('H', 0), 'yg'], writes=['ho'])
                S.dma('pool', lambda e: e.dma_start(out=hlT[cc * 128:(cc + 1) * 128, :], in_=ho[:]), reads=['ho'], writes=[('hlT', cc)])
        S.barrier()

    def phase_E3():
        NLN16 = -2.772588722239781
        with contextlib.ExitStack() as es:
            def al(name, shape, dt):
                return es.enter_context(SBT(name, shape, dt))
            G = al("G", [128, NT, 16], F32)
            LF = al("LF", [128, 2, NT, 4], F32)
            CF = al("CF", [128, 2, NT, 4], F32)
            EB16 = al("EB16", [128, 2, NT, 4], F32)
            ECF = al("ECF", [128, 2, NT, 4], F32)
            DEC = al("DEC", [128, 2, NT, 4], F32)
            WS16 = al("WS16", [128, 2, NT, 4], F32)
            qT0 = al("qT0", [128, 2, T], BF16)
            qT1 = al("qT1", [128, 2, T], BF16)
            kT0 = al("kT0", [128, 2, T], BF16)
            kT1 = al("kT1", [128, 2, T], BF16)
            V0 = al("V0", [128, NT, 257], BF16)
            V1 = al("V1", [128, NT, 257], BF16)
            CTs = al("CTs", [128, 4, 2, 257], F32)
            CTbs = al("CTbs", [128, 4, 2, 257], BF16)
            PTs = al("PTs", [128, 4, 128], BF16)
            ktms = al("ktms", [128, 4, 256], BF16)
            vws = al("vws", [128, 4, 257], BF16)
            sms = al("sms", [128, 4, 8], F32)
            houts = al("houts", [128, 4, 256], F32)
            S.dma('sp', lambda e: e.dma_start(out=G[:], in_=g_tm.rearrange("(n p) c -> p n c", p=128)), writes=['G'])
            Gv = G[:].rearrange("p n (d g h) -> p d g n h", d=2, g=2, h=4)
            fl = lambda t, d: t[:, d].rearrange("p n h -> p (n h)")
            for d in range(2):
                S.op('act', lambda e: e.activation(out=LF[:, d], in_=Gv[:, d, 1], func=AF.Exp, scale=-1.0), reads=['G'], writes=['LF'])
                S.op('act', lambda e: e.activation(out=LF[:, d], in_=LF[:, d], func=AF.Ln, bias=1.0, scale=1.0), writes=['LF'])
                S.op('dve', lambda e: e.tensor_scalar(out=LF[:, d], in0=LF[:, d], scalar1=-1.0, scalar2=None, op0=ALU.mult), writes=['LF'])
                tri = triF if d == 0 else triB
                sel = selF if d == 0 else selB
                S.op('pe', lambda e: e.matmul(psum[0][:, 0:NT * 4], tri[:], fl(LF, d), start=True, stop=True),
                     reads=['LF', tri.name], writes=[PB(0)])
                S.op('act', lambda e: e.copy(fl(CF, d), psum[0][:, 0:NT * 4]), writes=[PB(0), 'CF'])
                S.op('pe', lambda e: e.matmul(psum[1][:, 0:NT * 4], sel[:], fl(CF, d), start=True, stop=True),
                     reads=['CF', sel.name], writes=[PB(1)])
                S.op('act', lambda e: e.activation(out=fl(DEC, d), in_=psum[1][:, 0:NT * 4], func=AF.Exp), writes=[PB(1), 'DEC'])
                S.op('dve', lambda e: e.tensor_tensor(out=EB16[:, d], in0=Gv[:, d, 0], in1=CF[:, d], op=ALU.subtract), reads=['G', 'CF'], writes=['EB16'])
                S.op('dve', lambda e: e.tensor_scalar(out=EB16[:, d], in0=EB16[:, d], scalar1=NLN16, scalar2=None, op0=ALU.add), writes=['EB16'])
                S.op('act', lambda e: e.activation(out=EB16[:, d], in_=EB16[:, d], func=AF.Exp), writes=['EB16'])
                S.op('act', lambda e: e.activation(out=ECF[:, d], in_=CF[:, d], func=AF.Exp), reads=['CF'], writes=['ECF'])
                S.op('dve', lambda e: e.tensor_tensor(out=WS16[:, d], in0=EB16[:, d], in1=DEC[:, d], op=ALU.mult), reads=['EB16', 'DEC'], writes=['WS16'])
            S.barrier()
            qTs, kTs, Vs = [qT0, qT1], [kT0, kT1], [V0, V1]
            order = [list(range(NT)), [1, 0] + list(range(NT - 1, 1, -1))]
            cnt = {'st': 0, 'num': 0}
            for hp in range(2):
                for hh in range(2):
                    h = hp * 2 + hh
                    S.dma('sp', lambda e: e.dma_start(out=qTs[hh][:], in_=qkT[h * 256:(h + 1) * 256, :].rearrange("(dh p) t -> p dh t", p=128)),
                          writes=[('qT', hh)])
                    S.dma('sp', lambda e: e.dma_start(out=kTs[hh][:], in_=qkT[1024 + h * 256:1024 + (h + 1) * 256, :].rearrange("(dh p) t -> p dh t", p=128)),
                          writes=[('kT', hh)])
                    S.dma('sp', lambda e: e.dma_start(out=Vs[hh][:, :, 0:256], in_=v_tm[:, h * 256:(h + 1) * 256].rearrange("(n p) e -> p n e", p=128)),
                          writes=[('V', hh)])
                    S.op('pool', lambda e: e.memset(Vs[hh][:, :, 256:257], 1.0), writes=[('V1', hh)])
                S.op('pool', lambda e: e.memset(CTs[:], 0.0), writes=[('CT', i) for i in range(4)])
                S.op('pool', lambda e: e.memset(CTbs[:], 0.0), writes=[('CTb', i) for i in range(4)])
                for step in range(NT):
                    for ci, (z, hh) in enumerate([(0, 0), (0, 1), (1, 0), (1, 1)]):
                        h = hp * 2 + hh
                        c = order[z][step]
                        sl = slice(c * 128, (c + 1) * 128)
                        qT, kT, V = qTs[hh], kTs[hh], Vs[hh]
                        rk = [('qT', hh), ('kT', hh), ('V', hh), ('V1', hh)]
                        tri = triF if z == 0 else triB
                        pst = cnt['st'] % 2
                        cnt['st'] += 1
                        for dh in range(2):
                            S.op('pe', lambda e: e.matmul(psum[pst][:, 0:128], kT[:, dh, sl], qT[:, dh, sl], start=(dh == 0), stop=(dh == 1)),
                                 reads=rk, writes=[PB(pst)])
                        S.op('dve', lambda e: e.scalar_tensor_tensor(out=PTs[:, ci, :], in0=psum[pst][:, 0:128], scalar=EB16[:, z, c, h:h + 1],
                                                                     in1=tri[:], op0=ALU.mult, op1=ALU.mult), writes=[PB(pst), ('PT', ci)])
                        for dh in range(2):
                            S.op('pe', lambda e: e.transpose(psb[0][:, dh * 128:(dh + 1) * 128], kT[:, dh, sl], identb[:]),
                                 reads=rk, writes=[PBB(0)])
                        S.op('act', lambda e: e.copy(ktms[:, ci, :], psb[0][:, 0:256]), writes=[PBB(0), ('ktm', ci)])
                        S.op('act', lambda e: e.activation(out=vws[:, ci, :], in_=V[:, c, :], func=AF.Identity, scale=WS16[:, z, c, h:h + 1]),
                             reads=rk, writes=[('vw', ci)])
                        pn = 2 + cnt['num'] % 2
                        cnt['num'] += 1
                        S.op('pe', lambda e: e.matmul(psum[pn][:, 0:257], PTs[:, ci, :], V[:, c, :], start=True, stop=False),
                             reads=rk + [('PT', ci)], writes=[PB(pn)])
                        for dh in range(2):
                            S.op('pe', lambda e: e.matmul(psum[pn][:, 0:257], qT[:, dh, sl], CTbs[:, ci, dh, :], start=False, stop=(dh == 1)),
                                 reads=rk + [('CTb', ci)], writes=[PB(pn)])
                        sm = sms[:, ci, :]
                        ecf = ECF[:, z, c, h:h + 1]
                        S.op('dve', lambda e: e.tensor_tensor(out=sm[:, 0:1], in0=psum[pn][:, 256:257], in1=ecf, op=ALU.mult), writes=[PB(pn), ('sm', ci)])
                        S.op('dve', lambda e: e.tensor_scalar(out=sm[:, 2:3], in0=sm[:, 0:1], scalar1=1.0, scalar2=None, op0=ALU.max), writes=[('sm', ci)])
                        S.op('dve', lambda e: e.tensor_scalar(out=sm[:, 3:4], in0=sm[:, 0:1], scalar1=-1.0, scalar2=sm[:, 2:3], op0=ALU.mult, op1=ALU.max),
                             writes=[('sm', ci)])
                        S.op('dve', lambda e: e.reciprocal(sm[:, 4:5], sm[:, 3:4]), writes=[('sm', ci)])
                        S.op('dve', lambda e: e.tensor_tensor(out=sm[:, 5:6], in0=sm[:, 4:5], in1=ecf, op=ALU.mult), writes=[('sm', ci)])
                        S.op('act', lambda e: e.activation(out=houts[:, ci, :], in_=psum[pn][:, 0:256], func=AF.Identity, scale=sm[:, 5:6]),
                             reads=[('sm', ci)], writes=[PB(pn), ('hout', ci)])
                        S.dma('sp', lambda e: e.dma_start(out=hm_d[z][sl, h * 256:(h + 1) * 256], in_=houts[:, ci, :]), reads=[('hout', ci)],
                              writes=[('hm', z, h, c)])
                        for dh in range(2):
                            S.op('pe', lambda e: e.matmul(psum[4 + dh][:, 0:257], ktms[:, ci, dh * 128:(dh + 1) * 128], vws[:, ci, :], start=True, stop=True),
                                 reads=[('ktm', ci), ('vw', ci)], writes=[PB(4 + dh)])
                            S.op('dve', lambda e: e.scalar_tensor_tensor(out=CTs[:, ci, dh, :], in0=CTs[:, ci, dh, :], scalar=DEC[:, z, c, h:h + 1],
                                                                         in1=psum[4 + dh][:, 0:257], op0=ALU.mult, op1=ALU.add),
                                 writes=[PB(4 + dh), ('CT', ci)])
                        S.op('act', lambda e: e.copy(CTbs[:, ci], CTs[:, ci]), reads=[('CT', ci)], writes=[('CTb', ci)])
        S.barrier()

    def mixer_out(i, r, G1, wo, nk, lhs_list, src_rows, pbase):
        return

    def phase_E4():
        with contextlib.ExitStack() as es:
            def al(name, shape, dt):
                return es.enter_context(SBT(name, shape, dt))
            wo = al("wo", [128, 16, D], BF16)
            MG = al("MG", [128, D], F32)
            G1 = al("G1", [128, 2, D], F32)
            hf = [al("hf%d" % j, [128, D], F32) for j in range(2)]
            hb = [al("hb%d" % j, [128, D], F32) for j in range(2)]
            so = [al("so%d" % j, [128, D], BF16) for j in range(2)]
            sq2 = [al("e4sq%d" % j_, [128, 256], F32) for j_ in range(2)]
            st2 = [al("e4st%d" % j_, [128, 16], F32) for j_ in range(2)]
            hmb2 = [al("hmb%d" % j_, [128, D], BF16) for j_ in range(2)]
            hmT = [al("hmT%d" % j, [128, 8, 128], BF16) for j in range(2)]
            hl = [al("hl%d" % j, [128, 8, 128], BF16) for j in range(2)]
            xt = [al("e4x%d" % j, [128, D], F32) for j in range(2)]
            tmp2 = [al("e4tmp%d" % j_, [128, D], F32) for j_ in range(2)]
            wov = I("ev_w_out").rearrange("(kc p) n -> p kc n", p=128)
            for hlf in range(2):
                S.dma('pool', lambda e: e.dma_start(out=wo[:, hlf * 8:(hlf + 1) * 8, :], in_=wov[:, hlf * 8:(hlf + 1) * 8, :]), writes=[('wo', hlf)])
            bcast_row(MG[:], I("ev_mnorm_g")[0:1, :], 'MG')
            for r in range(2):
                bcast_row(G1[:, r, :], modrow[0, r:r + 1, 2 * D:3 * D], ('G1', r))
            hlv = hlT.rearrange("(cc p) t -> p cc t", p=128)
            xin = I("xin")
            def stL(i):
                r = 1 if i < 2 else 0
                j = i % 2
                rows = slice(i * 128, (i + 1) * 128)
                sq, st, hmb, tmp = sq2[j], st2[j], hmb2[j], tmp2[j]
                KSQ, KST, KHMB = ('e4sq', j), ('e4st', j), ('hmb', j)
                S.dma('sp', lambda e: e.dma_start(out=hf[j][:], in_=hm_d[0][rows, :]), writes=[('hf', j)])
                S.dma('sp', lambda e: e.dma_start(out=hb[j][:], in_=hm_d[1][rows, :]), writes=[('hb', j)])
                S.dma('sp', lambda e: e.dma_start(out=so[j][:], in_=so_tm[rows, :]), writes=[('so', j)])
                S.dma('sp', lambda e: e.dma_start(out=hl[j][:], in_=hlv[:, :, rows]), writes=[('hl', j)])
                S.dma('sp', lambda e: e.dma_start(out=xt[j][:], in_=xin[rows, :]), writes=[('xt', j)])

            def stB(i):
                r = 1 if i < 2 else 0
                j = i % 2
                rows = slice(i * 128, (i + 1) * 128)
                sq, st, hmb, tmp = sq2[j], st2[j], hmb2[j], tmp2[j]
                KSQ, KST, KHMB = ('e4sq', j), ('e4st', j), ('hmb', j)
                S.op('pool', lambda e: e.tensor_tensor(out=hf[j][:], in0=hf[j][:], in1=hb[j][:], op=ALU.add), reads=[('hb', j)], writes=[('hf', j)])
                for h in range(4):
                    S.op('act', lambda e: e.activation(out=sq[:], in_=hf[j][:, h * 256:(h + 1) * 256], func=AF.Square, accum_out=st[:, h:h + 1]),
                         reads=[('hf', j)], writes=[KSQ, KST])
                S.op('dve', lambda e: e.tensor_scalar(out=st[:, 4:8], in0=st[:, 0:4], scalar1=1.0 / 256, scalar2=EPS, op0=ALU.mult, op1=ALU.add), writes=[KST])
                S.op('act', lambda e: e.activation(out=st[:, 8:12], in_=st[:, 4:8], func=AF.Sqrt), writes=[KST])
                S.op('dve', lambda e: e.reciprocal(st[:, 12:16], st[:, 8:12]), writes=[KST])
                for h in range(4):
                    hs = slice(h * 256, (h + 1) * 256)
                    S.op('dve', lambda e: e.scalar_tensor_tensor(out=hf[j][:, hs], in0=hf[j][:, hs], scalar=st[:, 12 + h:13 + h], in1=MG[:, hs],
                                                                 op0=ALU.mult, op1=ALU.mult), reads=['MG'], writes=[('hf', j), KST])
                S.op('pool', lambda e: e.tensor_tensor(out=hmb[:], in0=hf[j][:], in1=so[j][:], op=ALU.mult), reads=[('hf', j), ('so', j)], writes=[KHMB])

            def stCD(i):
                r = 1 if i < 2 else 0
                j = i % 2
                rows = slice(i * 128, (i + 1) * 128)
                sq, st, hmb, tmp = sq2[j], st2[j], hmb2[j], tmp2[j]
                KSQ, KST, KHMB = ('e4sq', j), ('e4st', j), ('hmb', j)
                for kc in range(8):
                    S.op('pe', lambda e: e.transpose(psb[j][:, kc * 128:(kc + 1) * 128], hmb[:, kc * 128:(kc + 1) * 128], identb[:]),
                         reads=[KHMB], writes=[PBB(j)])
                S.op('act', lambda e: e.copy(hmT[j][:].rearrange("p a b -> p (a b)"), psb[j][:]), writes=[PBB(j), ('hmT', j)])
                for nb in range(2):
                    pb = (2 * i + nb) % 4
                    for kc in range(16):
                        lhs = hmT[j][:, kc, :] if kc < 8 else hl[j][:, kc - 8, :]
                        S.op('pe', lambda e: e.matmul(psum[pb][:], lhs, wo[:, kc, nb * 512:(nb + 1) * 512], start=(kc == 0), stop=(kc == 15)),
                             reads=[('hmT', j), ('hl', j), ('wo', 0), ('wo', 1)], writes=[PB(pb)])
                    cs = slice(nb * 512, (nb + 1) * 512)
                    S.op('dve', lambda e: e.tensor_tensor(out=tmp[:, cs], in0=psum[pb][:], in1=G1[:, r, cs], op=ALU.mult), reads=[('G1', r)],
                         writes=[PB(pb), ('e4tmp', j, nb)])
                    S.op('pool', lambda e: e.tensor_tensor(out=tmp[:, cs], in0=tmp[:, cs], in1=xt[j][:, cs], op=ALU.add), reads=[('xt', j)],
                         writes=[('e4tmp', j, nb)])
                S.dma('pool', lambda e: e.dma_start(out=xres[rows, :], in_=tmp[:]), reads=[('e4tmp', j, 0), ('e4tmp', j, 1)], writes=[('xres', i)])

            stL(0)
            stB(0)
            for i in range(NT):
                if i + 1 < NT:
                    stL(i + 1)
                    stB(i + 1)
                stCD(i)
        S.barrier()

    v2_tm = dscr("v2_tm", [T, D], BF16)

    def phase_N2(layer, tiles, LOG):
        with contextlib.ExitStack() as es:
            def al(name, shape, dt):
                return es.enter_context(SBT(name, shape, dt))
            wr = al("wr", [128, 8, 32], F32)
            br = al("br", [1, 32], F32)
            vT32 = [al("vT32_%d" % j, [128, 8, 128], F32) for j in range(2)]
            vb = [al("vb%d" % j, [128, D], BF16) for j in range(2)]
            S.dma('sp', lambda e: e.dma_start(out=wr[:], in_=I("moe_w_r")[layer].rearrange("(kc p) n -> p kc n", p=128)), writes=['wr'])
            S.dma('sp', lambda e: e.dma_start(out=br[:], in_=I("moe_b_r")[layer:layer + 1, :]), writes=['br'])
            cnt = [0]

            def consume(i, ut, uk):
                j = cnt[0] % 2
                cnt[0] += 1
                S.op('pool', lambda e: e.tensor_copy(vb[j][:], ut[:]), reads=[uk], writes=[('vb', j)])
                S.dma('pool', lambda e: e.dma_start(out=v2_tm[i * 128:(i + 1) * 128, :], in_=vb[j][:]), reads=[('vb', j)], writes=[('v2', i)])
                for half in range(2):
                    pb = (i * 2 + half) % 4
                    for jj in range(4):
                        kc = half * 4 + jj
                        S.op('pe', lambda e: e.transpose(psum[pb][:, jj * 128:(jj + 1) * 128], ut[:, kc * 128:(kc + 1) * 128], ident[:]),
                             reads=[uk, 'ident'], writes=[PB(pb)])
                    S.op('act', lambda e: e.copy(vT32[j][:, half * 4:half * 4 + 4, :].rearrange("p a b -> p (a b)"), psum[pb][:]),
                         writes=[PB(pb), ('vT32', j, half)])
                pl = 4 + i % 2
                for kc in range(8):
                    S.op('pe', lambda e: e.matmul(psum[pl][:, 0:32], vT32[j][:, kc, :], wr[:, kc, :], start=(kc == 0), stop=False),
                         reads=[('vT32', j, 0), ('vT32', j, 1), 'wr'], writes=[PB(pl)])
                S.op('pe', lambda e: e.matmul(psum[pl][:, 0:32], ones1[0:1, :], br[:], start=False, stop=True), reads=['br', 'ones1'], writes=[PB(pl)])
                S.op('dve', lambda e: e.tensor_copy(LOG[:, i, :], psum[pl][:, 0:32]), writes=[PB(pl), 'LOG'])
            norm_tiles(layer, 1, xres, tiles, consume)
        S.barrier()

    SB = 512
    SHIFT = 9
    NSUB = SB // 128
    NBMAX = 66
    xs_d = dscr("xs_d", [NBMAX * SB, D], BF16)
    ys_d = dscr("ys_d", [NBMAX * SB, D], F32)
    RT = {}
    for nm, shp, dt in (("LOG", [128, NT, 32], F32), ("TOP8", [128, NT, 8], F32), ("WG", [128, NT, 4], F32),
                        ("SLOTI", [128, NT, 4], I32), ("IDXI", [128, NBMAX], I32), ("BIDX", [128, NBMAX], I32),
                        ("IDX8", [128, NBMAX, 8], I32), ("IDX4", [128, NBMAX, 4], I32)):
        RT[nm] = nc.alloc_sbuf_tensor("rt_" + nm, shp, dt)

    def phase_route(tiles, NB):
        LOG, TOP8, WG, SLOTI, IDXI, BIDX = (RT[k] for k in ("LOG", "TOP8", "WG", "SLOTI", "IDXI", "BIDX"))
        with contextlib.ExitStack() as es:
            def al(name, shape, dt):
                return es.enter_context(SBT(name, shape, dt))
            MASK = al("MASK", [128, NT, 32], F32)
            RANK = al("RANK", [128, NT, 32], F32)
            CAR = al("CAR", [128, 32], F32)
            sm = al("rsm", [128, 8], F32)
            ci = al("rci", [128, 32], I32)
            PADDED = al("PADDED", [128, 32], F32)
            PEND = al("PEND", [128, 32], F32)
            BASE = al("BASE", [128, 32], F32)
            ones32 = al("ones32", [128, 32], F32)
            junk = al("rjunk", [128, 32], F32)
            SLOTF = al("SLOTF", [128, NT, 4], F32)
            BSTi = al("BSTi", [128, NBMAX], I32)
            BST = al("BST", [128, NBMAX], F32)
            BLKE = al("BLKE", [128, NBMAX], F32)
            IDXF = al("IDXF", [128, NBMAX], F32)
            IDXF2 = al("IDXF2", [128, NBMAX], F32)
            S.op('pool', lambda e: e.memset(CAR[:], 0.0), writes=['CAR'])
            S.op('pool', lambda e: e.memset(ones32[:], 1.0), writes=['ones32'])
            S.op('pool', lambda e: e.memset(SLOTF[:], 0.0), writes=['SLOTF'])
            for i in tiles:
                S.op('dve', lambda e: e.max(out=TOP8[:, i, :], in_=LOG[:, i, :]), writes=['TOP8'])
                S.op('dve', lambda e: e.tensor_scalar(out=MASK[:, i, :], in0=LOG[:, i, :], scalar1=TOP8[:, i, 3:4], scalar2=None, op0=ALU.is_ge),
                     reads=['TOP8'], writes=[('MASK', i)])
                S.op('dve', lambda e: e.tensor_scalar(out=sm[:, 0:1], in0=TOP8[:, i, 0:1], scalar1=-1.0, scalar2=None, op0=ALU.mult), reads=['TOP8'], writes=['rsm'])
                S.op('act', lambda e: e.activation(out=WG[:, i, :], in_=TOP8[:, i, 0:4], func=AF.Exp, bias=sm[:, 0:1], scale=1.0, accum_out=sm[:, 1:2]),
                     reads=['TOP8'], writes=['rsm', 'WG'])
                S.op('dve', lambda e: e.reciprocal(sm[:, 2:3], sm[:, 1:2]), writes=['rsm'])
                S.op('dve', lambda e: e.tensor_scalar(out=WG[:, i, :], in0=WG[:, i, :], scalar1=sm[:, 2:3], scalar2=None, op0=ALU.mult), writes=['rsm', 'WG'])
                S.op('pe', lambda e: e.matmul(psum[0][:, 0:32], triS[:], MASK[:, i, :], start=True, stop=True), reads=[('MASK', i)], writes=[PB(0)])
                S.op('pe', lambda e: e.matmul(psum[1][:, 0:32], onesm[:], MASK[:, i, :], start=True, stop=True), reads=[('MASK', i)], writes=[PB(1)])
                S.op('dve', lambda e: e.tensor_tensor(out=RANK[:, i, :], in0=psum[0][:, 0:32], in1=CAR[:], op=ALU.add), reads=['CAR'], writes=[PB(0), 'RANK'])
                S.op('dve', lambda e: e.tensor_tensor(out=CAR[:], in0=psum[1][:, 0:32], in1=CAR[:], op=ALU.add), writes=[PB(1), 'CAR'])
            S.op('dve', lambda e: e.tensor_scalar(out=junk[:], in0=CAR[:], scalar1=float(SB - 1), scalar2=None, op0=ALU.add), reads=['CAR'], writes=['rjunk'])
            S.op('dve', lambda e: e.tensor_copy(ci[:], junk[:]), reads=['rjunk'], writes=['rci'])
            S.op('dve', lambda e: e.tensor_scalar(out=ci[:], in0=ci[:], scalar1=SHIFT, scalar2=SHIFT, op0=ALU.arith_shift_right, op1=ALU.logical_shift_left),
                 writes=['rci'])
            S.op('dve', lambda e: e.tensor_copy(PADDED[:], ci[:]), reads=['rci'], writes=['PADDED'])
            S.op('dve', lambda e: e.tensor_tensor_scan(PEND[:], ones32[:], PADDED[:], 0.0, ALU.mult, ALU.add), reads=['ones32', 'PADDED'], writes=['PEND'])
            S.op('dve', lambda e: e.tensor_tensor(out=BASE[:], in0=PEND[:], in1=PADDED[:], op=ALU.subtract), reads=['PEND', 'PADDED'], writes=['BASE'])
            for i in tiles:
                S.op('dve', lambda e: e.tensor_tensor(out=RANK[:, i, :], in0=RANK[:, i, :], in1=BASE[:], op=ALU.add), reads=['BASE'], writes=['RANK'])
                for k in range(4):
                    S.op('dve', lambda e: e.scalar_tensor_tensor(out=junk[:], in0=LOG[:, i, :], scalar=TOP8[:, i, k:k + 1], in1=RANK[:, i, :],
                                                                 op0=ALU.is_equal, op1=ALU.mult, accum_out=SLOTF[:, i, k:k + 1]),
                         reads=['TOP8'], writes=['rjunk', 'RANK', 'SLOTF'])
            S.op('dve', lambda e: e.tensor_copy(SLOTI[:], SLOTF[:]), reads=['SLOTF'], writes=['SLOTI'])
            S.op('pool', lambda e: e.iota(BSTi[:], pattern=[[SB, NBMAX]], base=0, channel_multiplier=0), writes=['BSTi'])
            S.op('dve', lambda e: e.tensor_copy(BST[:], BSTi[:]), reads=['BSTi'], writes=['BST'])
            S.op('pool', lambda e: e.memset(BLKE[:], 0.0), writes=['BLKE'])
            for ex in range(32):
                S.op('dve', lambda e: e.scalar_tensor_tensor(out=BLKE[:], in0=BST[:], scalar=PEND[:, ex:ex + 1], in1=BLKE[:], op0=ALU.is_ge, op1=ALU.add),
                     reads=['BST', 'PEND'], writes=['BLKE'])
            S.op('dve', lambda e: e.tensor_scalar(out=BLKE[:], in0=BLKE[:], scalar1=31.0, scalar2=None, op0=ALU.min), writes=['BLKE'])
            S.op('dve', lambda e: e.tensor_scalar(out=IDXF[:], in0=BLKE[:], scalar1=128.0, scalar2=pidx[:, 0:1], op0=ALU.mult, op1=ALU.add),
                 reads=['pidx'], writes=['IDXF'])
            S.op('dve', lambda e: e.tensor_copy(IDXI[:], IDXF[:]), reads=['IDXF'], writes=['IDXI'])
            S.op('dve', lambda e: e.tensor_copy(BIDX[:], BLKE[:]), reads=['BLKE'], writes=['BIDX'])
            S.op('pool', lambda e: e.memset(BSTi[:], 0), writes=['BSTi'])
            S.op('dve', lambda e: e.tensor_copy(IDXF2[:], BSTi[:]), reads=['BSTi'], writes=['IDXF2'])
            S.op('dve', lambda e: e.tensor_tensor(out=IDXF2[:, 2:NBMAX], in0=BLKE[:, 2:NBMAX], in1=BLKE[:, 0:NBMAX - 2], op=ALU.is_equal),
                 reads=['BLKE'], writes=['IDXF2'])
            S.op('dve', lambda e: e.tensor_scalar(out=IDXF2[:], in0=IDXF2[:], scalar1=float(1 << 20), scalar2=None, op0=ALU.mult), writes=['IDXF2'])
            for kc in range(8):
                S.op('dve', lambda e: e.tensor_scalar(out=BST[:], in0=IDXF[:], scalar1=8.0, scalar2=float(kc), op0=ALU.mult, op1=ALU.add),
                     reads=['IDXF'], writes=['BST'])
                S.op('dve', lambda e: e.tensor_tensor(out=BST[:], in0=BST[:], in1=IDXF2[:], op=ALU.add), reads=['IDXF2'], writes=['BST'])
                S.op('dve', lambda e: e.tensor_copy(RT["IDX8"][:, :, kc], BST[:]), reads=['BST'], writes=['IDX8'])
            for q in range(4):
                S.op('dve', lambda e: e.tensor_scalar(out=BST[:], in0=IDXF[:], scalar1=4.0, scalar2=float(q), op0=ALU.mult, op1=ALU.add),
                     reads=['IDXF'], writes=['BST'])
                S.op('dve', lambda e: e.tensor_tensor(out=BST[:], in0=BST[:], in1=IDXF2[:], op=ALU.add), reads=['IDXF2'], writes=['BST'])
                S.op('dve', lambda e: e.tensor_copy(RT["IDX4"][:, :, q], BST[:]), reads=['BST'], writes=['IDX4'])
            if RT.get('dbg') is not None:
                dd = RT['dbg']
                S.op('dve', lambda e: e.tensor_copy(dd[:, 0:32], CAR[:]), reads=['CAR'], writes=['dd'])
                S.op('dve', lambda e: e.tensor_copy(dd[:, 32:64], PADDED[:]), reads=['PADDED'], writes=['dd'])
                S.op('dve', lambda e: e.tensor_copy(dd[:, 64:96], PEND[:]), reads=['PEND'], writes=['dd'])
                S.op('dve', lambda e: e.tensor_copy(dd[:, 96:128], MASK[:, 0, :]), writes=['dd'])
                S.op('dve', lambda e: e.tensor_copy(dd[:, 128:160], RANK[:, 1, :]), writes=['dd'])
                S.op('dve', lambda e: e.tensor_copy(dd[:, 160:192], ci[:]), writes=['dd'])
        S.barrier()

    def phase_scatter(tiles, NB):
        SLOTI = RT["SLOTI"]
        with contextlib.ExitStack() as es:
            def al(name, shape, dt):
                return es.enter_context(SBT(name, shape, dt))
            zt = al("zt", [128, D], BF16)
            vt = [al("svt%d" % j, [128, D], BF16) for j in range(3)]
            S.op('pool', lambda e: e.memset(zt[:], 0.0), writes=['zt'])
            for b in range(NB * NSUB):
                S.dma('sp', lambda e: e.dma_start(out=xs_d[b * 128:(b + 1) * 128, :], in_=zt[:]), reads=['zt'], writes=[('xsz', b)])
            S.barrier()
            for n, i in enumerate(tiles):
                j = n % 3
                S.dma('sp', lambda e: e.dma_start(out=vt[j][:], in_=v2_tm[i * 128:(i + 1) * 128, :]), writes=[('svt', j)])
                for k in range(4):
                    S.dma('pool', lambda e: e.indirect_dma_start(out=xs_d, out_offset=bass.IndirectOffsetOnAxis(ap=SLOTI[:, i, k:k + 1], axis=0),
                                                                 in_=vt[j][:], in_offset=None), reads=[('svt', j)], writes=[('xs', i, k)])
        S.barrier()

    def phase_moe(layer, NB):
        w1d = I("moe_w1_%d" % layer)
        w2d = I("moe_w2_%d" % layer)
        b1d = I("moe_b1_%d" % layer)
        b2d = I("moe_b2_%d" % layer)
        IDXI, BIDX = RT["IDXI"], RT["BIDX"]
        w1v = w1d.rearrange("a b c -> (a b) c")
        w2v = w2d.rearrange("a (q t) c -> (a q) (t c)", t=2)
        with contextlib.ExitStack() as es:
            def al(name, shape, dt):
                return es.enter_context(SBT(name, shape, dt))
            W1 = [al("W1_%d" % j, [128, 8, 2048], BF16) for j in range(2)]
            W2 = [al("W2_%d" % j, [128, 8, 1024], BF16) for j in range(2)]
            B1C = [al("B1C_%d" % j, [128, 16], F32) for j in range(2)]
            B2R = [al("B2R_%d" % j, [128, D], F32) for j in range(2)]
            XS = [al("XS_%d" % j, [128, D], BF16) for j in range(2 * NSUB)]
            XST2 = [al("XST%d" % j_, [128, 8, SB], BF16) for j_ in range(2)]
            ACTT = al("ACTT", [128, 8, SB], BF16)
            tg = [al("tg_%d" % j, [128, SB], F32) for j in range(2)]
            sg = [al("sg_%d" % j, [128, SB], F32) for j in range(2)]
            tu = [al("tu_%d" % j, [128, SB], F32) for j in range(2)]
            YT = [al("YT_%d" % j, [128, D], F32) for j in range(2)]
            ec = [0]
            xc = [0]
            yc = [0]
            bc8 = nc.gpsimd.to_reg(32 * 128 * 8 - 1)
            bc4 = nc.gpsimd.to_reg(32 * 128 * 4 - 1)

            def gathers(b):
                j = b % 2
                idx = bass.IndirectOffsetOnAxis(ap=IDXI[:, b:b + 1], axis=0)
                bidx = bass.IndirectOffsetOnAxis(ap=BIDX[:, b:b + 1], axis=0)
                for kc in range(8):
                    i8 = bass.IndirectOffsetOnAxis(ap=RT["IDX8"][:, b, kc:kc + 1], axis=0)
                    S.dma('pool', lambda e: e.indirect_dma_start(out=W1[j][:, kc, :], out_offset=None, in_=w1v, in_offset=i8, bounds_check=bc8, oob_is_err=False), writes=[('W1', j, kc)])
                for q in range(4):
                    i4 = bass.IndirectOffsetOnAxis(ap=RT["IDX4"][:, b, q:q + 1], axis=0)
                    S.dma('pool', lambda e: e.indirect_dma_start(out=W2[j][:, 2 * q:2 * q + 2, :].rearrange("p a b -> p (a b)"), out_offset=None,
                                                                 in_=w2v, in_offset=i4, bounds_check=bc4, oob_is_err=False), writes=[('W2', j, q)])
                S.dma('pool', lambda e: e.indirect_dma_start(out=B1C[j][:], out_offset=None, in_=b1d, in_offset=idx), writes=[('B1C', j)])
                S.dma('pool', lambda e: e.indirect_dma_start(out=B2R[j][:], out_offset=None, in_=b2d, in_offset=bidx), writes=[('B2R', j)])

            def xloads(b):
                for st_ in range(NSUB):
                    xj = (b % 2) * NSUB + st_
                    r0 = b * SB + st_ * 128
                    S.dma('sp', lambda e: e.dma_start(out=XS[xj][:], in_=xs_d[r0:r0 + 128, :]), writes=[('XS', xj)])

            def do_tr(b):
                j = b % 2
                for st_ in range(NSUB):
                    xj = (b % 2) * NSUB + st_
                    pbb = st_ % 2
                    for kc in range(8):
                        S.op('pe', lambda e: e.transpose(psb[pbb][:, kc * 128:(kc + 1) * 128], XS[xj][:, kc::8], identb[:]), reads=[('XS', xj)], writes=[PBB(pbb)])
                    S.op('act', lambda e: e.copy(XST2[b % 2][:, :, st_ * 128:(st_ + 1) * 128], psb[pbb][:].rearrange("p (a b) -> p a b", a=8)),
                         writes=[PBB(pbb), ('XST', b % 2, st_)])

            def do_mm1(b):
                j = b % 2
                xk = [('XST', b % 2, s_) for s_ in range(NSUB)]
                for jj in range(8):
                    t = ec[0] % 2
                    ec[0] += 1
                    for half in range(2):
                        bank = 2 * t + half
                        for kc in range(8):
                            S.op('pe', lambda e: e.matmul(psum[bank][:, 0:SB], W1[j][:, kc, half * 1024:(half + 1) * 1024][:, jj::8], XST2[b % 2][:, kc, :],
                                                          start=(kc == 0), stop=(kc == 7)), reads=[('W1', j, kc)] + xk, writes=[PB(bank)])
                    S.op('dve', lambda e: e.tensor_scalar(out=tg[t][:], in0=psum[2 * t][:, 0:SB], scalar1=B1C[j][:, jj:jj + 1], scalar2=7.0,
                                                          op0=ALU.add, op1=ALU.min), reads=[('B1C', j)], writes=[PB(2 * t), ('tg', t)])
                    S.op('dve', lambda e: e.tensor_scalar(out=tu[t][:], in0=psum[2 * t + 1][:, 0:SB], scalar1=B1C[j][:, 8 + jj:9 + jj], scalar2=7.0,
                                                          op0=ALU.add, op1=ALU.min), reads=[('B1C', j)], writes=[PB(2 * t + 1), ('tu', t)])
                    S.op('act', lambda e: e.activation(out=sg[t][:], in_=tg[t][:], func=AF.Sigmoid, scale=1.702), reads=[('tg', t)], writes=[('sg', t)])
                    S.op('dve', lambda e: e.tensor_scalar(out=tu[t][:], in0=tu[t][:], scalar1=-7.0, scalar2=1.0, op0=ALU.max, op1=ALU.add), writes=[('tu', t)])
                    S.op('dve', lambda e: e.tensor_tensor(out=tu[t][:], in0=tu[t][:], in1=tg[t][:], op=ALU.mult), reads=[('tg', t)], writes=[('tu', t)])
                    S.op('dve', lambda e: e.tensor_tensor(out=ACTT[:, jj, :], in0=tu[t][:], in1=sg[t][:], op=ALU.mult),
                         reads=[('tu', t), ('sg', t)], writes=[('ACTT', jj)])

            def do_mm2(b):
                j = b % 2
                ak = [('ACTT', jj) for jj in range(8)]
                for st_ in range(NSUB):
                    yj = yc[0] % 2
                    yc[0] += 1
                    for nb in range(2):
                        for jj in range(8):
                            S.op('pe', lambda e: e.matmul(psum[4 + nb][:], ACTT[:, jj, st_ * 128:(st_ + 1) * 128], W2[j][:, jj, nb * 512:(nb + 1) * 512],
                                                          start=(jj == 0), stop=(jj == 7)), reads=ak + [('W2', j, jj // 2)], writes=[PB(4 + nb)])
                        cs = slice(nb * 512, (nb + 1) * 512)
                        S.op('dve', lambda e: e.tensor_tensor(out=YT[yj][:, cs], in0=psum[4 + nb][:], in1=B2R[j][:, cs], op=ALU.add), reads=[('B2R', j)],
                             writes=[PB(4 + nb), ('YT', yj, nb)])
                    r0 = b * SB + st_ * 128
                    S.dma('sp', lambda e: e.dma_start(out=ys_d[r0:r0 + 128, :], in_=YT[yj][:]), reads=[('YT', yj, 0), ('YT', yj, 1)], writes=[('ys', b, st_)])

            gathers(0)
            xloads(0)
            do_tr(0)
            for b in range(NB):
                if b + 1 < NB:
                    gathers(b + 1)
                    xloads(b + 1)
                do_mm1(b)
                if b + 1 < NB:
                    do_tr(b + 1)
                do_mm2(b)
        S.barrier()

    def phase_combine(layer, tiles, final):
        WG, SLOTI = RT["WG"], RT["SLOTI"]
        with contextlib.ExitStack() as es:
            def al(name, shape, dt):
                return es.enter_context(SBT(name, shape, dt))
            G2 = al("G2", [128, 2, D], F32)
            FG = al("FG", [128, D], F32)
            xt = [al("cxt%d" % j, [128, D], F32) for j in range(2)]
            Y = [[al("cY%d_%d" % (j, k), [128, D], F32) for k in range(4)] for j in range(2)]
            acc = [al("cacc%d" % j, [128, D], F32) for j in range(2)]
            sq = al("csq", [128, D], F32)
            st = al("cst", [128, 8], F32)
            for r in range(2):
                bcast_row(G2[:, r, :], modrow[layer, r:r + 1, 5 * D:6 * D], ('G2', r))
            if final:
                bcast_row(FG[:], I("final_g")[0:1, :], 'FG')
            for n, i in enumerate(tiles):
                j = n % 2
                r = 1 if i < 2 else 0
                rows = slice(i * 128, (i + 1) * 128)
                S.dma('sp', lambda e: e.dma_start(out=xt[j][:], in_=xres[rows, :]), writes=[('cxt', j)])
                for k in range(4):
                    off = bass.IndirectOffsetOnAxis(ap=SLOTI[:, i, k:k + 1], axis=0)
                    S.dma('pool', lambda e: e.indirect_dma_start(out=Y[j][k][:], out_offset=None, in_=ys_d, in_offset=off), writes=[('cY', j, k)])
                S.op('dve', lambda e: e.tensor_scalar(out=acc[j][:], in0=Y[j][0][:], scalar1=WG[:, i, 0:1], scalar2=None, op0=ALU.mult),
                     reads=[('cY', j, 0)], writes=[('cacc', j)])
                for k in range(1, 4):
                    S.op('dve', lambda e: e.scalar_tensor_tensor(out=acc[j][:], in0=Y[j][k][:], scalar=WG[:, i, k:k + 1], in1=acc[j][:],
                                                                 op0=ALU.mult, op1=ALU.add), reads=[('cY', j, k)], writes=[('cacc', j)])
                S.op('dve', lambda e: e.tensor_tensor(out=acc[j][:], in0=acc[j][:], in1=G2[:, r, :], op=ALU.mult), reads=[('G2', r)], writes=[('cacc', j)])
                S.op('dve', lambda e: e.tensor_tensor(out=acc[j][:], in0=acc[j][:], in1=xt[j][:], op=ALU.add), reads=[('cxt', j)], writes=[('cacc', j)])
                if not final:
                    S.dma('act', lambda e: e.dma_start(out=xres[rows, :], in_=acc[j][:]), reads=[('cacc', j)], writes=[('xres', i)])
                else:
                    S.op('act', lambda e: e.activation(out=sq[:], in_=acc[j][:], func=AF.Square, accum_out=st[:, 0:1]), reads=[('cacc', j)], writes=['csq', 'cst'])
                    S.op('dve', lambda e: e.tensor_scalar(out=st[:, 1:2], in0=st[:, 0:1], scalar1=1.0 / D, scalar2=EPS, op0=ALU.mult, op1=ALU.add), writes=['cst'])
                    S.op('act', lambda e: e.activation(out=st[:, 2:3], in_=st[:, 1:2], func=AF.Sqrt), writes=['cst'])
                    S.op('dve', lambda e: e.reciprocal(st[:, 3:4], st[:, 2:3]), writes=['cst'])
                    S.op('dve', lambda e: e.scalar_tensor_tensor(out=acc[j][:], in0=acc[j][:], scalar=st[:, 3:4], in1=FG[:], op0=ALU.mult, op1=ALU.mult),
                         reads=['FG'], writes=[('cacc', j), 'cst'])
                    S.dma('act', lambda e: e.dma_start(out=out_d[(i - 2) * 128:(i - 1) * 128, :], in_=acc[j][:]), reads=[('cacc', j)], writes=[('out', i)])
        S.barrier()

    qT_d = dscr("qT_d", [D, 4096], BF16)
    kT_d = dscr("kT_d", [256, T], BF16)
    vA_d = dscr("vA_d", [T, 256], BF16)
    oT_d = dscr("oT_d", [D, 4096], BF16)

    def phase_A1(uT):
        wv = I("od_w_in").rearrange("(kc p) n -> p kc n", p=128)
        with contextlib.ExitStack() as es:
            def al(name, shape, dt):
                return es.enter_context(SBT(name, shape, dt))
            W = al("aW", [128, 8, 1536], BF16)
            GQ = al("aGQ", [128, 128], F32)
            GK = al("aGK", [128, 128], F32)
            COS = [al("aCOS%d" % j, [128, 128], F32) for j in range(2)]
            SIN = [al("aSIN%d" % j, [128, 128], F32) for j in range(2)]
            xf = [al("axf%d" % j, [128, 1536], F32) for j in range(2)]
            sq2 = [al("asq%d" % j_, [128, 1280], F32) for j_ in range(2)]
            st2 = [al("ast%d" % j_, [128, 40], F32) for j_ in range(2)]
            kn2 = [al("akn%d" % j_, [128, 1280], F32) for j_ in range(2)]
            t12 = [al("at1%d" % j_, [128, 1280], F32) for j_ in range(2)]
            t22 = [al("at2%d" % j_, [128, 1280], F32) for j_ in range(2)]
            kr = [al("akr%d" % j, [128, 1280], BF16) for j in range(2)]
            vb = [al("avb%d" % j, [128, 256], BF16) for j in range(2)]
            xT = [al("axT%d" % j, [128, 10, 128], BF16) for j in range(2)]
            for blk in range(3):
                S.dma('pool', lambda e: e.dma_start(out=W[:, :, blk * 512:(blk + 1) * 512], in_=wv[:, :, blk * 512:(blk + 1) * 512]), writes=[('aW', blk)])
            bcast_row(GQ[:], I("od_qk_g")[0:1, :], 'aGQ')
            bcast_row(GK[:], I("od_qk_g")[1:2, :], 'aGK')
            def hdrvars(i):
                lat = i >= 2
                j = i % 2
                rows = slice(i * 128, (i + 1) * 128)
                blks = [0, 1, 2] if lat else [2]
                h0 = 0 if lat else 8
                sq, st, kn, t1, t2 = sq2[j], st2[j], kn2[j], t12[j], t22[j]
                KSQ, KST = ('asq', j), ('ast', j)
                return lat, j, rows, blks, h0, sq, st, kn, t1, t2, KSQ, KST

            def stageA(i):
                lat, j, rows, blks, h0, sq, st, kn, t1, t2, KSQ, KST = hdrvars(i)
                if lat:
                    tr = slice((i - 2) * 128, (i - 1) * 128)
                    S.dma('pool', lambda e: e.dma_start(out=COS[j][:], in_=I("rope_cos")[tr, :]), writes=[('aCOS', j)])
                    S.dma('pool', lambda e: e.dma_start(out=SIN[j][:], in_=I("rope_sin")[tr, :]), writes=[('aSIN', j)])
                for blk in blks:
                    pb = blk
                    for kc in range(8):
                        S.op('pe', lambda e: e.matmul(psum[pb][:], uT[:, kc, rows], W[:, kc, blk * 512:(blk + 1) * 512], start=(kc == 0), stop=(kc == 7)),
                             reads=[('aW', blk)], writes=[PB(pb)])
                    S.op('act', lambda e: e.copy(xf[j][:, blk * 512:(blk + 1) * 512], psum[pb][:]), writes=[PB(pb), ('axf', j, blk)])
                S.op('pool', lambda e: e.tensor_copy(vb[j][:], xf[j][:, 1280:1536]), reads=[('axf', j, 2)], writes=[('avb', j)])
                S.dma('sp', lambda e: e.dma_start(out=vA_d[rows, :], in_=vb[j][:]), reads=[('avb', j)], writes=[('vA', i)])

            def stageB(i):
                lat, j, rows, blks, h0, sq, st, kn, t1, t2, KSQ, KST = hdrvars(i)
                c0 = h0 * 128
                nh = 10 - h0
                S.op('act', lambda e: e.activation(out=sq[:, c0:1280], in_=xf[j][:, c0:1280], func=AF.Square),
                     reads=[('axf', j, 0), ('axf', j, 1), ('axf', j, 2)], writes=[KSQ])
                S.op('dve', lambda e: e.tensor_reduce(out=st[:, h0:10], in_=sq[:, c0:1280].rearrange("p (h d) -> p h d", d=128), axis=AX.X, op=ALU.add),
                     reads=[KSQ], writes=[KST])
                S.op('dve', lambda e: e.tensor_scalar(out=st[:, 10 + h0:20], in0=st[:, h0:10], scalar1=1.0 / 128, scalar2=EPS, op0=ALU.mult, op1=ALU.add), writes=[KST])
                S.op('act', lambda e: e.activation(out=st[:, 20 + h0:30], in_=st[:, 10 + h0:20], func=AF.Sqrt), writes=[KST])
                S.op('dve', lambda e: e.reciprocal(st[:, 30 + h0:40], st[:, 20 + h0:30]), writes=[KST])
                for h in range(h0, 10):
                    hs = slice(h * 128, (h + 1) * 128)
                    G = GQ if h < 8 else GK
                    dst = kn[:, hs] if lat else kr[j][:, hs]
                    dk = ('akn', j, h) if lat else ('akr', j, h)
                    S.op('dve', lambda e: e.scalar_tensor_tensor(out=dst, in0=xf[j][:, hs], scalar=st[:, 30 + h:31 + h], in1=G[:], op0=ALU.mult, op1=ALU.mult),
                         reads=['aGQ', 'aGK', KST], writes=[dk])
                if lat:
                    snv = SIN[j][:].rearrange("p (b c) -> p b c", b=2)
                    for h in range(h0, 10):
                        hs = slice(h * 128, (h + 1) * 128)
                        knv = kn[:, hs].rearrange("p (b c) -> p b c", b=2)
                        t2v = t2[:, hs].rearrange("p (b c) -> p b c", b=2)
                        S.op('dve', lambda e: e.tensor_tensor(out=t1[:, hs], in0=kn[:, hs], in1=COS[j][:], op=ALU.mult), reads=[('akn', j, h), ('aCOS', j)],
                             writes=[('at1', j, h)])
                        S.op('pool', lambda e: e.tensor_tensor(out=t2v[:, :, 0:32], in0=knv[:, :, 32:64], in1=snv[:, :, 0:32], op=ALU.mult),
                             reads=[('akn', j, h), ('aSIN', j)], writes=[('at2', j, h)])
                        S.op('pool', lambda e: e.tensor_tensor(out=t2v[:, :, 32:64], in0=knv[:, :, 0:32], in1=snv[:, :, 32:64], op=ALU.mult),
                             reads=[('akn', j, h), ('aSIN', j)], writes=[('at2', j, h)])
                    for h in range(h0, 10):
                        hs = slice(h * 128, (h + 1) * 128)
                        S.op('dve', lambda e: e.tensor_tensor(out=kr[j][:, hs], in0=t1[:, hs], in1=t2[:, hs], op=ALU.add), reads=[('at1', j, h), ('at2', j, h)],
                             writes=[('akr', j, h)])

            def stageC(i):
                lat, j, rows, blks, h0, sq, st, kn, t1, t2, KSQ, KST = hdrvars(i)
                if lat:
                    for h in range(8):
                        S.op('pe', lambda e: e.transpose(psb[0][:, h * 128:(h + 1) * 128], kr[j][:, h * 128:(h + 1) * 128], identb[:]),
                             reads=[('akr', j, h)], writes=[PBB(0)])
                    S.op('act', lambda e: e.copy(xT[j][:, 0:8, :].rearrange("p a b -> p (a b)"), psb[0][:]), writes=[PBB(0), ('axT', j, 0)])
                    S.dma('sp', lambda e: e.dma_start(out=qT_d[:, (i - 2) * 128:(i - 1) * 128].rearrange("(h d) t -> d h t", d=128), in_=xT[j][:, 0:8, :]),
                          reads=[('axT', j, 0)], writes=[('qT_d', i)])
                for h in range(8, 10):
                    S.op('pe', lambda e: e.transpose(psb[1][:, (h - 8) * 128:(h - 7) * 128], kr[j][:, h * 128:(h + 1) * 128], identb[:]),
                         reads=[('akr', j, h)], writes=[PBB(1)])
                S.op('act', lambda e: e.copy(xT[j][:, 8:10, :].rearrange("p a b -> p (a b)"), psb[1][:, 0:256]), writes=[PBB(1), ('axT', j, 1)])
                S.dma('sp', lambda e: e.dma_start(out=kT_d[:, rows].rearrange("(h d) t -> d h t", d=128), in_=xT[j][:, 8:10, :]),
                      reads=[('axT', j, 1)], writes=[('kT_d', i)])

            stageA(0)
            for i in range(NT):
                if i + 1 < NT:
                    stageA(i + 1)
                stageB(i)
                stageC(i)
        S.barrier()

    def phase_A2():
        scale = 128.0 ** -0.5
        with contextlib.ExitStack() as es:
            def al(name, shape, dt):
                return es.enter_context(SBT(name, shape, dt))
            KT = al("bKT", [128, T], BF16)
            V = al("bV", [128, NT, 128], BF16)
            onesb = al("bones", [128, 128], BF16)
            QT4 = [al("bQT%d" % j, [128, 4, 128], BF16) for j in range(2)]
            PT = [al("bPT%d" % j, [128, 512], BF16) for j in range(4)]
            rs = [al("brs%d" % j, [128, 512], F32) for j in range(2)]
            ot = [al("bot%d" % j, [128, 4, 128], BF16) for j in range(2)]
            S.op('pool', lambda e: e.memset(onesb[:], 1.0), writes=['bones'])
            pc = 0
            it = 0
            for g in range(2):
                S.dma('sp', lambda e: e.dma_start(out=KT[:], in_=kT_d[g * 128:(g + 1) * 128, :]), writes=['bKT'])
                S.dma('sp', lambda e: e.dma_start(out=V[:], in_=vA_d[:, g * 128:(g + 1) * 128].rearrange("(n p) d -> p n d", p=128)), writes=['bV'])
                for qi in range(32):
                    j = it % 2
                    it += 1
                    bo, bs_ = 2 + 2 * j, 3 + 2 * j
                    S.dma('sp', lambda e: e.dma_start(out=QT4[j][:], in_=qT_d[g * 512:(g + 1) * 512, qi * 128:(qi + 1) * 128].rearrange("(h d) t -> d h t", d=128)),
                          writes=[('bQT', j)])

                    def st_mm(kt):
                        bank = kt % 2
                        S.op('pe', lambda e: e.matmul(psum[bank][:], KT[:, kt * 128:(kt + 1) * 128], QT4[j][:].rearrange("p a b -> p (a b)"), start=True, stop=True),
                             reads=['bKT', ('bQT', j)], writes=[PB(bank)])
                    st_mm(0)
                    for kt in range(NT):
                        bank = kt % 2
                        p_ = pc % 4
                        pc += 1
                        S.op('act', lambda e: e.activation(out=PT[p_][:], in_=psum[bank][:], func=AF.Exp, scale=scale), writes=[PB(bank), ('bPT', p_)])
                        if kt + 1 < NT:
                            st_mm(kt + 1)
                        S.op('pe', lambda e: e.matmul(psum[bo][:], V[:, kt, :], PT[p_][:], start=(kt == 0), stop=(kt == NT - 1)),
                             reads=[('bPT', p_), 'bV'], writes=[PB(bo)])
                        S.op('pe', lambda e: e.matmul(psum[bs_][:], onesb[:], PT[p_][:], start=(kt == 0), stop=(kt == NT - 1)),
                             reads=[('bPT', p_), 'bones'], writes=[PB(bs_)])
                    S.op('dve', lambda e: e.reciprocal(rs[j][:], psum[bs_][:]), writes=[PB(bs_), ('brs', j)])
                    S.op('dve', lambda e: e.tensor_tensor(out=ot[j][:].rearrange("p a b -> p (a b)"), in0=psum[bo][:], in1=rs[j][:], op=ALU.mult),
                         reads=[('brs', j)], writes=[PB(bo), ('bot', j)])
                    S.dma('pool', lambda e: e.dma_start(out=oT_d[g * 512:(g + 1) * 512, qi * 128:(qi + 1) * 128].rearrange("(h d) t -> d h t", d=128), in_=ot[j][:]),
                          reads=[('bot', j)], writes=[('oT_d', g, qi)])
        S.barrier()

    def phase_A3():
        with contextlib.ExitStack() as es:
            def al(name, shape, dt):
                return es.enter_context(SBT(name, shape, dt))
            WO = al("cWO", [128, 8, D], BF16)
            G1 = al("cG1", [128, D], F32)
            OT = [al("cOT%d" % j, [128, 8, 128], BF16) for j in range(2)]
            xt = [al("cx%d" % j, [128, D], F32) for j in range(2)]
            tmp = al("ctmp", [128, D], F32)
            S.dma('pool', lambda e: e.dma_start(out=WO[:], in_=I("od_w_out").rearrange("(kc p) n -> p kc n", p=128)), writes=['cWO'])
            bcast_row(G1[:], modrow[1, 0:1, 2 * D:3 * D], 'cG1')
            for qi in range(32):
                j = qi % 2
                rows = slice((qi + 2) * 128, (qi + 3) * 128)
                S.dma('sp', lambda e: e.dma_start(out=OT[j][:], in_=oT_d[:, qi * 128:(qi + 1) * 128].rearrange("(h d) t -> d h t", d=128)), writes=[('cOT', j)])
                S.dma('sp', lambda e: e.dma_start(out=xt[j][:], in_=xres[rows, :]), writes=[('cx', j)])
                for nb in range(2):
                    pb = (2 * qi + nb) % 4
                    for kc in range(8):
                        S.op('pe', lambda e: e.matmul(psum[pb][:], OT[j][:, kc, :], WO[:, kc, nb * 512:(nb + 1) * 512], start=(kc == 0), stop=(kc == 7)),
                             reads=[('cOT', j), 'cWO'], writes=[PB(pb)])
                    cs = slice(nb * 512, (nb + 1) * 512)
                    S.op('dve', lambda e: e.tensor_tensor(out=tmp[:, cs], in0=psum[pb][:], in1=G1[:, cs], op=ALU.mult), reads=['cG1'], writes=[PB(pb), ('ctmp', nb)])
                    S.op('pool', lambda e: e.tensor_tensor(out=tmp[:, cs], in0=tmp[:, cs], in1=xt[j][:, cs], op=ALU.add), reads=[('cx', j)], writes=[('ctmp', nb)])
                S.dma('pool', lambda e: e.dma_start(out=xres[rows, :], in_=tmp[:]), reads=[('ctmp', 0), ('ctmp', 1)], writes=[('xres', qi)])
        S.barrier()

    def small_dump():
        W_ = NT * 32 + NT * 4 + NT * 4 + 2 * NBMAX
        dl = dscr("dsmall_scr", [128, W_], F32)
        with SBT("dsm", [128, W_], F32) as dsm:
            o = 0
            for nm, n in (("LOG", NT * 32), ("WG", NT * 4), ("SLOTI", NT * 4)):
                S.op('dve', lambda e: e.tensor_copy(dsm[:, o:o + n], RT[nm][:].rearrange("p a b -> p (a b)")), writes=['dsm'])
                o += n
            for nm in ("IDXI", "BIDX"):
                S.op('dve', lambda e: e.tensor_copy(dsm[:, o:o + NBMAX], RT[nm][:]), writes=['dsm'])
                o += NBMAX
            S.dma('sp', lambda e: e.dma_start(out=dl, in_=dsm[:]), reads=['dsm'], writes=['dl'])
            S.barrier()
        return ('small', dl, [128, W_], F32)

    def copy_xin_to_xres():
        with SBT("cpx", [128, D], F32) as cpx:
            for i in range(NT):
                S.dma('sp', lambda e: e.dma_start(out=cpx[:], in_=I("xin")[i * 128:(i + 1) * 128, :]), writes=['cpx'])
                S.dma('sp', lambda e: e.dma_start(out=xres[i * 128:(i + 1) * 128, :], in_=cpx[:]), reads=['cpx'], writes=[('xres', i)])
        S.barrier()

    phase_mod()
    if stop_after == 'mod':
        return finish_dbg([('modrow', modrow.rearrange("a b c -> (a b) c"), [4, 6 * D], F32)])

    if stop_after not in ('A_only', 'M1_only'):
        uT_guard = SBT("uT", [128, 8, T], BF16)
        uT = uT_guard.__enter__()
        phase_norm_T(0, I("xin"), uT)
        phase_E1(uT)
        if stop_after == 'E1':
            return finish_dbg([('qkT', qkT, [2048, T], BF16), ('v_tm', v_tm, [T, D], BF16), ('so_tm', so_tm, [T, D], BF16),
                               ('g_tm', g_tm, [T, 16], F32), ('xrT', xrT, [D, T], F32), ('ygT', ygT, [D, T], BF16)])
        uT_guard.__exit__(None, None, None)
        phase_E2()
        if stop_after == 'E2':
            return finish_dbg([('hlT', hlT, [D, T], BF16)])
        phase_E3()
        if stop_after == 'E3':
            return finish_dbg([('hm_f', hm_d[0], [T, D], F32), ('hm_b', hm_d[1], [T, D], F32)])
        phase_E4()
        if stop_after == 'E4':
            return finish_dbg([('xres', xres, [T, D], F32)])
        NB0 = (4 * T) // SB + 32
        phase_N2(0, range(NT), RT["LOG"])
        phase_route(range(NT), NB0)
        phase_scatter(range(NT), NB0)
        if stop_after == 'R0':
            return finish_dbg([small_dump(), ('xs', xs_d, [NBMAX * SB, D], BF16), ('v2', v2_tm, [T, D], BF16)])
        phase_moe(0, NB0)
        phase_combine(0, range(NT), False)
        if stop_after == 'M0':
            return finish_dbg([('xres', xres, [T, D], F32)])
    else:
        copy_xin_to_xres()

    if stop_after != 'M1_only':
        uT_guard = SBT("uT", [128, 8, T], BF16)
        uT = uT_guard.__enter__()
        phase_norm_T(1, xres, uT)
        phase_A1(uT)
        uT_guard.__exit__(None, None, None)
        phase_A2()
        phase_A3()
        if stop_after in ('A', 'A_only'):
            return finish_dbg([('xres', xres, [T, D], F32)])
    NB1 = (4 * 4096) // SB + 32
    lat_tiles = range(2, NT)
    phase_N2(1, lat_tiles, RT["LOG"])
    phase_route(lat_tiles, NB1)
    phase_scatter(lat_tiles, NB1)
    phase_moe(1, NB1)
    phase_combine(1, lat_tiles, True)
    if stop_after == 'M1_only':
        return finish_dbg([('outc', out_d, [4096, D], F32)])
    S.barrier()
    return nc, dbg_out, used_inputs


def host_inputs(b, inp, names=None):
    f = lambda a: np.ascontiguousarray(a, dtype=np.float32)
    want = lambda k: names is None or k in names
    m = {}
    if want("xin"):
        m["xin"] = f(np.concatenate([inp["ctx"][b], inp["x"][b]], axis=0))
    if want("ccols"):
        m["ccols"] = f(np.concatenate([inp["c"][b].reshape(8, 128).T, inp["c_ctx"].reshape(8, 128).T], axis=1))
    for k in ["mod_w", "mod_b", "norm1_g", "norm2_g", "moe_w_r", "moe_b_r"]:
        if want(k):
            m[k] = f(inp[k])
    if want("final_g"):
        m["final_g"] = f(inp["final_g"].reshape(1, D))
    if want("ev_w_in"):
        m["ev_w_in"] = f(inp["ev_w_in"][0])
    if want("ev_qkcw"):
        qk = np.concatenate([inp["ev_qk_conv_w"][0], inp["ev_qk_conv_b"][0][None]], axis=0)
        m["ev_qkcw"] = f(qk.T.reshape(16, 128, 5).transpose(1, 0, 2))
    if want("ev_gate_b"):
        m["ev_gate_b"] = f(inp["ev_gate_b"][0].reshape(1, 16))
    if want("ev_mnorm_g"):
        m["ev_mnorm_g"] = f(inp["ev_mnorm_g"][0].reshape(1, D))
    if want("ev_lrucw"):
        lc = np.concatenate([inp["ev_lru_conv_w"][0], inp["ev_lru_conv_b"][0][None]], axis=0)
        m["ev_lrucw"] = f(lc.T.reshape(8, 128, 5).transpose(1, 0, 2))
    if want("ev_lru_w") or want("ev_lru_b"):
        lw = np.zeros((4, 8, 128, 128), np.float32)
        lb = np.zeros((128, 8, 4), np.float32)
        for z in range(2):
            for gi, (wk, bk) in enumerate([("ev_lru_wa", "ev_lru_ba"), ("ev_lru_wx", "ev_lru_bx")]):
                for cc_ in range(8):
                    for h in range(2):
                        n = cc_ * 2 + h
                        lw[z * 2 + gi, cc_, h * 64:(h + 1) * 64, h * 64:(h + 1) * 64] = inp[wk][0, z, n]
                        lb[h * 64:(h + 1) * 64, cc_, z * 2 + gi] = inp[bk][0, z, n]
        m["ev_lru_w"] = lw
        m["ev_lru_b"] = lb
    if want("ev_lru_lam"):
        m["ev_lru_lam"] = f(inp["ev_lru_lam"][0].reshape(2, 8, 128).transpose(2, 1, 0))
    if want("ev_w_out"):
        m["ev_w_out"] = f(inp["ev_w_out"][0])
    if want("od_w_in"):
        m["od_w_in"] = f(inp["od_w_in"][0])
    if want("od_qk_g"):
        m["od_qk_g"] = f(np.stack([inp["od_q_norm_g"][0], inp["od_k_norm_g"][0]]))
    if want("od_w_out"):
        m["od_w_out"] = f(inp["od_w_out"][0])
    if want("rope_cos") or want("rope_sin"):
        pos = np.arange(4096)
        row = (pos // 64).astype(np.float32)
        col = (pos % 64).astype(np.float32)
        inv = (10000.0 ** (-np.arange(0, 64, 2, dtype=np.float32) / 64)).astype(np.float32)
        ar = row[:, None] * inv[None]
        ac = col[:, None] * inv[None]
        m["rope_cos"] = f(np.concatenate([np.cos(ar), np.cos(ar), np.cos(ac), np.cos(ac)], axis=1))
        m["rope_sin"] = f(np.concatenate([-np.sin(ar), np.sin(ar), -np.sin(ac), np.sin(ac)], axis=1))
    for l in range(2):
        if want("moe_w1_%d" % l):
            m["moe_w1_%d" % l] = f(inp["moe_w1"][l]).reshape(32 * 128, 8, 2048)
        if want("moe_w2_%d" % l):
            m["moe_w2_%d" % l] = f(inp["moe_w2"][l]).reshape(32 * 128, 8, 1024)
    for l in range(2):
        if want("moe_b1_%d" % l):
            m["moe_b1_%d" % l] = f(inp["moe_b1"][l].reshape(32, 2, 128, 8).transpose(0, 2, 1, 3).reshape(32 * 128, 16))
        if want("moe_b2_%d" % l):
            m["moe_b2_%d" % l] = f(inp["moe_b2"][l])
    if names is not None:
        m = {k: v for k, v in m.items() if k in names}
    return m


_CACHE = {}


def kernel(**inputs):
    if 'nc' not in _CACHE:
        _CACHE['nc'] = build()
    nc, _, used = _CACHE['nc']
    names = set(used.keys())
    in_maps = [host_inputs(b, inputs, names) for b in range(8)]
    res = run_bass_kernel_spmd(nc, in_maps, core_ids=list(range(8)))
    return np.stack([r["out"] for r in res.results], axis=0).astype(np.float32)
```

```python
import contextlib
import numpy as np
import concourse.bass as bass
import concourse.mybir as mybir
from concourse.bass_utils import run_bass_kernel_spmd

F32 = mybir.dt.float32
BF16 = mybir.dt.bfloat16
I32 = mybir.dt.int32
U32 = mybir.dt.uint32
ALU = mybir.AluOpType
AF = mybir.ActivationFunctionType

T = 4352
NT = 34
D = 1024
NCTX = 256
EPS = 1e-6


class Sched:
    NSLOT = 6

    def __init__(self, nc):
        self.nc = nc
        self.engs = {'pe': nc.tensor, 'act': nc.scalar, 'dve': nc.vector,
                     'pool': nc.gpsimd, 'sp': nc.sync}
        self.sem = {}
        self.cnt = {}
        for k in self.engs:
            self.sem[k] = nc.alloc_semaphore('s_' + k)
            self.cnt[k] = 0
        self.dslots = {}
        self.dcnt = {}
        self.dnext = {}
        self.nslot = {'sp': 8, 'pool': 8, 'act': 2}
        for q in ('sp', 'pool', 'act'):
            self.dslots[q] = [nc.alloc_semaphore('d_%s%d' % (q, i)) for i in range(self.nslot[q])]
            self.dcnt[q] = [0] * self.nslot[q]
            self.dnext[q] = 0
        self.waited = {k: {} for k in self.engs}
        self.res = {}

    def _semof(self, tok):
        if tok[0] == 'e':
            return self.sem[tok[1]]
        return self.dslots[tok[1][0]][tok[1][1]]

    def _wait(self, e, tok):
        key = (tok[0], tok[1])
        if self.waited[e].get(key, 0) >= tok[2]:
            return
        self.engs[e].wait_ge(self._semof(tok), tok[2])
        self.waited[e][key] = tok[2]

    def _deps(self, e, reads, writes):
        deps = []
        for r in reads:
            st = self.res.get(r)
            if st and st['w'] is not None:
                deps.append(st['w'])
        for w in writes:
            st = self.res.get(w)
            if st:
                if st['w'] is not None:
                    deps.append(st['w'])
                deps.extend(st['r'].values())
        for tok in deps:
            if e == 'pe' and tok[0] == 'e' and tok[1] == 'pe':
                continue
            self._wait(e, tok)

    def _commit(self, tok, reads, writes):
        for r in reads:
            st = self.res.setdefault(r, {'w': None, 'r': {}})
            st['r'][(tok[0], tok[1])] = tok
        for w in writes:
            self.res[w] = {'w': tok, 'r': {}}

    def op(self, e, fn, reads=(), writes=()):
        self._deps(e, reads, writes)
        inst = fn(self.engs[e])
        self.cnt[e] += 1
        inst.then_inc(self.sem[e], 1)
        self._commit(('e', e, self.cnt[e]), reads, writes)
        return inst

    def dma(self, q, fn, reads=(), writes=()):
        s = self.dnext[q]
        self.dnext[q] = (s + 1) % self.nslot[q]
        if self.dcnt[q][s] > 0:
            self._wait(q, ('d', (q, s), 16 * self.dcnt[q][s]))
        self._deps(q, reads, writes)
        inst = fn(self.engs[q])
        self.dcnt[q][s] += 1
        inst.then_inc(self.dslots[q][s], 16)
        self._commit(('d', (q, s), 16 * self.dcnt[q][s]), reads, writes)
        return inst

    def barrier(self):
        toks = []
        for q in self.dslots:
            for s in range(self.nslot[q]):
                if self.dcnt[q][s] > 0:
                    toks.append(('d', (q, s), 16 * self.dcnt[q][s]))
        for k in self.engs:
            if self.cnt[k] > 0:
                toks.append(('e', k, self.cnt[k]))
        for e in self.engs:
            for tok in toks:
                self._wait(e, tok)
        self.res = {}


class Rot:
    def __init__(self, bufs, name):
        self.bufs = bufs
        self.name = name
        self.i = 0

    def next(self):
        b = self.bufs[self.i % len(self.bufs)]
        k = (self.name, self.i % len(self.bufs))
        self.i += 1
        return b, k


AX = mybir.AxisListType


def build(stop_after=None, layers=(0, 1)):
    nc = bass.Bass("TRN2", target_bir_lowering=False)
    S = Sched(nc)
    used_inputs = {}
    _ctr = [0]

    def SBT(name, shape, dt):
        _ctr[0] += 1
        return nc.sbuf_tensor("%s_u%d" % (name, _ctr[0]), shape, dt)

    IN_SPECS = {
        "xin": [T, D], "ccols": [128, 16], "mod_w": [2, D, 6 * D], "mod_b": [2, 6 * D],
        "norm1_g": [2, D], "norm2_g": [2, D], "final_g": [1, D],
        "ev_w_in": [D, 6160], "ev_qkcw": [128, 16, 5], "ev_gate_b": [1, 16], "ev_mnorm_g": [1, D],
        "ev_lrucw": [128, 8, 5], "ev_lru_w": [4, 8, 128, 128], "ev_lru_b": [128, 8, 4], "ev_lru_lam": [128, 8, 2],
        "ev_w_out": [2 * D, D], "od_w_in": [D, 1536], "od_qk_g": [2, 128], "od_w_out": [D, D],
        "rope_cos": [4096, 128], "rope_sin": [4096, 128],
        "moe_w_r": [2, D, 32], "moe_b_r": [2, 32],
        "moe_w1_0": [32 * 128, 8, 2048], "moe_w1_1": [32 * 128, 8, 2048],
        "moe_b1_0": [32 * 128, 16], "moe_b1_1": [32 * 128, 16],
        "moe_w2_0": [32 * 128, 8, 1024], "moe_w2_1": [32 * 128, 8, 1024],
        "moe_b2_0": [32, 1024], "moe_b2_1": [32, 1024],
    }

    def I(name):
        if name not in used_inputs:
            used_inputs[name] = nc.dram_tensor(name, list(IN_SPECS[name]), F32, kind="ExternalInput").ap()
        return used_inputs[name]

    def dscr(name, shape, dt=F32):
        return nc.dram_tensor(name, list(shape), dt, kind="Internal").ap()

    dbg_out = {}

    def dbgt(name, shape, dt=F32):
        dbg_out[name] = nc.dram_tensor("dbg_" + name, list(shape), dt, kind="ExternalOutput").ap()
        return dbg_out[name]

    def finish_dbg(pairs):
        S.barrier()
        for name, src, shape, dt in pairs:
            d = dbgt(name, shape, dt)
            rows, cols = shape[0], int(np.prod(shape[1:]))
            s2 = src if len(shape) == 2 else src.rearrange("a b c -> a (b c)")
            d2 = d if len(shape) == 2 else d.rearrange("a b c -> a (b c)")
            with SBT("dbgbuf_" + name, [128, cols], dt) as buf:
                for r0 in range(0, rows, 128):
                    n = min(128, rows - r0)
                    S.dma('sp', lambda e: e.dma_start(out=buf[0:n, :], in_=s2[r0:r0 + n, :]), writes=['dbgbuf'])
                    S.dma('sp', lambda e: e.dma_start(out=d2[r0:r0 + n, :], in_=buf[0:n, :]), reads=['dbgbuf'], writes=[('dbgo', name, r0)])
                S.barrier()
        return nc, dbg_out, used_inputs

    out_d = nc.dram_tensor("out", [4096, D], F32, kind="ExternalOutput").ap()
    modrow = dscr("modrow", [2, 2, 6 * D])
    xres = dscr("xres", [T, D])

    ident = nc.alloc_sbuf_tensor("ident", [128, 128], F32)
    identb = nc.alloc_sbuf_tensor("identb", [128, 128], BF16)
    ones1 = nc.alloc_sbuf_tensor("ones1", [1, 128], F32)
    onesm = nc.alloc_sbuf_tensor("onesm", [128, 128], F32)
    triF = nc.alloc_sbuf_tensor("triF", [128, 128], F32)
    triB = nc.alloc_sbuf_tensor("triB", [128, 128], F32)
    triS = nc.alloc_sbuf_tensor("triS", [128, 128], F32)
    selF = nc.alloc_sbuf_tensor("selF", [128, 128], F32)
    selB = nc.alloc_sbuf_tensor("selB", [128, 128], F32)
    pidx = nc.alloc_sbuf_tensor("pidx", [128, 1], F32)
    pidx_i = nc.alloc_sbuf_tensor("pidx_i", [128, 1], I32)
    S.op('pool', lambda e: e.memset(ident[:], 0.0), writes=['ident'])
    S.op('pool', lambda e: e.affine_select(ident[:], ident[:], pattern=[[-1, 128]], compare_op=ALU.not_equal,
                                          fill=1.0, base=0, channel_multiplier=1), reads=['ident'], writes=['ident'])
    S.op('dve', lambda e: e.tensor_copy(identb[:], ident[:]), reads=['ident'], writes=['identb'])
    S.op('pool', lambda e: e.memset(ones1[:], 1.0), writes=['ones1'])
    S.op('pool', lambda e: e.memset(onesm[:], 1.0), writes=['onesm'])

    def mk_mask(t, pattern, cm, base, cmp):
        S.op('pool', lambda e: e.memset(t[:], 1.0), writes=[t.name])
        S.op('pool', lambda e: e.affine_select(t[:], t[:], pattern=pattern, compare_op=cmp, fill=0.0, base=base,
                                              channel_multiplier=cm), writes=[t.name])

    mk_mask(triF, [[1, 128]], -1, 0, ALU.is_ge)
    mk_mask(triB, [[-1, 128]], 1, 0, ALU.is_ge)
    mk_mask(triS, [[1, 128]], -1, 0, ALU.is_gt)
    mk_mask(selF, [[0, 128]], 1, -127, ALU.is_equal)
    mk_mask(selB, [[0, 128]], 1, 0, ALU.is_equal)
    S.op('pool', lambda e: e.iota(pidx_i[:], pattern=[[0, 1]], base=0, channel_multiplier=1), writes=['pidx_i'])
    S.op('dve', lambda e: e.tensor_copy(pidx[:], pidx_i[:]), reads=['pidx_i'], writes=['pidx'])

    psum = [nc.alloc_psum_tensor("ps%d" % i, [128, 512], F32) for i in range(6)]
    psb = [nc.alloc_psum_tensor("psb%d" % i, [128, 1024], BF16) for i in range(2)]

    def PB(i):
        return ('psum', i)

    def PBB(i):
        return ('psb', i)

    def bcast_row(dst, src_row, key, q='sp'):
        S.dma(q, lambda e: e.dma_start(out=dst, in_=src_row.partition_broadcast(dst.shape[0])), writes=[key])

    def phase_mod():
        mod_w, mod_b = I("mod_w"), I("mod_b")
        with SBT("cc", [128, 16], F32) as cc, SBT("csil", [128, 16], BF16) as csil, \
                SBT("mw0", [128, 8, 512], BF16) as mw0, SBT("mw1", [128, 8, 512], BF16) as mw1, SBT("mw2", [128, 8, 512], BF16) as mw2, \
                SBT("mb0", [1, 512], F32) as mb0, SBT("mb1", [1, 512], F32) as mb1, \
                SBT("mo0", [1, 512], F32) as mo0, SBT("mo1", [1, 512], F32) as mo1:
            S.dma('sp', lambda e: e.dma_start(out=cc[:], in_=I("ccols")), writes=['cc'])
            S.op('act', lambda e: e.activation(out=csil[:], in_=cc[:], func=AF.Silu), reads=['cc'], writes=['csil'])
            mws = Rot([mw0, mw1, mw2], 'mw')
            mbs = Rot([mb0, mb1], 'mb')
            mos = Rot([mo0, mo1], 'mo')
            pi = 0
            for layer in range(2):
                mwv = mod_w[layer].rearrange("(kc p) n -> p kc n", p=128)
                for cb in range(12):
                    mw, mwk = mws.next()
                    mb, mbk = mbs.next()
                    S.dma('pool', lambda e: e.dma_start(out=mw[:], in_=mwv[:, :, cb * 512:(cb + 1) * 512]), writes=[mwk])
                    S.dma('sp', lambda e: e.dma_start(out=mb[:], in_=mod_b[layer:layer + 1, cb * 512:(cb + 1) * 512]), writes=[mbk])
                    for r in range(2):
                        ps = psum[pi % 4]
                        pk = PB(pi % 4)
                        pi += 1
                        for kc in range(8):
                            S.op('pe', lambda e: e.matmul(ps[0:1, :], csil[:, r * 8 + kc:r * 8 + kc + 1], mw[:, kc, :],
                                                          start=(kc == 0), stop=(kc == 7)), reads=['csil', mwk], writes=[pk])
                        mo, mok = mos.next()
                        S.op('dve', lambda e: e.tensor_tensor(out=mo[:], in0=ps[0:1, :], in1=mb[:], op=ALU.add), reads=[mbk], writes=[pk, mok])
                        S.dma('sp', lambda e: e.dma_start(out=modrow[layer, r:r + 1, cb * 512:(cb + 1) * 512], in_=mo[:]),
                              reads=[mok], writes=[('modrow', layer, r, cb)])
        S.barrier()

    def norm_tiles(layer, which, src, tiles, consume):
        gsrc = I("norm1_g") if which == 0 else I("norm2_g")
        sh_off = 0 if which == 0 else 3 * D
        sc_off = D if which == 0 else 4 * D
        with SBT("nA", [128, 2, D], F32) as A, SBT("nSH", [128, 2, D], F32) as SH, \
                SBT("nG", [128, D], F32) as G, \
                SBT("nx0", [128, D], F32) as x0, SBT("nx1", [128, D], F32) as x1, \
                SBT("nu0", [128, D], F32) as u0, SBT("nu1", [128, D], F32) as u1, \
                SBT("nsq", [128, 2, D], F32) as sq2, SBT("nst", [128, 2, 8], F32) as st2:
            bcast_row(G[:], gsrc[layer:layer + 1, :], 'nG')
            for r in range(2):
                bcast_row(A[:, r, :], modrow[layer, r:r + 1, sc_off:sc_off + D], ('nA', r))
                bcast_row(SH[:, r, :], modrow[layer, r:r + 1, sh_off:sh_off + D], ('nSH', r))
                S.op('dve', lambda e: e.scalar_tensor_tensor(out=A[:, r, :], in0=A[:, r, :], scalar=1.0, in1=G[:],
                                                             op0=ALU.add, op1=ALU.mult), reads=['nG'], writes=[('nA', r)])
            xs = Rot([x0, x1], 'nx')
            us = Rot([u0, u1], 'nu')
            tl = list(tiles)
            pend = {}

            def pre(n_):
                i = tl[n_]
                r = 1 if i < 2 else 0
                xt, xk = xs.next()
                ut, uk = us.next()
                sq = sq2[:, n_ % 2, :]
                st = st2[:, n_ % 2, :]
                NSQ, NST = ('nsq', n_ % 2), ('nst', n_ % 2)
                S.dma('sp', lambda e: e.dma_start(out=xt[:], in_=src[i * 128:(i + 1) * 128, :]), writes=[xk])
                S.op('act', lambda e: e.activation(out=sq, in_=xt[:], func=AF.Square, accum_out=st[:, 0:1]),
                     reads=[xk], writes=[NSQ, NST])
                S.op('dve', lambda e: e.tensor_scalar(out=st[:, 1:2], in0=st[:, 0:1], scalar1=1.0 / D, scalar2=EPS,
                                                      op0=ALU.mult, op1=ALU.add), writes=[NST])
                S.op('act', lambda e: e.activation(out=st[:, 2:3], in_=st[:, 1:2], func=AF.Sqrt), writes=[NST])
                S.op('dve', lambda e: e.reciprocal(st[:, 3:4], st[:, 2:3]), writes=[NST])
                S.op('dve', lambda e: e.scalar_tensor_tensor(out=ut[:], in0=xt[:], scalar=st[:, 3:4], in1=A[:, r, :],
                                                             op0=ALU.mult, op1=ALU.mult), reads=[xk, ('nA', r)], writes=[uk, NST])
                S.op('pool', lambda e: e.tensor_tensor(out=ut[:], in0=ut[:], in1=SH[:, r, :], op=ALU.add),
                     reads=[('nSH', r)], writes=[uk])
                pend[n_] = (i, ut, uk)

            pre(0)
            for n_ in range(len(tl)):
                if n_ + 1 < len(tl):
                    pre(n_ + 1)
                consume(*pend.pop(n_))

    def phase_norm_T(layer, src, uT, tiles=range(NT)):
        def consume(i, ut, uk):
            for half in range(2):
                pb = (i * 2 + half) % 4
                for j in range(4):
                    kc = half * 4 + j
                    S.op('pe', lambda e: e.transpose(psum[pb][:, j * 128:(j + 1) * 128], ut[:, kc * 128:(kc + 1) * 128], ident[:]),
                         reads=[uk, 'ident'], writes=[PB(pb)])
                S.op('act', lambda e: e.copy(uT[:, half * 4:half * 4 + 4, i * 128:(i + 1) * 128],
                                             psum[pb][:].rearrange("p (a b) -> p a b", a=4)),
                     writes=[PB(pb), ('uT', i, half)])
        norm_tiles(layer, 0, src, tiles, consume)
        S.barrier()

    qkT = dscr("qkT", [2048, T], BF16)
    v_tm = dscr("v_tm", [T, D], BF16)
    so_tm = dscr("so_tm", [T, D], BF16)
    g_tm = dscr("g_tm", [T, 16])
    xrT = dscr("xrT", [D, T])
    ygT = dscr("ygT", [D, T], BF16)
    hlT = dscr("hlT", [D, T], BF16)
    hm_d = [dscr("hm_f", [T, D]), dscr("hm_b", [T, D])]
    TG = [(0, 256)] + [(256 + 512 * j, 512) for j in range(8)]
    ZW = 4358
    CN = 4355

    def zcol(t0):
        return 2 + t0 if t0 < 256 else t0 + 5

    def phase_E1(uT):
        wv = I("ev_w_in").rearrange("(kc p) n -> p kc n", p=128)
        with SBT("wb0", [128, 8, 512], BF16) as wb0, SBT("wb1", [128, 8, 512], BF16) as wb1, \
                SBT("zp0", [128, ZW], F32) as zp0, SBT("zp1", [128, ZW], F32) as zp1, \
                SBT("co", [128, ZW], F32) as co, SBT("co_b", [128, ZW], F32) as co_b, \
                SBT("ob0", [128, ZW], BF16) as ob0, SBT("ob1", [128, ZW], BF16) as ob1, \
                SBT("cwq", [128, 16, 5], F32) as cwq, SBT("cwl", [128, 8, 5], F32) as cwl, \
                SBT("tms0", [128, 512], BF16) as tms0, SBT("tms1", [128, 512], BF16) as tms1, \
                SBT("wg", [128, 8, 16], BF16) as wg, SBT("gbr", [128, 16], F32) as gbr, \
                SBT("GT", [128, NT, 16], F32) as GT:
            S.dma('sp', lambda e: e.dma_start(out=cwq[:], in_=I("ev_qkcw")), writes=['cwq'])
            S.dma('sp', lambda e: e.dma_start(out=cwl[:], in_=I("ev_lrucw")), writes=['cwl'])
            S.op('pool', lambda e: e.memset(zp0[:], 0.0), writes=[('zp', 0)])
            S.op('pool', lambda e: e.memset(zp1[:], 0.0), writes=[('zp', 1)])
            wbs = Rot([wb0, wb1], 'wb')
            zps = Rot([zp0, zp1], 'zp')
            obs = Rot([ob0, ob1], 'ob')
            pi = [0]
            fm_blocks = [(c0, 'qk', c0 // 128) for c0 in range(0, 2048, 512)] + \
                        [(4112 + j * 512, 'xr', j * 4) for j in range(2)] + \
                        [(5136 + j * 512, 'yg', j * 4) for j in range(2)]
            chunks = []
            for c0, kind, cbase in fm_blocks:
                for jj in range(4):
                    chunks.append((c0, kind, cbase + jj, jj))
            cos_ = [co, co_b]
            state = {}

            def stage1(n):
                c0, kind, cidx, jj = chunks[n]
                if jj == 0:
                    wb, wbk = wbs.next()
                    S.dma('pool', lambda e: e.dma_start(out=wb[:], in_=wv[:, :, c0:c0 + 512]), writes=[wbk])
                    state['wb'] = (wb, wbk)
                wb, wbk = state['wb']
                zp, zpk = zps.next()
                cq = cos_[n % 2]
                ck = ('co', n % 2)
                for (t0, n_) in TG:
                    pb = pi[0] % 2
                    pi[0] += 1
                    for kc in range(8):
                        S.op('pe', lambda e: e.matmul(psum[pb][:, 0:n_], wb[:, kc, jj * 128:(jj + 1) * 128], uT[:, kc, t0:t0 + n_],
                                                      start=(kc == 0), stop=(kc == 7)), reads=[wbk], writes=[PB(pb)])
                    z0 = zcol(t0)
                    S.op('act', lambda e: e.copy(zp[:, z0:z0 + n_], psum[pb][:, 0:n_]), writes=[PB(pb), zpk])
                if kind in ('qk', 'xr'):
                    cw = cwq if kind == 'qk' else cwl
                    S.op('dve', lambda e: e.tensor_scalar(out=cq[:, 0:CN], in0=zp[:, 0:CN], scalar1=cw[:, cidx, 0:1], scalar2=None,
                                                          op0=ALU.mult), reads=[zpk, 'cwq', 'cwl'], writes=[ck])
                    for j in range(1, 4):
                        S.op('dve', lambda e: e.scalar_tensor_tensor(out=cq[:, 0:CN], in0=zp[:, j:j + CN], scalar=cw[:, cidx, j:j + 1],
                                                                     in1=cq[:, 0:CN], op0=ALU.mult, op1=ALU.add),
                             reads=[zpk], writes=[ck])
                state[n] = (zp, zpk, cq, ck)

            def stage2(n):
                c0, kind, cidx, jj = chunks[n]
                zp, zpk, cq, ck = state.pop(n)
                ob, obk = obs.next()
                if kind == 'qk':
                    S.op('act', lambda e: e.activation(out=ob[:, 0:CN], in_=cq[:, 0:CN], func=AF.Silu, bias=cwq[:, cidx, 4:5], scale=1.0),
                         reads=[ck], writes=[obk])
                    rows = qkT[cidx * 128:(cidx + 1) * 128, :]
                    S.dma('sp', lambda e: e.dma_start(out=rows[:, 0:256], in_=ob[:, 0:256]), reads=[obk], writes=[('qkT', cidx, 0)])
                    S.dma('sp', lambda e: e.dma_start(out=rows[:, 256:T], in_=ob[:, 259:CN]), reads=[obk], writes=[('qkT', cidx, 1)])
                elif kind == 'xr':
                    S.op('act', lambda e: e.activation(out=cq[:, 0:CN], in_=cq[:, 0:CN], func=AF.Identity, bias=cwl[:, cidx, 4:5], scale=1.0),
                         writes=[ck])
                    rows = xrT[cidx * 128:(cidx + 1) * 128, :]
                    S.dma('sp', lambda e: e.dma_start(out=rows[:, 0:256], in_=cq[:, 0:256]), reads=[ck], writes=[('xrT', cidx, 0)])
                    S.dma('sp', lambda e: e.dma_start(out=rows[:, 256:T], in_=cq[:, 259:CN]), reads=[ck], writes=[('xrT', cidx, 1)])
                else:
                    S.op('act', lambda e: e.activation(out=cq[:], in_=zp[:], func=AF.Square), reads=[zpk], writes=[ck])
                    S.op('dve', lambda e: e.tensor_scalar(out=cq[:], in0=cq[:], scalar1=0.044715, scalar2=1.0, op0=ALU.mult, op1=ALU.add),
                         writes=[ck])
                    S.op('dve', lambda e: e.tensor_tensor(out=cq[:], in0=cq[:], in1=zp[:], op=ALU.mult), reads=[zpk], writes=[ck])
                    S.op('act', lambda e: e.activation(out=cq[:], in_=cq[:], func=AF.Sigmoid, scale=1.5957691216), writes=[ck])
                    S.op('pool', lambda e: e.tensor_tensor(out=ob[:], in0=cq[:], in1=zp[:], op=ALU.mult), reads=[ck, zpk], writes=[obk])
                    rows = ygT[cidx * 128:(cidx + 1) * 128, :]
                    S.dma('sp', lambda e: e.dma_start(out=rows[:, 0:256], in_=ob[:, 2:258]), reads=[obk], writes=[('ygT', cidx, 0)])
                    S.dma('sp', lambda e: e.dma_start(out=rows[:, 256:T], in_=ob[:, 261:4357]), reads=[obk], writes=[('ygT', cidx, 1)])

            stage1(0)
            for n in range(len(chunks)):
                if n + 1 < len(chunks):
                    stage1(n + 1)
                stage2(n)
            tms = Rot([tms0, tms1], 'tms')
            for blk in range(4):
                c0 = 2048 + blk * 512
                wb, wbk = wbs.next()
                S.dma('pool', lambda e: e.dma_start(out=wb[:], in_=wv[:, :, c0:c0 + 512]), writes=[wbk])
                dst = v_tm if blk < 2 else so_tm
                dc0 = (blk % 2) * 512
                for i in range(NT):
                    pb = 2 + i % 2
                    for kc in range(8):
                        S.op('pe', lambda e: e.matmul(psum[pb][:], uT[:, kc, i * 128:(i + 1) * 128], wb[:, kc, :],
                                                      start=(kc == 0), stop=(kc == 7)), reads=[wbk], writes=[PB(pb)])
                    st_, stk = tms.next()
                    if blk < 2:
                        S.op('act', lambda e: e.copy(st_[:], psum[pb][:]), writes=[PB(pb), stk])
                    else:
                        S.op('act', lambda e: e.activation(out=st_[:], in_=psum[pb][:], func=AF.Sigmoid), writes=[PB(pb), stk])
                    S.dma('sp', lambda e: e.dma_start(out=dst[i * 128:(i + 1) * 128, dc0:dc0 + 512], in_=st_[:]), reads=[stk],
                          writes=[('tmout', blk, i)])
            S.dma('pool', lambda e: e.dma_start(out=wg[:], in_=wv[:, :, 4096:4112]), writes=['wg'])
            bcast_row(gbr[:], I("ev_gate_b")[0:1, :], 'gbr')
            for i in range(NT):
                pb = 4 + i % 2
                for kc in range(8):
                    S.op('pe', lambda e: e.matmul(psum[pb][:, 0:16], uT[:, kc, i * 128:(i + 1) * 128], wg[:, kc, :],
                                                  start=(kc == 0), stop=(kc == 7)), reads=['wg'], writes=[PB(pb)])
                S.op('dve', lambda e: e.tensor_tensor(out=GT[:, i, :], in0=psum[pb][:, 0:16], in1=gbr[:], op=ALU.add),
                     reads=['gbr'], writes=[PB(pb), 'GT'])
            S.dma('sp', lambda e: e.dma_start(out=g_tm.rearrange("(n p) c -> p n c", p=128), in_=GT[:]), reads=['GT'], writes=['g_tm'])
        S.barrier()

    def phase_E2():
        lw_d, lb_d, lam_d = I("ev_lru_w"), I("ev_lru_b"), I("ev_lru_lam")
        with SBT("lxr", [128, T], F32) as xr, SBT("lyg", [128, T], BF16) as yg, \
                SBT("lA", [128, T], F32) as A0, SBT("lB", [128, T], F32) as Bx0, SBT("ltmp", [128, T], F32) as tmp0, \
                SBT("lA1", [128, T], F32) as A1_, SBT("lB1", [128, T], F32) as Bx1, SBT("ltmp1", [128, T], F32) as tmp1, \
                SBT("lH0", [128, T], F32) as H0, SBT("lH1", [128, T], F32) as H1, \
                SBT("lho", [128, T], BF16) as ho, \
                SBT("lw", [128, 4, 128], F32) as lw, SBT("lwb", [128, 4, 128], BF16) as lwb, SBT("xrb", [128, T], BF16) as xrb, SBT("lb", [128, 8, 4], F32) as lb, \
                SBT("lam", [128, 8, 2], F32) as lam, SBT("cA", [128, 8, 2], F32) as cA:
            S.dma('sp', lambda e: e.dma_start(out=lb[:], in_=lb_d), writes=['lb'])
            S.dma('sp', lambda e: e.dma_start(out=lam[:], in_=lam_d), writes=['lam'])
            S.op('act', lambda e: e.activation(out=cA[:], in_=lam[:], func=AF.Exp, scale=-1.0), reads=['lam'], writes=['cA'])
            S.op('act', lambda e: e.activation(out=cA[:], in_=cA[:], func=AF.Ln, bias=1.0, scale=1.0), writes=['cA'])
            S.op('dve', lambda e: e.tensor_scalar(out=cA[:], in0=cA[:], scalar1=-8.0, scalar2=None, op0=ALU.mult), writes=['cA'])
            pi = 0
            for cc in range(8):
                S.dma('sp', lambda e: e.dma_start(out=xr[:], in_=xrT[cc * 128:(cc + 1) * 128, :]), writes=['xr'])
                S.dma('sp', lambda e: e.dma_start(out=yg[:], in_=ygT[cc * 128:(cc + 1) * 128, :]), writes=['yg'])
                S.dma('sp', lambda e: e.dma_start(out=lw[:], in_=lw_d[:, cc].rearrange("g k m -> k g m")), writes=['lw'])
                S.op('pool', lambda e: e.tensor_copy(lwb[:], lw[:]), reads=['lw'], writes=['lwb'])
                S.op('pool', lambda e: e.tensor_copy(xrb[:], xr[:]), reads=['xr'], writes=['xrb'])
                for z in range(2):
                    H = H0 if z == 0 else H1
                    hk = ('H', z)
                    A, Bx, tmp = (A0, Bx0, tmp0) if z == 0 else (A1_, Bx1, tmp1)
                    KA, KB, KT_ = ('A', z), ('Bx', z), ('tmp', z)
                    for gi, dst, dk in ((0, A, KA), (1, Bx, KB)):
                        for (t0, n) in TG:
                            pb = pi % 2
                            pi += 1
                            S.op('pe', lambda e: e.matmul(psum[pb][:, 0:n], lwb[:, z * 2 + gi, :], xrb[:, t0:t0 + n], start=True, stop=True),
                                 reads=['lwb', 'xrb'], writes=[PB(pb)])
                            S.op('act', lambda e: e.activation(out=dst[:, t0:t0 + n], in_=psum[pb][:, 0:n], func=AF.Sigmoid,
                                                               bias=lb[:, cc, z * 2 + gi:z * 2 + gi + 1], scale=1.0),
                                 reads=['lb'], writes=[PB(pb), dk])
                    S.op('act', lambda e: e.activation(out=A[:], in_=A[:], func=AF.Exp, scale=cA[:, cc, z:z + 1]), reads=['cA'], writes=[KA])
                    S.op('act', lambda e: e.activation(out=tmp[:], in_=A[:], func=AF.Square), reads=[KA], writes=[KT_])
                    S.op('act', lambda e: e.activation(out=tmp[:], in_=tmp[:], func=AF.Sqrt, bias=1.0, scale=-1.0), writes=[KT_])
                    S.op('pool', lambda e: e.tensor_tensor(out=Bx[:], in0=Bx[:], in1=xr[:], op=ALU.mult), reads=['xr'], writes=[KB])
                    S.op('dve', lambda e: e.tensor_tensor(out=Bx[:], in0=Bx[:], in1=tmp[:], op=ALU.mult), reads=[KT_], writes=[KB])
                    if z == 0:
                        S.op('dve', lambda e: e.tensor_tensor_scan(H[:], A[:], Bx[:], 0.0, ALU.mult, ALU.add), reads=[KA, KB], writes=[hk])
                    else:
                        S.op('dve', lambda e: e.tensor_tensor_scan(H[:, 0:256][:, ::-1], A[:, 0:256][:, ::-1], Bx[:, 0:256][:, ::-1], 0.0,
                                                                   ALU.mult, ALU.add), reads=[KA, KB], writes=[hk])
                        S.op('dve', lambda e: e.tensor_tensor_scan(H[:, 256:T][:, ::-1], A[:, 256:T][:, ::-1], Bx[:, 256:T][:, ::-1], H[:, 0:1],
                                                                   ALU.mult, ALU.add), reads=[KA, KB], writes=[hk])
                S.op('pool', lambda e: e.tensor_tensor(out=H0[:], in0=H0[:], in1=H1[:], op=ALU.add), reads=[('H', 1)], writes=[('H', 0)])
                S.op('dve', lambda e: e.tensor_tensor(out=ho[:], in0=H0[:], in1=yg[:], op=ALU.mult), reads=[('H', 0), 'yg'], writes=['ho'])
                S.dma('pool', lambda e: e.dma_start(out=hlT[cc * 128:(cc + 1) * 128, :], in_=ho[:]), reads=['ho'], writes=[('hlT', cc)])
        S.barrier()

    def phase_E3():
        NLN16 = -2.772588722239781
        with contextlib.ExitStack() as es:
            def al(name, shape, dt):
                return es.enter_context(SBT(name, shape, dt))
            G = al("G", [128, NT, 16], F32)
            LF = al("LF", [128, 2, NT, 4], F32)
            CF = al("CF", [128, 2, NT, 4], F32)
            EB16 = al("EB16", [128, 2, NT, 4], F32)
            ECF = al("ECF", [128, 2, NT, 4], F32)
            DEC = al("DEC", [128, 2, NT, 4], F32)
            WS16 = al("WS16", [128, 2, NT, 4], F32)
            qT0 = al("qT0", [128, 2, T], BF16)
            qT1 = al("qT1", [128, 2, T], BF16)
            kT0 = al("kT0", [128, 2, T], BF16)
            kT1 = al("kT1", [128, 2, T], BF16)
            V0 = al("V0", [128, NT, 257], BF16)
            V1 = al("V1", [128, NT, 257], BF16)
            CTs = al("CTs", [128, 4, 2, 257], F32)
            CTbs = al("CTbs", [128, 4, 2, 257], BF16)
            PTs = al("PTs", [128, 4, 128], BF16)
            ktms = al("ktms", [128, 4, 256], BF16)
            vws = al("vws", [128, 4, 257], BF16)
            sms = al("sms", [128, 4, 8], F32)
            houts = al("houts", [128, 4, 256], F32)
            S.dma('sp', lambda e: e.dma_start(out=G[:], in_=g_tm.rearrange("(n p) c -> p n c", p=128)), writes=['G'])
            Gv = G[:].rearrange("p n (d g h) -> p d g n h", d=2, g=2, h=4)
            fl = lambda t, d: t[:, d].rearrange("p n h -> p (n h)")
            for d in range(2):
                S.op('act', lambda e: e.activation(out=LF[:, d], in_=Gv[:, d, 1], func=AF.Exp, scale=-1.0), reads=['G'], writes=['LF'])
                S.op('act', lambda e: e.activation(out=LF[:, d], in_=LF[:, d], func=AF.Ln, bias=1.0, scale=1.0), writes=['LF'])
                S.op('dve', lambda e: e.tensor_scalar(out=LF[:, d], in0=LF[:, d], scalar1=-1.0, scalar2=None, op0=ALU.mult), writes=['LF'])
                tri = triF if d == 0 else triB
                sel = selF if d == 0 else selB
                S.op('pe', lambda e: e.matmul(psum[0][:, 0:NT * 4], tri[:], fl(LF, d), start=True, stop=True),
                     reads=['LF', tri.name], writes=[PB(0)])
                S.op('act', lambda e: e.copy(fl(CF, d), psum[0][:, 0:NT * 4]), writes=[PB(0), 'CF'])
                S.op('pe', lambda e: e.matmul(psum[1][:, 0:NT * 4], sel[:], fl(CF, d), start=True, stop=True),
                     reads=['CF', sel.name], writes=[PB(1)])
                S.op('act', lambda e: e.activation(out=fl(DEC, d), in_=psum[1][:, 0:NT * 4], func=AF.Exp), writes=[PB(1), 'DEC'])
                S.op('dve', lambda e: e.tensor_tensor(out=EB16[:, d], in0=Gv[:, d, 0], in1=CF[:, d], op=ALU.subtract), reads=['G', 'CF'], writes=['EB16'])
                S.op('dve', lambda e: e.tensor_scalar(out=EB16[:, d], in0=EB16[:, d], scalar1=NLN16, scalar2=None, op0=ALU.add), writes=['EB16'])
                S.op('act', lambda e: e.activation(out=EB16[:, d], in_=EB16[:, d], func=AF.Exp), writes=['EB16'])
                S.op('act', lambda e: e.activation(out=ECF[:, d], in_=CF[:, d], func=AF.Exp), reads=['CF'], writes=['ECF'])
                S.op('dve', lambda e: e.tensor_tensor(out=WS16[:, d], in0=EB16[:, d], in1=DEC[:, d], op=ALU.mult), reads=['EB16', 'DEC'], writes=['WS16'])
            S.barrier()
            qTs, kTs, Vs = [qT0, qT1], [kT0, kT1], [V0, V1]
            order = [list(range(NT)), [1, 0] + list(range(NT - 1, 1, -1))]
            cnt = {'st': 0, 'num': 0}
            for hp in range(2):
                for hh in range(2):
                    h = hp * 2 + hh
                    S.dma('sp', lambda e: e.dma_start(out=qTs[hh][:], in_=qkT[h * 256:(h + 1) * 256, :].rearrange("(dh p) t -> p dh t", p=128)),
                          writes=[('qT', hh)])
                    S.dma('sp', lambda e: e.dma_start(out=kTs[hh][:], in_=qkT[1024 + h * 256:1024 + (h + 1) * 256, :].rearrange("(dh p) t -> p dh t", p=128)),
                          writes=[('kT', hh)])
                    S.dma('sp', lambda e: e.dma_start(out=Vs[hh][:, :, 0:256], in_=v_tm[:, h * 256:(h + 1) * 256].rearrange("(n p) e -> p n e", p=128)),
                          writes=[('V', hh)])
                    S.op('pool', lambda e: e.memset(Vs[hh][:, :, 256:257], 1.0), writes=[('V1', hh)])
                S.op('pool', lambda e: e.memset(CTs[:], 0.0), writes=[('CT', i) for i in range(4)])
                S.op('pool', lambda e: e.memset(CTbs[:], 0.0), writes=[('CTb', i) for i in range(4)])
                for step in range(NT):
                    for ci, (z, hh) in enumerate([(0, 0), (0, 1), (1, 0), (1, 1)]):
                        h = hp * 2 + hh
                        c = order[z][step]
                        sl = slice(c * 128, (c + 1) * 128)
                        qT, kT, V = qTs[hh], kTs[hh], Vs[hh]
                        rk = [('qT', hh), ('kT', hh), ('V', hh), ('V1', hh)]
                        tri = triF if z == 0 else triB
                        pst = cnt['st'] % 2
                        cnt['st'] += 1
                        for dh in range(2):
                            S.op('pe', lambda e: e.matmul(psum[pst][:, 0:128], kT[:, dh, sl], qT[:, dh, sl], start=(dh == 0), stop=(dh == 1)),
                                 reads=rk, writes=[PB(pst)])
                        S.op('dve', lambda e: e.scalar_tensor_tensor(out=PTs[:, ci, :], in0=psum[pst][:, 0:128], scalar=EB16[:, z, c, h:h + 1],
                                                                     in1=tri[:], op0=ALU.mult, op1=ALU.mult), writes=[PB(pst), ('PT', ci)])
                        for dh in range(2):
                            S.op('pe', lambda e: e.transpose(psb[0][:, dh * 128:(dh + 1) * 128], kT[:, dh, sl], identb[:]),
                                 reads=rk, writes=[PBB(0)])
                        S.op('act', lambda e: e.copy(ktms[:, ci, :], psb[0][:, 0:256]), writes=[PBB(0), ('ktm', ci)])
                        S.op('act', lambda e: e.activation(out=vws[:, ci, :], in_=V[:, c, :], func=AF.Identity, scale=WS16[:, z, c, h:h + 1]),
                             reads=rk, writes=[('vw', ci)])
                        pn = 2 + cnt['num'] % 2
                        cnt['num'] += 1
                        S.op('pe', lambda e: e.matmul(psum[pn][:, 0:257], PTs[:, ci, :], V[:, c, :], start=True, stop=False),
                             reads=rk + [('PT', ci)], writes=[PB(pn)])
                        for dh in range(2):
                            S.op('pe', lambda e: e.matmul(psum[pn][:, 0:257], qT[:, dh, sl], CTbs[:, ci, dh, :], start=False, stop=(dh == 1)),
                                 reads=rk + [('CTb', ci)], writes=[PB(pn)])
                        sm = sms[:, ci, :]
                        ecf = ECF[:, z, c, h:h + 1]
                        S.op('dve', lambda e: e.tensor_tensor(out=sm[:, 0:1], in0=psum[pn][:, 256:257], in1=ecf, op=ALU.mult), writes=[PB(pn), ('sm', ci)])
                        S.op('dve', lambda e: e.tensor_scalar(out=sm[:, 2:3], in0=sm[:, 0:1], scalar1=1.0, scalar2=None, op0=ALU.max), writes=[('sm', ci)])
                        S.op('dve', lambda e: e.tensor_scalar(out=sm[:, 3:4], in0=sm[:, 0:1], scalar1=-1.0, scalar2=sm[:, 2:3], op0=ALU.mult, op1=ALU.max),
                             writes=[('sm', ci)])
                        S.op('dve', lambda e: e.reciprocal(sm[:, 4:5], sm[:, 3:4]), writes=[('sm', ci)])
                        S.op('dve', lambda e: e.tensor_tensor(out=sm[:, 5:6], in0=sm[:, 4:5], in1=ecf, op=ALU.mult), writes=[('sm', ci)])
                        S.op('act', lambda e: e.activation(out=houts[:, ci, :], in_=psum[pn][:, 0:256], func=AF.Identity, scale=sm[:, 5:6]),
                             reads=[('sm', ci)], writes=[PB(pn), ('hout', ci)])
                        S.dma('sp', lambda e: e.dma_start(out=hm_d[z][sl, h * 256:(h + 1) * 256], in_=houts[:, ci, :]), reads=[('hout', ci)],
                              writes=[('hm', z, h, c)])
                        for dh in range(2):
                            S.op('pe', lambda e: e.matmul(psum[4 + dh][:, 0:257], ktms[:, ci, dh * 128:(dh + 1) * 128], vws[:, ci, :], start=True, stop=True),
                                 reads=[('ktm', ci), ('vw', ci)], writes=[PB(4 + dh)])
                            S.op('dve', lambda e: e.scalar_tensor_tensor(out=CTs[:, ci, dh, :], in0=CTs[:, ci, dh, :], scalar=DEC[:, z, c, h:h + 1],
                                                                         in1=psum[4 + dh][:, 0:257], op0=ALU.mult, op1=ALU.add),
                                 writes=[PB(4 + dh), ('CT', ci)])
                        S.op('act', lambda e: e.copy(CTbs[:, ci], CTs[:, ci]), reads=[('CT', ci)], writes=[('CTb', ci)])
        S.barrier()

    def mixer_out(i, r, G1, wo, nk, lhs_list, src_rows, pbase):
        return

    def phase_E4():
        with contextlib.ExitStack() as es:
            def al(name, shape, dt):
                return es.enter_context(SBT(name, shape, dt))
            wo = al("wo", [128, 16, D], BF16)
            MG = al("MG", [128, D], F32)
            G1 = al("G1", [128, 2, D], F32)
            hf = [al("hf%d" % j, [128, D], F32) for j in range(2)]
            hb = [al("hb%d" % j, [128, D], F32) for j in range(2)]
            so = [al("so%d" % j, [128, D], BF16) for j in range(2)]
            sq2 = [al("e4sq%d" % j_, [128, 256], F32) for j_ in range(2)]
            st2 = [al("e4st%d" % j_, [128, 16], F32) for j_ in range(2)]
            hmb2 = [al("hmb%d" % j_, [128, D], BF16) for j_ in range(2)]
            hmT = [al("hmT%d" % j, [128, 8, 128], BF16) for j in range(2)]
            hl = [al("hl%d" % j, [128, 8, 128], BF16) for j in range(2)]
            xt = [al("e4x%d" % j, [128, D], F32) for j in range(2)]
            tmp2 = [al("e4tmp%d" % j_, [128, D], F32) for j_ in range(2)]
            wov = I("ev_w_out").rearrange("(kc p) n -> p kc n", p=128)
            for hlf in range(2):
                S.dma('pool', lambda e: e.dma_start(out=wo[:, hlf * 8:(hlf + 1) * 8, :], in_=wov[:, hlf * 8:(hlf + 1) * 8, :]), writes=[('wo', hlf)])
            bcast_row(MG[:], I("ev_mnorm_g")[0:1, :], 'MG')
            for r in range(2):
                bcast_row(G1[:, r, :], modrow[0, r:r + 1, 2 * D:3 * D], ('G1', r))
            hlv = hlT.rearrange("(cc p) t -> p cc t", p=128)
            xin = I("xin")
            def stL(i):
                r = 1 if i < 2 else 0
                j = i % 2
                rows = slice(i * 128, (i + 1) * 128)
                sq, st, hmb, tmp = sq2[j], st2[j], hmb2[j], tmp2[j]
                KSQ, KST, KHMB = ('e4sq', j), ('e4st', j), ('hmb', j)
                S.dma('sp', lambda e: e.dma_start(out=hf[j][:], in_=hm_d[0][rows, :]), writes=[('hf', j)])
                S.dma('sp', lambda e: e.dma_start(out=hb[j][:], in_=hm_d[1][rows, :]), writes=[('hb', j)])
                S.dma('sp', lambda e: e.dma_start(out=so[j][:], in_=so_tm[rows, :]), writes=[('so', j)])
                S.dma('sp', lambda e: e.dma_start(out=hl[j][:], in_=hlv[:, :, rows]), writes=[('hl', j)])
                S.dma('sp', lambda e: e.dma_start(out=xt[j][:], in_=xin[rows, :]), writes=[('xt', j)])

            def stB(i):
                r = 1 if i < 2 else 0
                j = i % 2
                rows = slice(i * 128, (i + 1) * 128)
                sq, st, hmb, tmp = sq2[j], st2[j], hmb2[j], tmp2[j]
                KSQ, KST, KHMB = ('e4sq', j), ('e4st', j), ('hmb', j)
                S.op('pool', lambda e: e.tensor_tensor(out=hf[j][:], in0=hf[j][:], in1=hb[j][:], op=ALU.add), reads=[('hb', j)], writes=[('hf', j)])
                for h in range(4):
                    S.op('act', lambda e: e.activation(out=sq[:], in_=hf[j][:, h * 256:(h + 1) * 256], func=AF.Square, accum_out=st[:, h:h + 1]),
                         reads=[('hf', j)], writes=[KSQ, KST])
                S.op('dve', lambda e: e.tensor_scalar(out=st[:, 4:8], in0=st[:, 0:4], scalar1=1.0 / 256, scalar2=EPS, op0=ALU.mult, op1=ALU.add), writes=[KST])
                S.op('act', lambda e: e.activation(out=st[:, 8:12], in_=st[:, 4:8], func=AF.Sqrt), writes=[KST])
                S.op('dve', lambda e: e.reciprocal(st[:, 12:16], st[:, 8:12]), writes=[KST])
                for h in range(4):
                    hs = slice(h * 256, (h + 1) * 256)
                    S.op('dve', lambda e: e.scalar_tensor_tensor(out=hf[j][:, hs], in0=hf[j][:, hs], scalar=st[:, 12 + h:13 + h], in1=MG[:, hs],
                                                                 op0=ALU.mult, op1=ALU.mult), reads=['MG'], writes=[('hf', j), KST])
                S.op('pool', lambda e: e.tensor_tensor(out=hmb[:], in0=hf[j][:], in1=so[j][:], op=ALU.mult), reads=[('hf', j), ('so', j)], writes=[KHMB])

            def stCD(i):
                r = 1 if i < 2 else 0
                j = i % 2
                rows = slice(i * 128, (i + 1) * 128)
                sq, st, hmb, tmp = sq2[j], st2[j], hmb2[j], tmp2[j]
                KSQ, KST, KHMB = ('e4sq', j), ('e4st', j), ('hmb', j)
                for kc in range(8):
                    S.op('pe', lambda e: e.transpose(psb[j][:, kc * 128:(kc + 1) * 128], hmb[:, kc * 128:(kc + 1) * 128], identb[:]),
                         reads=[KHMB], writes=[PBB(j)])
                S.op('act', lambda e: e.copy(hmT[j][:].rearrange("p a b -> p (a b)"), psb[j][:]), writes=[PBB(j), ('hmT', j)])
                for nb in range(2):
                    pb = (2 * i + nb) % 4
                    for kc in range(16):
                        lhs = hmT[j][:, kc, :] if kc < 8 else hl[j][:, kc - 8, :]
                        S.op('pe', lambda e: e.matmul(psum[pb][:], lhs, wo[:, kc, nb * 512:(nb + 1) * 512], start=(kc == 0), stop=(kc == 15)),
                             reads=[('hmT', j), ('hl', j), ('wo', 0), ('wo', 1)], writes=[PB(pb)])
                    cs = slice(nb * 512, (nb + 1) * 512)
                    S.op('dve', lambda e: e.tensor_tensor(out=tmp[:, cs], in0=psum[pb][:], in1=G1[:, r, cs], op=ALU.mult), reads=[('G1', r)],
                         writes=[PB(pb), ('e4tmp', j, nb)])
                    S.op('pool', lambda e: e.tensor_tensor(out=tmp[:, cs], in0=tmp[:, cs], in1=xt[j][:, cs], op=ALU.add), reads=[('xt', j)],
                         writes=[('e4tmp', j, nb)])
                S.dma('pool', lambda e: e.dma_start(out=xres[rows, :], in_=tmp[:]), reads=[('e4tmp', j, 0), ('e4tmp', j, 1)], writes=[('xres', i)])

            stL(0)
            stB(0)
            for i in range(NT):
                if i + 1 < NT:
                    stL(i + 1)
                    stB(i + 1)
                stCD(i)
        S.barrier()

    v2_tm = dscr("v2_tm", [T, D], BF16)

    def phase_N2(layer, tiles, LOG):
        with contextlib.ExitStack() as es:
            def al(name, shape, dt):
                return es.enter_context(SBT(name, shape, dt))
            wr = al("wr", [128, 8, 32], F32)
            br = al("br", [1, 32], F32)
            vT32 = [al("vT32_%d" % j, [128, 8, 128], F32) for j in range(2)]
            vb = [al("vb%d" % j, [128, D], BF16) for j in range(2)]
            S.dma('sp', lambda e: e.dma_start(out=wr[:], in_=I("moe_w_r")[layer].rearrange("(kc p) n -> p kc n", p=128)), writes=['wr'])
            S.dma('sp', lambda e: e.dma_start(out=br[:], in_=I("moe_b_r")[layer:layer + 1, :]), writes=['br'])
            cnt = [0]

            def consume(i, ut, uk):
                j = cnt[0] % 2
                cnt[0] += 1
                S.op('pool', lambda e: e.tensor_copy(vb[j][:], ut[:]), reads=[uk], writes=[('vb', j)])
                S.dma('pool', lambda e: e.dma_start(out=v2_tm[i * 128:(i + 1) * 128, :], in_=vb[j][:]), reads=[('vb', j)], writes=[('v2', i)])
                for half in range(2):
                    pb = (i * 2 + half) % 4
                    for jj in range(4):
                        kc = half * 4 + jj
                        S.op('pe', lambda e: e.transpose(psum[pb][:, jj * 128:(jj + 1) * 128], ut[:, kc * 128:(kc + 1) * 128], ident[:]),
                             reads=[uk, 'ident'], writes=[PB(pb)])
                    S.op('act', lambda e: e.copy(vT32[j][:, half * 4:half * 4 + 4, :].rearrange("p a b -> p (a b)"), psum[pb][:]),
                         writes=[PB(pb), ('vT32', j, half)])
                pl = 4 + i % 2
                for kc in range(8):
                    S.op('pe', lambda e: e.matmul(psum[pl][:, 0:32], vT32[j][:, kc, :], wr[:, kc, :], start=(kc == 0), stop=False),
                         reads=[('vT32', j, 0), ('vT32', j, 1), 'wr'], writes=[PB(pl)])
                S.op('pe', lambda e: e.matmul(psum[pl][:, 0:32], ones1[0:1, :], br[:], start=False, stop=True), reads=['br', 'ones1'], writes=[PB(pl)])
                S.op('dve', lambda e: e.tensor_copy(LOG[:, i, :], psum[pl][:, 0:32]), writes=[PB(pl), 'LOG'])
            norm_tiles(layer, 1, xres, tiles, consume)
        S.barrier()

    SB = 512
    SHIFT = 9
    NSUB = SB // 128
    NBMAX = 66
    xs_d = dscr("xs_d", [NBMAX * SB, D], BF16)
    ys_d = dscr("ys_d", [NBMAX * SB, D], F32)
    RT = {}
    for nm, shp, dt in (("LOG", [128, NT, 32], F32), ("TOP8", [128, NT, 8], F32), ("WG", [128, NT, 4], F32),
                        ("SLOTI", [128, NT, 4], I32), ("IDXI", [128, NBMAX], I32), ("BIDX", [128, NBMAX], I32),
                        ("IDX8", [128, NBMAX, 8], I32), ("IDX4", [128, NBMAX, 4], I32)):
        RT[nm] = nc.alloc_sbuf_tensor("rt_" + nm, shp, dt)

    def phase_route(tiles, NB):
        LOG, TOP8, WG, SLOTI, IDXI, BIDX = (RT[k] for k in ("LOG", "TOP8", "WG", "SLOTI", "IDXI", "BIDX"))
        with contextlib.ExitStack() as es:
            def al(name, shape, dt):
                return es.enter_context(SBT(name, shape, dt))
            MASK = al("MASK", [128, NT, 32], F32)
            RANK = al("RANK", [128, NT, 32], F32)
            CAR = al("CAR", [128, 32], F32)
            sm = al("rsm", [128, 8], F32)
            ci = al("rci", [128, 32], I32)
            PADDED = al("PADDED", [128, 32], F32)
            PEND = al("PEND", [128, 32], F32)
            BASE = al("BASE", [128, 32], F32)
            ones32 = al("ones32", [128, 32], F32)
            junk = al("rjunk", [128, 32], F32)
            SLOTF = al("SLOTF", [128, NT, 4], F32)
            BSTi = al("BSTi", [128, NBMAX], I32)
            BST = al("BST", [128, NBMAX], F32)
            BLKE = al("BLKE", [128, NBMAX], F32)
            IDXF = al("IDXF", [128, NBMAX], F32)
            IDXF2 = al("IDXF2", [128, NBMAX], F32)
            S.op('pool', lambda e: e.memset(CAR[:], 0.0), writes=['CAR'])
            S.op('pool', lambda e: e.memset(ones32[:], 1.0), writes=['ones32'])
            S.op('pool', lambda e: e.memset(SLOTF[:], 0.0), writes=['SLOTF'])
            for i in tiles:
                S.op('dve', lambda e: e.max(out=TOP8[:, i, :], in_=LOG[:, i, :]), writes=['TOP8'])
                S.op('dve', lambda e: e.tensor_scalar(out=MASK[:, i, :], in0=LOG[:, i, :], scalar1=TOP8[:, i, 3:4], scalar2=None, op0=ALU.is_ge),
                     reads=['TOP8'], writes=[('MASK', i)])
                S.op('dve', lambda e: e.tensor_scalar(out=sm[:, 0:1], in0=TOP8[:, i, 0:1], scalar1=-1.0, scalar2=None, op0=ALU.mult), reads=['TOP8'], writes=['rsm'])
                S.op('act', lambda e: e.activation(out=WG[:, i, :], in_=TOP8[:, i, 0:4], func=AF.Exp, bias=sm[:, 0:1], scale=1.0, accum_out=sm[:, 1:2]),
                     reads=['TOP8'], writes=['rsm', 'WG'])
                S.op('dve', lambda e: e.reciprocal(sm[:, 2:3], sm[:, 1:2]), writes=['rsm'])
                S.op('dve', lambda e: e.tensor_scalar(out=WG[:, i, :], in0=WG[:, i, :], scalar1=sm[:, 2:3], scalar2=None, op0=ALU.mult), writes=['rsm', 'WG'])
                S.op('pe', lambda e: e.matmul(psum[0][:, 0:32], triS[:], MASK[:, i, :], start=True, stop=True), reads=[('MASK', i)], writes=[PB(0)])
                S.op('pe', lambda e: e.matmul(psum[1][:, 0:32], onesm[:], MASK[:, i, :], start=True, stop=True), reads=[('MASK', i)], writes=[PB(1)])
                S.op('dve', lambda e: e.tensor_tensor(out=RANK[:, i, :], in0=psum[0][:, 0:32], in1=CAR[:], op=ALU.add), reads=['CAR'], writes=[PB(0), 'RANK'])
                S.op('dve', lambda e: e.tensor_tensor(out=CAR[:], in0=psum[1][:, 0:32], in1=CAR[:], op=ALU.add), writes=[PB(1), 'CAR'])
            S.op('dve', lambda e: e.tensor_scalar(out=junk[:], in0=CAR[:], scalar1=float(SB - 1), scalar2=None, op0=ALU.add), reads=['CAR'], writes=['rjunk'])
            S.op('dve', lambda e: e.tensor_copy(ci[:], junk[:]), reads=['rjunk'], writes=['rci'])
            S.op('dve', lambda e: e.tensor_scalar(out=ci[:], in0=ci[:], scalar1=SHIFT, scalar2=SHIFT, op0=ALU.arith_shift_right, op1=ALU.logical_shift_left),
                 writes=['rci'])
            S.op('dve', lambda e: e.tensor_copy(PADDED[:], ci[:]), reads=['rci'], writes=['PADDED'])
            S.op('dve', lambda e: e.tensor_tensor_scan(PEND[:], ones32[:], PADDED[:], 0.0, ALU.mult, ALU.add), reads=['ones32', 'PADDED'], writes=['PEND'])
            S.op('dve', lambda e: e.tensor_tensor(out=BASE[:], in0=PEND[:], in1=PADDED[:], op=ALU.subtract), reads=['PEND', 'PADDED'], writes=['BASE'])
            for i in tiles:
                S.op('dve', lambda e: e.tensor_tensor(out=RANK[:, i, :], in0=RANK[:, i, :], in1=BASE[:], op=ALU.add), reads=['BASE'], writes=['RANK'])
                for k in range(4):
                    S.op('dve', lambda e: e.scalar_tensor_tensor(out=junk[:], in0=LOG[:, i, :], scalar=TOP8[:, i, k:k + 1], in1=RANK[:, i, :],
                                                                 op0=ALU.is_equal, op1=ALU.mult, accum_out=SLOTF[:, i, k:k + 1]),
                         reads=['TOP8'], writes=['rjunk', 'RANK', 'SLOTF'])
            S.op('dve', lambda e: e.tensor_copy(SLOTI[:], SLOTF[:]), reads=['SLOTF'], writes=['SLOTI'])
            S.op('pool', lambda e: e.iota(BSTi[:], pattern=[[SB, NBMAX]], base=0, channel_multiplier=0), writes=['BSTi'])
            S.op('dve', lambda e: e.tensor_copy(BST[:], BSTi[:]), reads=['BSTi'], writes=['BST'])
            S.op('pool', lambda e: e.memset(BLKE[:], 0.0), writes=['BLKE'])
            for ex in range(32):
                S.op('dve', lambda e: e.scalar_tensor_tensor(out=BLKE[:], in0=BST[:], scalar=PEND[:, ex:ex + 1], in1=BLKE[:], op0=ALU.is_ge, op1=ALU.add),
                     reads=['BST', 'PEND'], writes=['BLKE'])
            S.op('dve', lambda e: e.tensor_scalar(out=BLKE[:], in0=BLKE[:], scalar1=31.0, scalar2=None, op0=ALU.min), writes=['BLKE'])
            S.op('dve', lambda e: e.tensor_scalar(out=IDXF[:], in0=BLKE[:], scalar1=128.0, scalar2=pidx[:, 0:1], op0=ALU.mult, op1=ALU.add),
                 reads=['pidx'], writes=['IDXF'])
            S.op('dve', lambda e: e.tensor_copy(IDXI[:], IDXF[:]), reads=['IDXF'], writes=['IDXI'])
            S.op('dve', lambda e: e.tensor_copy(BIDX[:], BLKE[:]), reads=['BLKE'], writes=['BIDX'])
            S.op('pool', lambda e: e.memset(BSTi[:], 0), writes=['BSTi'])
            S.op('dve', lambda e: e.tensor_copy(IDXF2[:], BSTi[:]), reads=['BSTi'], writes=['IDXF2'])
            S.op('dve', lambda e: e.tensor_tensor(out=IDXF2[:, 2:NBMAX], in0=BLKE[:, 2:NBMAX], in1=BLKE[:, 0:NBMAX - 2], op=ALU.is_equal),
                 reads=['BLKE'], writes=['IDXF2'])
            S.op('dve', lambda e: e.tensor_scalar(out=IDXF2[:], in0=IDXF2[:], scalar1=float(1 << 20), scalar2=None, op0=ALU.mult), writes=['IDXF2'])
            for kc in range(8):
                S.op('dve', lambda e: e.tensor_scalar(out=BST[:], in0=IDXF[:], scalar1=8.0, scalar2=float(kc), op0=ALU.mult, op1=ALU.add),
                     reads=['IDXF'], writes=['BST'])
                S.op('dve', lambda e: e.tensor_tensor(out=BST[:], in0=BST[:], in1=IDXF2[:], op=ALU.add), reads=['IDXF2'], writes=['BST'])
                S.op('dve', lambda e: e.tensor_copy(RT["IDX8"][:, :, kc], BST[:]), reads=['BST'], writes=['IDX8'])
            for q in range(4):
                S.op('dve', lambda e: e.tensor_scalar(out=BST[:], in0=IDXF[:], scalar1=4.0, scalar2=float(q), op0=ALU.mult, op1=ALU.add),
                     reads=['IDXF'], writes=['BST'])
                S.op('dve', lambda e: e.tensor_tensor(out=BST[:], in0=BST[:], in1=IDXF2[:], op=ALU.add), reads=['IDXF2'], writes=['BST'])
                S.op('dve', lambda e: e.tensor_copy(RT["IDX4"][:, :, q], BST[:]), reads=['BST'], writes=['IDX4'])
            if RT.get('dbg') is not None:
                dd = RT['dbg']
                S.op('dve', lambda e: e.tensor_copy(dd[:, 0:32], CAR[:]), reads=['CAR'], writes=['dd'])
                S.op('dve', lambda e: e.tensor_copy(dd[:, 32:64], PADDED[:]), reads=['PADDED'], writes=['dd'])
                S.op('dve', lambda e: e.tensor_copy(dd[:, 64:96], PEND[:]), reads=['PEND'], writes=['dd'])
                S.op('dve', lambda e: e.tensor_copy(dd[:, 96:128], MASK[:, 0, :]), writes=['dd'])
                S.op('dve', lambda e: e.tensor_copy(dd[:, 128:160], RANK[:, 1, :]), writes=['dd'])
                S.op('dve', lambda e: e.tensor_copy(dd[:, 160:192], ci[:]), writes=['dd'])
        S.barrier()

    def phase_scatter(tiles, NB):
        SLOTI = RT["SLOTI"]
        with contextlib.ExitStack() as es:
            def al(name, shape, dt):
                return es.enter_context(SBT(name, shape, dt))
            zt = al("zt", [128, D], BF16)
            vt = [al("svt%d" % j, [128, D], BF16) for j in range(3)]
            S.op('pool', lambda e: e.memset(zt[:], 0.0), writes=['zt'])
            for b in range(NB * NSUB):
                S.dma('sp', lambda e: e.dma_start(out=xs_d[b * 128:(b + 1) * 128, :], in_=zt[:]), reads=['zt'], writes=[('xsz', b)])
            S.barrier()
            for n, i in enumerate(tiles):
                j = n % 3
                S.dma('sp', lambda e: e.dma_start(out=vt[j][:], in_=v2_tm[i * 128:(i + 1) * 128, :]), writes=[('svt', j)])
                for k in range(4):
                    S.dma('pool', lambda e: e.indirect_dma_start(out=xs_d, out_offset=bass.IndirectOffsetOnAxis(ap=SLOTI[:, i, k:k + 1], axis=0),
                                                                 in_=vt[j][:], in_offset=None), reads=[('svt', j)], writes=[('xs', i, k)])
        S.barrier()

    def phase_moe(layer, NB):
        w1d = I("moe_w1_%d" % layer)
        w2d = I("moe_w2_%d" % layer)
        b1d = I("moe_b1_%d" % layer)
        b2d = I("moe_b2_%d" % layer)
        IDXI, BIDX = RT["IDXI"], RT["BIDX"]
        w1v = w1d.rearrange("a b c -> (a b) c")
        w2v = w2d.rearrange("a (q t) c -> (a q) (t c)", t=2)
        with contextlib.ExitStack() as es:
            def al(name, shape, dt):
                return es.enter_context(SBT(name, shape, dt))
            W1 = [al("W1_%d" % j, [128, 8, 2048], BF16) for j in range(2)]
            W2 = [al("W2_%d" % j, [128, 8, 1024], BF16) for j in range(2)]
            B1C = [al("B1C_%d" % j, [128, 16], F32) for j in range(2)]
            B2R = [al("B2R_%d" % j, [128, D], F32) for j in range(2)]
            XS = [al("XS_%d" % j, [128, D], BF16) for j in range(2 * NSUB)]
            XST2 = [al("XST%d" % j_, [128, 8, SB], BF16) for j_ in range(2)]
            ACTT = al("ACTT", [128, 8, SB], BF16)
            tg = [al("tg_%d" % j, [128, SB], F32) for j in range(2)]
            sg = [al("sg_%d" % j, [128, SB], F32) for j in range(2)]
            tu = [al("tu_%d" % j, [128, SB], F32) for j in range(2)]
            YT = [al("YT_%d" % j, [128, D], F32) for j in range(2)]
            ec = [0]
            xc = [0]
            yc = [0]
            bc8 = nc.gpsimd.to_reg(32 * 128 * 8 - 1)
            bc4 = nc.gpsimd.to_reg(32 * 128 * 4 - 1)

            def gathers(b):
                j = b % 2
                idx = bass.IndirectOffsetOnAxis(ap=IDXI[:, b:b + 1], axis=0)
                bidx = bass.IndirectOffsetOnAxis(ap=BIDX[:, b:b + 1], axis=0)
                for kc in range(8):
                    i8 = bass.IndirectOffsetOnAxis(ap=RT["IDX8"][:, b, kc:kc + 1], axis=0)
                    S.dma('pool', lambda e: e.indirect_dma_start(out=W1[j][:, kc, :], out_offset=None, in_=w1v, in_offset=i8, bounds_check=bc8, oob_is_err=False), writes=[('W1', j, kc)])
                for q in range(4):
                    i4 = bass.IndirectOffsetOnAxis(ap=RT["IDX4"][:, b, q:q + 1], axis=0)
                    S.dma('pool', lambda e: e.indirect_dma_start(out=W2[j][:, 2 * q:2 * q + 2, :].rearrange("p a b -> p (a b)"), out_offset=None,
                                                                 in_=w2v, in_offset=i4, bounds_check=bc4, oob_is_err=False), writes=[('W2', j, q)])
                S.dma('pool', lambda e: e.indirect_dma_start(out=B1C[j][:], out_offset=None, in_=b1d, in_offset=idx), writes=[('B1C', j)])
                S.dma('pool', lambda e: e.indirect_dma_start(out=B2R[j][:], out_offset=None, in_=b2d, in_offset=bidx), writes=[('B2R', j)])

            def xloads(b):
                for st_ in range(NSUB):
                    xj = (b % 2) * NSUB + st_
                    r0 = b * SB + st_ * 128
                    S.dma('sp', lambda e: e.dma_start(out=XS[xj][:], in_=xs_d[r0:r0 + 128, :]), writes=[('XS', xj)])

            def do_tr(b):
                j = b % 2
                for st_ in range(NSUB):
                    xj = (b % 2) * NSUB + st_
                    pbb = st_ % 2
                    for kc in range(8):
                        S.op('pe', lambda e: e.transpose(psb[pbb][:, kc * 128:(kc + 1) * 128], XS[xj][:, kc::8], identb[:]), reads=[('XS', xj)], writes=[PBB(pbb)])
                    S.op('act', lambda e: e.copy(XST2[b % 2][:, :, st_ * 128:(st_ + 1) * 128], psb[pbb][:].rearrange("p (a b) -> p a b", a=8)),
                         writes=[PBB(pbb), ('XST', b % 2, st_)])

            def do_mm1(b):
                j = b % 2
                xk = [('XST', b % 2, s_) for s_ in range(NSUB)]
                for jj in range(8):
                    t = ec[0] % 2
                    ec[0] += 1
                    for half in range(2):
                        bank = 2 * t + half
                        for kc in range(8):
                            S.op('pe', lambda e: e.matmul(psum[bank][:, 0:SB], W1[j][:, kc, half * 1024:(half + 1) * 1024][:, jj::8], XST2[b % 2][:, kc, :],
                                                          start=(kc == 0), stop=(kc == 7)), reads=[('W1', j, kc)] + xk, writes=[PB(bank)])
                    S.op('dve', lambda e: e.tensor_scalar(out=tg[t][:], in0=psum[2 * t][:, 0:SB], scalar1=B1C[j][:, jj:jj + 1], scalar2=7.0,
                                                          op0=ALU.add, op1=ALU.min), reads=[('B1C', j)], writes=[PB(2 * t), ('tg', t)])
                    S.op('dve', lambda e: e.tensor_scalar(out=tu[t][:], in0=psum[2 * t + 1][:, 0:SB], scalar1=B1C[j][:, 8 + jj:9 + jj], scalar2=7.0,
                                                          op0=ALU.add, op1=ALU.min), reads=[('B1C', j)], writes=[PB(2 * t + 1), ('tu', t)])
                    S.op('act', lambda e: e.activation(out=sg[t][:], in_=tg[t][:], func=AF.Sigmoid, scale=1.702), reads=[('tg', t)], writes=[('sg', t)])
                    S.op('dve', lambda e: e.tensor_scalar(out=tu[t][:], in0=tu[t][:], scalar1=-7.0, scalar2=1.0, op0=ALU.max, op1=ALU.add), writes=[('tu', t)])
                    S.op('dve', lambda e: e.tensor_tensor(out=tu[t][:], in0=tu[t][:], in1=tg[t][:], op=ALU.mult), reads=[('tg', t)], writes=[('tu', t)])
                    S.op('dve', lambda e: e.tensor_tensor(out=ACTT[:, jj, :], in0=tu[t][:], in1=sg[t][:], op=ALU.mult),
                         reads=[('tu', t), ('sg', t)], writes=[('ACTT', jj)])

            def do_mm2(b):
                j = b % 2
                ak = [('ACTT', jj) for jj in range(8)]
                for st_ in range(NSUB):
                    yj = yc[0] % 2
                    yc[0] += 1
                    for nb in range(2):
                        for jj in range(8):
                            S.op('pe', lambda e: e.matmul(psum[4 + nb][:], ACTT[:, jj, st_ * 128:(st_ + 1) * 128], W2[j][:, jj, nb * 512:(nb + 1) * 512],
                                                          start=(jj == 0), stop=(jj == 7)), reads=ak + [('W2', j, jj // 2)], writes=[PB(4 + nb)])
                        cs = slice(nb * 512, (nb + 1) * 512)
                        S.op('dve', lambda e: e.tensor_tensor(out=YT[yj][:, cs], in0=psum[4 + nb][:], in1=B2R[j][:, cs], op=ALU.add), reads=[('B2R', j)],
                             writes=[PB(4 + nb), ('YT', yj, nb)])
                    r0 = b * SB + st_ * 128
                    S.dma('sp', lambda e: e.dma_start(out=ys_d[r0:r0 + 128, :], in_=YT[yj][:]), reads=[('YT', yj, 0), ('YT', yj, 1)], writes=[('ys', b, st_)])

            gathers(0)
            xloads(0)
            do_tr(0)
            for b in range(NB):
                if b + 1 < NB:
                    gathers(b + 1)
                    xloads(b + 1)
                do_mm1(b)
                if b + 1 < NB:
                    do_tr(b + 1)
                do_mm2(b)
        S.barrier()

    def phase_combine(layer, tiles, final):
        WG, SLOTI = RT["WG"], RT["SLOTI"]
        with contextlib.ExitStack() as es:
            def al(name, shape, dt):
                return es.enter_context(SBT(name, shape, dt))
            G2 = al("G2", [128, 2, D], F32)
            FG = al("FG", [128, D], F32)
            xt = [al("cxt%d" % j, [128, D], F32) for j in range(2)]
            Y = [[al("cY%d_%d" % (j, k), [128, D], F32) for k in range(4)] for j in range(2)]
            acc = [al("cacc%d" % j, [128, D], F32) for j in range(2)]
            sq = al("csq", [128, D], F32)
            st = al("cst", [128, 8], F32)
            for r in range(2):
                bcast_row(G2[:, r, :], modrow[layer, r:r + 1, 5 * D:6 * D], ('G2', r))
            if final:
                bcast_row(FG[:], I("final_g")[0:1, :], 'FG')
            for n, i in enumerate(tiles):
                j = n % 2
                r = 1 if i < 2 else 0
                rows = slice(i * 128, (i + 1) * 128)
                S.dma('sp', lambda e: e.dma_start(out=xt[j][:], in_=xres[rows, :]), writes=[('cxt', j)])
                for k in range(4):
                    off = bass.IndirectOffsetOnAxis(ap=SLOTI[:, i, k:k + 1], axis=0)
                    S.dma('pool', lambda e: e.indirect_dma_start(out=Y[j][k][:], out_offset=None, in_=ys_d, in_offset=off), writes=[('cY', j, k)])
                S.op('dve', lambda e: e.tensor_scalar(out=acc[j][:], in0=Y[j][0][:], scalar1=WG[:, i, 0:1], scalar2=None, op0=ALU.mult),
                     reads=[('cY', j, 0)], writes=[('cacc', j)])
                for k in range(1, 4):
                    S.op('dve', lambda e: e.scalar_tensor_tensor(out=acc[j][:], in0=Y[j][k][:], scalar=WG[:, i, k:k + 1], in1=acc[j][:],
                                                                 op0=ALU.mult, op1=ALU.add), reads=[('cY', j, k)], writes=[('cacc', j)])
                S.op('dve', lambda e: e.tensor_tensor(out=acc[j][:], in0=acc[j][:], in1=G2[:, r, :], op=ALU.mult), reads=[('G2', r)], writes=[('cacc', j)])
                S.op('dve', lambda e: e.tensor_tensor(out=acc[j][:], in0=acc[j][:], in1=xt[j][:], op=ALU.add), reads=[('cxt', j)], writes=[('cacc', j)])
                if not final:
                    S.dma('act', lambda e: e.dma_start(out=xres[rows, :], in_=acc[j][:]), reads=[('cacc', j)], writes=[('xres', i)])
                else:
                    S.op('act', lambda e: e.activation(out=sq[:], in_=acc[j][:], func=AF.Square, accum_out=st[:, 0:1]), reads=[('cacc', j)], writes=['csq', 'cst'])
                    S.op('dve', lambda e: e.tensor_scalar(out=st[:, 1:2], in0=st[:, 0:1], scalar1=1.0 / D, scalar2=EPS, op0=ALU.mult, op1=ALU.add), writes=['cst'])
                    S.op('act', lambda e: e.activation(out=st[:, 2:3], in_=st[:, 1:2], func=AF.Sqrt), writes=['cst'])
                    S.op('dve', lambda e: e.reciprocal(st[:, 3:4], st[:, 2:3]), writes=['cst'])
                    S.op('dve', lambda e: e.scalar_tensor_tensor(out=acc[j][:], in0=acc[j][:], scalar=st[:, 3:4], in1=FG[:], op0=ALU.mult, op1=ALU.mult),
                         reads=['FG'], writes=[('cacc', j), 'cst'])
                    S.dma('act', lambda e: e.dma_start(out=out_d[(i - 2) * 128:(i - 1) * 128, :], in_=acc[j][:]), reads=[('cacc', j)], writes=[('out', i)])
        S.barrier()

    qT_d = dscr("qT_d", [D, 4096], BF16)
    kT_d = dscr("kT_d", [256, T], BF16)
    vA_d = dscr("vA_d", [T, 256], BF16)
    oT_d = dscr("oT_d", [D, 4096], BF16)

    def phase_A1(uT):
        wv = I("od_w_in").rearrange("(kc p) n -> p kc n", p=128)
        with contextlib.ExitStack() as es:
            def al(name, shape, dt):
                return es.enter_context(SBT(name, shape, dt))
            W = al("aW", [128, 8, 1536], BF16)
            GQ = al("aGQ", [128, 128], F32)
            GK = al("aGK", [128, 128], F32)
            COS = [al("aCOS%d" % j, [128, 128], F32) for j in range(2)]
            SIN = [al("aSIN%d" % j, [128, 128], F32) for j in range(2)]
            xf = [al("axf%d" % j, [128, 1536], F32) for j in range(2)]
            sq2 = [al("asq%d" % j_, [128, 1280], F32) for j_ in range(2)]
            st2 = [al("ast%d" % j_, [128, 40], F32) for j_ in range(2)]
            kn2 = [al("akn%d" % j_, [128, 1280], F32) for j_ in range(2)]
            t12 = [al("at1%d" % j_, [128, 1280], F32) for j_ in range(2)]
            t22 = [al("at2%d" % j_, [128, 1280], F32) for j_ in range(2)]
            kr = [al("akr%d" % j, [128, 1280], BF16) for j in range(2)]
            vb = [al("avb%d" % j, [128, 256], BF16) for j in range(2)]
            xT = [al("axT%d" % j, [128, 10, 128], BF16) for j in range(2)]
            for blk in range(3):
                S.dma('pool', lambda e: e.dma_start(out=W[:, :, blk * 512:(blk + 1) * 512], in_=wv[:, :, blk * 512:(blk + 1) * 512]), writes=[('aW', blk)])
            bcast_row(GQ[:], I("od_qk_g")[0:1, :], 'aGQ')
            bcast_row(GK[:], I("od_qk_g")[1:2, :], 'aGK')
            def hdrvars(i):
                lat = i >= 2
                j = i % 2
                rows = slice(i * 128, (i + 1) * 128)
                blks = [0, 1, 2] if lat else [2]
                h0 = 0 if lat else 8
                sq, st, kn, t1, t2 = sq2[j], st2[j], kn2[j], t12[j], t22[j]
                KSQ, KST = ('asq', j), ('ast', j)
                return lat, j, rows, blks, h0, sq, st, kn, t1, t2, KSQ, KST

            def stageA(i):
                lat, j, rows, blks, h0, sq, st, kn, t1, t2, KSQ, KST = hdrvars(i)
                if lat:
                    tr = slice((i - 2) * 128, (i - 1) * 128)
                    S.dma('pool', lambda e: e.dma_start(out=COS[j][:], in_=I("rope_cos")[tr, :]), writes=[('aCOS', j)])
                    S.dma('pool', lambda e: e.dma_start(out=SIN[j][:], in_=I("rope_sin")[tr, :]), writes=[('aSIN', j)])
                for blk in blks:
                    pb = blk
                    for kc in range(8):
                        S.op('pe', lambda e: e.matmul(psum[pb][:], uT[:, kc, rows], W[:, kc, blk * 512:(blk + 1) * 512], start=(kc == 0), stop=(kc == 7)),
                             reads=[('aW', blk)], writes=[PB(pb)])
                    S.op('act', lambda e: e.copy(xf[j][:, blk * 512:(blk + 1) * 512], psum[pb][:]), writes=[PB(pb), ('axf', j, blk)])
                S.op('pool', lambda e: e.tensor_copy(vb[j][:], xf[j][:, 1280:1536]), reads=[('axf', j, 2)], writes=[('avb', j)])
                S.dma('sp', lambda e: e.dma_start(out=vA_d[rows, :], in_=vb[j][:]), reads=[('avb', j)], writes=[('vA', i)])

            def stageB(i):
                lat, j, rows, blks, h0, sq, st, kn, t1, t2, KSQ, KST = hdrvars(i)
                c0 = h0 * 128
                nh = 10 - h0
                S.op('act', lambda e: e.activation(out=sq[:, c0:1280], in_=xf[j][:, c0:1280], func=AF.Square),
                     reads=[('axf', j, 0), ('axf', j, 1), ('axf', j, 2)], writes=[KSQ])
                S.op('dve', lambda e: e.tensor_reduce(out=st[:, h0:10], in_=sq[:, c0:1280].rearrange("p (h d) -> p h d", d=128), axis=AX.X, op=ALU.add),
                     reads=[KSQ], writes=[KST])
                S.op('dve', lambda e: e.tensor_scalar(out=st[:, 10 + h0:20], in0=st[:, h0:10], scalar1=1.0 / 128, scalar2=EPS, op0=ALU.mult, op1=ALU.add), writes=[KST])
                S.op('act', lambda e: e.activation(out=st[:, 20 + h0:30], in_=st[:, 10 + h0:20], func=AF.Sqrt), writes=[KST])
                S.op('dve', lambda e: e.reciprocal(st[:, 30 + h0:40], st[:, 20 + h0:30]), writes=[KST])
                for h in range(h0, 10):
                    hs = slice(h * 128, (h + 1) * 128)
                    G = GQ if h < 8 else GK
                    dst = kn[:, hs] if lat else kr[j][:, hs]
                    dk = ('akn', j, h) if lat else ('akr', j, h)
                    S.op('dve', lambda e: e.scalar_tensor_tensor(out=dst, in0=xf[j][:, hs], scalar=st[:, 30 + h:31 + h], in1=G[:], op0=ALU.mult, op1=ALU.mult),
                         reads=['aGQ', 'aGK', KST], writes=[dk])
                if lat:
                    snv = SIN[j][:].rearrange("p (b c) -> p b c", b=2)
                    for h in range(h0, 10):
                        hs = slice(h * 128, (h + 1) * 128)
                        knv = kn[:, hs].rearrange("p (b c) -> p b c", b=2)
                        t2v = t2[:, hs].rearrange("p (b c) -> p b c", b=2)
                        S.op('dve', lambda e: e.tensor_tensor(out=t1[:, hs], in0=kn[:, hs], in1=COS[j][:], op=ALU.mult), reads=[('akn', j, h), ('aCOS', j)],
                             writes=[('at1', j, h)])
                        S.op('pool', lambda e: e.tensor_tensor(out=t2v[:, :, 0:32], in0=knv[:, :, 32:64], in1=snv[:, :, 0:32], op=ALU.mult),
                             reads=[('akn', j, h), ('aSIN', j)], writes=[('at2', j, h)])
                        S.op('pool', lambda e: e.tensor_tensor(out=t2v[:, :, 32:64], in0=knv[:, :, 0:32], in1=snv[:, :, 32:64], op=ALU.mult),
                             reads=[('akn', j, h), ('aSIN', j)], writes=[('at2', j, h)])
                    for h in range(h0, 10):
                        hs = slice(h * 128, (h + 1) * 128)
                        S.op('dve', lambda e: e.tensor_tensor(out=kr[j][:, hs], in0=t1[:, hs], in1=t2[:, hs], op=ALU.add), reads=[('at1', j, h), ('at2', j, h)],
                             writes=[('akr', j, h)])

            def stageC(i):
                lat, j, rows, blks, h0, sq, st, kn, t1, t2, KSQ, KST = hdrvars(i)
                if lat:
                    for h in range(8):
                        S.op('pe', lambda e: e.transpose(psb[0][:, h * 128:(h + 1) * 128], kr[j][:, h * 128:(h + 1) * 128], identb[:]),
                             reads=[('akr', j, h)], writes=[PBB(0)])
                    S.op('act', lambda e: e.copy(xT[j][:, 0:8, :].rearrange("p a b -> p (a b)"), psb[0][:]), writes=[PBB(0), ('axT', j, 0)])
                    S.dma('sp', lambda e: e.dma_start(out=qT_d[:, (i - 2) * 128:(i - 1) * 128].rearrange("(h d) t -> d h t", d=128), in_=xT[j][:, 0:8, :]),
                          reads=[('axT', j, 0)], writes=[('qT_d', i)])
                for h in range(8, 10):
                    S.op('pe', lambda e: e.transpose(psb[1][:, (h - 8) * 128:(h - 7) * 128], kr[j][:, h * 128:(h + 1) * 128], identb[:]),
                         reads=[('akr', j, h)], writes=[PBB(1)])
                S.op('act', lambda e: e.copy(xT[j][:, 8:10, :].rearrange("p a b -> p (a b)"), psb[1][:, 0:256]), writes=[PBB(1), ('axT', j, 1)])
                S.dma('sp', lambda e: e.dma_start(out=kT_d[:, rows].rearrange("(h d) t -> d h t", d=128), in_=xT[j][:, 8:10, :]),
                      reads=[('axT', j, 1)], writes=[('kT_d', i)])

            stageA(0)
            for i in range(NT):
                if i + 1 < NT:
                    stageA(i + 1)
                stageB(i)
                stageC(i)
        S.barrier()

    def phase_A2():
        scale = 128.0 ** -0.5
        with contextlib.ExitStack() as es:
            def al(name, shape, dt):
                return es.enter_context(SBT(name, shape, dt))
            KT = al("bKT", [128, T], BF16)
            V = al("bV", [128, NT, 128], BF16)
            onesb = al("bones", [128, 128], BF16)
            QT4 = [al("bQT%d" % j, [128, 4, 128], BF16) for j in range(2)]
            PT = [al("bPT%d" % j, [128, 512], BF16) for j in range(4)]
            rs = [al("brs%d" % j, [128, 512], F32) for j in range(2)]
            ot = [al("bot%d" % j, [128, 4, 128], BF16) for j in range(2)]
            S.op('pool', lambda e: e.memset(onesb[:], 1.0), writes=['bones'])
            pc = 0
            it = 0
            for g in range(2):
                S.dma('sp', lambda e: e.dma_start(out=KT[:], in_=kT_d[g * 128:(g + 1) * 128, :]), writes=['bKT'])
                S.dma('sp', lambda e: e.dma_start(out=V[:], in_=vA_d[:, g * 128:(g + 1) * 128].rearrange("(n p) d -> p n d", p=128)), writes=['bV'])
                for qi in range(32):
                    j = it % 2
                    it += 1
                    bo, bs_ = 2 + 2 * j, 3 + 2 * j
                    S.dma('sp', lambda e: e.dma_start(out=QT4[j][:], in_=qT_d[g * 512:(g + 1) * 512, qi * 128:(qi + 1) * 128].rearrange("(h d) t -> d h t", d=128)),
                          writes=[('bQT', j)])

                    def st_mm(kt):
                        bank = kt % 2
                        S.op('pe', lambda e: e.matmul(psum[bank][:], KT[:, kt * 128:(kt + 1) * 128], QT4[j][:].rearrange("p a b -> p (a b)"), start=True, stop=True),
                             reads=['bKT', ('bQT', j)], writes=[PB(bank)])
                    st_mm(0)
                    for kt in range(NT):
                        bank = kt % 2
                        p_ = pc % 4
                        pc += 1
                        S.op('act', lambda e: e.activation(out=PT[p_][:], in_=psum[bank][:], func=AF.Exp, scale=scale), writes=[PB(bank), ('bPT', p_)])
                        if kt + 1 < NT:
                            st_mm(kt + 1)
                        S.op('pe', lambda e: e.matmul(psum[bo][:], V[:, kt, :], PT[p_][:], start=(kt == 0), stop=(kt == NT - 1)),
                             reads=[('bPT', p_), 'bV'], writes=[PB(bo)])
                        S.op('pe', lambda e: e.matmul(psum[bs_][:], onesb[:], PT[p_][:], start=(kt == 0), stop=(kt == NT - 1)),
                             reads=[('bPT', p_), 'bones'], writes=[PB(bs_)])
                    S.op('dve', lambda e: e.reciprocal(rs[j][:], psum[bs_][:]), writes=[PB(bs_), ('brs', j)])
                    S.op('dve', lambda e: e.tensor_tensor(out=ot[j][:].rearrange("p a b -> p (a b)"), in0=psum[bo][:], in1=rs[j][:], op=ALU.mult),
                         reads=[('brs', j)], writes=[PB(bo), ('bot', j)])
                    S.dma('pool', lambda e: e.dma_start(out=oT_d[g * 512:(g + 1) * 512, qi * 128:(qi + 1) * 128].rearrange("(h d) t -> d h t", d=128), in_=ot[j][:]),
                          reads=[('bot', j)], writes=[('oT_d', g, qi)])
        S.barrier()

    def phase_A3():
        with contextlib.ExitStack() as es:
            def al(name, shape, dt):
                return es.enter_context(SBT(name, shape, dt))
            WO = al("cWO", [128, 8, D], BF16)
            G1 = al("cG1", [128, D], F32)
            OT = [al("cOT%d" % j, [128, 8, 128], BF16) for j in range(2)]
            xt = [al("cx%d" % j, [128, D], F32) for j in range(2)]
            tmp = al("ctmp", [128, D], F32)
            S.dma('pool', lambda e: e.dma_start(out=WO[:], in_=I("od_w_out").rearrange("(kc p) n -> p kc n", p=128)), writes=['cWO'])
            bcast_row(G1[:], modrow[1, 0:1, 2 * D:3 * D], 'cG1')
            for qi in range(32):
                j = qi % 2
                rows = slice((qi + 2) * 128, (qi + 3) * 128)
                S.dma('sp', lambda e: e.dma_start(out=OT[j][:], in_=oT_d[:, qi * 128:(qi + 1) * 128].rearrange("(h d) t -> d h t", d=128)), writes=[('cOT', j)])
                S.dma('sp', lambda e: e.dma_start(out=xt[j][:], in_=xres[rows, :]), writes=[('cx', j)])
                for nb in range(2):
                    pb = (2 * qi + nb) % 4
                    for kc in range(8):
                        S.op('pe', lambda e: e.matmul(psum[pb][:], OT[j][:, kc, :], WO[:, kc, nb * 512:(nb + 1) * 512], start=(kc == 0), stop=(kc == 7)),
                             reads=[('cOT', j), 'cWO'], writes=[PB(pb)])
                    cs = slice(nb * 512, (nb + 1) * 512)
                    S.op('dve', lambda e: e.tensor_tensor(out=tmp[:, cs], in0=psum[pb][:], in1=G1[:, cs], op=ALU.mult), reads=['cG1'], writes=[PB(pb), ('ctmp', nb)])
                    S.op('pool', lambda e: e.tensor_tensor(out=tmp[:, cs], in0=tmp[:, cs], in1=xt[j][:, cs], op=ALU.add), reads=[('cx', j)], writes=[('ctmp', nb)])
                S.dma('pool', lambda e: e.dma_start(out=xres[rows, :], in_=tmp[:]), reads=[('ctmp', 0), ('ctmp', 1)], writes=[('xres', qi)])
        S.barrier()

    def small_dump():
        W_ = NT * 32 + NT * 4 + NT * 4 + 2 * NBMAX
        dl = dscr("dsmall_scr", [128, W_], F32)
        with SBT("dsm", [128, W_], F32) as dsm:
            o = 0
            for nm, n in (("LOG", NT * 32), ("WG", NT * 4), ("SLOTI", NT * 4)):
                S.op('dve', lambda e: e.tensor_copy(dsm[:, o:o + n], RT[nm][:].rearrange("p a b -> p (a b)")), writes=['dsm'])
                o += n
            for nm in ("IDXI", "BIDX"):
                S.op('dve', lambda e: e.tensor_copy(dsm[:, o:o + NBMAX], RT[nm][:]), writes=['dsm'])
                o += NBMAX
            S.dma('sp', lambda e: e.dma_start(out=dl, in_=dsm[:]), reads=['dsm'], writes=['dl'])
            S.barrier()
        return ('small', dl, [128, W_], F32)

    def copy_xin_to_xres():
        with SBT("cpx", [128, D], F32) as cpx:
            for i in range(NT):
                S.dma('sp', lambda e: e.dma_start(out=cpx[:], in_=I("xin")[i * 128:(i + 1) * 128, :]), writes=['cpx'])
                S.dma('sp', lambda e: e.dma_start(out=xres[i * 128:(i + 1) * 128, :], in_=cpx[:]), reads=['cpx'], writes=[('xres', i)])
        S.barrier()

    phase_mod()
    if stop_after == 'mod':
        return finish_dbg([('modrow', modrow.rearrange("a b c -> (a b) c"), [4, 6 * D], F32)])

    if stop_after not in ('A_only', 'M1_only'):
        uT_guard = SBT("uT", [128, 8, T], BF16)
        uT = uT_guard.__enter__()
        phase_norm_T(0, I("xin"), uT)
        phase_E1(uT)
        if stop_after == 'E1':
            return finish_dbg([('qkT', qkT, [2048, T], BF16), ('v_tm', v_tm, [T, D], BF16), ('so_tm', so_tm, [T, D], BF16),
                               ('g_tm', g_tm, [T, 16], F32), ('xrT', xrT, [D, T], F32), ('ygT', ygT, [D, T], BF16)])
        uT_guard.__exit__(None, None, None)
        phase_E2()
        if stop_after == 'E2':
            return finish_dbg([('hlT', hlT, [D, T], BF16)])
        phase_E3()
        if stop_after == 'E3':
            return finish_dbg([('hm_f', hm_d[0], [T, D], F32), ('hm_b', hm_d[1], [T, D], F32)])
        phase_E4()
        if stop_after == 'E4':
            return finish_dbg([('xres', xres, [T, D], F32)])
        NB0 = (4 * T) // SB + 32
        phase_N2(0, range(NT), RT["LOG"])
        phase_route(range(NT), NB0)
        phase_scatter(range(NT), NB0)
        if stop_after == 'R0':
            return finish_dbg([small_dump(), ('xs', xs_d, [NBMAX * SB, D], BF16), ('v2', v2_tm, [T, D], BF16)])
        phase_moe(0, NB0)
        phase_combine(0, range(NT), False)
        if stop_after == 'M0':
            return finish_dbg([('xres', xres, [T, D], F32)])
    else:
        copy_xin_to_xres()

    if stop_after != 'M1_only':
        uT_guard = SBT("uT", [128, 8, T], BF16)
        uT = uT_guard.__enter__()
        phase_norm_T(1, xres, uT)
        phase_A1(uT)
        uT_guard.__exit__(None, None, None)
        phase_A2()
        phase_A3()
        if stop_after in ('A', 'A_only'):
            return finish_dbg([('xres', xres, [T, D], F32)])
    NB1 = (4 * 4096) // SB + 32
    lat_tiles = range(2, NT)
    phase_N2(1, lat_tiles, RT["LOG"])
    phase_route(lat_tiles, NB1)
    phase_scatter(lat_tiles, NB1)
    phase_moe(1, NB1)
    phase_combine(1, lat_tiles, True)
    if stop_after == 'M1_only':
        return finish_dbg([('outc', out_d, [4096, D], F32)])
    S.barrier()
    return nc, dbg_out, used_inputs


def host_inputs(b, inp, names=None):
    f = lambda a: np.ascontiguousarray(a, dtype=np.float32)
    want = lambda k: names is None or k in names
    m = {}
    if want("xin"):
        m["xin"] = f(np.concatenate([inp["ctx"][b], inp["x"][b]], axis=0))
    if want("ccols"):
        m["ccols"] = f(np.concatenate([inp["c"][b].reshape(8, 128).T, inp["c_ctx"].reshape(8, 128).T], axis=1))
    for k in ["mod_w", "mod_b", "norm1_g", "norm2_g", "moe_w_r", "moe_b_r"]:
        if want(k):
            m[k] = f(inp[k])
    if want("final_g"):
        m["final_g"] = f(inp["final_g"].reshape(1, D))
    if want("ev_w_in"):
        m["ev_w_in"] = f(inp["ev_w_in"][0])
    if want("ev_qkcw"):
        qk = np.concatenate([inp["ev_qk_conv_w"][0], inp["ev_qk_conv_b"][0][None]], axis=0)
        m["ev_qkcw"] = f(qk.T.reshape(16, 128, 5).transpose(1, 0, 2))
    if want("ev_gate_b"):
        m["ev_gate_b"] = f(inp["ev_gate_b"][0].reshape(1, 16))
    if want("ev_mnorm_g"):
        m["ev_mnorm_g"] = f(inp["ev_mnorm_g"][0].reshape(1, D))
    if want("ev_lrucw"):
        lc = np.concatenate([inp["ev_lru_conv_w"][0], inp["ev_lru_conv_b"][0][None]], axis=0)
        m["ev_lrucw"] = f(lc.T.reshape(8, 128, 5).transpose(1, 0, 2))
    if want("ev_lru_w") or want("ev_lru_b"):
        lw = np.zeros((4, 8, 128, 128), np.float32)
        lb = np.zeros((128, 8, 4), np.float32)
        for z in range(2):
            for gi, (wk, bk) in enumerate([("ev_lru_wa", "ev_lru_ba"), ("ev_lru_wx", "ev_lru_bx")]):
                for cc_ in range(8):
                    for h in range(2):
                        n = cc_ * 2 + h
                        lw[z * 2 + gi, cc_, h * 64:(h + 1) * 64, h * 64:(h + 1) * 64] = inp[wk][0, z, n]
                        lb[h * 64:(h + 1) * 64, cc_, z * 2 + gi] = inp[bk][0, z, n]
        m["ev_lru_w"] = lw
        m["ev_lru_b"] = lb
    if want("ev_lru_lam"):
        m["ev_lru_lam"] = f(inp["ev_lru_lam"][0].reshape(2, 8, 128).transpose(2, 1, 0))
    if want("ev_w_out"):
        m["ev_w_out"] = f(inp["ev_w_out"][0])
    if want("od_w_in"):
        m["od_w_in"] = f(inp["od_w_in"][0])
    if want("od_qk_g"):
        m["od_qk_g"] = f(np.stack([inp["od_q_norm_g"][0], inp["od_k_norm_g"][0]]))
    if want("od_w_out"):
        m["od_w_out"] = f(inp["od_w_out"][0])
    if want("rope_cos") or want("rope_sin"):
        pos = np.arange(4096)
        row = (pos // 64).astype(np.float32)
        col = (pos % 64).astype(np.float32)
        inv = (10000.0 ** (-np.arange(0, 64, 2, dtype=np.float32) / 64)).astype(np.float32)
        ar = row[:, None] * inv[None]
        ac = col[:, None] * inv[None]
        m["rope_cos"] = f(np.concatenate([np.cos(ar), np.cos(ar), np.cos(ac), np.cos(ac)], axis=1))
        m["rope_sin"] = f(np.concatenate([-np.sin(ar), np.sin(ar), -np.sin(ac), np.sin(ac)], axis=1))
    for l in range(2):
        if want("moe_w1_%d" % l):
            m["moe_w1_%d" % l] = f(inp["moe_w1"][l]).reshape(32 * 128, 8, 2048)
        if want("moe_w2_%d" % l):
            m["moe_w2_%d" % l] = f(inp["moe_w2"][l]).reshape(32 * 128, 8, 1024)
    for l in range(2):
        if want("moe_b1_%d" % l):
            m["moe_b1_%d" % l] = f(inp["moe_b1"][l].reshape(32, 2, 128, 8).transpose(0, 2, 1, 3).reshape(32 * 128, 16))
        if want("moe_b2_%d" % l):
            m["moe_b2_%d" % l] = f(inp["moe_b2"][l])
    if names is not None:
        m = {k: v for k, v in m.items() if k in names}
    return m


_CACHE = {}


def kernel(**inputs):
    if 'nc' not in _CACHE:
        _CACHE['nc'] = build()
    nc, _, used = _CACHE['nc']
    names = set(used.keys())
    in_maps = [host_inputs(b, inputs, names) for b in range(8)]
    res = run_bass_kernel_spmd(nc, in_maps, core_ids=list(range(8)))
    return np.stack([r["out"] for r in res.results], axis=0).astype(np.float32)
```

```python
import contextlib
import numpy as np
import concourse.bass as bass
import concourse.mybir as mybir
from concourse.bass_utils import run_bass_kernel_spmd

F32 = mybir.dt.float32
BF16 = mybir.dt.bfloat16
I32 = mybir.dt.int32
U32 = mybir.dt.uint32
ALU = mybir.AluOpType
AF = mybir.ActivationFunctionType

T = 4352
NT = 34
D = 1024
NCTX = 256
EPS = 1e-6


class Sched:
    NSLOT = 6

    def __init__(self, nc):
        self.nc = nc
        self.engs = {'pe': nc.tensor, 'act': nc.scalar, 'dve': nc.vector,
                     'pool': nc.gpsimd, 'sp': nc.sync}
        self.sem = {}
        self.cnt = {}
        for k in self.engs:
            self.sem[k] = nc.alloc_semaphore('s_' + k)
            self.cnt[k] = 0
        self.dslots = {}
        self.dcnt = {}
        self.dnext = {}
        self.nslot = {'sp': 8, 'pool': 8, 'act': 2}
        for q in ('sp', 'pool', 'act'):
            self.dslots[q] = [nc.alloc_semaphore('d_%s%d' % (q, i)) for i in range(self.nslot[q])]
            self.dcnt[q] = [0] * self.nslot[q]
            self.dnext[q] = 0
        self.waited = {k: {} for k in self.engs}
        self.res = {}

    def _semof(self, tok):
        if tok[0] == 'e':
            return self.sem[tok[1]]
        return self.dslots[tok[1][0]][tok[1][1]]

    def _wait(self, e, tok):
        key = (tok[0], tok[1])
        if self.waited[e].get(key, 0) >= tok[2]:
            return
        self.engs[e].wait_ge(self._semof(tok), tok[2])
        self.waited[e][key] = tok[2]

    def _deps(self, e, reads, writes):
        deps = []
        for r in reads:
            st = self.res.get(r)
            if st and st['w'] is not None:
                deps.append(st['w'])
        for w in writes:
            st = self.res.get(w)
            if st:
                if st['w'] is not None:
                    deps.append(st['w'])
                deps.extend(st['r'].values())
        for tok in deps:
            if e == 'pe' and tok[0] == 'e' and tok[1] == 'pe':
                continue
            self._wait(e, tok)

    def _commit(self, tok, reads, writes):
        for r in reads:
            st = self.res.setdefault(r, {'w': None, 'r': {}})
            st['r'][(tok[0], tok[1])] = tok
        for w in writes:
            self.res[w] = {'w': tok, 'r': {}}

    def op(self, e, fn, reads=(), writes=()):
        self._deps(e, reads, writes)
        inst = fn(self.engs[e])
        self.cnt[e] += 1
        inst.then_inc(self.sem[e], 1)
        self._commit(('e', e, self.cnt[e]), reads, writes)
        return inst

    def dma(self, q, fn, reads=(), writes=()):
        s = self.dnext[q]
        self.dnext[q] = (s + 1) % self.nslot[q]
        if self.dcnt[q][s] > 0:
            self._wait(q, ('d', (q, s), 16 * self.dcnt[q][s]))
        self._deps(q, reads, writes)
        inst = fn(self.engs[q])
        self.dcnt[q][s] += 1
        inst.then_inc(self.dslots[q][s], 16)
        self._commit(('d', (q, s), 16 * self.dcnt[q][s]), reads, writes)
        return inst

    def barrier(self):
        toks = []
        for q in self.dslots:
            for s in range(self.nslot[q]):
                if self.dcnt[q][s] > 0:
                    toks.append(('d', (q, s), 16 * self.dcnt[q][s]))
        for k in self.engs:
            if self.cnt[k] > 0:
                toks.append(('e', k, self.cnt[k]))
        for e in self.engs:
            for tok in toks:
                self._wait(e, tok)
        self.res = {}


class Rot:
    def __init__(self, bufs, name):
        self.bufs = bufs
        self.name = name
        self.i = 0

    def next(self):
        b = self.bufs[self.i % len(self.bufs)]
        k = (self.name, self.i % len(self.bufs))
        self.i += 1
        return b, k


AX = mybir.AxisListType


def build(stop_after=None, layers=(0, 1)):
    nc = bass.Bass("TRN2", target_bir_lowering=False)
    S = Sched(nc)
    used_inputs = {}
    _ctr = [0]

    def SBT(name, shape, dt):
        _ctr[0] += 1
        return nc.sbuf_tensor("%s_u%d" % (name, _ctr[0]), shape, dt)

    IN_SPECS = {
        "xin": [T, D], "ccols": [128, 16], "mod_w": [2, D, 6 * D], "mod_b": [2, 6 * D],
        "norm1_g": [2, D], "norm2_g": [2, D], "final_g": [1, D],
        "ev_w_in": [D, 6160], "ev_qkcw": [128, 16, 5], "ev_gate_b": [1, 16], "ev_mnorm_g": [1, D],
        "ev_lrucw": [128, 8, 5], "ev_lru_w": [4, 8, 128, 128], "ev_lru_b": [128, 8, 4], "ev_lru_lam": [128, 8, 2],
        "ev_w_out": [2 * D, D], "od_w_in": [D, 1536], "od_qk_g": [2, 128], "od_w_out": [D, D],
        "rope_cos": [4096, 128], "rope_sin": [4096, 128],
        "moe_w_r": [2, D, 32], "moe_b_r": [2, 32],
        "moe_w1_0": [32 * 128, 8, 2048], "moe_w1_1": [32 * 128, 8, 2048],
        "moe_b1_0": [32 * 128, 16], "moe_b1_1": [32 * 128, 16],
        "moe_w2_0": [32 * 128, 8, 1024], "moe_w2_1": [32 * 128, 8, 1024],
        "moe_b2_0": [32, 1024], "moe_b2_1": [32, 1024],
    }

    def I(name):
        if name not in used_inputs:
            used_inputs[name] = nc.dram_tensor(name, list(IN_SPECS[name]), F32, kind="ExternalInput").ap()
        return used_inputs[name]

    def dscr(name, shape, dt=F32):
        return nc.dram_tensor(name, list(shape), dt, kind="Internal").ap()

    dbg_out = {}

    def dbgt(name, shape, dt=F32):
        dbg_out[name] = nc.dram_tensor("dbg_" + name, list(shape), dt, kind="ExternalOutput").ap()
        return dbg_out[name]

    def finish_dbg(pairs):
        S.barrier()
        for name, src, shape, dt in pairs:
            d = dbgt(name, shape, dt)
            rows, cols = shape[0], int(np.prod(shape[1:]))
            s2 = src if len(shape) == 2 else src.rearrange("a b c -> a (b c)")
            d2 = d if len(shape) == 2 else d.rearrange("a b c -> a (b c)")
            with SBT("dbgbuf_" + name, [128, cols], dt) as buf:
                for r0 in range(0, rows, 128):
                    n = min(128, rows - r0)
                    S.dma('sp', lambda e: e.dma_start(out=buf[0:n, :], in_=s2[r0:r0 + n, :]), writes=['dbgbuf'])
                    S.dma('sp', lambda e: e.dma_start(out=d2[r0:r0 + n, :], in_=buf[0:n, :]), reads=['dbgbuf'], writes=[('dbgo', name, r0)])
                S.barrier()
        return nc, dbg_out, used_inputs

    out_d = nc.dram_tensor("out", [4096, D], F32, kind="ExternalOutput").ap()
    modrow = dscr("modrow", [2, 2, 6 * D])
    xres = dscr("xres", [T, D])

    ident = nc.alloc_sbuf_tensor("ident", [128, 128], F32)
    identb = nc.alloc_sbuf_tensor("identb", [128, 128], BF16)
    ones1 = nc.alloc_sbuf_tensor("ones1", [1, 128], F32)
    onesm = nc.alloc_sbuf_tensor("onesm", [128, 128], F32)
    triF = nc.alloc_sbuf_tensor("triF", [128, 128], F32)
    triB = nc.alloc_sbuf_tensor("triB", [128, 128], F32)
    triS = nc.alloc_sbuf_tensor("triS", [128, 128], F32)
    selF = nc.alloc_sbuf_tensor("selF", [128, 128], F32)
    selB = nc.alloc_sbuf_tensor("selB", [128, 128], F32)
    pidx = nc.alloc_sbuf_tensor("pidx", [128, 1], F32)
    pidx_i = nc.alloc_sbuf_tensor("pidx_i", [128, 1], I32)
    S.op('pool', lambda e: e.memset(ident[:], 0.0), writes=['ident'])
    S.op('pool', lambda e: e.affine_select(ident[:], ident[:], pattern=[[-1, 128]], compare_op=ALU.not_equal,
                                          fill=1.0, base=0, channel_multiplier=1), reads=['ident'], writes=['ident'])
    S.op('dve', lambda e: e.tensor_copy(identb[:], ident[:]), reads=['ident'], writes=['identb'])
    S.op('pool', lambda e: e.memset(ones1[:], 1.0), writes=['ones1'])
    S.op('pool', lambda e: e.memset(onesm[:], 1.0), writes=['onesm'])

    def mk_mask(t, pattern, cm, base, cmp):
        S.op('pool', lambda e: e.memset(t[:], 1.0), writes=[t.name])
        S.op('pool', lambda e: e.affine_select(t[:], t[:], pattern=pattern, compare_op=cmp, fill=0.0, base=base,
                                              channel_multiplier=cm), writes=[t.name])

    mk_mask(triF, [[1, 128]], -1, 0, ALU.is_ge)
    mk_mask(triB, [[-1, 128]], 1, 0, ALU.is_ge)
    mk_mask(triS, [[1, 128]], -1, 0, ALU.is_gt)
    mk_mask(selF, [[0, 128]], 1, -127, ALU.is_equal)
    mk_mask(selB, [[0, 128]], 1, 0, ALU.is_equal)
    S.op('pool', lambda e: e.iota(pidx_i[:], pattern=[[0, 1]], base=0, channel_multiplier=1), writes=['pidx_i'])
    S.op('dve', lambda e: e.tensor_copy(pidx[:], pidx_i[:]), reads=['pidx_i'], writes=['pidx'])

    psum = [nc.alloc_psum_tensor("ps%d" % i, [128, 512], F32) for i in range(6)]
    psb = [nc.alloc_psum_tensor("psb%d" % i, [128, 1024], BF16) for i in range(2)]

    def PB(i):
        return ('psum', i)

    def PBB(i):
        return ('psb', i)

    def bcast_row(dst, src_row, key, q='sp'):
        S.dma(q, lambda e: e.dma_start(out=dst, in_=src_row.partition_broadcast(dst.shape[0])), writes=[key])

    def phase_mod():
        mod_w, mod_b = I("mod_w"), I("mod_b")
        with SBT("cc", [128, 16], F32) as cc, SBT("csil", [128, 16], BF16) as csil, \
                SBT("mw0", [128, 8, 512], BF16) as mw0, SBT("mw1", [128, 8, 512], BF16) as mw1, SBT("mw2", [128, 8, 512], BF16) as mw2, \
                SBT("mb0", [1, 512], F32) as mb0, SBT("mb1", [1, 512], F32) as mb1, \
                SBT("mo0", [1, 512], F32) as mo0, SBT("mo1", [1, 512], F32) as mo1:
            S.dma('sp', lambda e: e.dma_start(out=cc[:], in_=I("ccols")), writes=['cc'])
            S.op('act', lambda e: e.activation(out=csil[:], in_=cc[:], func=AF.Silu), reads=['cc'], writes=['csil'])
            mws = Rot([mw0, mw1, mw2], 'mw')
            mbs = Rot([mb0, mb1], 'mb')
            mos = Rot([mo0, mo1], 'mo')
            pi = 0
            for layer in range(2):
                mwv = mod_w[layer].rearrange("(kc p) n -> p kc n", p=128)
                for cb in range(12):
                    mw, mwk = mws.next()
                    mb, mbk = mbs.next()
                    S.dma('pool', lambda e: e.dma_start(out=mw[:], in_=mwv[:, :, cb * 512:(cb + 1) * 512]), writes=[mwk])
                    S.dma('sp', lambda e: e.dma_start(out=mb[:], in_=mod_b[layer:layer + 1, cb * 512:(cb + 1) * 512]), writes=[mbk])
                    for r in range(2):
                        ps = psum[pi % 4]
                        pk = PB(pi % 4)
                        pi += 1
                        for kc in range(8):
                            S.op('pe', lambda e: e.matmul(ps[0:1, :], csil[:, r * 8 + kc:r * 8 + kc + 1], mw[:, kc, :],
                                                          start=(kc == 0), stop=(kc == 7)), reads=['csil', mwk], writes=[pk])
                        mo, mok = mos.next()
                        S.op('dve', lambda e: e.tensor_tensor(out=mo[:], in0=ps[0:1, :], in1=mb[:], op=ALU.add), reads=[mbk], writes=[pk, mok])
                        S.dma('sp', lambda e: e.dma_start(out=modrow[layer, r:r + 1, cb * 512:(cb + 1) * 512], in_=mo[:]),
                              reads=[mok], writes=[('modrow', layer, r, cb)])
        S.barrier()

    def norm_tiles(layer, which, src, tiles, consume):
        gsrc = I("norm1_g") if which == 0 else I("norm2_g")
        sh_off = 0 if which == 0 else 3 * D
        sc_off = D if which == 0 else 4 * D
        with SBT("nA", [128, 2, D], F32) as A, SBT("nSH", [128, 2, D], F32) as SH, \
                SBT("nG", [128, D], F32) as G, \
                SBT("nx0", [128, D], F32) as x0, SBT("nx1", [128, D], F32) as x1, \
                SBT("nu0", [128, D], F32) as u0, SBT("nu1", [128, D], F32) as u1, \
                SBT("nsq", [128, 2, D], F32) as sq2, SBT("nst", [128, 2, 8], F32) as st2:
            bcast_row(G[:], gsrc[layer:layer + 1, :], 'nG')
            for r in range(2):
                bcast_row(A[:, r, :], modrow[layer, r:r + 1, sc_off:sc_off + D], ('nA', r))
                bcast_row(SH[:, r, :], modrow[layer, r:r + 1, sh_off:sh_off + D], ('nSH', r))
                S.op('dve', lambda e: e.scalar_tensor_tensor(out=A[:, r, :], in0=A[:, r, :], scalar=1.0, in1=G[:],
                                                             op0=ALU.add, op1=ALU.mult), reads=['nG'], writes=[('nA', r)])
            xs = Rot([x0, x1], 'nx')
            us = Rot([u0, u1], 'nu')
            tl = list(tiles)
            pend = {}

            def pre(n_):
                i = tl[n_]
                r = 1 if i < 2 else 0
                xt, xk = xs.next()
                ut, uk = us.next()
                sq = sq2[:, n_ % 2, :]
                st = st2[:, n_ % 2, :]
                NSQ, NST = ('nsq', n_ % 2), ('nst', n_ % 2)
                S.dma('sp', lambda e: e.dma_start(out=xt[:], in_=src[i * 128:(i + 1) * 128, :]), writes=[xk])
                S.op('act', lambda e: e.activation(out=sq, in_=xt[:], func=AF.Square, accum_out=st[:, 0:1]),
                     reads=[xk], writes=[NSQ, NST])
                S.op('dve', lambda e: e.tensor_scalar(out=st[:, 1:2], in0=st[:, 0:1], scalar1=1.0 / D, scalar2=EPS,
                                                      op0=ALU.mult, op1=ALU.add), writes=[NST])
                S.op('act', lambda e: e.activation(out=st[:, 2:3], in_=st[:, 1:2], func=AF.Sqrt), writes=[NST])
                S.op('dve', lambda e: e.reciprocal(st[:, 3:4], st[:, 2:3]), writes=[NST])
                S.op('dve', lambda e: e.scalar_tensor_tensor(out=ut[:], in0=xt[:], scalar=st[:, 3:4], in1=A[:, r, :],
                                                             op0=ALU.mult, op1=ALU.mult), reads=[xk, ('nA', r)], writes=[uk, NST])
                S.op('pool', lambda e: e.tensor_tensor(out=ut[:], in0=ut[:], in1=SH[:, r, :], op=ALU.add),
                     reads=[('nSH', r)], writes=[uk])
                pend[n_] = (i, ut, uk)

            pre(0)
            for n_ in range(len(tl)):
                if n_ + 1 < len(tl):
                    pre(n_ + 1)
                consume(*pend.pop(n_))

    def phase_norm_T(layer, src, uT, tiles=range(NT)):
        def consume(i, ut, uk):
            for half in range(2):
                pb = (i * 2 + half) % 4
                for j in range(4):
                    kc = half * 4 + j
                    S.op('pe', lambda e: e.transpose(psum[pb][:, j * 128:(j + 1) * 128], ut[:, kc * 128:(kc + 1) * 128], ident[:]),
                         reads=[uk, 'ident'], writes=[PB(pb)])
                S.op('act', lambda e: e.copy(uT[:, half * 4:half * 4 + 4, i * 128:(i + 1) * 128],
                                             psum[pb][:].rearrange("p (a b) -> p a b", a=4)),
                     writes=[PB(pb), ('uT', i, half)])
        norm_tiles(layer, 0, src, tiles, consume)
        S.barrier()

    qkT = dscr("qkT", [2048, T], BF16)
    v_tm = dscr("v_tm", [T, D], BF16)
    so_tm = dscr("so_tm", [T, D], BF16)
    g_tm = dscr("g_tm", [T, 16])
    xrT = dscr("xrT", [D, T])
    ygT = dscr("ygT", [D, T], BF16)
    hlT = dscr("hlT", [D, T], BF16)
    hm_d = [dscr("hm_f", [T, D]), dscr("hm_b", [T, D])]
    TG = [(0, 256)] + [(256 + 512 * j, 512) for j in range(8)]
    ZW = 4358
    CN = 4355

    def zcol(t0):
        return 2 + t0 if t0 < 256 else t0 + 5

    def phase_E1(uT):
        wv = I("ev_w_in").rearrange("(kc p) n -> p kc n", p=128)
        with SBT("wb0", [128, 8, 512], BF16) as wb0, SBT("wb1", [128, 8, 512], BF16) as wb1, \
                SBT("zp0", [128, ZW], F32) as zp0, SBT("zp1", [128, ZW], F32) as zp1, \
                SBT("co", [128, ZW], F32) as co, SBT("co_b", [128, ZW], F32) as co_b, \
                SBT("ob0", [128, ZW], BF16) as ob0, SBT("ob1", [128, ZW], BF16) as ob1, \
                SBT("cwq", [128, 16, 5], F32) as cwq, SBT("cwl", [128, 8, 5], F32) as cwl, \
                SBT("tms0", [128, 512], BF16) as tms0, SBT("tms1", [128, 512], BF16) as tms1, \
                SBT("wg", [128, 8, 16], BF16) as wg, SBT("gbr", [128, 16], F32) as gbr, \
                SBT("GT", [128, NT, 16], F32) as GT:
            S.dma('sp', lambda e: e.dma_start(out=cwq[:], in_=I("ev_qkcw")), writes=['cwq'])
            S.dma('sp', lambda e: e.dma_start(out=cwl[:], in_=I("ev_lrucw")), writes=['cwl'])
            S.op('pool', lambda e: e.memset(zp0[:], 0.0), writes=[('zp', 0)])
            S.op('pool', lambda e: e.memset(zp1[:], 0.0), writes=[('zp', 1)])
            wbs = Rot([wb0, wb1], 'wb')
            zps = Rot([zp0, zp1], 'zp')
            obs = Rot([ob0, ob1], 'ob')
            pi = [0]
            fm_blocks = [(c0, 'qk', c0 // 128) for c0 in range(0, 2048, 512)] + \
                        [(4112 + j * 512, 'xr', j * 4) for j in range(2)] + \
                        [(5136 + j * 512, 'yg', j * 4) for j in range(2)]
            chunks = []
            for c0, kind, cbase in fm_blocks:
                for jj in range(4):
                    chunks.append((c0, kind, cbase + jj, jj))
            cos_ = [co, co_b]
            state = {}

            def stage1(n):
                c0, kind, cidx, jj = chunks[n]
                if jj == 0:
                    wb, wbk = wbs.next()
                    S.dma('pool', lambda e: e.dma_start(out=wb[:], in_=wv[:, :, c0:c0 + 512]), writes=[wbk])
                    state['wb'] = (wb, wbk)
                wb, wbk = state['wb']
                zp, zpk = zps.next()
                cq = cos_[n % 2]
                ck = ('co', n % 2)
                for (t0, n_) in TG:
                    pb = pi[0] % 2
                    pi[0] += 1
                    for kc in range(8):
                        S.op('pe', lambda e: e.matmul(psum[pb][:, 0:n_], wb[:, kc, jj * 128:(jj + 1) * 128], uT[:, kc, t0:t0 + n_],
                                                      start=(kc == 0), stop=(kc == 7)), reads=[wbk], writes=[PB(pb)])
                    z0 = zcol(t0)
                    S.op('act', lambda e: e.copy(zp[:, z0:z0 + n_], psum[pb][:, 0:n_]), writes=[PB(pb), zpk])
                if kind in ('qk', 'xr'):
                    cw = cwq if kind == 'qk' else cwl
                    S.op('dve', lambda e: e.tensor_scalar(out=cq[:, 0:CN], in0=zp[:, 0:CN], scalar1=cw[:, cidx, 0:1], scalar2=None,
                                                          op0=ALU.mult), reads=[zpk, 'cwq', 'cwl'], writes=[ck])
                    for j in range(1, 4):
                        S.op('dve', lambda e: e.scalar_tensor_tensor(out=cq[:, 0:CN], in0=zp[:, j:j + CN], scalar=cw[:, cidx, j:j + 1],
                                                                     in1=cq[:, 0:CN], op0=ALU.mult, op1=ALU.add),
                             reads=[zpk], writes=[ck])
                state[n] = (zp, zpk, cq, ck)

            def stage2(n):
                c0, kind, cidx, jj = chunks[n]
                zp, zpk, cq, ck = state.pop(n)
                ob, obk = obs.next()
                if kind == 'qk':
                    S.op('act', lambda e: e.activation(out=ob[:, 0:CN], in_=cq[:, 0:CN], func=AF.Silu, bias=cwq[:, cidx, 4:5], scale=1.0),
                         reads=[ck], writes=[obk])
                    rows = qkT[cidx * 128:(cidx + 1) * 128, :]
                    S.dma('sp', lambda e: e.dma_start(out=rows[:, 0:256], in_=ob[:, 0:256]), reads=[obk], writes=[('qkT', cidx, 0)])
                    S.dma('sp', lambda e: e.dma_start(out=rows[:, 256:T], in_=ob[:, 259:CN]), reads=[obk], writes=[('qkT', cidx, 1)])
                elif kind == 'xr':
                    S.op('act', lambda e: e.activation(out=cq[:, 0:CN], in_=cq[:, 0:CN], func=AF.Identity, bias=cwl[:, cidx, 4:5], scale=1.0),
                         writes=[ck])
                    rows = xrT[cidx * 128:(cidx + 1) * 128, :]
                    S.dma('sp', lambda e: e.dma_start(out=rows[:, 0:256], in_=cq[:, 0:256]), reads=[ck], writes=[('xrT', cidx, 0)])
                    S.dma('sp', lambda e: e.dma_start(out=rows[:, 256:T], in_=cq[:, 259:CN]), reads=[ck], writes=[('xrT', cidx, 1)])
                else:
                    S.op('act', lambda e: e.activation(out=cq[:], in_=zp[:], func=AF.Square), reads=[zpk], writes=[ck])
                    S.op('dve', lambda e: e.tensor_scalar(out=cq[:], in0=cq[:], scalar1=0.044715, scalar2=1.0, op0=ALU.mult, op1=ALU.add),
                         writes=[ck])
                    S.op('dve', lambda e: e.tensor_tensor(out=cq[:], in0=cq[:], in1=zp[:], op=ALU.mult), reads=[zpk], writes=[ck])
                    S.op('act', lambda e: e.activation(out=cq[:], in_=cq[:], func=AF.Sigmoid, scale=1.5957691216), writes=[ck])
                    S.op('pool', lambda e: e.tensor_tensor(out=ob[:], in0=cq[:], in1=zp[:], op=ALU.mult), reads=[ck, zpk], writes=[obk])
                    rows = ygT[cidx * 128:(cidx + 1) * 128, :]
                    S.dma('sp', lambda e: e.dma_start(out=rows[:, 0:256], in_=ob[:, 2:258]), reads=[obk], writes=[('ygT', cidx, 0)])
                    S.dma('sp', lambda e: e.dma_start(out=rows[:, 256:T], in_=ob[:, 261:4357]), reads=[obk], writes=[('ygT', cidx, 1)])

            stage1(0)
            for n in range(len(chunks)):
                if n + 1 < len(chunks):
                    stage1(n + 1)
                stage2(n)
            tms = Rot([tms0, tms1], 'tms')
            for blk in range(4):
                c0 = 2048 + blk * 512
                wb, wbk = wbs.next()
                S.dma('pool', lambda e: e.dma_start(out=wb[:], in_=wv[:, :, c0:c0 + 512]), writes=[wbk])
                dst = v_tm if blk < 2 else so_tm
                dc0 = (blk % 2) * 512
                for i in range(NT):
                    pb = 2 + i % 2
                    for kc in range(8):
                        S.op('pe', lambda e: e.matmul(psum[pb][:], uT[:, kc, i * 128:(i + 1) * 128], wb[:, kc, :],
                                                      start=(kc == 0), stop=(kc == 7)), reads=[wbk], writes=[PB(pb)])
                    st_, stk = tms.next()
                    if blk < 2:
                        S.op('act', lambda e: e.copy(st_[:], psum[pb][:]), writes=[PB(pb), stk])
                    else:
                        S.op('act', lambda e: e.activation(out=st_[:], in_=psum[pb][:], func=AF.Sigmoid), writes=[PB(pb), stk])
                    S.dma('sp', lambda e: e.dma_start(out=dst[i * 128:(i + 1) * 128, dc0:dc0 + 512], in_=st_[:]), reads=[stk],
                          writes=[('tmout', blk, i)])
            S.dma('pool', lambda e: e.dma_start(out=wg[:], in_=wv[:, :, 4096:4112]), writes=['wg'])
            bcast_row(gbr[:], I("ev_gate_b")[0:1, :], 'gbr')
            for i in range(NT):
                pb = 4 + i % 2
                for kc in range(8):
                    S.op('pe', lambda e: e.matmul(psum[pb][:, 0:16], uT[:, kc, i * 128:(i + 1) * 128], wg[:, kc, :],
                                                  start=(kc == 0), stop=(kc == 7)), reads=['wg'], writes=[PB(pb)])
                S.op('dve', lambda e: e.tensor_tensor(out=GT[:, i, :], in0=psum[pb][:, 0:16], in1=gbr[:], op=ALU.add),
                     reads=['gbr'], writes=[PB(pb), 'GT'])
            S.dma('sp', lambda e: e.dma_start(out=g_tm.rearrange("(n p) c -> p n c", p=128), in_=GT[:]), reads=['GT'], writes=['g_tm'])
        S.barrier()

    def phase_E2():
        lw_d, lb_d, lam_d = I("ev_lru_w"), I("ev_lru_b"), I("ev_lru_lam")
        with SBT("lxr", [128, T], F32) as xr, SBT("lyg", [128, T], BF16) as yg, \
                SBT("lA", [128, T], F32) as A0, SBT("lB", [128, T], F32) as Bx0, SBT("ltmp", [128, T], F32) as tmp0, \
                SBT("lA1", [128, T], F32) as A1_, SBT("lB1", [128, T], F32) as Bx1, SBT("ltmp1", [128, T], F32) as tmp1, \
                SBT("lH0", [128, T], F32) as H0, SBT("lH1", [128, T], F32) as H1, \
                SBT("lho", [128, T], BF16) as ho, \
                SBT("lw", [128, 4, 128], F32) as lw, SBT("lwb", [128, 4, 128], BF16) as lwb, SBT("xrb", [128, T], BF16) as xrb, SBT("lb", [128, 8, 4], F32) as lb, \
                SBT("lam", [128, 8, 2], F32) as lam, SBT("cA", [128, 8, 2], F32) as cA:
            S.dma('sp', lambda e: e.dma_start(out=lb[:], in_=lb_d), writes=['lb'])
            S.dma('sp', lambda e: e.dma_start(out=lam[:], in_=lam_d), writes=['lam'])
            S.op('act', lambda e: e.activation(out=cA[:], in_=lam[:], func=AF.Exp, scale=-1.0), reads=['lam'], writes=['cA'])
            S.op('act', lambda e: e.activation(out=cA[:], in_=cA[:], func=AF.Ln, bias=1.0, scale=1.0), writes=['cA'])
            S.op('dve', lambda e: e.tensor_scalar(out=cA[:], in0=cA[:], scalar1=-8.0, scalar2=None, op0=ALU.mult), writes=['cA'])
            pi = 0
            for cc in range(8):
                S.dma('sp', lambda e: e.dma_start(out=xr[:], in_=xrT[cc * 128:(cc + 1) * 128, :]), writes=['xr'])
                S.dma('sp', lambda e: e.dma_start(out=yg[:], in_=ygT[cc * 128:(cc + 1) * 128, :]), writes=['yg'])
                S.dma('sp', lambda e: e.dma_start(out=lw[:], in_=lw_d[:, cc].rearrange("g k m -> k g m")), writes=['lw'])
                S.op('pool', lambda e: e.tensor_copy(lwb[:], lw[:]), reads=['lw'], writes=['lwb'])
                S.op('pool', lambda e: e.tensor_copy(xrb[:], xr[:]), reads=['xr'], writes=['xrb'])
                for z in range(2):
                    H = H0 if z == 0 else H1
                    hk = ('H', z)
                    A, Bx, tmp = (A0, Bx0, tmp0) if z == 0 else (A1_, Bx1, tmp1)
                    KA, KB, KT_ = ('A', z), ('Bx', z), ('tmp', z)
                    for gi, dst, dk in ((0, A, KA), (1, Bx, KB)):
                        for (t0, n) in TG:
                            pb = pi % 2
                            pi += 1
                            S.op('pe', lambda e: e.matmul(psum[pb][:, 0:n], lwb[:, z * 2 + gi, :], xrb[:, t0:t0 + n], start=True, stop=True),
                                 reads=['lwb', 'xrb'], writes=[PB(pb)])
                            S.op('act', lambda e: e.activation(out=dst[:, t0:t0 + n], in_=psum[pb][:, 0:n], func=AF.Sigmoid,
                                                               bias=lb[:, cc, z * 2 + gi:z * 2 + gi + 1], scale=1.0),
                                 reads=['lb'], writes=[PB(pb), dk])
                    S.op('act', lambda e: e.activation(out=A[:], in_=A[:], func=AF.Exp, scale=cA[:, cc, z:z + 1]), reads=['cA'], writes=[KA])
                    S.op('act', lambda e: e.activation(out=tmp[:], in_=A[:], func=AF.Square), reads=[KA], writes=[KT_])
                    S.op('act', lambda e: e.activation(out=tmp[:], in_=tmp[:], func=AF.Sqrt, bias=1.0, scale=-1.0), writes=[KT_])
                    S.op('pool', lambda e: e.tensor_tensor(out=Bx[:], in0=Bx[:], in1=xr[:], op=ALU.mult), reads=['xr'], writes=[KB])
                    S.op('dve', lambda e: e.tensor_tensor(out=Bx[:], in0=Bx[:], in1=tmp[:], op=ALU.mult), reads=[KT_], writes=[KB])
                    if z == 0:
                        S.op('dve', lambda e: e.tensor_tensor_scan(H[:], A[:], Bx[:], 0.0, ALU.mult, ALU.add), reads=[KA, KB], writes=[hk])
                    else:
                        S.op('dve', lambda e: e.tensor_tensor_scan(H[:, 0:256][:, ::-1], A[:, 0:256][:, ::-1], Bx[:, 0:256][:, ::-1], 0.0,
                                                                   ALU.mult, ALU.add), reads=[KA, KB], writes=[hk])
                        S.op('dve', lambda e: e.tensor_tensor_scan(H[:, 256:T][:, ::-1], A[:, 256:T][:, ::-1], Bx[:, 256:T][:, ::-1], H[:, 0:1],
                                                                   ALU.mult, ALU.add), reads=[KA, KB], writes=[hk])
                S.op('pool', lambda e: e.tensor_tensor(out=H0[:], in0=H0[:], in1=H1[:], op=ALU.add), reads=[('H', 1)], writes=[('H', 0)])
                S.op('dve', lambda e: e.tensor_tensor(out=ho[:], in0=H0[:], in1=yg[:], op=ALU.mult), reads=[('H', 0), 'yg'], writes=['ho'])
                S.dma('pool', lambda e: e.dma_start(out=hlT[cc * 128:(cc + 1) * 128, :], in_=ho[:]), reads=['ho'], writes=[('hlT', cc)])
        S.barrier()

    def phase_E3():
        NLN16 = -2.772588722239781
        with contextlib.ExitStack() as es:
            def al(name, shape, dt):
                return es.enter_context(SBT(name, shape, dt))
            G = al("G", [128, NT, 16], F32)
            LF = al("LF", [128, 2, NT, 4], F32)
            CF = al("CF", [128, 2, NT, 4], F32)
            EB16 = al("EB16", [128, 2, NT, 4], F32)
            ECF = al("ECF", [128, 2, NT, 4], F32)
            DEC = al("DEC", [128, 2, NT, 4], F32)
            WS16 = al("WS16", [128, 2, NT, 4], F32)
            qT0 = al("qT0", [128, 2, T], BF16)
            qT1 = al("qT1", [128, 2, T], BF16)
            kT0 = al("kT0", [128, 2, T], BF16)
            kT1 = al("kT1", [128, 2, T], BF16)
            V0 = al("V0", [128, NT, 257], BF16)
            V1 = al("V1", [128, NT, 257], BF16)
            CTs = al("CTs", [128, 4, 2, 257], F32)
            CTbs = al("CTbs", [128, 4, 2, 257], BF16)
            PTs = al("PTs", [128, 4, 128], BF16)
            ktms = al("ktms", [128, 4, 256], BF16)
            vws = al("vws", [128, 4, 257], BF16)
            sms = al("sms", [128, 4, 8], F32)
            houts = al("houts", [128, 4, 256], F32)
            S.dma('sp', lambda e: e.dma_start(out=G[:], in_=g_tm.rearrange("(n p) c -> p n c", p=128)), writes=['G'])
            Gv = G[:].rearrange("p n (d g h) -> p d g n h", d=2, g=2, h=4)
            fl = lambda t, d: t[:, d].rearrange("p n h -> p (n h)")
            for d in range(2):
                S.op('act', lambda e: e.activation(out=LF[:, d], in_=Gv[:, d, 1], func=AF.Exp, scale=-1.0), reads=['G'], writes=['LF'])
                S.op('act', lambda e: e.activation(out=LF[:, d], in_=LF[:, d], func=AF.Ln, bias=1.0, scale=1.0), writes=['LF'])
                S.op('dve', lambda e: e.tensor_scalar(out=LF[:, d], in0=LF[:, d], scalar1=-1.0, scalar2=None, op0=ALU.mult), writes=['LF'])
                tri = triF if d == 0 else triB
                sel = selF if d == 0 else selB
                S.op('pe', lambda e: e.matmul(psum[0][:, 0:NT * 4], tri[:], fl(LF, d), start=True, stop=True),
                     reads=['LF', tri.name], writes=[PB(0)])
                S.op('act', lambda e: e.copy(fl(CF, d), psum[0][:, 0:NT * 4]), writes=[PB(0), 'CF'])
                S.op('pe', lambda e: e.matmul(psum[1][:, 0:NT * 4], sel[:], fl(CF, d), start=True, stop=True),
                     reads=['CF', sel.name], writes=[PB(1)])
                S.op('act', lambda e: e.activation(out=fl(DEC, d), in_=psum[1][:, 0:NT * 4], func=AF.Exp), writes=[PB(1), 'DEC'])
                S.op('dve', lambda e: e.tensor_tensor(out=EB16[:, d], in0=Gv[:, d, 0], in1=CF[:, d], op=ALU.subtract), reads=['G', 'CF'], writes=['EB16'])
                S.op('dve', lambda e: e.tensor_scalar(out=EB16[:, d], in0=EB16[:, d], scalar1=NLN16, scalar2=None, op0=ALU.add), writes=['EB16'])
                S.op('act', lambda e: e.activation(out=EB16[:, d], in_=EB16[:, d], func=AF.Exp), writes=['EB16'])
                S.op('act', lambda e: e.activation(out=ECF[:, d], in_=CF[:, d], func=AF.Exp), reads=['CF'], writes=['ECF'])
                S.op('dve', lambda e: e.tensor_tensor(out=WS16[:, d], in0=EB16[:, d], in1=DEC[:, d], op=ALU.mult), reads=['EB16', 'DEC'], writes=['WS16'])
            S.barrier()
            qTs, kTs, Vs = [qT0, qT1], [kT0, kT1], [V0, V1]
            order = [list(range(NT)), [1, 0] + list(range(NT - 1, 1, -1))]
            cnt = {'st': 0, 'num': 0}
            for hp in range(2):
                for hh in range(2):
                    h = hp * 2 + hh
                    S.dma('sp', lambda e: e.dma_start(out=qTs[hh][:], in_=qkT[h * 256:(h + 1) * 256, :].rearrange("(dh p) t -> p dh t", p=128)),
                          writes=[('qT', hh)])
                    S.dma('sp', lambda e: e.dma_start(out=kTs[hh][:], in_=qkT[1024 + h * 256:1024 + (h + 1) * 256, :].rearrange("(dh p) t -> p dh t", p=128)),
                          writes=[('kT', hh)])
                    S.dma('sp', lambda e: e.dma_start(out=Vs[hh][:, :, 0:256], in_=v_tm[:, h * 256:(h + 1) * 256].rearrange("(n p) e -> p n e", p=128)),
                          writes=[('V', hh)])
                    S.op('pool', lambda e: e.memset(Vs[hh][:, :, 256:257], 1.0), writes=[('V1', hh)])
                S.op('pool', lambda e: e.memset(CTs[:], 0.0), writes=[('CT', i) for i in range(4)])
                S.op('pool', lambda e: e.memset(CTbs[:], 0.0), writes=[('CTb', i) for i in range(4)])
                for step in range(NT):
                    for ci, (z, hh) in enumerate([(0, 0), (0, 1), (1, 0), (1, 1)]):
                        h = hp * 2 + hh
                        c = order[z][step]
                        sl = slice(c * 128, (c + 1) * 128)
                        qT, kT, V = qTs[hh], kTs[hh], Vs[hh]
                        rk = [('qT', hh), ('kT', hh), ('V', hh), ('V1', hh)]
                        tri = triF if z == 0 else triB
                        pst = cnt['st'] % 2
                        cnt['st'] += 1
                        for dh in range(2):
                            S.op('pe', lambda e: e.matmul(psum[pst][:, 0:128], kT[:, dh, sl], qT[:, dh, sl], start=(dh == 0), stop=(dh == 1)),
                                 reads=rk, writes=[PB(pst)])
                        S.op('dve', lambda e: e.scalar_tensor_tensor(out=PTs[:, ci, :], in0=psum[pst][:, 0:128], scalar=EB16[:, z, c, h:h + 1],
                                                                     in1=tri[:], op0=ALU.mult, op1=ALU.mult), writes=[PB(pst), ('PT', ci)])
                        for dh in range(2):
                            S.op('pe', lambda e: e.transpose(psb[0][:, dh * 128:(dh + 1) * 128], kT[:, dh, sl], identb[:]),
                                 reads=rk, writes=[PBB(0)])
                        S.op('act', lambda e: e.copy(ktms[:, ci, :], psb[0][:, 0:256]), writes=[PBB(0), ('ktm', ci)])
                        S.op('act', lambda e: e.activation(out=vws[:, ci, :], in_=V[:, c, :], func=AF.Identity, scale=WS16[:, z, c, h:h + 1]),
                             reads=rk, writes=[('vw', ci)])
                        pn = 2 + cnt['num'] % 2
                        cnt['num'] += 1
                        S.op('pe', lambda e: e.matmul(psum[pn][:, 0:257], PTs[:, ci, :], V[:, c, :], start=True, stop=False),
                             reads=rk + [('PT', ci)], writes=[PB(pn)])
                        for dh in range(2):
                            S.op('pe', lambda e: e.matmul(psum[pn][:, 0:257], qT[:, dh, sl], CTbs[:, ci, dh, :], start=False, stop=(dh == 1)),
                                 reads=rk + [('CTb', ci)], writes=[PB(pn)])
                        sm = sms[:, ci, :]
                        ecf = ECF[:, z, c, h:h + 1]
                        S.op('dve', lambda e: e.tensor_tensor(out=sm[:, 0:1], in0=psum[pn][:, 256:257], in1=ecf, op=ALU.mult), writes=[PB(pn), ('sm', ci)])
                        S.op('dve', lambda e: e.tensor_scalar(out=sm[:, 2:3], in0=sm[:, 0:1], scalar1=1.0, scalar2=None, op0=ALU.max), writes=[('sm', ci)])
                        S.op('dve', lambda e: e.tensor_scalar(out=sm[:, 3:4], in0=sm[:, 0:1], scalar1=-1.0, scalar2=sm[:, 2:3], op0=ALU.mult, op1=ALU.max),
                             writes=[('sm', ci)])
                        S.op('dve', lambda e: e.reciprocal(sm[:, 4:5], sm[:, 3:4]), writes=[('sm', ci)])
                        S.op('dve', lambda e: e.tensor_tensor(out=sm[:, 5:6], in0=sm[:, 4:5], in1=ecf, op=ALU.mult), writes=[('sm', ci)])
                        S.op('act', lambda e: e.activation(out=houts[:, ci, :], in_=psum[pn][:, 0:256], func=AF.Identity, scale=sm[:, 5:6]),
                             reads=[('sm', ci)], writes=[PB(pn), ('hout', ci)])
                        S.dma('sp', lambda e: e.dma_start(out=hm_d[z][sl, h * 256:(h + 1) * 256], in_=houts[:, ci, :]), reads=[('hout', ci)],
                              writes=[('hm', z, h, c)])
                        for dh in range(2):
                            S.op('pe', lambda e: e.matmul(psum[4 + dh][:, 0:257], ktms[:, ci, dh * 128:(dh + 1) * 128], vws[:, ci, :], start=True, stop=True),
                                 reads=[('ktm', ci), ('vw', ci)], writes=[PB(4 + dh)])
                            S.op('dve', lambda e: e.scalar_tensor_tensor(out=CTs[:, ci, dh, :], in0=CTs[:, ci, dh, :], scalar=DEC[:, z, c, h:h + 1],
                                                                         in1=psum[4 + dh][:, 0:257], op0=ALU.mult, op1=ALU.add),
                                 writes=[PB(4 + dh), ('CT', ci)])
                        S.op('act', lambda e: e.copy(CTbs[:, ci], CTs[:, ci]), reads=[('CT', ci)], writes=[('CTb', ci)])
        S.barrier()

    def mixer_out(i, r, G1, wo, nk, lhs_list, src_rows, pbase):
        return

    def phase_E4():
        with contextlib.ExitStack() as es:
            def al(name, shape, dt):
                return es.enter_context(SBT(name, shape, dt))
            wo = al("wo", [128, 16, D], BF16)
            MG = al("MG", [128, D], F32)
            G1 = al("G1", [128, 2, D], F32)
            hf = [al("hf%d" % j, [128, D], F32) for j in range(2)]
            hb = [al("hb%d" % j, [128, D], F32) for j in range(2)]
            so = [al("so%d" % j, [128, D], BF16) for j in range(2)]
            sq2 = [al("e4sq%d" % j_, [128, 256], F32) for j_ in range(2)]
            st2 = [al("e4st%d" % j_, [128, 16], F32) for j_ in range(2)]
            hmb2 = [al("hmb%d" % j_, [128, D], BF16) for j_ in range(2)]
            hmT = [al("hmT%d" % j, [128, 8, 128], BF16) for j in range(2)]
            hl = [al("hl%d" % j, [128, 8, 128], BF16) for j in range(2)]
            xt = [al("e4x%d" % j, [128, D], F32) for j in range(2)]
            tmp2 = [al("e4tmp%d" % j_, [128, D], F32) for j_ in range(2)]
            wov = I("ev_w_out").rearrange("(kc p) n -> p kc n", p=128)
            for hlf in range(2):
                S.dma('pool', lambda e: e.dma_start(out=wo[:, hlf * 8:(hlf + 1) * 8, :], in_=wov[:, hlf * 8:(hlf + 1) * 8, :]), writes=[('wo', hlf)])
            bcast_row(MG[:], I("ev_mnorm_g")[0:1, :], 'MG')
            for r in range(2):
                bcast_row(G1[:, r, :], modrow[0, r:r + 1, 2 * D:3 * D], ('G1', r))
            hlv = hlT.rearrange("(cc p) t -> p cc t", p=128)
            xin = I("xin")
            def stL(i):
                r = 1 if i < 2 else 0
                j = i % 2
                rows = slice(i * 128, (i + 1) * 128)
                sq, st, hmb, tmp = sq2[j], st2[j], hmb2[j], tmp2[j]
                KSQ, KST, KHMB = ('e4sq', j), ('e4st', j), ('hmb', j)
                S.dma('sp', lambda e: e.dma_start(out=hf[j][:], in_=hm_d[0][rows, :]), writes=[('hf', j)])
                S.dma('sp', lambda e: e.dma_start(out=hb[j][:], in_=hm_d[1][rows, :]), writes=[('hb', j)])
                S.dma('sp', lambda e: e.dma_start(out=so[j][:], in_=so_tm[rows, :]), writes=[('so', j)])
                S.dma('sp', lambda e: e.dma_start(out=hl[j][:], in_=hlv[:, :, rows]), writes=[('hl', j)])
                S.dma('sp', lambda e: e.dma_start(out=xt[j][:], in_=xin[rows, :]), writes=[('xt', j)])

            def stB(i):
                r = 1 if i < 2 else 0
                j = i % 2
                rows = slice(i * 128, (i + 1) * 128)
                sq, st, hmb, tmp = sq2[j], st2[j], hmb2[j], tmp2[j]
                KSQ, KST, KHMB = ('e4sq', j), ('e4st', j), ('hmb', j)
                S.op('pool', lambda e: e.tensor_tensor(out=hf[j][:], in0=hf[j][:], in1=hb[j][:], op=ALU.add), reads=[('hb', j)], writes=[('hf', j)])
                for h in range(4):
                    S.op('act', lambda e: e.activation(out=sq[:], in_=hf[j][:, h * 256:(h + 1) * 256], func=AF.Square, accum_out=st[:, h:h + 1]),
                         reads=[('hf', j)], writes=[KSQ, KST])
                S.op('dve', lambda e: e.tensor_scalar(out=st[:, 4:8], in0=st[:, 0:4], scalar1=1.0 / 256, scalar2=EPS, op0=ALU.mult, op1=ALU.add), writes=[KST])
                S.op('act', lambda e: e.activation(out=st[:, 8:12], in_=st[:, 4:8], func=AF.Sqrt), writes=[KST])
                S.op('dve', lambda e: e.reciprocal(st[:, 12:16], st[:, 8:12]), writes=[KST])
                for h in range(4):
                    hs = slice(h * 256, (h + 1) * 256)
                    S.op('dve', lambda e: e.scalar_tensor_tensor(out=hf[j][:, hs], in0=hf[j][:, hs], scalar=st[:, 12 + h:13 + h], in1=MG[:, hs],
                                                                 op0=ALU.mult, op1=ALU.mult), reads=['MG'], writes=[('hf', j), KST])
                S.op('pool', lambda e: e.tensor_tensor(out=hmb[:], in0=hf[j][:], in1=so[j][:], op=ALU.mult), reads=[('hf', j), ('so', j)], writes=[KHMB])

            def stCD(i):
                r = 1 if i < 2 else 0
                j = i % 2
                rows = slice(i * 128, (i + 1) * 128)
                sq, st, hmb, tmp = sq2[j], st2[j], hmb2[j], tmp2[j]
                KSQ, KST, KHMB = ('e4sq', j), ('e4st', j), ('hmb', j)
                for kc in range(8):
                    S.op('pe', lambda e: e.transpose(psb[j][:, kc * 128:(kc + 1) * 128], hmb[:, kc * 128:(kc + 1) * 128], identb[:]),
                         reads=[KHMB], writes=[PBB(j)])
                S.op('act', lambda e: e.copy(hmT[j][:].rearrange("p a b -> p (a b)"), psb[j][:]), writes=[PBB(j), ('hmT', j)])
                for nb in range(2):
                    pb = (2 * i + nb) % 4
                    for kc in range(16):
                        lhs = hmT[j][:, kc, :] if kc < 8 else hl[j][:, kc - 8, :]
                        S.op('pe', lambda e: e.matmul(psum[pb][:], lhs, wo[:, kc, nb * 512:(nb + 1) * 512], start=(kc == 0), stop=(kc == 15)),
                             reads=[('hmT', j), ('hl', j), ('wo', 0), ('wo', 1)], writes=[PB(pb)])
                    cs = slice(nb * 512, (nb + 1) * 512)
                    S.op('dve', lambda e: e.tensor_tensor(out=tmp[:, cs], in0=psum[pb][:], in1=G1[:, r, cs], op=ALU.mult), reads=[('G1', r)],
                         writes=[PB(pb), ('e4tmp', j, nb)])
                    S.op('pool', lambda e: e.tensor_tensor(out=tmp[:, cs], in0=tmp[:, cs], in1=xt[j][:, cs], op=ALU.add), reads=[('xt', j)],
                         writes=[('e4tmp', j, nb)])
                S.dma('pool', lambda e: e.dma_start(out=xres[rows, :], in_=tmp[:]), reads=[('e4tmp', j, 0), ('e4tmp', j, 1)], writes=[('xres', i)])

            stL(0)
            stB(0)
            for i in range(NT):
                if i + 1 < NT:
                    stL(i + 1)
                    stB(i + 1)
                stCD(i)
        S.barrier()

    v2_tm = dscr("v2_tm", [T, D], BF16)

    def phase_N2(layer, tiles, LOG):
        with contextlib.ExitStack() as es:
            def al(name, shape, dt):
                return es.enter_context(SBT(name, shape, dt))
            wr = al("wr", [128, 8, 32], F32)
            br = al("br", [1, 32], F32)
            vT32 = [al("vT32_%d" % j, [128, 8, 128], F32) for j in range(2)]
            vb = [al("vb%d" % j, [128, D], BF16) for j in range(2)]
            S.dma('sp', lambda e: e.dma_start(out=wr[:], in_=I("moe_w_r")[layer].rearrange("(kc p) n -> p kc n", p=128)), writes=['wr'])
            S.dma('sp', lambda e: e.dma_start(out=br[:], in_=I("moe_b_r")[layer:layer + 1, :]), writes=['br'])
            cnt = [0]

            def consume(i, ut, uk):
                j = cnt[0] % 2
                cnt[0] += 1
                S.op('pool', lambda e: e.tensor_copy(vb[j][:], ut[:]), reads=[uk], writes=[('vb', j)])
                S.dma('pool', lambda e: e.dma_start(out=v2_tm[i * 128:(i + 1) * 128, :], in_=vb[j][:]), reads=[('vb', j)], writes=[('v2', i)])
                for half in range(2):
                    pb = (i * 2 + half) % 4
                    for jj in range(4):
                        kc = half * 4 + jj
                        S.op('pe', lambda e: e.transpose(psum[pb][:, jj * 128:(jj + 1) * 128], ut[:, kc * 128:(kc + 1) * 128], ident[:]),
                             reads=[uk, 'ident'], writes=[PB(pb)])
                    S.op('act', lambda e: e.copy(vT32[j][:, half * 4:half * 4 + 4, :].rearrange("p a b -> p (a b)"), psum[pb][:]),
                         writes=[PB(pb), ('vT32', j, half)])
                pl = 4 + i % 2
                for kc in range(8):
                    S.op('pe', lambda e: e.matmul(psum[pl][:, 0:32], vT32[j][:, kc, :], wr[:, kc, :], start=(kc == 0), stop=False),
                         reads=[('vT32', j, 0), ('vT32', j, 1), 'wr'], writes=[PB(pl)])
                S.op('pe', lambda e: e.matmul(psum[pl][:, 0:32], ones1[0:1, :], br[:], start=False, stop=True), reads=['br', 'ones1'], writes=[PB(pl)])
                S.op('dve', lambda e: e.tensor_copy(LOG[:, i, :], psum[pl][:, 0:32]), writes=[PB(pl), 'LOG'])
            norm_tiles(layer, 1, xres, tiles, consume)
        S.barrier()

    SB = 512
    SHIFT = 9
    NSUB = SB // 128
    NBMAX = 66
    xs_d = dscr("xs_d", [NBMAX * SB, D], BF16)
    ys_d = dscr("ys_d", [NBMAX * SB, D], F32)
    RT = {}
    for nm, shp, dt in (("LOG", [128, NT, 32], F32), ("TOP8", [128, NT, 8], F32), ("WG", [128, NT, 4], F32),
                        ("SLOTI", [128, NT, 4], I32), ("IDXI", [128, NBMAX], I32), ("BIDX", [128, NBMAX], I32),
                        ("IDX8", [128, NBMAX, 8], I32), ("IDX4", [128, NBMAX, 4], I32)):
        RT[nm] = nc.alloc_sbuf_tensor("rt_" + nm, shp, dt)

    def phase_route(tiles, NB):
        LOG, TOP8, WG, SLOTI, IDXI, BIDX = (RT[k] for k in ("LOG", "TOP8", "WG", "SLOTI", "IDXI", "BIDX"))
        with contextlib.ExitStack() as es:
            def al(name, shape, dt):
                return es.enter_context(SBT(name, shape, dt))
            MASK = al("MASK", [128, NT, 32], F32)
            RANK = al("RANK", [128, NT, 32], F32)
            CAR = al("CAR", [128, 32], F32)
            sm = al("rsm", [128, 8], F32)
            ci = al("rci", [128, 32], I32)
            PADDED = al("PADDED", [128, 32], F32)
            PEND = al("PEND", [128, 32], F32)
            BASE = al("BASE", [128, 32], F32)
            ones32 = al("ones32", [128, 32], F32)
            junk = al("rjunk", [128, 32], F32)
            SLOTF = al("SLOTF", [128, NT, 4], F32)
            BSTi = al("BSTi", [128, NBMAX], I32)
            BST = al("BST", [128, NBMAX], F32)
            BLKE = al("BLKE", [128, NBMAX], F32)
            IDXF = al("IDXF", [128, NBMAX], F32)
            IDXF2 = al("IDXF2", [128, NBMAX], F32)
            S.op('pool', lambda e: e.memset(CAR[:], 0.0), writes=['CAR'])
            S.op('pool', lambda e: e.memset(ones32[:], 1.0), writes=['ones32'])
            S.op('pool', lambda e: e.memset(SLOTF[:], 0.0), writes=['SLOTF'])
            for i in tiles:
                S.op('dve', lambda e: e.max(out=TOP8[:, i, :], in_=LOG[:, i, :]), writes=['TOP8'])
                S.op('dve', lambda e: e.tensor_scalar(out=MASK[:, i, :], in0=LOG[:, i, :], scalar1=TOP8[:, i, 3:4], scalar2=None, op0=ALU.is_ge),
                     reads=['TOP8'], writes=[('MASK', i)])
                S.op('dve', lambda e: e.tensor_scalar(out=sm[:, 0:1], in0=TOP8[:, i, 0:1], scalar1=-1.0, scalar2=None, op0=ALU.mult), reads=['TOP8'], writes=['rsm'])
                S.op('act', lambda e: e.activation(out=WG[:, i, :], in_=TOP8[:, i, 0:4], func=AF.Exp, bias=sm[:, 0:1], scale=1.0, accum_out=sm[:, 1:2]),
                     reads=['TOP8'], writes=['rsm', 'WG'])
                S.op('dve', lambda e: e.reciprocal(sm[:, 2:3], sm[:, 1:2]), writes=['rsm'])
                S.op('dve', lambda e: e.tensor_scalar(out=WG[:, i, :], in0=WG[:, i, :], scalar1=sm[:, 2:3], scalar2=None, op0=ALU.mult), writes=['rsm', 'WG'])
                S.op('pe', lambda e: e.matmul(psum[0][:, 0:32], triS[:], MASK[:, i, :], start=True, stop=True), reads=[('MASK', i)], writes=[PB(0)])
                S.op('pe', lambda e: e.matmul(psum[1][:, 0:32], onesm[:], MASK[:, i, :], start=True, stop=True), reads=[('MASK', i)], writes=[PB(1)])
                S.op('dve', lambda e: e.tensor_tensor(out=RANK[:, i, :], in0=psum[0][:, 0:32], in1=CAR[:], op=ALU.add), reads=['CAR'], writes=[PB(0), 'RANK'])
                S.op('dve', lambda e: e.tensor_tensor(out=CAR[:], in0=psum[1][:, 0:32], in1=CAR[:], op=ALU.add), writes=[PB(1), 'CAR'])
            S.op('dve', lambda e: e.tensor_scalar(out=junk[:], in0=CAR[:], scalar1=float(SB - 1), scalar2=None, op0=ALU.add), reads=['CAR'], writes=['rjunk'])
            S.op('dve', lambda e: e.tensor_copy(ci[:], junk[:]), reads=['rjunk'], writes=['rci'])
            S.op('dve', lambda e: e.tensor_scalar(out=ci[:], in0=ci[:], scalar1=SHIFT, scalar2=SHIFT, op0=ALU.arith_shift_right, op1=ALU.logical_shift_left),
                 writes=['rci'])
            S.op('dve', lambda e: e.tensor_copy(PADDED[:], ci[:]), reads=['rci'], writes=['PADDED'])
            S.op('dve', lambda e: e.tensor_tensor_scan(PEND[:], ones32[:], PADDED[:], 0.0, ALU.mult, ALU.add), reads=['ones32', 'PADDED'], writes=['PEND'])
            S.op('dve', lambda e: e.tensor_tensor(out=BASE[:], in0=PEND[:], in1=PADDED[:], op=ALU.subtract), reads=['PEND', 'PADDED'], writes=['BASE'])
            for i in tiles:
                S.op('dve', lambda e: e.tensor_tensor(out=RANK[:, i, :], in0=RANK[:, i, :], in1=BASE[:], op=ALU.add), reads=['BASE'], writes=['RANK'])
                for k in range(4):
                    S.op('dve', lambda e: e.scalar_tensor_tensor(out=junk[:], in0=LOG[:, i, :], scalar=TOP8[:, i, k:k + 1], in1=RANK[:, i, :],
                                                                 op0=ALU.is_equal, op1=ALU.mult, accum_out=SLOTF[:, i, k:k + 1]),
                         reads=['TOP8'], writes=['rjunk', 'RANK', 'SLOTF'])
            S.op('dve', lambda e: e.tensor_copy(SLOTI[:], SLOTF[:]), reads=['SLOTF'], writes=['SLOTI'])
            S.op('pool', lambda e: e.iota(BSTi[:], pattern=[[SB, NBMAX]], base=0, channel_multiplier=0), writes=['BSTi'])
            S.op('dve', lambda e: e.tensor_copy(BST[:], BSTi[:]), reads=['BSTi'], writes=['BST'])
            S.op('pool', lambda e: e.memset(BLKE[:], 0.0), writes=['BLKE'])
            for ex in range(32):
                S.op('dve', lambda e: e.scalar_tensor_tensor(out=BLKE[:], in0=BST[:], scalar=PEND[:, ex:ex + 1], in1=BLKE[:], op0=ALU.is_ge, op1=ALU.add),
                     reads=['BST', 'PEND'], writes=['BLKE'])
            S.op('dve', lambda e: e.tensor_scalar(out=BLKE[:], in0=BLKE[:], scalar1=31.0, scalar2=None, op0=ALU.min), writes=['BLKE'])
            S.op('dve', lambda e: e.tensor_scalar(out=IDXF[:], in0=BLKE[:], scalar1=128.0, scalar2=pidx[:, 0:1], op0=ALU.mult, op1=ALU.add),
                 reads=['pidx'], writes=['IDXF'])
            S.op('dve', lambda e: e.tensor_copy(IDXI[:], IDXF[:]), reads=['IDXF'], writes=['IDXI'])
            S.op('dve', lambda e: e.tensor_copy(BIDX[:], BLKE[:]), reads=['BLKE'], writes=['BIDX'])
            S.op('pool', lambda e: e.memset(BSTi[:], 0), writes=['BSTi'])
            S.op('dve', lambda e: e.tensor_copy(IDXF2[:], BSTi[:]), reads=['BSTi'], writes=['IDXF2'])
            S.op('dve', lambda e: e.tensor_tensor(out=IDXF2[:, 2:NBMAX], in0=BLKE[:, 2:NBMAX], in1=BLKE[:, 0:NBMAX - 2], op=ALU.is_equal),
                 reads=['BLKE'], writes=['IDXF2'])
            S.op('dve', lambda e: e.tensor_scalar(out=IDXF2[:], in0=IDXF2[:], scalar1=float(1 << 20), scalar2=None, op0=ALU.mult), writes=['IDXF2'])
            for kc in range(8):
                S.op('dve', lambda e: e.tensor_scalar(out=BST[:], in0=IDXF[:], scalar1=8.0, scalar2=float(kc), op0=ALU.mult, op1=ALU.add),
                     reads=['IDXF'], writes=['BST'])
                S.op('dve', lambda e: e.tensor_tensor(out=BST[:], in0=BST[:], in1=IDXF2[:], op=ALU.add), reads=['IDXF2'], writes=['BST'])
                S.op('dve', lambda e: e.tensor_copy(RT["IDX8"][:, :, kc], BST[:]), reads=['BST'], writes=['IDX8'])
            for q in range(4):
                S.op('dve', lambda e: e.tensor_scalar(out=BST[:], in0=IDXF[:], scalar1=4.0, scalar2=float(q), op0=ALU.mult, op1=ALU.add),
                     reads=['IDXF'], writes=['BST'])
                S.op('dve', lambda e: e.tensor_tensor(out=BST[:], in0=BST[:], in1=IDXF2[:], op=ALU.add), reads=['IDXF2'], writes=['BST'])
                S.op('dve', lambda e: e.tensor_copy(RT["IDX4"][:, :, q], BST[:]), reads=['BST'], writes=['IDX4'])
            if RT.get('dbg') is not None:
                dd = RT['dbg']
                S.op('dve', lambda e: e.tensor_copy(dd[:, 0:32], CAR[:]), reads=['CAR'], writes=['dd'])
                S.op('dve', lambda e: e.tensor_copy(dd[:, 32:64], PADDED[:]), reads=['PADDED'], writes=['dd'])
                S.op('dve', lambda e: e.tensor_copy(dd[:, 64:96], PEND[:]), reads=['PEND'], writes=['dd'])
                S.op('dve', lambda e: e.tensor_copy(dd[:, 96:128], MASK[:, 0, :]), writes=['dd'])
                S.op('dve', lambda e: e.tensor_copy(dd[:, 128:160], RANK[:, 1, :]), writes=['dd'])
                S.op('dve', lambda e: e.tensor_copy(dd[:, 160:192], ci[:]), writes=['dd'])
        S.barrier()

    def phase_scatter(tiles, NB):
        SLOTI = RT["SLOTI"]
        with contextlib.ExitStack() as es:
            def al(name, shape, dt):
                return es.enter_context(SBT(name, shape, dt))
            zt = al("zt", [128, D], BF16)
            vt = [al("svt%d" % j, [128, D], BF16) for j in range(3)]
            for n, i in enumerate(tiles):
                j = n % 3
                S.dma('sp', lambda e: e.dma_start(out=vt[j][:], in_=v2_tm[i * 128:(i + 1) * 128, :]), writes=[('svt', j)])
                for k in range(4):
                    S.dma('pool', lambda e: e.indirect_dma_start(out=xs_d, out_offset=bass.IndirectOffsetOnAxis(ap=SLOTI[:, i, k:k + 1], axis=0),
                                                                 in_=vt[j][:], in_offset=None), reads=[('svt', j)], writes=[('xs', i, k)])
        S.barrier()

    def phase_moe(layer, NB):
        w1d = I("moe_w1_%d" % layer)
        w2d = I("moe_w2_%d" % layer)
        b1d = I("moe_b1_%d" % layer)
        b2d = I("moe_b2_%d" % layer)
        IDXI, BIDX = RT["IDXI"], RT["BIDX"]
        w1v = w1d.rearrange("a b c -> (a b) c")
        w2v = w2d.rearrange("a (q t) c -> (a q) (t c)", t=2)
        with contextlib.ExitStack() as es:
            def al(name, shape, dt):
                return es.enter_context(SBT(name, shape, dt))
            W1 = [al("W1_%d" % j, [128, 8, 2048], BF16) for j in range(2)]
            W2 = [al("W2_%d" % j, [128, 8, 1024], BF16) for j in range(2)]
            B1C = [al("B1C_%d" % j, [128, 16], F32) for j in range(2)]
            B2R = [al("B2R_%d" % j, [128, D], F32) for j in range(2)]
            XS = [al("XS_%d" % j, [128, D], BF16) for j in range(2 * NSUB)]
            XST2 = [al("XST%d" % j_, [128, 8, SB], BF16) for j_ in range(2)]
            ACTT = al("ACTT", [128, 8, SB], BF16)
            tg = [al("tg_%d" % j, [128, SB], F32) for j in range(2)]
            sg = [al("sg_%d" % j, [128, SB], F32) for j in range(2)]
            tu = [al("tu_%d" % j, [128, SB], F32) for j in range(2)]
            YT = [al("YT_%d" % j, [128, D], F32) for j in range(2)]
            ec = [0]
            xc = [0]
            yc = [0]
            bc8 = nc.gpsimd.to_reg(32 * 128 * 8 - 1)
            bc4 = nc.gpsimd.to_reg(32 * 128 * 4 - 1)

            def gathers(b):
                j = b % 2
                idx = bass.IndirectOffsetOnAxis(ap=IDXI[:, b:b + 1], axis=0)
                bidx = bass.IndirectOffsetOnAxis(ap=BIDX[:, b:b + 1], axis=0)
                for kc in range(8):
                    i8 = bass.IndirectOffsetOnAxis(ap=RT["IDX8"][:, b, kc:kc + 1], axis=0)
                    S.dma('pool', lambda e: e.indirect_dma_start(out=W1[j][:, kc, :], out_offset=None, in_=w1v, in_offset=i8, bounds_check=bc8, oob_is_err=False), writes=[('W1', j, kc)])
                for q in range(4):
                    i4 = bass.IndirectOffsetOnAxis(ap=RT["IDX4"][:, b, q:q + 1], axis=0)
                    S.dma('pool', lambda e: e.indirect_dma_start(out=W2[j][:, 2 * q:2 * q + 2, :].rearrange("p a b -> p (a b)"), out_offset=None,
                                                                 in_=w2v, in_offset=i4, bounds_check=bc4, oob_is_err=False), writes=[('W2', j, q)])
                S.dma('pool', lambda e: e.indirect_dma_start(out=B1C[j][:], out_offset=None, in_=b1d, in_offset=idx), writes=[('B1C', j)])
                S.dma('pool', lambda e: e.indirect_dma_start(out=B2R[j][:], out_offset=None, in_=b2d, in_offset=bidx), writes=[('B2R', j)])

            def xloads(b):
                for st_ in range(NSUB):
                    xj = (b % 2) * NSUB + st_
                    r0 = b * SB + st_ * 128
                    S.dma('sp', lambda e: e.dma_start(out=XS[xj][:], in_=xs_d[r0:r0 + 128, :]), writes=[('XS', xj)])

            def do_tr(b):
                j = b % 2
                for st_ in range(NSUB):
                    xj = (b % 2) * NSUB + st_
                    pbb = st_ % 2
                    for kc in range(8):
                        S.op('pe', lambda e: e.transpose(psb[pbb][:, kc * 128:(kc + 1) * 128], XS[xj][:, kc::8], identb[:]), reads=[('XS', xj)], writes=[PBB(pbb)])
                    S.op('act', lambda e: e.copy(XST2[b % 2][:, :, st_ * 128:(st_ + 1) * 128], psb[pbb][:].rearrange("p (a b) -> p a b", a=8)),
                         writes=[PBB(pbb), ('XST', b % 2, st_)])

            def do_mm1(b):
                j = b % 2
                xk = [('XST', b % 2, s_) for s_ in range(NSUB)]
                for jj in range(8):
                    t = ec[0] % 2
                    ec[0] += 1
                    for half in range(2):
                        bank = 2 * t + half
                        for kc in range(8):
                            S.op('pe', lambda e: e.matmul(psum[bank][:, 0:SB], W1[j][:, kc, half * 1024:(half + 1) * 1024][:, jj::8], XST2[b % 2][:, kc, :],
                                                          start=(kc == 0), stop=(kc == 7)), reads=[('W1', j, kc)] + xk, writes=[PB(bank)])
                    S.op('dve', lambda e: e.tensor_scalar(out=tg[t][:], in0=psum[2 * t][:, 0:SB], scalar1=B1C[j][:, jj:jj + 1], scalar2=7.0,
                                                          op0=ALU.add, op1=ALU.min), reads=[('B1C', j)], writes=[PB(2 * t), ('tg', t)])
                    S.op('dve', lambda e: e.tensor_scalar(out=tu[t][:], in0=psum[2 * t + 1][:, 0:SB], scalar1=B1C[j][:, 8 + jj:9 + jj], scalar2=7.0,
                                                          op0=ALU.add, op1=ALU.min), reads=[('B1C', j)], writes=[PB(2 * t + 1), ('tu', t)])
                    S.op('act', lambda e: e.activation(out=sg[t][:], in_=tg[t][:], func=AF.Sigmoid, scale=1.702), reads=[('tg', t)], writes=[('sg', t)])
                    S.op('dve', lambda e: e.tensor_scalar(out=tu[t][:], in0=tu[t][:], scalar1=-7.0, scalar2=1.0, op0=ALU.max, op1=ALU.add), writes=[('tu', t)])
                    S.op('dve', lambda e: e.tensor_tensor(out=tu[t][:], in0=tu[t][:], in1=tg[t][:], op=ALU.mult), reads=[('tg', t)], writes=[('tu', t)])
                    S.op('dve', lambda e: e.tensor_tensor(out=ACTT[:, jj, :], in0=tu[t][:], in1=sg[t][:], op=ALU.mult),
                         reads=[('tu', t), ('sg', t)], writes=[('ACTT', jj)])

            def do_mm2(b):
                j = b % 2
                ak = [('ACTT', jj) for jj in range(8)]
                for st_ in range(NSUB):
                    yj = yc[0] % 2
                    yc[0] += 1
                    def mm2_mm(nb, jj):
                        S.op('pe', lambda e: e.matmul(psum[4 + nb][:], ACTT[:, jj, st_ * 128:(st_ + 1) * 128], W2[j][:, jj, nb * 512:(nb + 1) * 512],
                                                      start=(jj == 0), stop=(jj == 7)), reads=[('ACTT', jj), ('W2', j, jj // 2)], writes=[PB(4 + nb)])
                    if st_ == 0:
                        for nb in range(2):
                            for jj in range(7):
                                mm2_mm(nb, jj)
                        for nb in range(2):
                            mm2_mm(nb, 7)
                    else:
                        for nb in range(2):
                            for jj in range(8):
                                mm2_mm(nb, jj)
                    for nb in range(2):
                        cs = slice(nb * 512, (nb + 1) * 512)
                        S.op('dve', lambda e: e.tensor_tensor(out=YT[yj][:, cs], in0=psum[4 + nb][:], in1=B2R[j][:, cs], op=ALU.add), reads=[('B2R', j)],
                             writes=[PB(4 + nb), ('YT', yj, nb)])
                    r0 = b * SB + st_ * 128
                    S.dma('sp', lambda e: e.dma_start(out=ys_d[r0:r0 + 128, :], in_=YT[yj][:]), reads=[('YT', yj, 0), ('YT', yj, 1)], writes=[('ys', b, st_)])

            gathers(0)
            xloads(0)
            do_tr(0)
            for b in range(NB):
                if b + 1 < NB:
                    gathers(b + 1)
                    xloads(b + 1)
                do_mm1(b)
                if b + 1 < NB:
                    do_tr(b + 1)
                do_mm2(b)
        S.barrier()

    def phase_combine(layer, tiles, final):
        WG, SLOTI = RT["WG"], RT["SLOTI"]
        with contextlib.ExitStack() as es:
            def al(name, shape, dt):
                return es.enter_context(SBT(name, shape, dt))
            G2 = al("G2", [128, 2, D], F32)
            FG = al("FG", [128, D], F32)
            xt = [al("cxt%d" % j, [128, D], F32) for j in range(2)]
            Y = [[al("cY%d_%d" % (j, k), [128, D], F32) for k in range(4)] for j in range(2)]
            acc = [al("cacc%d" % j, [128, D], F32) for j in range(2)]
            sq = al("csq", [128, D], F32)
            st = al("cst", [128, 8], F32)
            for r in range(2):
                bcast_row(G2[:, r, :], modrow[layer, r:r + 1, 5 * D:6 * D], ('G2', r))
            if final:
                bcast_row(FG[:], I("final_g")[0:1, :], 'FG')
            for n, i in enumerate(tiles):
                j = n % 2
                r = 1 if i < 2 else 0
                rows = slice(i * 128, (i + 1) * 128)
                S.dma('sp', lambda e: e.dma_start(out=xt[j][:], in_=xres[rows, :]), writes=[('cxt', j)])
                for k in range(4):
                    off = bass.IndirectOffsetOnAxis(ap=SLOTI[:, i, k:k + 1], axis=0)
                    S.dma('pool', lambda e: e.indirect_dma_start(out=Y[j][k][:], out_offset=None, in_=ys_d, in_offset=off), writes=[('cY', j, k)])
                S.op('dve', lambda e: e.tensor_scalar(out=acc[j][:], in0=Y[j][0][:], scalar1=WG[:, i, 0:1], scalar2=None, op0=ALU.mult),
                     reads=[('cY', j, 0)], writes=[('cacc', j)])
                for k in range(1, 4):
                    S.op('dve', lambda e: e.scalar_tensor_tensor(out=acc[j][:], in0=Y[j][k][:], scalar=WG[:, i, k:k + 1], in1=acc[j][:],
                                                                 op0=ALU.mult, op1=ALU.add), reads=[('cY', j, k)], writes=[('cacc', j)])
                S.op('dve', lambda e: e.tensor_tensor(out=acc[j][:], in0=acc[j][:], in1=G2[:, r, :], op=ALU.mult), reads=[('G2', r)], writes=[('cacc', j)])
                S.op('dve', lambda e: e.tensor_tensor(out=acc[j][:], in0=acc[j][:], in1=xt[j][:], op=ALU.add), reads=[('cxt', j)], writes=[('cacc', j)])
                if not final:
                    S.dma('act', lambda e: e.dma_start(out=xres[rows, :], in_=acc[j][:]), reads=[('cacc', j)], writes=[('xres', i)])
                else:
                    S.op('act', lambda e: e.activation(out=sq[:], in_=acc[j][:], func=AF.Square, accum_out=st[:, 0:1]), reads=[('cacc', j)], writes=['csq', 'cst'])
                    S.op('dve', lambda e: e.tensor_scalar(out=st[:, 1:2], in0=st[:, 0:1], scalar1=1.0 / D, scalar2=EPS, op0=ALU.mult, op1=ALU.add), writes=['cst'])
                    S.op('act', lambda e: e.activation(out=st[:, 2:3], in_=st[:, 1:2], func=AF.Sqrt), writes=['cst'])
                    S.op('dve', lambda e: e.reciprocal(st[:, 3:4], st[:, 2:3]), writes=['cst'])
                    S.op('dve', lambda e: e.scalar_tensor_tensor(out=acc[j][:], in0=acc[j][:], scalar=st[:, 3:4], in1=FG[:], op0=ALU.mult, op1=ALU.mult),
                         reads=['FG'], writes=[('cacc', j), 'cst'])
                    S.dma('act', lambda e: e.dma_start(out=out_d[(i - 2) * 128:(i - 1) * 128, :], in_=acc[j][:]), reads=[('cacc', j)], writes=[('out', i)])
        S.barrier()

    qT_d = dscr("qT_d", [D, 4096], BF16)
    kT_d = dscr("kT_d", [256, T], BF16)
    vA_d = dscr("vA_d", [T, 256], BF16)
    oT_d = dscr("oT_d", [D, 4096], BF16)

    def phase_A1(uT):
        wv = I("od_w_in").rearrange("(kc p) n -> p kc n", p=128)
        with contextlib.ExitStack() as es:
            def al(name, shape, dt):
                return es.enter_context(SBT(name, shape, dt))
            W = al("aW", [128, 8, 1536], BF16)
            GQ = al("aGQ", [128, 128], F32)
            GK = al("aGK", [128, 128], F32)
            COS = [al("aCOS%d" % j, [128, 128], F32) for j in range(2)]
            SIN = [al("aSIN%d" % j, [128, 128], F32) for j in range(2)]
            xf = [al("axf%d" % j, [128, 1536], F32) for j in range(2)]
            sq2 = [al("asq%d" % j_, [128, 1280], F32) for j_ in range(2)]
            st2 = [al("ast%d" % j_, [128, 40], F32) for j_ in range(2)]
            kn2 = [al("akn%d" % j_, [128, 1280], F32) for j_ in range(2)]
            t12 = [al("at1%d" % j_, [128, 1280], F32) for j_ in range(2)]
            t22 = [al("at2%d" % j_, [128, 1280], F32) for j_ in range(2)]
            kr = [al("akr%d" % j, [128, 1280], BF16) for j in range(2)]
            vb = [al("avb%d" % j, [128, 256], BF16) for j in range(2)]
            xT = [al("axT%d" % j, [128, 10, 128], BF16) for j in range(2)]
            for blk in range(3):
                S.dma('pool', lambda e: e.dma_start(out=W[:, :, blk * 512:(blk + 1) * 512], in_=wv[:, :, blk * 512:(blk + 1) * 512]), writes=[('aW', blk)])
            bcast_row(GQ[:], I("od_qk_g")[0:1, :], 'aGQ')
            bcast_row(GK[:], I("od_qk_g")[1:2, :], 'aGK')
            def hdrvars(i):
                lat = i >= 2
                j = i % 2
                rows = slice(i * 128, (i + 1) * 128)
                blks = [0, 1, 2] if lat else [2]
                h0 = 0 if lat else 8
                sq, st, kn, t1, t2 = sq2[j], st2[j], kn2[j], t12[j], t22[j]
                KSQ, KST = ('asq', j), ('ast', j)
                return lat, j, rows, blks, h0, sq, st, kn, t1, t2, KSQ, KST

            def stageA(i):
                lat, j, rows, blks, h0, sq, st, kn, t1, t2, KSQ, KST = hdrvars(i)
                if lat:
                    tr = slice((i - 2) * 128, (i - 1) * 128)
                    S.dma('pool', lambda e: e.dma_start(out=COS[j][:], in_=I("rope_cos")[tr, :]), writes=[('aCOS', j)])
                    S.dma('pool', lambda e: e.dma_start(out=SIN[j][:], in_=I("rope_sin")[tr, :]), writes=[('aSIN', j)])
                for blk in blks:
                    pb = blk
                    for kc in range(8):
                        S.op('pe', lambda e: e.matmul(psum[pb][:], uT[:, kc, rows], W[:, kc, blk * 512:(blk + 1) * 512], start=(kc == 0), stop=(kc == 7)),
                             reads=[('aW', blk)], writes=[PB(pb)])
                    S.op('act', lambda e: e.copy(xf[j][:, blk * 512:(blk + 1) * 512], psum[pb][:]), writes=[PB(pb), ('axf', j, blk)])
                S.op('pool', lambda e: e.tensor_copy(vb[j][:], xf[j][:, 1280:1536]), reads=[('axf', j, 2)], writes=[('avb', j)])
                S.dma('sp', lambda e: e.dma_start(out=vA_d[rows, :], in_=vb[j][:]), reads=[('avb', j)], writes=[('vA', i)])

            def stageB(i):
                lat, j, rows, blks, h0, sq, st, kn, t1, t2, KSQ, KST = hdrvars(i)
                c0 = h0 * 128
                nh = 10 - h0
                S.op('act', lambda e: e.activation(out=sq[:, c0:1280], in_=xf[j][:, c0:1280], func=AF.Square),
                     reads=[('axf', j, 0), ('axf', j, 1), ('axf', j, 2)], writes=[KSQ])
                S.op('dve', lambda e: e.tensor_reduce(out=st[:, h0:10], in_=sq[:, c0:1280].rearrange("p (h d) -> p h d", d=128), axis=AX.X, op=ALU.add),
                     reads=[KSQ], writes=[KST])
                S.op('dve', lambda e: e.tensor_scalar(out=st[:, 10 + h0:20], in0=st[:, h0:10], scalar1=1.0 / 128, scalar2=EPS, op0=ALU.mult, op1=ALU.add), writes=[KST])
                S.op('act', lambda e: e.activation(out=st[:, 20 + h0:30], in_=st[:, 10 + h0:20], func=AF.Sqrt), writes=[KST])
                S.op('dve', lambda e: e.reciprocal(st[:, 30 + h0:40], st[:, 20 + h0:30]), writes=[KST])
                for h in range(h0, 10):
                    hs = slice(h * 128, (h + 1) * 128)
                    G = GQ if h < 8 else GK
                    dst = kn[:, hs] if lat else kr[j][:, hs]
                    dk = ('akn', j, h) if lat else ('akr', j, h)
                    S.op('dve', lambda e: e.scalar_tensor_tensor(out=dst, in0=xf[j][:, hs], scalar=st[:, 30 + h:31 + h], in1=G[:], op0=ALU.mult, op1=ALU.mult),
                         reads=['aGQ', 'aGK', KST], writes=[dk])
                if lat:
                    snv = SIN[j][:].rearrange("p (b c) -> p b c", b=2)
                    for h in range(h0, 10):
                        hs = slice(h * 128, (h + 1) * 128)
                        knv = kn[:, hs].rearrange("p (b c) -> p b c", b=2)
                        t2v = t2[:, hs].rearrange("p (b c) -> p b c", b=2)
                        S.op('dve', lambda e: e.tensor_tensor(out=t1[:, hs], in0=kn[:, hs], in1=COS[j][:], op=ALU.mult), reads=[('akn', j, h), ('aCOS', j)],
                             writes=[('at1', j, h)])
                        S.op('pool', lambda e: e.tensor_tensor(out=t2v[:, :, 0:32], in0=knv[:, :, 32:64], in1=snv[:, :, 0:32], op=ALU.mult),
                             reads=[('akn', j, h), ('aSIN', j)], writes=[('at2', j, h)])
                        S.op('pool', lambda e: e.tensor_tensor(out=t2v[:, :, 32:64], in0=knv[:, :, 0:32], in1=snv[:, :, 32:64], op=ALU.mult),
                             reads=[('akn', j, h), ('aSIN', j)], writes=[('at2', j, h)])
                    for h in range(h0, 10):
                        hs = slice(h * 128, (h + 1) * 128)
                        S.op('dve', lambda e: e.tensor_tensor(out=kr[j][:, hs], in0=t1[:, hs], in1=t2[:, hs], op=ALU.add), reads=[('at1', j, h), ('at2', j, h)],
                             writes=[('akr', j, h)])

            def stageC(i):
                lat, j, rows, blks, h0, sq, st, kn, t1, t2, KSQ, KST = hdrvars(i)
                if lat:
                    for h in range(8):
                        S.op('pe', lambda e: e.transpose(psb[0][:, h * 128:(h + 1) * 128], kr[j][:, h * 128:(h + 1) * 128], identb[:]),
                             reads=[('akr', j, h)], writes=[PBB(0)])
                    S.op('act', lambda e: e.copy(xT[j][:, 0:8, :].rearrange("p a b -> p (a b)"), psb[0][:]), writes=[PBB(0), ('axT', j, 0)])
                    S.dma('sp', lambda e: e.dma_start(out=qT_d[:, (i - 2) * 128:(i - 1) * 128].rearrange("(h d) t -> d h t", d=128), in_=xT[j][:, 0:8, :]),
                          reads=[('axT', j, 0)], writes=[('qT_d', i)])
                for h in range(8, 10):
                    S.op('pe', lambda e: e.transpose(psb[1][:, (h - 8) * 128:(h - 7) * 128], kr[j][:, h * 128:(h + 1) * 128], identb[:]),
                         reads=[('akr', j, h)], writes=[PBB(1)])
                S.op('act', lambda e: e.copy(xT[j][:, 8:10, :].rearrange("p a b -> p (a b)"), psb[1][:, 0:256]), writes=[PBB(1), ('axT', j, 1)])
                S.dma('sp', lambda e: e.dma_start(out=kT_d[:, rows].rearrange("(h d) t -> d h t", d=128), in_=xT[j][:, 8:10, :]),
                      reads=[('axT', j, 1)], writes=[('kT_d', i)])

            stageA(0)
            for i in range(NT):
                if i + 1 < NT:
                    stageA(i + 1)
                stageB(i)
                stageC(i)
        S.barrier()

    def phase_A2():
        scale = 128.0 ** -0.5
        with contextlib.ExitStack() as es:
            def al(name, shape, dt):
                return es.enter_context(SBT(name, shape, dt))
            KT = al("bKT", [128, T], BF16)
            V = al("bV", [128, NT, 128], BF16)
            onesb = al("bones", [128, 128], BF16)
            QT4 = [al("bQT%d" % j, [128, 4, 128], BF16) for j in range(2)]
            PT = [al("bPT%d" % j, [128, 512], BF16) for j in range(4)]
            rs = [al("brs%d" % j, [128, 512], F32) for j in range(2)]
            PTS = [al("bPTS%d" % j, [128, 512], BF16) for j in range(2)]
            ot = [al("bot%d" % j, [128, 4, 128], BF16) for j in range(2)]
            S.op('pool', lambda e: e.memset(onesb[:], 1.0), writes=['bones'])
            pc = 0
            it = 0
            for g in range(2):
                S.dma('sp', lambda e: e.dma_start(out=KT[:], in_=kT_d[g * 128:(g + 1) * 128, :]), writes=['bKT'])
                S.dma('sp', lambda e: e.dma_start(out=V[:], in_=vA_d[:, g * 128:(g + 1) * 128].rearrange("(n p) d -> p n d", p=128)), writes=['bV'])
                for qi in range(32):
                    j = it % 2
                    it += 1
                    bo, bs_ = 2 + 2 * j, 3 + 2 * j
                    S.dma('sp', lambda e: e.dma_start(out=QT4[j][:], in_=qT_d[g * 512:(g + 1) * 512, qi * 128:(qi + 1) * 128].rearrange("(h d) t -> d h t", d=128)),
                          writes=[('bQT', j)])

                    def st_mm(kt):
                        bank = kt % 2
                        S.op('pe', lambda e: e.matmul(psum[bank][:], KT[:, kt * 128:(kt + 1) * 128], QT4[j][:].rearrange("p a b -> p (a b)"), start=True, stop=True),
                             reads=['bKT', ('bQT', j)], writes=[PB(bank)])
                    st_mm(0)
                    for kt in range(NT):
                        bank = kt % 2
                        p_ = pc % 4
                        pc += 1
                        S.op('act', lambda e: e.activation(out=PT[p_][:], in_=psum[bank][:], func=AF.Exp, scale=scale), writes=[PB(bank), ('bPT', p_)])
                        if kt + 1 < NT:
                            st_mm(kt + 1)
                        S.op('pe', lambda e: e.matmul(psum[bo][:], V[:, kt, :], PT[p_][:], start=(kt == 0), stop=(kt == NT - 1)),
                             reads=[('bPT', p_), 'bV'], writes=[PB(bo)])
                        S.op('pe', lambda e: e.matmul(psum[bs_][:], onesb[:], PT[p_][:], start=(kt == 0), stop=(kt == NT - 1)),
                             reads=[('bPT', p_), 'bones'], writes=[PB(bs_)])
                    S.op('dve', lambda e: e.reciprocal(rs[j][:], psum[bs_][:]), writes=[PB(bs_), ('brs', j)])
                    S.op('dve', lambda e: e.tensor_tensor(out=ot[j][:].rearrange("p a b -> p (a b)"), in0=psum[bo][:], in1=rs[j][:], op=ALU.mult),
                         reads=[('brs', j)], writes=[PB(bo), ('bot', j)])
                    S.dma('pool', lambda e: e.dma_start(out=oT_d[g * 512:(g + 1) * 512, qi * 128:(qi + 1) * 128].rearrange("(h d) t -> d h t", d=128), in_=ot[j][:]),
                          reads=[('bot', j)], writes=[('oT_d', g, qi)])
        S.barrier()

    def phase_A3():
        with contextlib.ExitStack() as es:
            def al(name, shape, dt):
                return es.enter_context(SBT(name, shape, dt))
            WO = al("cWO", [128, 8, D], BF16)
            G1 = al("cG1", [128, D], F32)
            OT = [al("cOT%d" % j, [128, 8, 128], BF16) for j in range(2)]
            xt = [al("cx%d" % j, [128, D], F32) for j in range(2)]
            tmp = al("ctmp", [128, D], F32)
            S.dma('pool', lambda e: e.dma_start(out=WO[:], in_=I("od_w_out").rearrange("(kc p) n -> p kc n", p=128)), writes=['cWO'])
            bcast_row(G1[:], modrow[1, 0:1, 2 * D:3 * D], 'cG1')
            for qi in range(32):
                j = qi % 2
                rows = slice((qi + 2) * 128, (qi + 3) * 128)
                S.dma('sp', lambda e: e.dma_start(out=OT[j][:], in_=oT_d[:, qi * 128:(qi + 1) * 128].rearrange("(h d) t -> d h t", d=128)), writes=[('cOT', j)])
                S.dma('sp', lambda e: e.dma_start(out=xt[j][:], in_=xres[rows, :]), writes=[('cx', j)])
                for nb in range(2):
                    pb = (2 * qi + nb) % 4
                    for kc in range(8):
                        S.op('pe', lambda e: e.matmul(psum[pb][:], OT[j][:, kc, :], WO[:, kc, nb * 512:(nb + 1) * 512], start=(kc == 0), stop=(kc == 7)),
                             reads=[('cOT', j), 'cWO'], writes=[PB(pb)])
                    cs = slice(nb * 512, (nb + 1) * 512)
                    S.op('dve', lambda e: e.tensor_tensor(out=tmp[:, cs], in0=psum[pb][:], in1=G1[:, cs], op=ALU.mult), reads=['cG1'], writes=[PB(pb), ('ctmp', nb)])
                    S.op('pool', lambda e: e.tensor_tensor(out=tmp[:, cs], in0=tmp[:, cs], in1=xt[j][:, cs], op=ALU.add), reads=[('cx', j)], writes=[('ctmp', nb)])
                S.dma('pool', lambda e: e.dma_start(out=xres[rows, :], in_=tmp[:]), reads=[('ctmp', 0), ('ctmp', 1)], writes=[('xres', qi)])
        S.barrier()

    def small_dump():
        W_ = NT * 32 + NT * 4 + NT * 4 + 2 * NBMAX
        dl = dscr("dsmall_scr", [128, W_], F32)
        with SBT("dsm", [128, W_], F32) as dsm:
            o = 0
            for nm, n in (("LOG", NT * 32), ("WG", NT * 4), ("SLOTI", NT * 4)):
                S.op('dve', lambda e: e.tensor_copy(dsm[:, o:o + n], RT[nm][:].rearrange("p a b -> p (a b)")), writes=['dsm'])
                o += n
            for nm in ("IDXI", "BIDX"):
                S.op('dve', lambda e: e.tensor_copy(dsm[:, o:o + NBMAX], RT[nm][:]), writes=['dsm'])
                o += NBMAX
            S.dma('sp', lambda e: e.dma_start(out=dl, in_=dsm[:]), reads=['dsm'], writes=['dl'])
            S.barrier()
        return ('small', dl, [128, W_], F32)

    def copy_xin_to_xres():
        with SBT("cpx", [128, D], F32) as cpx:
            for i in range(NT):
                S.dma('sp', lambda e: e.dma_start(out=cpx[:], in_=I("xin")[i * 128:(i + 1) * 128, :]), writes=['cpx'])
                S.dma('sp', lambda e: e.dma_start(out=xres[i * 128:(i + 1) * 128, :], in_=cpx[:]), reads=['cpx'], writes=[('xres', i)])
        S.barrier()

    phase_mod()
    if stop_after == 'mod':
        return finish_dbg([('modrow', modrow.rearrange("a b c -> (a b) c"), [4, 6 * D], F32)])

    if stop_after not in ('A_only', 'M1_only'):
        uT_guard = SBT("uT", [128, 8, T], BF16)
        uT = uT_guard.__enter__()
        phase_norm_T(0, I("xin"), uT)
        phase_E1(uT)
        if stop_after == 'E1':
            return finish_dbg([('qkT', qkT, [2048, T], BF16), ('v_tm', v_tm, [T, D], BF16), ('so_tm', so_tm, [T, D], BF16),
                               ('g_tm', g_tm, [T, 16], F32), ('xrT', xrT, [D, T], F32), ('ygT', ygT, [D, T], BF16)])
        uT_guard.__exit__(None, None, None)
        phase_E2()
        if stop_after == 'E2':
            return finish_dbg([('hlT', hlT, [D, T], BF16)])
        phase_E3()
        if stop_after == 'E3':
            return finish_dbg([('hm_f', hm_d[0], [T, D], F32), ('hm_b', hm_d[1], [T, D], F32)])
        phase_E4()
        if stop_after == 'E4':
            return finish_dbg([('xres', xres, [T, D], F32)])
        NB0 = (4 * T) // SB + 32
        phase_N2(0, range(NT), RT["LOG"])
        phase_route(range(NT), NB0)
        phase_scatter(range(NT), NB0)
        if stop_after == 'R0':
            return finish_dbg([small_dump(), ('xs', xs_d, [NBMAX * SB, D], BF16), ('v2', v2_tm, [T, D], BF16)])
        phase_moe(0, NB0)
        phase_combine(0, range(NT), False)
        if stop_after == 'M0':
            return finish_dbg([('xres', xres, [T, D], F32)])
    else:
        copy_xin_to_xres()

    if stop_after != 'M1_only':
        uT_guard = SBT("uT", [128, 8, T], BF16)
        uT = uT_guard.__enter__()
        phase_norm_T(1, xres, uT)
        phase_A1(uT)
        uT_guard.__exit__(None, None, None)
        phase_A2()
        phase_A3()
        if stop_after in ('A', 'A_only'):
            return finish_dbg([('xres', xres, [T, D], F32)])
    NB1 = (4 * 4096) // SB + 32
    lat_tiles = range(2, NT)
    phase_N2(1, lat_tiles, RT["LOG"])
    phase_route(lat_tiles, NB1)
    phase_scatter(lat_tiles, NB1)
    phase_moe(1, NB1)
    phase_combine(1, lat_tiles, True)
    if stop_after == 'M1_only':
        return finish_dbg([('outc', out_d, [4096, D], F32)])
    S.barrier()
    return nc, dbg_out, used_inputs


def host_inputs(b, inp, names=None):
    f = lambda a: np.ascontiguousarray(a, dtype=np.float32)
    want = lambda k: names is None or k in names
    m = {}
    if want("xin"):
        m["xin"] = f(np.concatenate([inp["ctx"][b], inp["x"][b]], axis=0))
    if want("ccols"):
        m["ccols"] = f(np.concatenate([inp["c"][b].reshape(8, 128).T, inp["c_ctx"].reshape(8, 128).T], axis=1))
    for k in ["mod_w", "mod_b", "norm1_g", "norm2_g", "moe_w_r", "moe_b_r"]:
        if want(k):
            m[k] = f(inp[k])
    if want("final_g"):
        m["final_g"] = f(inp["final_g"].reshape(1, D))
    if want("ev_w_in"):
        m["ev_w_in"] = f(inp["ev_w_in"][0])
    if want("ev_qkcw"):
        qk = np.concatenate([inp["ev_qk_conv_w"][0], inp["ev_qk_conv_b"][0][None]], axis=0)
        m["ev_qkcw"] = f(qk.T.reshape(16, 128, 5).transpose(1, 0, 2))
    if want("ev_gate_b"):
        m["ev_gate_b"] = f(inp["ev_gate_b"][0].reshape(1, 16))
    if want("ev_mnorm_g"):
        m["ev_mnorm_g"] = f(inp["ev_mnorm_g"][0].reshape(1, D))
    if want("ev_lrucw"):
        lc = np.concatenate([inp["ev_lru_conv_w"][0], inp["ev_lru_conv_b"][0][None]], axis=0)
        m["ev_lrucw"] = f(lc.T.reshape(8, 128, 5).transpose(1, 0, 2))
    if want("ev_lru_w") or want("ev_lru_b"):
        lw = np.zeros((4, 8, 128, 128), np.float32)
        lb = np.zeros((128, 8, 4), np.float32)
        for z in range(2):
            for gi, (wk, bk) in enumerate([("ev_lru_wa", "ev_lru_ba"), ("ev_lru_wx", "ev_lru_bx")]):
                for cc_ in range(8):
                    for h in range(2):
                        n = cc_ * 2 + h
                        lw[z * 2 + gi, cc_, h * 64:(h + 1) * 64, h * 64:(h + 1) * 64] = inp[wk][0, z, n]
                        lb[h * 64:(h + 1) * 64, cc_, z * 2 + gi] = inp[bk][0, z, n]
        m["ev_lru_w"] = lw
        m["ev_lru_b"] = lb
    if want("ev_lru_lam"):
        m["ev_lru_lam"] = f(inp["ev_lru_lam"][0].reshape(2, 8, 128).transpose(2, 1, 0))
    if want("ev_w_out"):
        m["ev_w_out"] = f(inp["ev_w_out"][0])
    if want("od_w_in"):
        m["od_w_in"] = f(inp["od_w_in"][0])
    if want("od_qk_g"):
        m["od_qk_g"] = f(np.stack([inp["od_q_norm_g"][0], inp["od_k_norm_g"][0]]))
    if want("od_w_out"):
        m["od_w_out"] = f(inp["od_w_out"][0])
    if want("rope_cos") or want("rope_sin"):
        pos = np.arange(4096)
        row = (pos // 64).astype(np.float32)
        col = (pos % 64).astype(np.float32)
        inv = (10000.0 ** (-np.arange(0, 64, 2, dtype=np.float32) / 64)).astype(np.float32)
        ar = row[:, None] * inv[None]
        ac = col[:, None] * inv[None]
        m["rope_cos"] = f(np.concatenate([np.cos(ar), np.cos(ar), np.cos(ac), np.cos(ac)], axis=1))
        m["rope_sin"] = f(np.concatenate([-np.sin(ar), np.sin(ar), -np.sin(ac), np.sin(ac)], axis=1))
    for l in range(2):
        if want("moe_w1_%d" % l):
            m["moe_w1_%d" % l] = f(inp["moe_w1"][l]).reshape(32 * 128, 8, 2048)
        if want("moe_w2_%d" % l):
            m["moe_w2_%d" % l] = f(inp["moe_w2"][l]).reshape(32 * 128, 8, 1024)
    for l in range(2):
        if want("moe_b1_%d" % l):
            m["moe_b1_%d" % l] = f(inp["moe_b1"][l].reshape(32, 2, 128, 8).transpose(0, 2, 1, 3).reshape(32 * 128, 16))
        if want("moe_b2_%d" % l):
            m["moe_b2_%d" % l] = f(inp["moe_b2"][l])
    if names is not None:
        m = {k: v for k, v in m.items() if k in names}
    return m


_CACHE = {}


def kernel(**inputs):
    if 'nc' not in _CACHE:
        _CACHE['nc'] = build()
    nc, _, used = _CACHE['nc']
    names = set(used.keys())
    in_maps = [host_inputs(b, inputs, names) for b in range(8)]
    res = run_bass_kernel_spmd(nc, in_maps, core_ids=list(range(8)))
    return np.stack([r["out"] for r in res.results], axis=0).astype(np.float32)
```

```python
import contextlib
import numpy as np
import concourse.bass as bass
import concourse.mybir as mybir
from concourse.bass_utils import run_bass_kernel_spmd

F32 = mybir.dt.float32
BF16 = mybir.dt.bfloat16
I32 = mybir.dt.int32
U32 = mybir.dt.uint32
ALU = mybir.AluOpType
AF = mybir.ActivationFunctionType

T = 4352
NT = 34
D = 1024
NCTX = 256
EPS = 1e-6


class Sched:
    NSLOT = 6

    def __init__(self, nc):
        self.nc = nc
        self.engs = {'pe': nc.tensor, 'act': nc.scalar, 'dve': nc.vector,
                     'pool': nc.gpsimd, 'sp': nc.sync}
        self.sem = {}
        self.cnt = {}
        for k in self.engs:
            self.sem[k] = nc.alloc_semaphore('s_' + k)
            self.cnt[k] = 0
        self.dslots = {}
        self.dcnt = {}
        self.dnext = {}
        self.nslot = {'sp': 8, 'pool': 8, 'act': 2}
        for q in ('sp', 'pool', 'act'):
            self.dslots[q] = [nc.alloc_semaphore('d_%s%d' % (q, i)) for i in range(self.nslot[q])]
            self.dcnt[q] = [0] * self.nslot[q]
            self.dnext[q] = 0
        self.waited = {k: {} for k in self.engs}
        self.res = {}

    def _semof(self, tok):
        if tok[0] == 'e':
            return self.sem[tok[1]]
        return self.dslots[tok[1][0]][tok[1][1]]

    def _wait(self, e, tok):
        key = (tok[0], tok[1])
        if self.waited[e].get(key, 0) >= tok[2]:
            return
        self.engs[e].wait_ge(self._semof(tok), tok[2])
        self.waited[e][key] = tok[2]

    def _deps(self, e, reads, writes):
        deps = []
        for r in reads:
            st = self.res.get(r)
            if st and st['w'] is not None:
                deps.append(st['w'])
        for w in writes:
            st = self.res.get(w)
            if st:
                if st['w'] is not None:
                    deps.append(st['w'])
                deps.extend(st['r'].values())
        for tok in deps:
            if e == 'pe' and tok[0] == 'e' and tok[1] == 'pe':
                continue
            self._wait(e, tok)

    def _commit(self, tok, reads, writes):
        for r in reads:
            st = self.res.setdefault(r, {'w': None, 'r': {}})
            st['r'][(tok[0], tok[1])] = tok
        for w in writes:
            self.res[w] = {'w': tok, 'r': {}}

    def op(self, e, fn, reads=(), writes=()):
        self._deps(e, reads, writes)
        inst = fn(self.engs[e])
        self.cnt[e] += 1
        inst.then_inc(self.sem[e], 1)
        self._commit(('e', e, self.cnt[e]), reads, writes)
        return inst

    def dma(self, q, fn, reads=(), writes=()):
        s = self.dnext[q]
        self.dnext[q] = (s + 1) % self.nslot[q]
        if self.dcnt[q][s] > 0:
            self._wait(q, ('d', (q, s), 16 * self.dcnt[q][s]))
        self._deps(q, reads, writes)
        inst = fn(self.engs[q])
        self.dcnt[q][s] += 1
        inst.then_inc(self.dslots[q][s], 16)
        self._commit(('d', (q, s), 16 * self.dcnt[q][s]), reads, writes)
        return inst

    def barrier(self):
        toks = []
        for q in self.dslots:
            for s in range(self.nslot[q]):
                if self.dcnt[q][s] > 0:
                    toks.append(('d', (q, s), 16 * self.dcnt[q][s]))
        for k in self.engs:
            if self.cnt[k] > 0:
                toks.append(('e', k, self.cnt[k]))
        for e in self.engs:
            for tok in toks:
                self._wait(e, tok)
        self.res = {}


class Rot:
    def __init__(self, bufs, name):
        self.bufs = bufs
        self.name = name
        self.i = 0

    def next(self):
        b = self.bufs[self.i % len(self.bufs)]
        k = (self.name, self.i % len(self.bufs))
        self.i += 1
        return b, k


AX = mybir.AxisListType


def build(stop_after=None, layers=(0, 1)):
    nc = bass.Bass("TRN2", target_bir_lowering=False)
    S = Sched(nc)
    used_inputs = {}
    _ctr = [0]

    def SBT(name, shape, dt):
        _ctr[0] += 1
        return nc.sbuf_tensor("%s_u%d" % (name, _ctr[0]), shape, dt)

    IN_SPECS = {
        "xin": [T, D], "ccols": [128, 16], "mod_w": [2, D, 6 * D], "mod_b": [2, 6 * D],
        "norm1_g": [2, D], "norm2_g": [2, D], "final_g": [1, D],
        "ev_w_in": [D, 6160], "ev_qkcw": [128, 16, 5], "ev_gate_b": [1, 16], "ev_mnorm_g": [1, D],
        "ev_lrucw": [128, 8, 5], "ev_lru_w": [4, 8, 128, 128], "ev_lru_b": [128, 8, 4], "ev_lru_lam": [128, 8, 2],
        "ev_w_out": [2 * D, D], "od_w_in": [D, 1536], "od_qk_g": [2, 128], "od_w_out": [D, D],
        "rope_cos": [4096, 128], "rope_sin": [4096, 128],
        "moe_w_r": [2, D, 32], "moe_b_r": [2, 32],
        "moe_w1_0": [32 * 128, 8, 2048], "moe_w1_1": [32 * 128, 8, 2048],
        "moe_b1_0": [32 * 128, 16], "moe_b1_1": [32 * 128, 16],
        "moe_w2_0": [32 * 128, 8, 1024], "moe_w2_1": [32 * 128, 8, 1024],
        "moe_b2_0": [32, 1024], "moe_b2_1": [32, 1024],
    }

    def I(name):
        if name not in used_inputs:
            used_inputs[name] = nc.dram_tensor(name, list(IN_SPECS[name]), F32, kind="ExternalInput").ap()
        return used_inputs[name]

    def dscr(name, shape, dt=F32):
        return nc.dram_tensor(name, list(shape), dt, kind="Internal").ap()

    dbg_out = {}

    def dbgt(name, shape, dt=F32):
        dbg_out[name] = nc.dram_tensor("dbg_" + name, list(shape), dt, kind="ExternalOutput").ap()
        return dbg_out[name]

    def finish_dbg(pairs):
        S.barrier()
        for name, src, shape, dt in pairs:
            d = dbgt(name, shape, dt)
            rows, cols = shape[0], int(np.prod(shape[1:]))
            s2 = src if len(shape) == 2 else src.rearrange("a b c -> a (b c)")
            d2 = d if len(shape) == 2 else d.rearrange("a b c -> a (b c)")
            with SBT("dbgbuf_" + name, [128, cols], dt) as buf:
                for r0 in range(0, rows, 128):
                    n = min(128, rows - r0)
                    S.dma('sp', lambda e: e.dma_start(out=buf[0:n, :], in_=s2[r0:r0 + n, :]), writes=['dbgbuf'])
                    S.dma('sp', lambda e: e.dma_start(out=d2[r0:r0 + n, :], in_=buf[0:n, :]), reads=['dbgbuf'], writes=[('dbgo', name, r0)])
                S.barrier()
        return nc, dbg_out, used_inputs

    out_d = nc.dram_tensor("out", [4096, D], F32, kind="ExternalOutput").ap()
    modrow = dscr("modrow", [2, 2, 6 * D])
    xres = dscr("xres", [T, D])

    ident = nc.alloc_sbuf_tensor("ident", [128, 128], F32)
    identb = nc.alloc_sbuf_tensor("identb", [128, 128], BF16)
    ones1 = nc.alloc_sbuf_tensor("ones1", [1, 128], F32)
    onesm = nc.alloc_sbuf_tensor("onesm", [128, 128], F32)
    triF = nc.alloc_sbuf_tensor("triF", [128, 128], F32)
    triB = nc.alloc_sbuf_tensor("triB", [128, 128], F32)
    triS = nc.alloc_sbuf_tensor("triS", [128, 128], F32)
    selF = nc.alloc_sbuf_tensor("selF", [128, 128], F32)
    selB = nc.alloc_sbuf_tensor("selB", [128, 128], F32)
    pidx = nc.alloc_sbuf_tensor("pidx", [128, 1], F32)
    pidx_i = nc.alloc_sbuf_tensor("pidx_i", [128, 1], I32)
    S.op('pool', lambda e: e.memset(ident[:], 0.0), writes=['ident'])
    S.op('pool', lambda e: e.affine_select(ident[:], ident[:], pattern=[[-1, 128]], compare_op=ALU.not_equal,
                                          fill=1.0, base=0, channel_multiplier=1), reads=['ident'], writes=['ident'])
    S.op('dve', lambda e: e.tensor_copy(identb[:], ident[:]), reads=['ident'], writes=['identb'])
    S.op('pool', lambda e: e.memset(ones1[:], 1.0), writes=['ones1'])
    S.op('pool', lambda e: e.memset(onesm[:], 1.0), writes=['onesm'])

    def mk_mask(t, pattern, cm, base, cmp):
        S.op('pool', lambda e: e.memset(t[:], 1.0), writes=[t.name])
        S.op('pool', lambda e: e.affine_select(t[:], t[:], pattern=pattern, compare_op=cmp, fill=0.0, base=base,
                                              channel_multiplier=cm), writes=[t.name])

    mk_mask(triF, [[1, 128]], -1, 0, ALU.is_ge)
    mk_mask(triB, [[-1, 128]], 1, 0, ALU.is_ge)
    mk_mask(triS, [[1, 128]], -1, 0, ALU.is_gt)
    mk_mask(selF, [[0, 128]], 1, -127, ALU.is_equal)
    mk_mask(selB, [[0, 128]], 1, 0, ALU.is_equal)
    S.op('pool', lambda e: e.iota(pidx_i[:], pattern=[[0, 1]], base=0, channel_multiplier=1), writes=['pidx_i'])
    S.op('dve', lambda e: e.tensor_copy(pidx[:], pidx_i[:]), reads=['pidx_i'], writes=['pidx'])

    psum = [nc.alloc_psum_tensor("ps%d" % i, [128, 512], F32) for i in range(6)]
    psb = [nc.alloc_psum_tensor("psb%d" % i, [128, 1024], BF16) for i in range(2)]

    def PB(i):
        return ('psum', i)

    def PBB(i):
        return ('psb', i)

    def bcast_row(dst, src_row, key, q='sp'):
        S.dma(q, lambda e: e.dma_start(out=dst, in_=src_row.partition_broadcast(dst.shape[0])), writes=[key])

    def phase_mod():
        mod_w, mod_b = I("mod_w"), I("mod_b")
        with SBT("cc", [128, 16], F32) as cc, SBT("csil", [128, 16], BF16) as csil, \
                SBT("mw0", [128, 8, 512], BF16) as mw0, SBT("mw1", [128, 8, 512], BF16) as mw1, SBT("mw2", [128, 8, 512], BF16) as mw2, \
                SBT("mb0", [1, 512], F32) as mb0, SBT("mb1", [1, 512], F32) as mb1, \
                SBT("mo0", [1, 512], F32) as mo0, SBT("mo1", [1, 512], F32) as mo1:
            S.dma('sp', lambda e: e.dma_start(out=cc[:], in_=I("ccols")), writes=['cc'])
            S.op('act', lambda e: e.activation(out=csil[:], in_=cc[:], func=AF.Silu), reads=['cc'], writes=['csil'])
            mws = Rot([mw0, mw1, mw2], 'mw')
            mbs = Rot([mb0, mb1], 'mb')
            mos = Rot([mo0, mo1], 'mo')
            pi = 0
            for layer in range(2):
                mwv = mod_w[layer].rearrange("(kc p) n -> p kc n", p=128)
                for cb in range(12):
                    mw, mwk = mws.next()
                    mb, mbk = mbs.next()
                    S.dma('pool', lambda e: e.dma_start(out=mw[:], in_=mwv[:, :, cb * 512:(cb + 1) * 512]), writes=[mwk])
                    S.dma('sp', lambda e: e.dma_start(out=mb[:], in_=mod_b[layer:layer + 1, cb * 512:(cb + 1) * 512]), writes=[mbk])
                    for r in range(2):
                        ps = psum[pi % 4]
                        pk = PB(pi % 4)
                        pi += 1
                        for kc in range(8):
                            S.op('pe', lambda e: e.matmul(ps[0:1, :], csil[:, r * 8 + kc:r * 8 + kc + 1], mw[:, kc, :],
                                                          start=(kc == 0), stop=(kc == 7)), reads=['csil', mwk], writes=[pk])
                        mo, mok = mos.next()
                        S.op('dve', lambda e: e.tensor_tensor(out=mo[:], in0=ps[0:1, :], in1=mb[:], op=ALU.add), reads=[mbk], writes=[pk, mok])
                        S.dma('sp', lambda e: e.dma_start(out=modrow[layer, r:r + 1, cb * 512:(cb + 1) * 512], in_=mo[:]),
                              reads=[mok], writes=[('modrow', layer, r, cb)])
        S.barrier()

    def norm_tiles(layer, which, src, tiles, consume):
        gsrc = I("norm1_g") if which == 0 else I("norm2_g")
        sh_off = 0 if which == 0 else 3 * D
        sc_off = D if which == 0 else 4 * D
        with SBT("nA", [128, 2, D], F32) as A, SBT("nSH", [128, 2, D], F32) as SH, \
                SBT("nG", [128, D], F32) as G, \
                SBT("nx0", [128, D], F32) as x0, SBT("nx1", [128, D], F32) as x1, \
                SBT("nu0", [128, D], F32) as u0, SBT("nu1", [128, D], F32) as u1, \
                SBT("nsq", [128, 2, D], F32) as sq2, SBT("nst", [128, 2, 8], F32) as st2:
            bcast_row(G[:], gsrc[layer:layer + 1, :], 'nG')
            for r in range(2):
                bcast_row(A[:, r, :], modrow[layer, r:r + 1, sc_off:sc_off + D], ('nA', r))
                bcast_row(SH[:, r, :], modrow[layer, r:r + 1, sh_off:sh_off + D], ('nSH', r))
                S.op('dve', lambda e: e.scalar_tensor_tensor(out=A[:, r, :], in0=A[:, r, :], scalar=1.0, in1=G[:],
                                                             op0=ALU.add, op1=ALU.mult), reads=['nG'], writes=[('nA', r)])
            xs = Rot([x0, x1], 'nx')
            us = Rot([u0, u1], 'nu')
            tl = list(tiles)
            pend = {}

            def pre(n_):
                i = tl[n_]
                r = 1 if i < 2 else 0
                xt, xk = xs.next()
                ut, uk = us.next()
                sq = sq2[:, n_ % 2, :]
                st = st2[:, n_ % 2, :]
                NSQ, NST = ('nsq', n_ % 2), ('nst', n_ % 2)
                S.dma('sp', lambda e: e.dma_start(out=xt[:], in_=src[i * 128:(i + 1) * 128, :]), writes=[xk])
                S.op('act', lambda e: e.activation(out=sq, in_=xt[:], func=AF.Square, accum_out=st[:, 0:1]),
                     reads=[xk], writes=[NSQ, NST])
                S.op('dve', lambda e: e.tensor_scalar(out=st[:, 1:2], in0=st[:, 0:1], scalar1=1.0 / D, scalar2=EPS,
                                                      op0=ALU.mult, op1=ALU.add), writes=[NST])
                S.op('act', lambda e: e.activation(out=st[:, 2:3], in_=st[:, 1:2], func=AF.Sqrt), writes=[NST])
                S.op('dve', lambda e: e.reciprocal(st[:, 3:4], st[:, 2:3]), writes=[NST])
                S.op('dve', lambda e: e.scalar_tensor_tensor(out=ut[:], in0=xt[:], scalar=st[:, 3:4], in1=A[:, r, :],
                                                             op0=ALU.mult, op1=ALU.mult), reads=[xk, ('nA', r)], writes=[uk, NST])
                S.op('dve', lambda e: e.tensor_tensor(out=ut[:], in0=ut[:], in1=SH[:, r, :], op=ALU.add),
                     reads=[('nSH', r)], writes=[uk])
                pend[n_] = (i, ut, uk)

            pre(0)
            for n_ in range(len(tl)):
                if n_ + 1 < len(tl):
                    pre(n_ + 1)
                consume(*pend.pop(n_))

    def phase_norm_T(layer, src, uT, tiles=range(NT)):
        def consume(i, ut, uk):
            for half in range(2):
                pb = (i * 2 + half) % 4
                for j in range(4):
                    kc = half * 4 + j
                    S.op('pe', lambda e: e.transpose(psum[pb][:, j * 128:(j + 1) * 128], ut[:, kc * 128:(kc + 1) * 128], ident[:]),
                         reads=[uk, 'ident'], writes=[PB(pb)])
                S.op('act', lambda e: e.copy(uT[:, half * 4:half * 4 + 4, i * 128:(i + 1) * 128],
                                             psum[pb][:].rearrange("p (a b) -> p a b", a=4)),
                     writes=[PB(pb), ('uT', i, half)])
        norm_tiles(layer, 0, src, tiles, consume)
        S.barrier()

    qkT = dscr("qkT", [2048, T], BF16)
    v_tm = dscr("v_tm", [T, D], BF16)
    so_tm = dscr("so_tm", [T, D], BF16)
    g_tm = dscr("g_tm", [T, 16])
    xrT = dscr("xrT", [D, T])
    ygT = dscr("ygT", [D, T], BF16)
    hlT = dscr("hlT", [D, T], BF16)
    hm_d = [dscr("hm_f", [T, D]), dscr("hm_b", [T, D])]
    TG = [(0, 256)] + [(256 + 512 * j, 512) for j in range(8)]
    ZW = 4358
    CN = 4355

    def zcol(t0):
        return 2 + t0 if t0 < 256 else t0 + 5

    def phase_E1(uT):
        wv = I("ev_w_in").rearrange("(kc p) n -> p kc n", p=128)
        with SBT("wb0", [128, 8, 512], BF16) as wb0, SBT("wb1", [128, 8, 512], BF16) as wb1, \
                SBT("zp0", [128, ZW], F32) as zp0, SBT("zp1", [128, ZW], F32) as zp1, \
                SBT("co", [128, ZW], F32) as co, SBT("co_b", [128, ZW], F32) as co_b, \
                SBT("ob0", [128, ZW], BF16) as ob0, SBT("ob1", [128, ZW], BF16) as ob1, \
                SBT("cwq", [128, 16, 5], F32) as cwq, SBT("cwl", [128, 8, 5], F32) as cwl, \
                SBT("tms0", [128, 512], BF16) as tms0, SBT("tms1", [128, 512], BF16) as tms1, \
                SBT("wg", [128, 8, 16], BF16) as wg, SBT("gbr", [128, 16], F32) as gbr, \
                SBT("GT", [128, NT, 16], F32) as GT:
            S.dma('sp', lambda e: e.dma_start(out=cwq[:], in_=I("ev_qkcw")), writes=['cwq'])
            S.dma('sp', lambda e: e.dma_start(out=cwl[:], in_=I("ev_lrucw")), writes=['cwl'])
            S.op('pool', lambda e: e.memset(zp0[:], 0.0), writes=[('zp', 0)])
            S.op('pool', lambda e: e.memset(zp1[:], 0.0), writes=[('zp', 1)])
            wbs = Rot([wb0, wb1], 'wb')
            zps = Rot([zp0, zp1], 'zp')
            obs = Rot([ob0, ob1], 'ob')
            pi = [0]
            fm_blocks = [(c0, 'qk', c0 // 128) for c0 in range(0, 2048, 512)] + \
                        [(4112 + j * 512, 'xr', j * 4) for j in range(2)] + \
                        [(5136 + j * 512, 'yg', j * 4) for j in range(2)]
            chunks = []
            for c0, kind, cbase in fm_blocks:
                for jj in range(4):
                    chunks.append((c0, kind, cbase + jj, jj))
            cos_ = [co, co_b]
            state = {}

            def stage1(n):
                c0, kind, cidx, jj = chunks[n]
                if jj == 0:
                    wb, wbk = wbs.next()
                    S.dma('pool', lambda e: e.dma_start(out=wb[:], in_=wv[:, :, c0:c0 + 512]), writes=[wbk])
                    state['wb'] = (wb, wbk)
                wb, wbk = state['wb']
                zp, zpk = zps.next()
                cq = cos_[n % 2]
                ck = ('co', n % 2)
                for (t0, n_) in TG:
                    pb = pi[0] % 2
                    pi[0] += 1
                    for kc in range(8):
                        S.op('pe', lambda e: e.matmul(psum[pb][:, 0:n_], wb[:, kc, jj * 128:(jj + 1) * 128], uT[:, kc, t0:t0 + n_],
                                                      start=(kc == 0), stop=(kc == 7)), reads=[wbk], writes=[PB(pb)])
                    z0 = zcol(t0)
                    S.op('act', lambda e: e.copy(zp[:, z0:z0 + n_], psum[pb][:, 0:n_]), writes=[PB(pb), zpk])
                if kind in ('qk', 'xr'):
                    cw = cwq if kind == 'qk' else cwl
                    S.op('dve', lambda e: e.tensor_scalar(out=cq[:, 0:CN], in0=zp[:, 0:CN], scalar1=cw[:, cidx, 0:1], scalar2=None,
                                                          op0=ALU.mult), reads=[zpk, 'cwq', 'cwl'], writes=[ck])
                    for j in range(1, 4):
                        S.op('dve', lambda e: e.scalar_tensor_tensor(out=cq[:, 0:CN], in0=zp[:, j:j + CN], scalar=cw[:, cidx, j:j + 1],
                                                                     in1=cq[:, 0:CN], op0=ALU.mult, op1=ALU.add),
                             reads=[zpk], writes=[ck])
                state[n] = (zp, zpk, cq, ck)

            def stage2(n):
                c0, kind, cidx, jj = chunks[n]
                zp, zpk, cq, ck = state.pop(n)
                ob, obk = obs.next()
                if kind == 'qk':
                    S.op('act', lambda e: e.activation(out=ob[:, 0:CN], in_=cq[:, 0:CN], func=AF.Silu, bias=cwq[:, cidx, 4:5], scale=1.0),
                         reads=[ck], writes=[obk])
                    rows = qkT[cidx * 128:(cidx + 1) * 128, :]
                    S.dma('sp', lambda e: e.dma_start(out=rows[:, 0:256], in_=ob[:, 0:256]), reads=[obk], writes=[('qkT', cidx, 0)])
                    S.dma('sp', lambda e: e.dma_start(out=rows[:, 256:T], in_=ob[:, 259:CN]), reads=[obk], writes=[('qkT', cidx, 1)])
                elif kind == 'xr':
                    S.op('act', lambda e: e.activation(out=cq[:, 0:CN], in_=cq[:, 0:CN], func=AF.Identity, bias=cwl[:, cidx, 4:5], scale=1.0),
                         writes=[ck])
                    rows = xrT[cidx * 128:(cidx + 1) * 128, :]
                    S.dma('sp', lambda e: e.dma_start(out=rows[:, 0:256], in_=cq[:, 0:256]), reads=[ck], writes=[('xrT', cidx, 0)])
                    S.dma('sp', lambda e: e.dma_start(out=rows[:, 256:T], in_=cq[:, 259:CN]), reads=[ck], writes=[('xrT', cidx, 1)])
                else:
                    S.op('act', lambda e: e.activation(out=cq[:], in_=zp[:], func=AF.Square), reads=[zpk], writes=[ck])
                    S.op('dve', lambda e: e.tensor_scalar(out=cq[:], in0=cq[:], scalar1=0.044715, scalar2=1.0, op0=ALU.mult, op1=ALU.add),
                         writes=[ck])
                    S.op('dve', lambda e: e.tensor_tensor(out=cq[:], in0=cq[:], in1=zp[:], op=ALU.mult), reads=[zpk], writes=[ck])
                    S.op('act', lambda e: e.activation(out=cq[:], in_=cq[:], func=AF.Sigmoid, scale=1.5957691216), writes=[ck])
                    S.op('pool', lambda e: e.tensor_tensor(out=ob[:], in0=cq[:], in1=zp[:], op=ALU.mult), reads=[ck, zpk], writes=[obk])
                    rows = ygT[cidx * 128:(cidx + 1) * 128, :]
                    S.dma('sp', lambda e: e.dma_start(out=rows[:, 0:256], in_=ob[:, 2:258]), reads=[obk], writes=[('ygT', cidx, 0)])
                    S.dma('sp', lambda e: e.dma_start(out=rows[:, 256:T], in_=ob[:, 261:4357]), reads=[obk], writes=[('ygT', cidx, 1)])

            stage1(0)
            for n in range(len(chunks)):
                if n + 1 < len(chunks):
                    stage1(n + 1)
                stage2(n)
            tms = Rot([tms0, tms1], 'tms')
            for blk in range(4):
                c0 = 2048 + blk * 512
                wb, wbk = wbs.next()
                S.dma('pool', lambda e: e.dma_start(out=wb[:], in_=wv[:, :, c0:c0 + 512]), writes=[wbk])
                dst = v_tm if blk < 2 else so_tm
                dc0 = (blk % 2) * 512
                for i in range(NT):
                    pb = 2 + i % 2
                    for kc in range(8):
                        S.op('pe', lambda e: e.matmul(psum[pb][:], uT[:, kc, i * 128:(i + 1) * 128], wb[:, kc, :],
                                                      start=(kc == 0), stop=(kc == 7)), reads=[wbk], writes=[PB(pb)])
                    st_, stk = tms.next()
                    if blk < 2:
                        S.op('act', lambda e: e.copy(st_[:], psum[pb][:]), writes=[PB(pb), stk])
                    else:
                        S.op('act', lambda e: e.activation(out=st_[:], in_=psum[pb][:], func=AF.Sigmoid), writes=[PB(pb), stk])
                    S.dma('sp', lambda e: e.dma_start(out=dst[i * 128:(i + 1) * 128, dc0:dc0 + 512], in_=st_[:]), reads=[stk],
                          writes=[('tmout', blk, i)])
            S.dma('pool', lambda e: e.dma_start(out=wg[:], in_=wv[:, :, 4096:4112]), writes=['wg'])
            bcast_row(gbr[:], I("ev_gate_b")[0:1, :], 'gbr')
            for i in range(NT):
                pb = 4 + i % 2
                for kc in range(8):
                    S.op('pe', lambda e: e.matmul(psum[pb][:, 0:16], uT[:, kc, i * 128:(i + 1) * 128], wg[:, kc, :],
                                                  start=(kc == 0), stop=(kc == 7)), reads=['wg'], writes=[PB(pb)])
                S.op('dve', lambda e: e.tensor_tensor(out=GT[:, i, :], in0=psum[pb][:, 0:16], in1=gbr[:], op=ALU.add),
                     reads=['gbr'], writes=[PB(pb), 'GT'])
            S.dma('sp', lambda e: e.dma_start(out=g_tm.rearrange("(n p) c -> p n c", p=128), in_=GT[:]), reads=['GT'], writes=['g_tm'])
        S.barrier()

    def phase_E2():
        lw_d, lb_d, lam_d = I("ev_lru_w"), I("ev_lru_b"), I("ev_lru_lam")
        with SBT("lxr", [128, T], F32) as xr, SBT("lyg", [128, T], BF16) as yg, \
                SBT("lA", [128, T], F32) as A0, SBT("lB", [128, T], F32) as Bx0, SBT("ltmp", [128, T], F32) as tmp0, \
                SBT("lA1", [128, T], F32) as A1_, SBT("lB1", [128, T], F32) as Bx1, SBT("ltmp1", [128, T], F32) as tmp1, \
                SBT("lH0", [128, T], F32) as H0, SBT("lH1", [128, T], F32) as H1, \
                SBT("lho", [128, T], BF16) as ho, \
                SBT("lw", [128, 4, 128], F32) as lw, SBT("lwb", [128, 4, 128], BF16) as lwb, SBT("xrb", [128, T], BF16) as xrb, SBT("lb", [128, 8, 4], F32) as lb, \
                SBT("lam", [128, 8, 2], F32) as lam, SBT("cA", [128, 8, 2], F32) as cA:
            S.dma('sp', lambda e: e.dma_start(out=lb[:], in_=lb_d), writes=['lb'])
            S.dma('sp', lambda e: e.dma_start(out=lam[:], in_=lam_d), writes=['lam'])
            S.op('act', lambda e: e.activation(out=cA[:], in_=lam[:], func=AF.Exp, scale=-1.0), reads=['lam'], writes=['cA'])
            S.op('act', lambda e: e.activation(out=cA[:], in_=cA[:], func=AF.Ln, bias=1.0, scale=1.0), writes=['cA'])
            S.op('dve', lambda e: e.tensor_scalar(out=cA[:], in0=cA[:], scalar1=-8.0, scalar2=None, op0=ALU.mult), writes=['cA'])
            pi = 0
            for cc in range(8):
                S.dma('sp', lambda e: e.dma_start(out=xr[:], in_=xrT[cc * 128:(cc + 1) * 128, :]), writes=['xr'])
                S.dma('sp', lambda e: e.dma_start(out=yg[:], in_=ygT[cc * 128:(cc + 1) * 128, :]), writes=['yg'])
                S.dma('sp', lambda e: e.dma_start(out=lw[:], in_=lw_d[:, cc].rearrange("g k m -> k g m")), writes=['lw'])
                S.op('pool', lambda e: e.tensor_copy(lwb[:], lw[:]), reads=['lw'], writes=['lwb'])
                S.op('pool', lambda e: e.tensor_copy(xrb[:], xr[:]), reads=['xr'], writes=['xrb'])
                for z in range(2):
                    H = H0 if z == 0 else H1
                    hk = ('H', z)
                    A, Bx, tmp = (A0, Bx0, tmp0) if z == 0 else (A1_, Bx1, tmp1)
                    KA, KB, KT_ = ('A', z), ('Bx', z), ('tmp', z)
                    for gi, dst, dk in ((0, A, KA), (1, Bx, KB)):
                        for (t0, n) in TG:
                            pb = pi % 2
                            pi += 1
                            S.op('pe', lambda e: e.matmul(psum[pb][:, 0:n], lwb[:, z * 2 + gi, :], xrb[:, t0:t0 + n], start=True, stop=True),
                                 reads=['lwb', 'xrb'], writes=[PB(pb)])
                            S.op('act', lambda e: e.activation(out=dst[:, t0:t0 + n], in_=psum[pb][:, 0:n], func=AF.Sigmoid,
                                                               bias=lb[:, cc, z * 2 + gi:z * 2 + gi + 1], scale=1.0),
                                 reads=['lb'], writes=[PB(pb), dk])
                    S.op('act', lambda e: e.activation(out=A[:], in_=A[:], func=AF.Exp, scale=cA[:, cc, z:z + 1]), reads=['cA'], writes=[KA])
                    S.op('act', lambda e: e.activation(out=tmp[:], in_=A[:], func=AF.Square), reads=[KA], writes=[KT_])
                    S.op('act', lambda e: e.activation(out=tmp[:], in_=tmp[:], func=AF.Sqrt, bias=1.0, scale=-1.0), writes=[KT_])
                    S.op('pool', lambda e: e.tensor_tensor(out=Bx[:], in0=Bx[:], in1=xr[:], op=ALU.mult), reads=['xr'], writes=[KB])
                    S.op('dve', lambda e: e.tensor_tensor(out=Bx[:], in0=Bx[:], in1=tmp[:], op=ALU.mult), reads=[KT_], writes=[KB])
                    if z == 0:
                        S.op('dve', lambda e: e.tensor_tensor_scan(H[:], A[:], Bx[:], 0.0, ALU.mult, ALU.add), reads=[KA, KB], writes=[hk])
                    else:
                        S.op('dve', lambda e: e.tensor_tensor_scan(H[:, 0:256][:, ::-1], A[:, 0:256][:, ::-1], Bx[:, 0:256][:, ::-1], 0.0,
                                                                   ALU.mult, ALU.add), reads=[KA, KB], writes=[hk])
                        S.op('dve', lambda e: e.tensor_tensor_scan(H[:, 256:T][:, ::-1], A[:, 256:T][:, ::-1], Bx[:, 256:T][:, ::-1], H[:, 0:1],
                                                                   ALU.mult, ALU.add), reads=[KA, KB], writes=[hk])
                S.op('pool', lambda e: e.tensor_tensor(out=H0[:], in0=H0[:], in1=H1[:], op=ALU.add), reads=[('H', 1)], writes=[('H', 0)])
                S.op('dve', lambda e: e.tensor_tensor(out=ho[:], in0=H0[:], in1=yg[:], op=ALU.mult), reads=[('H', 0), 'yg'], writes=['ho'])
                S.dma('pool', lambda e: e.dma_start(out=hlT[cc * 128:(cc + 1) * 128, :], in_=ho[:]), reads=['ho'], writes=[('hlT', cc)])
        S.barrier()

    def phase_E3():
        NLN16 = -2.772588722239781
        with contextlib.ExitStack() as es:
            def al(name, shape, dt):
                return es.enter_context(SBT(name, shape, dt))
            G = al("G", [128, NT, 16], F32)
            LF = al("LF", [128, 2, NT, 4], F32)
            CF = al("CF", [128, 2, NT, 4], F32)
            EB16 = al("EB16", [128, 2, NT, 4], F32)
            ECF = al("ECF", [128, 2, NT, 4], F32)
            DEC = al("DEC", [128, 2, NT, 4], F32)
            WS16 = al("WS16", [128, 2, NT, 4], F32)
            qT0 = al("qT0", [128, 2, T], BF16)
            qT1 = al("qT1", [128, 2, T], BF16)
            kT0 = al("kT0", [128, 2, T], BF16)
            kT1 = al("kT1", [128, 2, T], BF16)
            V0 = al("V0", [128, NT, 257], BF16)
            V1 = al("V1", [128, NT, 257], BF16)
            CTs = al("CTs", [128, 4, 2, 257], F32)
            CTbs = al("CTbs", [128, 4, 2, 257], BF16)
            PTs = al("PTs", [128, 4, 128], BF16)
            ktms = al("ktms", [128, 4, 256], BF16)
            vws = al("vws", [128, 4, 257], BF16)
            sms = al("sms", [128, 4, 8], F32)
            houts = al("houts", [128, 4, 256], F32)
            S.dma('sp', lambda e: e.dma_start(out=G[:], in_=g_tm.rearrange("(n p) c -> p n c", p=128)), writes=['G'])
            Gv = G[:].rearrange("p n (d g h) -> p d g n h", d=2, g=2, h=4)
            fl = lambda t, d: t[:, d].rearrange("p n h -> p (n h)")
            for d in range(2):
                S.op('act', lambda e: e.activation(out=LF[:, d], in_=Gv[:, d, 1], func=AF.Exp, scale=-1.0), reads=['G'], writes=['LF'])
                S.op('act', lambda e: e.activation(out=LF[:, d], in_=LF[:, d], func=AF.Ln, bias=1.0, scale=1.0), writes=['LF'])
                S.op('dve', lambda e: e.tensor_scalar(out=LF[:, d], in0=LF[:, d], scalar1=-1.0, scalar2=None, op0=ALU.mult), writes=['LF'])
                tri = triF if d == 0 else triB
                sel = selF if d == 0 else selB
                S.op('pe', lambda e: e.matmul(psum[0][:, 0:NT * 4], tri[:], fl(LF, d), start=True, stop=True),
                     reads=['LF', tri.name], writes=[PB(0)])
                S.op('act', lambda e: e.copy(fl(CF, d), psum[0][:, 0:NT * 4]), writes=[PB(0), 'CF'])
                S.op('pe', lambda e: e.matmul(psum[1][:, 0:NT * 4], sel[:], fl(CF, d), start=True, stop=True),
                     reads=['CF', sel.name], writes=[PB(1)])
                S.op('act', lambda e: e.activation(out=fl(DEC, d), in_=psum[1][:, 0:NT * 4], func=AF.Exp), writes=[PB(1), 'DEC'])
                S.op('dve', lambda e: e.tensor_tensor(out=EB16[:, d], in0=Gv[:, d, 0], in1=CF[:, d], op=ALU.subtract), reads=['G', 'CF'], writes=['EB16'])
                S.op('dve', lambda e: e.tensor_scalar(out=EB16[:, d], in0=EB16[:, d], scalar1=NLN16, scalar2=None, op0=ALU.add), writes=['EB16'])
                S.op('act', lambda e: e.activation(out=EB16[:, d], in_=EB16[:, d], func=AF.Exp), writes=['EB16'])
                S.op('act', lambda e: e.activation(out=ECF[:, d], in_=CF[:, d], func=AF.Exp), reads=['CF'], writes=['ECF'])
                S.op('dve', lambda e: e.tensor_tensor(out=WS16[:, d], in0=EB16[:, d], in1=DEC[:, d], op=ALU.mult), reads=['EB16', 'DEC'], writes=['WS16'])
            S.barrier()
            qTs, kTs, Vs = [qT0, qT1], [kT0, kT1], [V0, V1]
            order = [list(range(NT)), [1, 0] + list(range(NT - 1, 1, -1))]
            cnt = {'st': 0, 'num': 0}
            for hp in range(2):
                for hh in range(2):
                    h = hp * 2 + hh
                    S.dma('sp', lambda e: e.dma_start(out=qTs[hh][:], in_=qkT[h * 256:(h + 1) * 256, :].rearrange("(dh p) t -> p dh t", p=128)),
                          writes=[('qT', hh)])
                    S.dma('sp', lambda e: e.dma_start(out=kTs[hh][:], in_=qkT[1024 + h * 256:1024 + (h + 1) * 256, :].rearrange("(dh p) t -> p dh t", p=128)),
                          writes=[('kT', hh)])
                    S.dma('sp', lambda e: e.dma_start(out=Vs[hh][:, :, 0:256], in_=v_tm[:, h * 256:(h + 1) * 256].rearrange("(n p) e -> p n e", p=128)),
                          writes=[('V', hh)])
                    S.op('pool', lambda e: e.memset(Vs[hh][:, :, 256:257], 1.0), writes=[('V1', hh)])
                S.op('pool', lambda e: e.memset(CTs[:], 0.0), writes=[('CT', i) for i in range(4)])
                S.op('pool', lambda e: e.memset(CTbs[:], 0.0), writes=[('CTb', i) for i in range(4)])
                for step in range(NT):
                    for ci, (z, hh) in enumerate([(0, 0), (0, 1), (1, 0), (1, 1)]):
                        h = hp * 2 + hh
                        c = order[z][step]
                        sl = slice(c * 128, (c + 1) * 128)
                        qT, kT, V = qTs[hh], kTs[hh], Vs[hh]
                        rk = [('qT', hh), ('kT', hh), ('V', hh), ('V1', hh)]
                        tri = triF if z == 0 else triB
                        pst = cnt['st'] % 2
                        cnt['st'] += 1
                        for dh in range(2):
                            S.op('pe', lambda e: e.matmul(psum[pst][:, 0:128], kT[:, dh, sl], qT[:, dh, sl], start=(dh == 0), stop=(dh == 1)),
                                 reads=rk, writes=[PB(pst)])
                        S.op('dve', lambda e: e.scalar_tensor_tensor(out=PTs[:, ci, :], in0=psum[pst][:, 0:128], scalar=EB16[:, z, c, h:h + 1],
                                                                     in1=tri[:], op0=ALU.mult, op1=ALU.mult), writes=[PB(pst), ('PT', ci)])
                        for dh in range(2):
                            S.op('pe', lambda e: e.transpose(psb[0][:, dh * 128:(dh + 1) * 128], kT[:, dh, sl], identb[:]),
                                 reads=rk, writes=[PBB(0)])
                        S.op('act', lambda e: e.copy(ktms[:, ci, :], psb[0][:, 0:256]), writes=[PBB(0), ('ktm', ci)])
                        S.op('act', lambda e: e.activation(out=vws[:, ci, :], in_=V[:, c, :], func=AF.Identity, scale=WS16[:, z, c, h:h + 1]),
                             reads=rk, writes=[('vw', ci)])
                        pn = 2 + cnt['num'] % 2
                        cnt['num'] += 1
                        S.op('pe', lambda e: e.matmul(psum[pn][:, 0:257], PTs[:, ci, :], V[:, c, :], start=True, stop=False),
                             reads=rk + [('PT', ci)], writes=[PB(pn)])
                        for dh in range(2):
                            S.op('pe', lambda e: e.matmul(psum[pn][:, 0:257], qT[:, dh, sl], CTbs[:, ci, dh, :], start=False, stop=(dh == 1)),
                                 reads=rk + [('CTb', ci)], writes=[PB(pn)])
                        sm = sms[:, ci, :]
                        ecf = ECF[:, z, c, h:h + 1]
                        S.op('dve', lambda e: e.tensor_tensor(out=sm[:, 0:1], in0=psum[pn][:, 256:257], in1=ecf, op=ALU.mult), writes=[PB(pn), ('sm', ci)])
                        S.op('dve', lambda e: e.tensor_scalar(out=sm[:, 2:3], in0=sm[:, 0:1], scalar1=1.0, scalar2=None, op0=ALU.max), writes=[('sm', ci)])
                        S.op('dve', lambda e: e.tensor_scalar(out=sm[:, 3:4], in0=sm[:, 0:1], scalar1=-1.0, scalar2=sm[:, 2:3], op0=ALU.mult, op1=ALU.max),
                             writes=[('sm', ci)])
                        S.op('dve', lambda e: e.reciprocal(sm[:, 4:5], sm[:, 3:4]), writes=[('sm', ci)])
                        S.op('dve', lambda e: e.tensor_tensor(out=sm[:, 5:6], in0=sm[:, 4:5], in1=ecf, op=ALU.mult), writes=[('sm', ci)])
                        S.op('act', lambda e: e.activation(out=houts[:, ci, :], in_=psum[pn][:, 0:256], func=AF.Identity, scale=sm[:, 5:6]),
                             reads=[('sm', ci)], writes=[PB(pn), ('hout', ci)])
                        S.dma('sp', lambda e: e.dma_start(out=hm_d[z][sl, h * 256:(h + 1) * 256], in_=houts[:, ci, :]), reads=[('hout', ci)],
                              writes=[('hm', z, h, c)])
                        for dh in range(2):
                            S.op('pe', lambda e: e.matmul(psum[4 + dh][:, 0:257], ktms[:, ci, dh * 128:(dh + 1) * 128], vws[:, ci, :], start=True, stop=True),
                                 reads=[('ktm', ci), ('vw', ci)], writes=[PB(4 + dh)])
                            S.op('dve', lambda e: e.scalar_tensor_tensor(out=CTs[:, ci, dh, :], in0=CTs[:, ci, dh, :], scalar=DEC[:, z, c, h:h + 1],
                                                                         in1=psum[4 + dh][:, 0:257], op0=ALU.mult, op1=ALU.add),
                                 writes=[PB(4 + dh), ('CT', ci)])
                        S.op('act', lambda e: e.copy(CTbs[:, ci], CTs[:, ci]), reads=[('CT', ci)], writes=[('CTb', ci)])
        S.barrier()

    def mixer_out(i, r, G1, wo, nk, lhs_list, src_rows, pbase):
        return

    def phase_E4():
        with contextlib.ExitStack() as es:
            def al(name, shape, dt):
                return es.enter_context(SBT(name, shape, dt))
            wo = al("wo", [128, 16, D], BF16)
            MG = al("MG", [128, D], F32)
            G1 = al("G1", [128, 2, D], F32)
            hf = [al("hf%d" % j, [128, D], F32) for j in range(2)]
            hb = [al("hb%d" % j, [128, D], F32) for j in range(2)]
            so = [al("so%d" % j, [128, D], BF16) for j in range(2)]
            sq2 = [al("e4sq%d" % j_, [128, 256], F32) for j_ in range(2)]
            st2 = [al("e4st%d" % j_, [128, 16], F32) for j_ in range(2)]
            hmb2 = [al("hmb%d" % j_, [128, D], BF16) for j_ in range(2)]
            hmT = [al("hmT%d" % j, [128, 8, 128], BF16) for j in range(2)]
            hl = [al("hl%d" % j, [128, 8, 128], BF16) for j in range(2)]
            xt = [al("e4x%d" % j, [128, D], F32) for j in range(2)]
            tmp2 = [al("e4tmp%d" % j_, [128, D], F32) for j_ in range(2)]
            wov = I("ev_w_out").rearrange("(kc p) n -> p kc n", p=128)
            for hlf in range(2):
                S.dma('pool', lambda e: e.dma_start(out=wo[:, hlf * 8:(hlf + 1) * 8, :], in_=wov[:, hlf * 8:(hlf + 1) * 8, :]), writes=[('wo', hlf)])
            bcast_row(MG[:], I("ev_mnorm_g")[0:1, :], 'MG')
            for r in range(2):
                bcast_row(G1[:, r, :], modrow[0, r:r + 1, 2 * D:3 * D], ('G1', r))
            hlv = hlT.rearrange("(cc p) t -> p cc t", p=128)
            xin = I("xin")
            def stL(i):
                r = 1 if i < 2 else 0
                j = i % 2
                rows = slice(i * 128, (i + 1) * 128)
                sq, st, hmb, tmp = sq2[j], st2[j], hmb2[j], tmp2[j]
                KSQ, KST, KHMB = ('e4sq', j), ('e4st', j), ('hmb', j)
                S.dma('sp', lambda e: e.dma_start(out=hf[j][:], in_=hm_d[0][rows, :]), writes=[('hf', j)])
                S.dma('sp', lambda e: e.dma_start(out=hb[j][:], in_=hm_d[1][rows, :]), writes=[('hb', j)])
                S.dma('sp', lambda e: e.dma_start(out=so[j][:], in_=so_tm[rows, :]), writes=[('so', j)])
                S.dma('sp', lambda e: e.dma_start(out=hl[j][:], in_=hlv[:, :, rows]), writes=[('hl', j)])
                S.dma('sp', lambda e: e.dma_start(out=xt[j][:], in_=xin[rows, :]), writes=[('xt', j)])

            def stB(i):
                r = 1 if i < 2 else 0
                j = i % 2
                rows = slice(i * 128, (i + 1) * 128)
                sq, st, hmb, tmp = sq2[j], st2[j], hmb2[j], tmp2[j]
                KSQ, KST, KHMB = ('e4sq', j), ('e4st', j), ('hmb', j)
                S.op('pool', lambda e: e.tensor_tensor(out=hf[j][:], in0=hf[j][:], in1=hb[j][:], op=ALU.add), reads=[('hb', j)], writes=[('hf', j)])
                for h in range(4):
                    S.op('act', lambda e: e.activation(out=sq[:], in_=hf[j][:, h * 256:(h + 1) * 256], func=AF.Square, accum_out=st[:, h:h + 1]),
                         reads=[('hf', j)], writes=[KSQ, KST])
                S.op('dve', lambda e: e.tensor_scalar(out=st[:, 4:8], in0=st[:, 0:4], scalar1=1.0 / 256, scalar2=EPS, op0=ALU.mult, op1=ALU.add), writes=[KST])
                S.op('act', lambda e: e.activation(out=st[:, 8:12], in_=st[:, 4:8], func=AF.Sqrt), writes=[KST])
                S.op('dve', lambda e: e.reciprocal(st[:, 12:16], st[:, 8:12]), writes=[KST])
                for h in range(4):
                    hs = slice(h * 256, (h + 1) * 256)
                    S.op('dve', lambda e: e.scalar_tensor_tensor(out=hf[j][:, hs], in0=hf[j][:, hs], scalar=st[:, 12 + h:13 + h], in1=MG[:, hs],
                                                                 op0=ALU.mult, op1=ALU.mult), reads=['MG'], writes=[('hf', j), KST])
                S.op('pool', lambda e: e.tensor_tensor(out=hmb[:], in0=hf[j][:], in1=so[j][:], op=ALU.mult), reads=[('hf', j), ('so', j)], writes=[KHMB])

            def stCD(i):
                r = 1 if i < 2 else 0
                j = i % 2
                rows = slice(i * 128, (i + 1) * 128)
                sq, st, hmb, tmp = sq2[j], st2[j], hmb2[j], tmp2[j]
                KSQ, KST, KHMB = ('e4sq', j), ('e4st', j), ('hmb', j)
                for kc in range(8):
                    S.op('pe', lambda e: e.transpose(psb[j][:, kc * 128:(kc + 1) * 128], hmb[:, kc * 128:(kc + 1) * 128], identb[:]),
                         reads=[KHMB], writes=[PBB(j)])
                S.op('act', lambda e: e.copy(hmT[j][:].rearrange("p a b -> p (a b)"), psb[j][:]), writes=[PBB(j), ('hmT', j)])
                for nb in range(2):
                    pb = (2 * i + nb) % 4
                    for kc in range(16):
                        lhs = hmT[j][:, kc, :] if kc < 8 else hl[j][:, kc - 8, :]
                        S.op('pe', lambda e: e.matmul(psum[pb][:], lhs, wo[:, kc, nb * 512:(nb + 1) * 512], start=(kc == 0), stop=(kc == 15)),
                             reads=[('hmT', j), ('hl', j), ('wo', 0), ('wo', 1)], writes=[PB(pb)])
                    cs = slice(nb * 512, (nb + 1) * 512)
                    S.op('dve', lambda e: e.tensor_tensor(out=tmp[:, cs], in0=psum[pb][:], in1=G1[:, r, cs], op=ALU.mult), reads=[('G1', r)],
                         writes=[PB(pb), ('e4tmp', j, nb)])
                    S.op('pool', lambda e: e.tensor_tensor(out=tmp[:, cs], in0=tmp[:, cs], in1=xt[j][:, cs], op=ALU.add), reads=[('xt', j)],
                         writes=[('e4tmp', j, nb)])
                S.dma('pool', lambda e: e.dma_start(out=xres[rows, :], in_=tmp[:]), reads=[('e4tmp', j, 0), ('e4tmp', j, 1)], writes=[('xres', i)])

            stL(0)
            stB(0)
            for i in range(NT):
                if i + 1 < NT:
                    stL(i + 1)
                    stB(i + 1)
                stCD(i)
        S.barrier()

    v2_tm = dscr("v2_tm", [T, D], BF16)

    def phase_N2(layer, tiles, LOG):
        with contextlib.ExitStack() as es:
            def al(name, shape, dt):
                return es.enter_context(SBT(name, shape, dt))
            wr = al("wr", [128, 8, 32], F32)
            br = al("br", [1, 32], F32)
            vT32 = [al("vT32_%d" % j, [128, 8, 128], F32) for j in range(2)]
            vb = [al("vb%d" % j, [128, D], BF16) for j in range(2)]
            S.dma('sp', lambda e: e.dma_start(out=wr[:], in_=I("moe_w_r")[layer].rearrange("(kc p) n -> p kc n", p=128)), writes=['wr'])
            S.dma('sp', lambda e: e.dma_start(out=br[:], in_=I("moe_b_r")[layer:layer + 1, :]), writes=['br'])
            cnt = [0]

            def consume(i, ut, uk):
                j = cnt[0] % 2
                cnt[0] += 1
                S.op('pool', lambda e: e.tensor_copy(vb[j][:], ut[:]), reads=[uk], writes=[('vb', j)])
                S.dma('pool', lambda e: e.dma_start(out=v2_tm[i * 128:(i + 1) * 128, :], in_=vb[j][:]), reads=[('vb', j)], writes=[('v2', i)])
                for half in range(2):
                    pb = (i * 2 + half) % 4
                    for jj in range(4):
                        kc = half * 4 + jj
                        S.op('pe', lambda e: e.transpose(psum[pb][:, jj * 128:(jj + 1) * 128], ut[:, kc * 128:(kc + 1) * 128], ident[:]),
                             reads=[uk, 'ident'], writes=[PB(pb)])
                    S.op('act', lambda e: e.copy(vT32[j][:, half * 4:half * 4 + 4, :].rearrange("p a b -> p (a b)"), psum[pb][:]),
                         writes=[PB(pb), ('vT32', j, half)])
                pl = 4 + i % 2
                for kc in range(8):
                    S.op('pe', lambda e: e.matmul(psum[pl][:, 0:32], vT32[j][:, kc, :], wr[:, kc, :], start=(kc == 0), stop=False),
                         reads=[('vT32', j, 0), ('vT32', j, 1), 'wr'], writes=[PB(pl)])
                S.op('pe', lambda e: e.matmul(psum[pl][:, 0:32], ones1[0:1, :], br[:], start=False, stop=True), reads=['br', 'ones1'], writes=[PB(pl)])
                S.op('dve', lambda e: e.tensor_copy(LOG[:, i, :], psum[pl][:, 0:32]), writes=[PB(pl), 'LOG'])
            norm_tiles(layer, 1, xres, tiles, consume)
        S.barrier()

    SB = 512
    SHIFT = 9
    NSUB = SB // 128
    NBMAX = 66
    xs_d = dscr("xs_d", [NBMAX * SB, D], BF16)
    ys_d = dscr("ys_d", [NBMAX * SB, D], F32)
    RT = {}
    for nm, shp, dt in (("LOG", [128, NT, 32], F32), ("TOP8", [128, NT, 8], F32), ("WG", [128, NT, 4], F32),
                        ("SLOTI", [128, NT, 4], I32), ("IDXI", [128, NBMAX], I32), ("BIDX", [128, NBMAX], I32),
                        ("IDX8", [128, NBMAX, 8], I32), ("IDX4", [128, NBMAX, 4], I32)):
        RT[nm] = nc.alloc_sbuf_tensor("rt_" + nm, shp, dt)

    def phase_route(tiles, NB):
        LOG, TOP8, WG, SLOTI, IDXI, BIDX = (RT[k] for k in ("LOG", "TOP8", "WG", "SLOTI", "IDXI", "BIDX"))
        with contextlib.ExitStack() as es:
            def al(name, shape, dt):
                return es.enter_context(SBT(name, shape, dt))
            MASK = al("MASK", [128, NT, 32], F32)
            RANK = al("RANK", [128, NT, 32], F32)
            CAR = al("CAR", [128, 32], F32)
            sm = al("rsm", [128, 8], F32)
            ci = al("rci", [128, 32], I32)
            PADDED = al("PADDED", [128, 32], F32)
            PEND = al("PEND", [128, 32], F32)
            BASE = al("BASE", [128, 32], F32)
            ones32 = al("ones32", [128, 32], F32)
            junk = al("rjunk", [128, 32], F32)
            SLOTF = al("SLOTF", [128, NT, 4], F32)
            BSTi = al("BSTi", [128, NBMAX], I32)
            BST = al("BST", [128, NBMAX], F32)
            BLKE = al("BLKE", [128, NBMAX], F32)
            IDXF = al("IDXF", [128, NBMAX], F32)
            IDXF2 = al("IDXF2", [128, NBMAX], F32)
            S.op('pool', lambda e: e.memset(CAR[:], 0.0), writes=['CAR'])
            S.op('pool', lambda e: e.memset(ones32[:], 1.0), writes=['ones32'])
            S.op('pool', lambda e: e.memset(SLOTF[:], 0.0), writes=['SLOTF'])
            for i in tiles:
                S.op('dve', lambda e: e.max(out=TOP8[:, i, :], in_=LOG[:, i, :]), writes=['TOP8'])
                S.op('dve', lambda e: e.tensor_scalar(out=MASK[:, i, :], in0=LOG[:, i, :], scalar1=TOP8[:, i, 3:4], scalar2=None, op0=ALU.is_ge),
                     reads=['TOP8'], writes=[('MASK', i)])
                S.op('dve', lambda e: e.tensor_scalar(out=sm[:, 0:1], in0=TOP8[:, i, 0:1], scalar1=-1.0, scalar2=None, op0=ALU.mult), reads=['TOP8'], writes=['rsm'])
                S.op('act', lambda e: e.activation(out=WG[:, i, :], in_=TOP8[:, i, 0:4], func=AF.Exp, bias=sm[:, 0:1], scale=1.0, accum_out=sm[:, 1:2]),
                     reads=['TOP8'], writes=['rsm', 'WG'])
                S.op('dve', lambda e: e.reciprocal(sm[:, 2:3], sm[:, 1:2]), writes=['rsm'])
                S.op('dve', lambda e: e.tensor_scalar(out=WG[:, i, :], in0=WG[:, i, :], scalar1=sm[:, 2:3], scalar2=None, op0=ALU.mult), writes=['rsm', 'WG'])
                S.op('pe', lambda e: e.matmul(psum[0][:, 0:32], triS[:], MASK[:, i, :], start=True, stop=True), reads=[('MASK', i)], writes=[PB(0)])
                S.op('pe', lambda e: e.matmul(psum[1][:, 0:32], onesm[:], MASK[:, i, :], start=True, stop=True), reads=[('MASK', i)], writes=[PB(1)])
                S.op('dve', lambda e: e.tensor_tensor(out=RANK[:, i, :], in0=psum[0][:, 0:32], in1=CAR[:], op=ALU.add), reads=['CAR'], writes=[PB(0), 'RANK'])
                S.op('dve', lambda e: e.tensor_tensor(out=CAR[:], in0=psum[1][:, 0:32], in1=CAR[:], op=ALU.add), writes=[PB(1), 'CAR'])
            S.op('dve', lambda e: e.tensor_scalar(out=junk[:], in0=CAR[:], scalar1=float(SB - 1), scalar2=None, op0=ALU.add), reads=['CAR'], writes=['rjunk'])
            S.op('dve', lambda e: e.tensor_copy(ci[:], junk[:]), reads=['rjunk'], writes=['rci'])
            S.op('dve', lambda e: e.tensor_scalar(out=ci[:], in0=ci[:], scalar1=SHIFT, scalar2=SHIFT, op0=ALU.arith_shift_right, op1=ALU.logical_shift_left),
                 writes=['rci'])
            S.op('dve', lambda e: e.tensor_copy(PADDED[:], ci[:]), reads=['rci'], writes=['PADDED'])
            S.op('dve', lambda e: e.tensor_tensor_scan(PEND[:], ones32[:], PADDED[:], 0.0, ALU.mult, ALU.add), reads=['ones32', 'PADDED'], writes=['PEND'])
            S.op('dve', lambda e: e.tensor_tensor(out=BASE[:], in0=PEND[:], in1=PADDED[:], op=ALU.subtract), reads=['PEND', 'PADDED'], writes=['BASE'])
            for i in tiles:
                S.op('dve', lambda e: e.tensor_tensor(out=RANK[:, i, :], in0=RANK[:, i, :], in1=BASE[:], op=ALU.add), reads=['BASE'], writes=['RANK'])
                for k in range(4):
                    S.op('dve', lambda e: e.scalar_tensor_tensor(out=junk[:], in0=LOG[:, i, :], scalar=TOP8[:, i, k:k + 1], in1=RANK[:, i, :],
                                                                 op0=ALU.is_equal, op1=ALU.mult, accum_out=SLOTF[:, i, k:k + 1]),
                         reads=['TOP8'], writes=['rjunk', 'RANK', 'SLOTF'])
            S.op('dve', lambda e: e.tensor_copy(SLOTI[:], SLOTF[:]), reads=['SLOTF'], writes=['SLOTI'])
            S.op('pool', lambda e: e.iota(BSTi[:], pattern=[[SB, NBMAX]], base=0, channel_multiplier=0), writes=['BSTi'])
            S.op('dve', lambda e: e.tensor_copy(BST[:], BSTi[:]), reads=['BSTi'], writes=['BST'])
            S.op('pool', lambda e: e.memset(BLKE[:], 0.0), writes=['BLKE'])
            for ex in range(32):
                S.op('dve', lambda e: e.scalar_tensor_tensor(out=BLKE[:], in0=BST[:], scalar=PEND[:, ex:ex + 1], in1=BLKE[:], op0=ALU.is_ge, op1=ALU.add),
                     reads=['BST', 'PEND'], writes=['BLKE'])
            S.op('dve', lambda e: e.tensor_scalar(out=BLKE[:], in0=BLKE[:], scalar1=31.0, scalar2=None, op0=ALU.min), writes=['BLKE'])
            S.op('dve', lambda e: e.tensor_scalar(out=IDXF[:], in0=BLKE[:], scalar1=128.0, scalar2=pidx[:, 0:1], op0=ALU.mult, op1=ALU.add),
                 reads=['pidx'], writes=['IDXF'])
            S.op('dve', lambda e: e.tensor_copy(IDXI[:], IDXF[:]), reads=['IDXF'], writes=['IDXI'])
            S.op('dve', lambda e: e.tensor_copy(BIDX[:], BLKE[:]), reads=['BLKE'], writes=['BIDX'])
            S.op('pool', lambda e: e.memset(BSTi[:], 0), writes=['BSTi'])
            S.op('dve', lambda e: e.tensor_copy(IDXF2[:], BSTi[:]), reads=['BSTi'], writes=['IDXF2'])
            S.op('dve', lambda e: e.tensor_tensor(out=IDXF2[:, 2:NBMAX], in0=BLKE[:, 2:NBMAX], in1=BLKE[:, 0:NBMAX - 2], op=ALU.is_equal),
                 reads=['BLKE'], writes=['IDXF2'])
            S.op('dve', lambda e: e.tensor_scalar(out=IDXF2[:], in0=IDXF2[:], scalar1=float(1 << 20), scalar2=None, op0=ALU.mult), writes=['IDXF2'])
            for kc in range(8):
                S.op('dve', lambda e: e.tensor_scalar(out=BST[:], in0=IDXF[:], scalar1=8.0, scalar2=float(kc), op0=ALU.mult, op1=ALU.add),
                     reads=['IDXF'], writes=['BST'])
                S.op('dve', lambda e: e.tensor_tensor(out=BST[:], in0=BST[:], in1=IDXF2[:], op=ALU.add), reads=['IDXF2'], writes=['BST'])
                S.op('dve', lambda e: e.tensor_copy(RT["IDX8"][:, :, kc], BST[:]), reads=['BST'], writes=['IDX8'])
            for q in range(4):
                S.op('dve', lambda e: e.tensor_scalar(out=BST[:], in0=IDXF[:], scalar1=4.0, scalar2=float(q), op0=ALU.mult, op1=ALU.add),
                     reads=['IDXF'], writes=['BST'])
                S.op('dve', lambda e: e.tensor_tensor(out=BST[:], in0=BST[:], in1=IDXF2[:], op=ALU.add), reads=['IDXF2'], writes=['BST'])
                S.op('dve', lambda e: e.tensor_copy(RT["IDX4"][:, :, q], BST[:]), reads=['BST'], writes=['IDX4'])
            if RT.get('dbg') is not None:
                dd = RT['dbg']
                S.op('dve', lambda e: e.tensor_copy(dd[:, 0:32], CAR[:]), reads=['CAR'], writes=['dd'])
                S.op('dve', lambda e: e.tensor_copy(dd[:, 32:64], PADDED[:]), reads=['PADDED'], writes=['dd'])
                S.op('dve', lambda e: e.tensor_copy(dd[:, 64:96], PEND[:]), reads=['PEND'], writes=['dd'])
                S.op('dve', lambda e: e.tensor_copy(dd[:, 96:128], MASK[:, 0, :]), writes=['dd'])
                S.op('dve', lambda e: e.tensor_copy(dd[:, 128:160], RANK[:, 1, :]), writes=['dd'])
                S.op('dve', lambda e: e.tensor_copy(dd[:, 160:192], ci[:]), writes=['dd'])
        S.barrier()

    def phase_scatter(tiles, NB):
        SLOTI = RT["SLOTI"]
        with contextlib.ExitStack() as es:
            def al(name, shape, dt):
                return es.enter_context(SBT(name, shape, dt))
            zt = al("zt", [128, D], BF16)
            vt = [al("svt%d" % j, [128, D], BF16) for j in range(3)]
            for n, i in enumerate(tiles):
                j = n % 3
                S.dma('sp', lambda e: e.dma_start(out=vt[j][:], in_=v2_tm[i * 128:(i + 1) * 128, :]), writes=[('svt', j)])
                for k in range(4):
                    S.dma('pool', lambda e: e.indirect_dma_start(out=xs_d, out_offset=bass.IndirectOffsetOnAxis(ap=SLOTI[:, i, k:k + 1], axis=0),
                                                                 in_=vt[j][:], in_offset=None), reads=[('svt', j)], writes=[('xs', i, k)])
        S.barrier()

    def phase_moe(layer, NB):
        w1d = I("moe_w1_%d" % layer)
        w2d = I("moe_w2_%d" % layer)
        b1d = I("moe_b1_%d" % layer)
        b2d = I("moe_b2_%d" % layer)
        IDXI, BIDX = RT["IDXI"], RT["BIDX"]
        w1v = w1d.rearrange("a b c -> (a b) c")
        w2v = w2d.rearrange("a (q t) c -> (a q) (t c)", t=2)
        with contextlib.ExitStack() as es:
            def al(name, shape, dt):
                return es.enter_context(SBT(name, shape, dt))
            W1 = [al("W1_%d" % j, [128, 8, 2048], BF16) for j in range(2)]
            W2 = [al("W2_%d" % j, [128, 8, 1024], BF16) for j in range(2)]
            B1C = [al("B1C_%d" % j, [128, 16], F32) for j in range(2)]
            B2R = [al("B2R_%d" % j, [128, D], F32) for j in range(2)]
            XS = [al("XS_%d" % j, [128, D], BF16) for j in range(2 * NSUB)]
            XST2 = [al("XST%d" % j_, [128, 8, SB], BF16) for j_ in range(2)]
            ACTT = al("ACTT", [128, 8, SB], BF16)
            tg = [al("tg_%d" % j, [128, SB], F32) for j in range(2)]
            sg = [al("sg_%d" % j, [128, SB], F32) for j in range(2)]
            tu = [al("tu_%d" % j, [128, SB], F32) for j in range(2)]
            YT = [al("YT_%d" % j, [128, D], F32) for j in range(2)]
            ec = [0]
            xc = [0]
            yc = [0]
            bc8 = nc.gpsimd.to_reg(32 * 128 * 8 - 1)
            bc4 = nc.gpsimd.to_reg(32 * 128 * 4 - 1)

            def gathers(b):
                j = b % 2
                idx = bass.IndirectOffsetOnAxis(ap=IDXI[:, b:b + 1], axis=0)
                bidx = bass.IndirectOffsetOnAxis(ap=BIDX[:, b:b + 1], axis=0)
                for kc in range(8):
                    i8 = bass.IndirectOffsetOnAxis(ap=RT["IDX8"][:, b, kc:kc + 1], axis=0)
                    S.dma('pool', lambda e: e.indirect_dma_start(out=W1[j][:, kc, :], out_offset=None, in_=w1v, in_offset=i8, bounds_check=bc8, oob_is_err=False), writes=[('W1', j, kc)])
                for q in range(4):
                    i4 = bass.IndirectOffsetOnAxis(ap=RT["IDX4"][:, b, q:q + 1], axis=0)
                    S.dma('pool', lambda e: e.indirect_dma_start(out=W2[j][:, 2 * q:2 * q + 2, :].rearrange("p a b -> p (a b)"), out_offset=None,
                                                                 in_=w2v, in_offset=i4, bounds_check=bc4, oob_is_err=False), writes=[('W2', j, q)])
                S.dma('pool', lambda e: e.indirect_dma_start(out=B1C[j][:], out_offset=None, in_=b1d, in_offset=idx), writes=[('B1C', j)])
                S.dma('pool', lambda e: e.indirect_dma_start(out=B2R[j][:], out_offset=None, in_=b2d, in_offset=bidx), writes=[('B2R', j)])

            def xloads(b):
                for st_ in range(NSUB):
                    xj = (b % 2) * NSUB + st_
                    r0 = b * SB + st_ * 128
                    S.dma('sp', lambda e: e.dma_start(out=XS[xj][:], in_=xs_d[r0:r0 + 128, :]), writes=[('XS', xj)])

            def do_tr(b):
                j = b % 2
                for st_ in range(NSUB):
                    xj = (b % 2) * NSUB + st_
                    pbb = st_ % 2
                    for kc in range(8):
                        S.op('pe', lambda e: e.transpose(psb[pbb][:, kc * 128:(kc + 1) * 128], XS[xj][:, kc::8], identb[:]), reads=[('XS', xj)], writes=[PBB(pbb)])
                    S.op('act', lambda e: e.copy(XST2[b % 2][:, :, st_ * 128:(st_ + 1) * 128], psb[pbb][:].rearrange("p (a b) -> p a b", a=8)),
                         writes=[PBB(pbb), ('XST', b % 2, st_)])

            def do_mm1(b):
                j = b % 2
                xk = [('XST', b % 2, s_) for s_ in range(NSUB)]
                for jj in range(8):
                    t = ec[0] % 2
                    ec[0] += 1
                    for half in range(2):
                        bank = 2 * t + half
                        for kc in range(8):
                            S.op('pe', lambda e: e.matmul(psum[bank][:, 0:SB], W1[j][:, kc, half * 1024:(half + 1) * 1024][:, jj::8], XST2[b % 2][:, kc, :],
                                                          start=(kc == 0), stop=(kc == 7)), reads=[('W1', j, kc)] + xk, writes=[PB(bank)])
                    S.op('dve', lambda e: e.tensor_scalar(out=tg[t][:], in0=psum[2 * t][:, 0:SB], scalar1=B1C[j][:, jj:jj + 1], scalar2=7.0,
                                                          op0=ALU.add, op1=ALU.min), reads=[('B1C', j)], writes=[PB(2 * t), ('tg', t)])
                    S.op('dve', lambda e: e.tensor_scalar(out=tu[t][:], in0=psum[2 * t + 1][:, 0:SB], scalar1=B1C[j][:, 8 + jj:9 + jj], scalar2=7.0,
                                                          op0=ALU.add, op1=ALU.min), reads=[('B1C', j)], writes=[PB(2 * t + 1), ('tu', t)])
                    S.op('act', lambda e: e.activation(out=sg[t][:], in_=tg[t][:], func=AF.Sigmoid, scale=1.702), reads=[('tg', t)], writes=[('sg', t)])
                    S.op('dve', lambda e: e.tensor_scalar(out=tu[t][:], in0=tu[t][:], scalar1=-7.0, scalar2=1.0, op0=ALU.max, op1=ALU.add), writes=[('tu', t)])
                    S.op('dve', lambda e: e.tensor_tensor(out=tu[t][:], in0=tu[t][:], in1=tg[t][:], op=ALU.mult), reads=[('tg', t)], writes=[('tu', t)])
                    S.op('dve', lambda e: e.tensor_tensor(out=ACTT[:, jj, :], in0=tu[t][:], in1=sg[t][:], op=ALU.mult),
                         reads=[('tu', t), ('sg', t)], writes=[('ACTT', jj)])

            def do_mm2(b):
                j = b % 2
                ak = [('ACTT', jj) for jj in range(8)]
                for st_ in range(NSUB):
                    yj = yc[0] % 2
                    yc[0] += 1
                    def mm2_mm(nb, jj):
                        S.op('pe', lambda e: e.matmul(psum[4 + nb][:], ACTT[:, jj, st_ * 128:(st_ + 1) * 128], W2[j][:, jj, nb * 512:(nb + 1) * 512],
                                                      start=(jj == 0), stop=(jj == 7)), reads=[('ACTT', jj), ('W2', j, jj // 2)], writes=[PB(4 + nb)])
                    if st_ == 0:
                        for nb in range(2):
                            for jj in range(7):
                                mm2_mm(nb, jj)
                        for nb in range(2):
                            mm2_mm(nb, 7)
                    else:
                        for nb in range(2):
                            for jj in range(8):
                                mm2_mm(nb, jj)
                    for nb in range(2):
                        cs = slice(nb * 512, (nb + 1) * 512)
                        S.op('dve', lambda e: e.tensor_tensor(out=YT[yj][:, cs], in0=psum[4 + nb][:], in1=B2R[j][:, cs], op=ALU.add), reads=[('B2R', j)],
                             writes=[PB(4 + nb), ('YT', yj, nb)])
                    r0 = b * SB + st_ * 128
                    S.dma('sp', lambda e: e.dma_start(out=ys_d[r0:r0 + 128, :], in_=YT[yj][:]), reads=[('YT', yj, 0), ('YT', yj, 1)], writes=[('ys', b, st_)])

            gathers(0)
            xloads(0)
            do_tr(0)
            for b in range(NB):
                if b + 1 < NB:
                    gathers(b + 1)
                    xloads(b + 1)
                do_mm1(b)
                if b + 1 < NB:
                    do_tr(b + 1)
                do_mm2(b)
        S.barrier()

    def phase_combine(layer, tiles, final):
        WG, SLOTI = RT["WG"], RT["SLOTI"]
        with contextlib.ExitStack() as es:
            def al(name, shape, dt):
                return es.enter_context(SBT(name, shape, dt))
            G2 = al("G2", [128, 2, D], F32)
            FG = al("FG", [128, D], F32)
            xt = [al("cxt%d" % j, [128, D], F32) for j in range(2)]
            Y = [[al("cY%d_%d" % (j, k), [128, D], F32) for k in range(4)] for j in range(2)]
            acc = [al("cacc%d" % j, [128, D], F32) for j in range(2)]
            sq = al("csq", [128, D], F32)
            st = al("cst", [128, 8], F32)
            for r in range(2):
                bcast_row(G2[:, r, :], modrow[layer, r:r + 1, 5 * D:6 * D], ('G2', r))
            if final:
                bcast_row(FG[:], I("final_g")[0:1, :], 'FG')
            for n, i in enumerate(tiles):
                j = n % 2
                r = 1 if i < 2 else 0
                rows = slice(i * 128, (i + 1) * 128)
                S.dma('sp', lambda e: e.dma_start(out=xt[j][:], in_=xres[rows, :]), writes=[('cxt', j)])
                for k in range(4):
                    off = bass.IndirectOffsetOnAxis(ap=SLOTI[:, i, k:k + 1], axis=0)
                    S.dma('pool', lambda e: e.indirect_dma_start(out=Y[j][k][:], out_offset=None, in_=ys_d, in_offset=off), writes=[('cY', j, k)])
                S.op('dve', lambda e: e.tensor_scalar(out=acc[j][:], in0=Y[j][0][:], scalar1=WG[:, i, 0:1], scalar2=None, op0=ALU.mult),
                     reads=[('cY', j, 0)], writes=[('cacc', j)])
                for k in range(1, 4):
                    S.op('dve', lambda e: e.scalar_tensor_tensor(out=acc[j][:], in0=Y[j][k][:], scalar=WG[:, i, k:k + 1], in1=acc[j][:],
                                                                 op0=ALU.mult, op1=ALU.add), reads=[('cY', j, k)], writes=[('cacc', j)])
                S.op('dve', lambda e: e.tensor_tensor(out=acc[j][:], in0=acc[j][:], in1=G2[:, r, :], op=ALU.mult), reads=[('G2', r)], writes=[('cacc', j)])
                S.op('dve', lambda e: e.tensor_tensor(out=acc[j][:], in0=acc[j][:], in1=xt[j][:], op=ALU.add), reads=[('cxt', j)], writes=[('cacc', j)])
                if not final:
                    S.dma('act', lambda e: e.dma_start(out=xres[rows, :], in_=acc[j][:]), reads=[('cacc', j)], writes=[('xres', i)])
                else:
                    S.op('act', lambda e: e.activation(out=sq[:], in_=acc[j][:], func=AF.Square, accum_out=st[:, 0:1]), reads=[('cacc', j)], writes=['csq', 'cst'])
                    S.op('dve', lambda e: e.tensor_scalar(out=st[:, 1:2], in0=st[:, 0:1], scalar1=1.0 / D, scalar2=EPS, op0=ALU.mult, op1=ALU.add), writes=['cst'])
                    S.op('act', lambda e: e.activation(out=st[:, 2:3], in_=st[:, 1:2], func=AF.Sqrt), writes=['cst'])
                    S.op('dve', lambda e: e.reciprocal(st[:, 3:4], st[:, 2:3]), writes=['cst'])
                    S.op('dve', lambda e: e.scalar_tensor_tensor(out=acc[j][:], in0=acc[j][:], scalar=st[:, 3:4], in1=FG[:], op0=ALU.mult, op1=ALU.mult),
                         reads=['FG'], writes=[('cacc', j), 'cst'])
                    S.dma('act', lambda e: e.dma_start(out=out_d[(i - 2) * 128:(i - 1) * 128, :], in_=acc[j][:]), reads=[('cacc', j)], writes=[('out', i)])
        S.barrier()

    qT_d = dscr("qT_d", [D, 4096], BF16)
    kT_d = dscr("kT_d", [256, T], BF16)
    vA_d = dscr("vA_d", [T, 256], BF16)
    oT_d = dscr("oT_d", [D, 4096], BF16)

    def phase_A1(uT):
        wv = I("od_w_in").rearrange("(kc p) n -> p kc n", p=128)
        with contextlib.ExitStack() as es:
            def al(name, shape, dt):
                return es.enter_context(SBT(name, shape, dt))
            W = al("aW", [128, 8, 1536], BF16)
            GQ = al("aGQ", [128, 128], F32)
            GK = al("aGK", [128, 128], F32)
            COS = [al("aCOS%d" % j, [128, 128], F32) for j in range(2)]
            SIN = [al("aSIN%d" % j, [128, 128], F32) for j in range(2)]
            xf = [al("axf%d" % j, [128, 1536], F32) for j in range(2)]
            sq2 = [al("asq%d" % j_, [128, 1280], F32) for j_ in range(2)]
            st2 = [al("ast%d" % j_, [128, 40], F32) for j_ in range(2)]
            kn2 = [al("akn%d" % j_, [128, 1280], F32) for j_ in range(2)]
            t12 = [al("at1%d" % j_, [128, 1280], F32) for j_ in range(2)]
            t22 = [al("at2%d" % j_, [128, 1280], F32) for j_ in range(2)]
            kr = [al("akr%d" % j, [128, 1280], BF16) for j in range(2)]
            vb = [al("avb%d" % j, [128, 256], BF16) for j in range(2)]
            xT = [al("axT%d" % j, [128, 10, 128], BF16) for j in range(2)]
            for blk in range(3):
                S.dma('pool', lambda e: e.dma_start(out=W[:, :, blk * 512:(blk + 1) * 512], in_=wv[:, :, blk * 512:(blk + 1) * 512]), writes=[('aW', blk)])
            bcast_row(GQ[:], I("od_qk_g")[0:1, :], 'aGQ')
            bcast_row(GK[:], I("od_qk_g")[1:2, :], 'aGK')
            def hdrvars(i):
                lat = i >= 2
                j = i % 2
                rows = slice(i * 128, (i + 1) * 128)
                blks = [0, 1, 2] if lat else [2]
                h0 = 0 if lat else 8
                sq, st, kn, t1, t2 = sq2[j], st2[j], kn2[j], t12[j], t22[j]
                KSQ, KST = ('asq', j), ('ast', j)
                return lat, j, rows, blks, h0, sq, st, kn, t1, t2, KSQ, KST

            def stageA(i):
                lat, j, rows, blks, h0, sq, st, kn, t1, t2, KSQ, KST = hdrvars(i)
                if lat:
                    tr = slice((i - 2) * 128, (i - 1) * 128)
                    S.dma('pool', lambda e: e.dma_start(out=COS[j][:], in_=I("rope_cos")[tr, :]), writes=[('aCOS', j)])
                    S.dma('pool', lambda e: e.dma_start(out=SIN[j][:], in_=I("rope_sin")[tr, :]), writes=[('aSIN', j)])
                for blk in blks:
                    pb = blk
                    for kc in range(8):
                        S.op('pe', lambda e: e.matmul(psum[pb][:], uT[:, kc, rows], W[:, kc, blk * 512:(blk + 1) * 512], start=(kc == 0), stop=(kc == 7)),
                             reads=[('aW', blk)], writes=[PB(pb)])
                    S.op('act', lambda e: e.copy(xf[j][:, blk * 512:(blk + 1) * 512], psum[pb][:]), writes=[PB(pb), ('axf', j, blk)])
                S.op('pool', lambda e: e.tensor_copy(vb[j][:], xf[j][:, 1280:1536]), reads=[('axf', j, 2)], writes=[('avb', j)])
                S.dma('sp', lambda e: e.dma_start(out=vA_d[rows, :], in_=vb[j][:]), reads=[('avb', j)], writes=[('vA', i)])

            def stageB(i):
                lat, j, rows, blks, h0, sq, st, kn, t1, t2, KSQ, KST = hdrvars(i)
                c0 = h0 * 128
                nh = 10 - h0
                S.op('act', lambda e: e.activation(out=sq[:, c0:1280], in_=xf[j][:, c0:1280], func=AF.Square),
                     reads=[('axf', j, 0), ('axf', j, 1), ('axf', j, 2)], writes=[KSQ])
                S.op('dve', lambda e: e.tensor_reduce(out=st[:, h0:10], in_=sq[:, c0:1280].rearrange("p (h d) -> p h d", d=128), axis=AX.X, op=ALU.add),
                     reads=[KSQ], writes=[KST])
                S.op('dve', lambda e: e.tensor_scalar(out=st[:, 10 + h0:20], in0=st[:, h0:10], scalar1=1.0 / 128, scalar2=EPS, op0=ALU.mult, op1=ALU.add), writes=[KST])
                S.op('act', lambda e: e.activation(out=st[:, 20 + h0:30], in_=st[:, 10 + h0:20], func=AF.Sqrt), writes=[KST])
                S.op('dve', lambda e: e.reciprocal(st[:, 30 + h0:40], st[:, 20 + h0:30]), writes=[KST])
                for h in range(h0, 10):
                    hs = slice(h * 128, (h + 1) * 128)
                    G = GQ if h < 8 else GK
                    dst = kn[:, hs] if lat else kr[j][:, hs]
                    dk = ('akn', j, h) if lat else ('akr', j, h)
                    S.op('dve', lambda e: e.scalar_tensor_tensor(out=dst, in0=xf[j][:, hs], scalar=st[:, 30 + h:31 + h], in1=G[:], op0=ALU.mult, op1=ALU.mult),
                         reads=['aGQ', 'aGK', KST], writes=[dk])
                if lat:
                    snv = SIN[j][:].rearrange("p (b c) -> p b c", b=2)
                    for h in range(h0, 10):
                        hs = slice(h * 128, (h + 1) * 128)
                        knv = kn[:, hs].rearrange("p (b c) -> p b c", b=2)
                        t2v = t2[:, hs].rearrange("p (b c) -> p b c", b=2)
                        S.op('dve', lambda e: e.tensor_tensor(out=t1[:, hs], in0=kn[:, hs], in1=COS[j][:], op=ALU.mult), reads=[('akn', j, h), ('aCOS', j)],
                             writes=[('at1', j, h)])
                        S.op('pool', lambda e: e.tensor_tensor(out=t2v[:, :, 0:32], in0=knv[:, :, 32:64], in1=snv[:, :, 0:32], op=ALU.mult),
                             reads=[('akn', j, h), ('aSIN', j)], writes=[('at2', j, h)])
                        S.op('pool', lambda e: e.tensor_tensor(out=t2v[:, :, 32:64], in0=knv[:, :, 0:32], in1=snv[:, :, 32:64], op=ALU.mult),
                             reads=[('akn', j, h), ('aSIN', j)], writes=[('at2', j, h)])
                    for h in range(h0, 10):
                        hs = slice(h * 128, (h + 1) * 128)
                        S.op('dve', lambda e: e.tensor_tensor(out=kr[j][:, hs], in0=t1[:, hs], in1=t2[:, hs], op=ALU.add), reads=[('at1', j, h), ('at2', j, h)],
                             writes=[('akr', j, h)])

            def stageC(i):
                lat, j, rows, blks, h0, sq, st, kn, t1, t2, KSQ, KST = hdrvars(i)
                if lat:
                    for h in range(8):
                        S.op('pe', lambda e: e.transpose(psb[0][:, h * 128:(h + 1) * 128], kr[j][:, h * 128:(h + 1) * 128], identb[:]),
                             reads=[('akr', j, h)], writes=[PBB(0)])
                    S.op('act', lambda e: e.copy(xT[j][:, 0:8, :].rearrange("p a b -> p (a b)"), psb[0][:]), writes=[PBB(0), ('axT', j, 0)])
                    S.dma('sp', lambda e: e.dma_start(out=qT_d[:, (i - 2) * 128:(i - 1) * 128].rearrange("(h d) t -> d h t", d=128), in_=xT[j][:, 0:8, :]),
                          reads=[('axT', j, 0)], writes=[('qT_d', i)])
                for h in range(8, 10):
                    S.op('pe', lambda e: e.transpose(psb[1][:, (h - 8) * 128:(h - 7) * 128], kr[j][:, h * 128:(h + 1) * 128], identb[:]),
                         reads=[('akr', j, h)], writes=[PBB(1)])
                S.op('act', lambda e: e.copy(xT[j][:, 8:10, :].rearrange("p a b -> p (a b)"), psb[1][:, 0:256]), writes=[PBB(1), ('axT', j, 1)])
                S.dma('sp', lambda e: e.dma_start(out=kT_d[:, rows].rearrange("(h d) t -> d h t", d=128), in_=xT[j][:, 8:10, :]),
                      reads=[('axT', j, 1)], writes=[('kT_d', i)])

            stageA(0)
            for i in range(NT):
                if i + 1 < NT:
                    stageA(i + 1)
                stageB(i)
                stageC(i)
        S.barrier()

    def phase_A2():
        scale = 128.0 ** -0.5
        with contextlib.ExitStack() as es:
            def al(name, shape, dt):
                return es.enter_context(SBT(name, shape, dt))
            KT = al("bKT", [128, T], BF16)
            V = al("bV", [128, NT, 128], BF16)
            onesb = al("bones", [128, 128], BF16)
            QT4 = [al("bQT%d" % j, [128, 4, 128], BF16) for j in range(2)]
            PT = [al("bPT%d" % j, [128, 512], BF16) for j in range(4)]
            rs = [al("brs%d" % j, [128, 512], F32) for j in range(2)]
            PTS = [al("bPTS%d" % j, [128, 512], BF16) for j in range(2)]
            ot = [al("bot%d" % j, [128, 4, 128], BF16) for j in range(2)]
            S.op('pool', lambda e: e.memset(onesb[:], 1.0), writes=['bones'])
            pc = 0
            it = 0
            for g in range(2):
                S.dma('sp', lambda e: e.dma_start(out=KT[:], in_=kT_d[g * 128:(g + 1) * 128, :]), writes=['bKT'])
                S.dma('sp', lambda e: e.dma_start(out=V[:], in_=vA_d[:, g * 128:(g + 1) * 128].rearrange("(n p) d -> p n d", p=128)), writes=['bV'])
                for qi in range(32):
                    j = it % 2
                    it += 1
                    bo, bs_ = 2 + 2 * j, 3 + 2 * j
                    S.dma('sp', lambda e: e.dma_start(out=QT4[j][:], in_=qT_d[g * 512:(g + 1) * 512, qi * 128:(qi + 1) * 128].rearrange("(h d) t -> d h t", d=128)),
                          writes=[('bQT', j)])

                    def st_mm(kt):
                        bank = kt % 2
                        S.op('pe', lambda e: e.matmul(psum[bank][:], KT[:, kt * 128:(kt + 1) * 128], QT4[j][:].rearrange("p a b -> p (a b)"), start=True, stop=True),
                             reads=['bKT', ('bQT', j)], writes=[PB(bank)])
                    st_mm(0)
                    for kt in range(NT):
                        bank = kt % 2
                        p_ = pc % 4
                        pc += 1
                        S.op('act', lambda e: e.activation(out=PT[p_][:], in_=psum[bank][:], func=AF.Exp, scale=scale), writes=[PB(bank), ('bPT', p_)])
                        if kt + 1 < NT:
                            st_mm(kt + 1)
                        S.op('pe', lambda e: e.matmul(psum[bo][:], V[:, kt, :], PT[p_][:], start=(kt == 0), stop=(kt == NT - 1)),
                             reads=[('bPT', p_), 'bV'], writes=[PB(bo)])
                        S.op('pe', lambda e: e.matmul(psum[bs_][:], onesb[:], PT[p_][:], start=(kt == 0), stop=(kt == NT - 1)),
                             reads=[('bPT', p_), 'bones'], writes=[PB(bs_)])
                    S.op('dve', lambda e: e.reciprocal(rs[j][:], psum[bs_][:]), writes=[PB(bs_), ('brs', j)])
                    S.op('dve', lambda e: e.tensor_tensor(out=ot[j][:].rearrange("p a b -> p (a b)"), in0=psum[bo][:], in1=rs[j][:], op=ALU.mult),
                         reads=[('brs', j)], writes=[PB(bo), ('bot', j)])
                    S.dma('pool', lambda e: e.dma_start(out=oT_d[g * 512:(g + 1) * 512, qi * 128:(qi + 1) * 128].rearrange("(h d) t -> d h t", d=128), in_=ot[j][:]),
                          reads=[('bot', j)], writes=[('oT_d', g, qi)])
        S.barrier()

    def phase_A3():
        with contextlib.ExitStack() as es:
            def al(name, shape, dt):
                return es.enter_context(SBT(name, shape, dt))
            WO = al("cWO", [128, 8, D], BF16)
            G1 = al("cG1", [128, D], F32)
            OT = [al("cOT%d" % j, [128, 8, 128], BF16) for j in range(2)]
            xt = [al("cx%d" % j, [128, D], F32) for j in range(2)]
            tmp = al("ctmp", [128, D], F32)
            S.dma('pool', lambda e: e.dma_start(out=WO[:], in_=I("od_w_out").rearrange("(kc p) n -> p kc n", p=128)), writes=['cWO'])
            bcast_row(G1[:], modrow[1, 0:1, 2 * D:3 * D], 'cG1')
            for qi in range(32):
                j = qi % 2
                rows = slice((qi + 2) * 128, (qi + 3) * 128)
                S.dma('sp', lambda e: e.dma_start(out=OT[j][:], in_=oT_d[:, qi * 128:(qi + 1) * 128].rearrange("(h d) t -> d h t", d=128)), writes=[('cOT', j)])
                S.dma('sp', lambda e: e.dma_start(out=xt[j][:], in_=xres[rows, :]), writes=[('cx', j)])
                for nb in range(2):
                    pb = (2 * qi + nb) % 4
                    for kc in range(8):
                        S.op('pe', lambda e: e.matmul(psum[pb][:], OT[j][:, kc, :], WO[:, kc, nb * 512:(nb + 1) * 512], start=(kc == 0), stop=(kc == 7)),
                             reads=[('cOT', j), 'cWO'], writes=[PB(pb)])
                    cs = slice(nb * 512, (nb + 1) * 512)
                    S.op('dve', lambda e: e.tensor_tensor(out=tmp[:, cs], in0=psum[pb][:], in1=G1[:, cs], op=ALU.mult), reads=['cG1'], writes=[PB(pb), ('ctmp', nb)])
                    S.op('pool', lambda e: e.tensor_tensor(out=tmp[:, cs], in0=tmp[:, cs], in1=xt[j][:, cs], op=ALU.add), reads=[('cx', j)], writes=[('ctmp', nb)])
                S.dma('pool', lambda e: e.dma_start(out=xres[rows, :], in_=tmp[:]), reads=[('ctmp', 0), ('ctmp', 1)], writes=[('xres', qi)])
        S.barrier()

    def small_dump():
        W_ = NT * 32 + NT * 4 + NT * 4 + 2 * NBMAX
        dl = dscr("dsmall_scr", [128, W_], F32)
        with SBT("dsm", [128, W_], F32) as dsm:
            o = 0
            for nm, n in (("LOG", NT * 32), ("WG", NT * 4), ("SLOTI", NT * 4)):
                S.op('dve', lambda e: e.tensor_copy(dsm[:, o:o + n], RT[nm][:].rearrange("p a b -> p (a b)")), writes=['dsm'])
                o += n
            for nm in ("IDXI", "BIDX"):
                S.op('dve', lambda e: e.tensor_copy(dsm[:, o:o + NBMAX], RT[nm][:]), writes=['dsm'])
                o += NBMAX
            S.dma('sp', lambda e: e.dma_start(out=dl, in_=dsm[:]), reads=['dsm'], writes=['dl'])
            S.barrier()
        return ('small', dl, [128, W_], F32)

    def copy_xin_to_xres():
        with SBT("cpx", [128, D], F32) as cpx:
            for i in range(NT):
                S.dma('sp', lambda e: e.dma_start(out=cpx[:], in_=I("xin")[i * 128:(i + 1) * 128, :]), writes=['cpx'])
                S.dma('sp', lambda e: e.dma_start(out=xres[i * 128:(i + 1) * 128, :], in_=cpx[:]), reads=['cpx'], writes=[('xres', i)])
        S.barrier()

    phase_mod()
    if stop_after == 'mod':
        return finish_dbg([('modrow', modrow.rearrange("a b c -> (a b) c"), [4, 6 * D], F32)])

    if stop_after not in ('A_only', 'M1_only'):
        uT_guard = SBT("uT", [128, 8, T], BF16)
        uT = uT_guard.__enter__()
        phase_norm_T(0, I("xin"), uT)
        phase_E1(uT)
        if stop_after == 'E1':
            return finish_dbg([('qkT', qkT, [2048, T], BF16), ('v_tm', v_tm, [T, D], BF16), ('so_tm', so_tm, [T, D], BF16),
                               ('g_tm', g_tm, [T, 16], F32), ('xrT', xrT, [D, T], F32), ('ygT', ygT, [D, T], BF16)])
        uT_guard.__exit__(None, None, None)
        phase_E2()
        if stop_after == 'E2':
            return finish_dbg([('hlT', hlT, [D, T], BF16)])
        phase_E3()
        if stop_after == 'E3':
            return finish_dbg([('hm_f', hm_d[0], [T, D], F32), ('hm_b', hm_d[1], [T, D], F32)])
        phase_E4()
        if stop_after == 'E4':
            return finish_dbg([('xres', xres, [T, D], F32)])
        NB0 = (4 * T) // SB + 32
        phase_N2(0, range(NT), RT["LOG"])
        phase_route(range(NT), NB0)
        phase_scatter(range(NT), NB0)
        if stop_after == 'R0':
            return finish_dbg([small_dump(), ('xs', xs_d, [NBMAX * SB, D], BF16), ('v2', v2_tm, [T, D], BF16)])
        phase_moe(0, NB0)
        phase_combine(0, range(NT), False)
        if stop_after == 'M0':
            return finish_dbg([('xres', xres, [T, D], F32)])
    else:
        copy_xin_to_xres()

    if stop_after != 'M1_only':
        uT_guard = SBT("uT", [128, 8, T], BF16)
        uT = uT_guard.__enter__()
        phase_norm_T(1, xres, uT)
        phase_A1(uT)
        uT_guard.__exit__(None, None, None)
        phase_A2()
        phase_A3()
        if stop_after in ('A', 'A_only'):
            return finish_dbg([('xres', xres, [T, D], F32)])
    NB1 = (4 * 4096) // SB + 32
    lat_tiles = range(2, NT)
    phase_N2(1, lat_tiles, RT["LOG"])
    phase_route(lat_tiles, NB1)
    phase_scatter(lat_tiles, NB1)
    phase_moe(1, NB1)
    phase_combine(1, lat_tiles, True)
    if stop_after == 'M1_only':
        return finish_dbg([('outc', out_d, [4096, D], F32)])
    S.barrier()
    return nc, dbg_out, used_inputs


def host_inputs(b, inp, names=None):
    f = lambda a: np.ascontiguousarray(a, dtype=np.float32)
    want = lambda k: names is None or k in names
    m = {}
    if want("xin"):
        m["xin"] = f(np.concatenate([inp["ctx"][b], inp["x"][b]], axis=0))
    if want("ccols"):
        m["ccols"] = f(np.concatenate([inp["c"][b].reshape(8, 128).T, inp["c_ctx"].reshape(8, 128).T], axis=1))
    for k in ["mod_w", "mod_b", "norm1_g", "norm2_g", "moe_w_r", "moe_b_r"]:
        if want(k):
            m[k] = f(inp[k])
    if want("final_g"):
        m["final_g"] = f(inp["final_g"].reshape(1, D))
    if want("ev_w_in"):
        m["ev_w_in"] = f(inp["ev_w_in"][0])
    if want("ev_qkcw"):
        qk = np.concatenate([inp["ev_qk_conv_w"][0], inp["ev_qk_conv_b"][0][None]], axis=0)
        m["ev_qkcw"] = f(qk.T.reshape(16, 128, 5).transpose(1, 0, 2))
    if want("ev_gate_b"):
        m["ev_gate_b"] = f(inp["ev_gate_b"][0].reshape(1, 16))
    if want("ev_mnorm_g"):
        m["ev_mnorm_g"] = f(inp["ev_mnorm_g"][0].reshape(1, D))
    if want("ev_lrucw"):
        lc = np.concatenate([inp["ev_lru_conv_w"][0], inp["ev_lru_conv_b"][0][None]], axis=0)
        m["ev_lrucw"] = f(lc.T.reshape(8, 128, 5).transpose(1, 0, 2))
    if want("ev_lru_w") or want("ev_lru_b"):
        lw = np.zeros((4, 8, 128, 128), np.float32)
        lb = np.zeros((128, 8, 4), np.float32)
        for z in range(2):
            for gi, (wk, bk) in enumerate([("ev_lru_wa", "ev_lru_ba"), ("ev_lru_wx", "ev_lru_bx")]):
                for cc_ in range(8):
                    for h in range(2):
                        n = cc_ * 2 + h
                        lw[z * 2 + gi, cc_, h * 64:(h + 1) * 64, h * 64:(h + 1) * 64] = inp[wk][0, z, n]
                        lb[h * 64:(h + 1) * 64, cc_, z * 2 + gi] = inp[bk][0, z, n]
        m["ev_lru_w"] = lw
        m["ev_lru_b"] = lb
    if want("ev_lru_lam"):
        m["ev_lru_lam"] = f(inp["ev_lru_lam"][0].reshape(2, 8, 128).transpose(2, 1, 0))
    if want("ev_w_out"):
        m["ev_w_out"] = f(inp["ev_w_out"][0])
    if want("od_w_in"):
        m["od_w_in"] = f(inp["od_w_in"][0])
    if want("od_qk_g"):
        m["od_qk_g"] = f(np.stack([inp["od_q_norm_g"][0], inp["od_k_norm_g"][0]]))
    if want("od_w_out"):
        m["od_w_out"] = f(inp["od_w_out"][0])
    if want("rope_cos") or want("rope_sin"):
        pos = np.arange(4096)
        row = (pos // 64).astype(np.float32)
        col = (pos % 64).astype(np.float32)
        inv = (10000.0 ** (-np.arange(0, 64, 2, dtype=np.float32) / 64)).astype(np.float32)
        ar = row[:, None] * inv[None]
        ac = col[:, None] * inv[None]
        m["rope_cos"] = f(np.concatenate([np.cos(ar), np.cos(ar), np.cos(ac), np.cos(ac)], axis=1))
        m["rope_sin"] = f(np.concatenate([-np.sin(ar), np.sin(ar), -np.sin(ac), np.sin(ac)], axis=1))
    for l in range(2):
        if want("moe_w1_%d" % l):
            m["moe_w1_%d" % l] = f(inp["moe_w1"][l]).reshape(32 * 128, 8, 2048)
        if want("moe_w2_%d" % l):
            m["moe_w2_%d" % l] = f(inp["moe_w2"][l]).reshape(32 * 128, 8, 1024)
    for l in range(2):
        if want("moe_b1_%d" % l):
            m["moe_b1_%d" % l] = f(inp["moe_b1"][l].reshape(32, 2, 128, 8).transpose(0, 2, 1, 3).reshape(32 * 128, 16))
        if want("moe_b2_%d" % l):
            m["moe_b2_%d" % l] = f(inp["moe_b2"][l])
    if names is not None:
        m = {k: v for k, v in m.items() if k in names}
    return m


_CACHE = {}


def kernel(**inputs):
    if 'nc' not in _CACHE:
        _CACHE['nc'] = build()
    nc, _, used = _CACHE['nc']
    names = set(used.keys())
    in_maps = [host_inputs(b, inputs, names) for b in range(8)]
    res = run_bass_kernel_spmd(nc, in_maps, core_ids=list(range(8)))
    return np.stack([r["out"] for r in res.results], axis=0).astype(np.float32)
```
